# Optimizing a Trainium2 kernel written in Bass

```python
import math
import jax
import jax.numpy as jnp
from jax import lax
import numpy as np

D_MODEL = 1024
BATCH = 8
SEQ = 2048
DEPTH = 4

HEAD_DIM = 64
A_HEADS = 6
A_PATTERNS = ((128, 1), (512, 4), (2048, 16))
A_WIDTH = A_HEADS * HEAD_DIM
B_HEADS = 4
B_NOPE = 64
B_ROPE = 32
B_V = 64
B_Q_RANK = 256
B_KV_RANK = 128
B_WIDTH = B_HEADS * B_V
C_HEADS = 6
C_KEY = 32
C_VAL = 64
C_WIDTH = C_HEADS * C_VAL
C_CHUNK = 128
MIX_WIDTH = A_WIDTH + B_WIDTH + C_WIDTH
IN_SIZES = (A_WIDTH, A_WIDTH, A_WIDTH, B_Q_RANK, B_KV_RANK, B_ROPE, C_HEADS * C_KEY, C_HEADS * C_KEY, C_WIDTH, C_WIDTH)
IN_WIDTH = sum(IN_SIZES)
FFN_HIDDEN = -(-8 * D_MODEL // (3 * 256)) * 256
N_BUCKETS = 32
MAX_DISTANCE = 2048
ROPE_BASE = 10000.0
EPS = 1e-6
Q_BLOCK = 128

kernel_name = 'hybrid_dilated_mla_retention_trunk'


def rmsnorm(x, g=None):
    xf = x.astype(jnp.float32)
    y = xf * lax.rsqrt(jnp.mean(xf * xf, axis=-1, keepdims=True) + EPS)
    if g is not None:
        y = y * g.astype(jnp.float32)
    return y.astype(x.dtype)


def group_norm_heads(x):
    xf = x.astype(jnp.float32)
    mu = jnp.mean(xf, axis=-1, keepdims=True)
    var = jnp.mean(jnp.square(xf - mu), axis=-1, keepdims=True)
    return ((xf - mu) * lax.rsqrt(var + EPS)).astype(x.dtype)


def rope(x, pos):
    half = x.shape[-1] // 2
    inv_freq = (1.0 / (ROPE_BASE ** (np.arange(half, dtype=np.float32) / half))).astype(np.float32)
    ang = pos[:, None, :, None].astype(jnp.float32) * inv_freq
    cos, sin = jnp.cos(ang), jnp.sin(ang)
    x1 = x[..., :half].astype(jnp.float32)
    x2 = x[..., half:].astype(jnp.float32)
    return jnp.concatenate([x1 * cos - x2 * sin, x1 * sin + x2 * cos], axis=-1).astype(x.dtype)


def t5_bucket(dist):
    max_exact = N_BUCKETS // 2
    safe = np.maximum(dist, 1).astype(np.float32)
    large = max_exact + (np.log(safe / max_exact) / np.log(MAX_DISTANCE / max_exact) * (N_BUCKETS - max_exact)).astype(np.int32)
    large = np.minimum(large, N_BUCKETS - 1)
    return np.where(dist < max_exact, dist, large).astype(np.int32)


def to_heads(t, h):
    b, s, _ = t.shape
    return t.reshape(b, s, h, -1).transpose(0, 2, 1, 3)


def from_heads(t):
    b, h, s, d = t.shape
    return t.transpose(0, 2, 1, 3).reshape(b, s, h * d)


def dilated_attention(q, k, v, rel_bias):
    b, h, s, dh = q.shape
    scale = dh ** -0.5
    patterns = []
    for (w, d) in A_PATTERNS:
        dist = np.arange(w // d + 1, dtype=np.int32) * d
        bias = jnp.transpose(rel_bias[t5_bucket(dist)]).astype(jnp.float32)
        patterns.append((jnp.asarray(dist), bias))

    def block(i):
        start = i * Q_BLOCK
        t = start + jnp.arange(Q_BLOCK, dtype=jnp.int32)
        q_blk = lax.dynamic_slice_in_dim(q, start, Q_BLOCK, axis=2).astype(jnp.float32) * scale
        ms, ls, outs = [], [], []
        for dist, bias in patterns:
            idx = t[:, None] - dist[None, :]
            valid = idx >= 0
            idx = jnp.maximum(idx, 0)
            k_g = k[:, :, idx].astype(jnp.float32)
            v_g = v[:, :, idx].astype(jnp.float32)
            sc = jnp.einsum('bhqd,bhqnd->bhqn', q_blk, k_g) + bias[None, :, None, :]
            sc = jnp.where(valid, sc, -jnp.inf)
            m = jnp.max(sc, axis=-1)
            p = jnp.exp(sc - m[..., None])
            l = jnp.sum(p, axis=-1)
            outs.append(jnp.einsum('bhqn,bhqnd->bhqd', p, v_g) / l[..., None])
            ms.append(m)
            ls.append(l)
        m_all = jnp.stack(ms)
        l_all = jnp.stack(ls)
        o_all = jnp.stack(outs)
        wts = l_all * jnp.exp(m_all - jnp.max(m_all, axis=0))
        out = jnp.sum(wts[..., None] * o_all, axis=0) / jnp.sum(wts, axis=0)[..., None]
        return out.astype(q.dtype)

    out = lax.map(block, jnp.arange(s // Q_BLOCK))
    return jnp.transpose(out, (1, 2, 0, 3, 4)).reshape(b, h, s, dh)


def mla(q_lat, kv_lat, k_rope, pos, q_norm_g, kv_norm_g, w_uq, w_ukv):
    b, s, _ = q_lat.shape
    q = to_heads(rmsnorm(q_lat, q_norm_g) @ w_uq, B_HEADS)
    q_nope = q[..., :B_NOPE]
    q_pe = rope(q[..., B_NOPE:], pos)
    kv = to_heads(rmsnorm(kv_lat, kv_norm_g) @ w_ukv, B_HEADS)
    k_nope = kv[..., :B_NOPE]
    v = kv[..., B_NOPE:]
    k_pe = rope(k_rope[:, None], pos)[:, 0]
    scale = (B_NOPE + B_ROPE) ** -0.5
    key_idx = jnp.arange(s, dtype=jnp.int32)

    def block(i):
        start = i * Q_BLOCK
        t = start + jnp.arange(Q_BLOCK, dtype=jnp.int32)
        qn = lax.dynamic_slice_in_dim(q_nope, start, Q_BLOCK, axis=2)
        qp = lax.dynamic_slice_in_dim(q_pe, start, Q_BLOCK, axis=2)
        sc = (jnp.einsum('bhqd,bhkd->bhqk', qn, k_nope) + jnp.einsum('bhqd,bkd->bhqk', qp, k_pe)).astype(jnp.float32) * scale
        sc = jnp.where(key_idx[None, :] <= t[:, None], sc, -jnp.inf)
        p = jax.nn.softmax(sc, axis=-1)
        return jnp.einsum('bhqk,bhkd->bhqd', p.astype(v.dtype), v)

    out = lax.map(block, jnp.arange(s // Q_BLOCK))
    return jnp.transpose(out, (1, 2, 0, 3, 4)).reshape(b, B_HEADS, s, B_V)


def retention(q, k, v, pos):
    b, h, s, dk = q.shape
    dv = v.shape[-1]
    n = s // C_CHUNK
    qf = rope(q, pos).astype(jnp.float32)
    kf = rope(k, pos).astype(jnp.float32) * (dk ** -0.5)
    vf = v.astype(jnp.float32)
    log_g = np.log(1.0 - 2.0 ** (-5.0 - np.arange(h))).astype(np.float32)
    i = np.arange(C_CHUNK, dtype=np.float32)
    rel = i[:, None] - i[None, :]
    decay_intra = jnp.asarray(np.exp(np.maximum(rel, 0.0)[None] * log_g[:, None, None]) * (rel >= 0)[None], dtype=jnp.float32)
    xi = jnp.asarray(np.exp((i + 1.0)[None, :] * log_g[:, None]), dtype=jnp.float32)
    zeta = jnp.asarray(np.exp((C_CHUNK - 1.0 - i)[None, :] * log_g[:, None]), dtype=jnp.float32)
    chunk_decay = jnp.asarray(np.exp(C_CHUNK * log_g), dtype=jnp.float32)

    def chunks(t):
        return t.reshape(b, h, n, C_CHUNK, t.shape[-1]).transpose(2, 0, 1, 3, 4)

    def step(state, qkv):
        qc, kc, vc = qkv
        sc = jnp.einsum('bhid,bhjd->bhij', qc, kc) * decay_intra
        o = jnp.einsum('bhij,bhjv->bhiv', sc, vc) + jnp.einsum('bhid,bhdv->bhiv', qc, state) * xi[:, :, None]
        state = state * chunk_decay[:, None, None] + jnp.einsum('bhjd,bhjv->bhdv', kc * zeta[:, :, None], vc)
        return state, o

    state0 = jnp.zeros((b, h, dk, dv), jnp.float32)
    _, o = lax.scan(step, state0, (chunks(qf), chunks(kf), chunks(vf)))
    return jnp.transpose(o, (1, 2, 0, 3, 4)).reshape(b, h, s, dv).astype(v.dtype)


def setup_inputs(seed: int = 0) -> dict:
    key = jax.random.key(seed)
    ks = jax.random.split(key, 20)

    def nrm(k, shape, sd):
        return jax.random.normal(k, shape, jnp.float32) * sd

    return {
        'x': nrm(ks[0], (BATCH, SEQ, D_MODEL), 1.0),
        'c': nrm(ks[1], (BATCH, D_MODEL), 1.0),
        'positions': jnp.arange(SEQ, dtype=jnp.int32)[None, :] + jax.random.randint(ks[2], (BATCH, 1), 0, 4096, dtype=jnp.int32),
        'rel_bias': nrm(ks[3], (N_BUCKETS, A_HEADS), 0.5),
        'ada_w': nrm(ks[4], (DEPTH, D_MODEL, 6 * D_MODEL), 0.5 * D_MODEL ** -0.5),
        'ada_b': nrm(ks[5], (DEPTH, 6 * D_MODEL), 0.02),
        'norm1_g': 1.0 + nrm(ks[6], (DEPTH, D_MODEL), 0.05),
        'w_in': nrm(ks[7], (DEPTH, D_MODEL, IN_WIDTH), D_MODEL ** -0.5),
        'mla_q_norm': 1.0 + nrm(ks[8], (DEPTH, B_Q_RANK), 0.05),
        'mla_kv_norm': 1.0 + nrm(ks[9], (DEPTH, B_KV_RANK), 0.05),
        'mla_w_uq': nrm(ks[10], (DEPTH, B_Q_RANK, B_HEADS * (B_NOPE + B_ROPE)), B_Q_RANK ** -0.5),
        'mla_w_ukv': nrm(ks[11], (DEPTH, B_KV_RANK, B_HEADS * (B_NOPE + B_V)), B_KV_RANK ** -0.5),
        'mix_gain': 1.0 + nrm(ks[12], (DEPTH, MIX_WIDTH), 0.05),
        'w_out': nrm(ks[13], (DEPTH, MIX_WIDTH, D_MODEL), MIX_WIDTH ** -0.5),
        'norm2_g': 1.0 + nrm(ks[14], (DEPTH, D_MODEL), 0.05),
        'ffn_w_gate': nrm(ks[15], (DEPTH, D_MODEL, FFN_HIDDEN), D_MODEL ** -0.5),
        'ffn_w_up': nrm(ks[16], (DEPTH, D_MODEL, FFN_HIDDEN), D_MODEL ** -0.5),
        'ffn_w_down': nrm(ks[17], (DEPTH, FFN_HIDDEN, D_MODEL), FFN_HIDDEN ** -0.5),
        'final_norm': 1.0 + nrm(ks[18], (D_MODEL,), 0.05),
    }


def reference(x, c, positions, rel_bias, ada_w, ada_b, norm1_g, w_in, mla_q_norm, mla_kv_norm, mla_w_uq, mla_w_ukv, mix_gain, w_out, norm2_g, ffn_w_gate, ffn_w_up, ffn_w_down, final_norm):
    split_at = [int(v) for v in np.cumsum(IN_SIZES)[:-1]]
    c_act = jax.nn.silu(c)
    for l in range(DEPTH):
        mod = (c_act @ ada_w[l] + ada_b[l])[:, None, :]
        sh1, sc1, g1, sh2, sc2, g2 = jnp.split(mod, 6, axis=-1)
        h = rmsnorm(x, norm1_g[l]) * (1.0 + sc1) + sh1
        proj = h @ w_in[l]
        aq, ak, av, bq_lat, bkv_lat, bk_rope, cq, ck, cv, cg = jnp.split(proj, split_at, axis=-1)
        oa = dilated_attention(to_heads(aq, A_HEADS), to_heads(ak, A_HEADS), to_heads(av, A_HEADS), rel_bias)
        ob = mla(bq_lat, bkv_lat, bk_rope, positions, mla_q_norm[l], mla_kv_norm[l], mla_w_uq[l], mla_w_ukv[l])
        oc = retention(to_heads(cq, C_HEADS), to_heads(ck, C_HEADS), to_heads(cv, C_HEADS), positions)
        mix = jnp.concatenate([
            from_heads(rmsnorm(oa)),
            from_heads(rmsnorm(ob)),
            from_heads(group_norm_heads(oc)) * jax.nn.silu(cg),
        ], axis=-1) * mix_gain[l]
        x = x + g1 * (mix @ w_out[l])
        h = rmsnorm(x, norm2_g[l]) * (1.0 + sc2) + sh2
        ffn = (jax.nn.silu(h @ ffn_w_gate[l]) * (h @ ffn_w_up[l])) @ ffn_w_down[l]
        x = x + g2 * ffn
    return rmsnorm(x, final_norm)
```

```python
import numpy as np
import concourse.bass as bass
import concourse.mybir as mybir
from concourse.bass_utils import run_bass_kernel_spmd
from contextlib import ExitStack

F32 = mybir.dt.float32
BF16 = mybir.dt.bfloat16
I32 = mybir.dt.int32
AF = mybir.ActivationFunctionType
ALU = mybir.AluOpType
AX = mybir.AxisListType

CE = ('pe', 'act', 'dve', 'pool')
SAME_ENGINE_SYNC = True


class Buf:
    __slots__ = ('t', 'name', 'w', 'r', 'semname')

    def __init__(self, t, name, semname=None):
        self.t = t
        self.name = name
        self.w = None
        self.r = {}
        self.semname = semname or name

    def __getitem__(self, idx):
        return V(self, self.t[idx])

    def v(self, ap):
        return V(self, ap)


class V:
    __slots__ = ('buf', 'ap')

    def __init__(self, buf, ap):
        self.buf = buf
        self.ap = ap


class Eng:
    def __init__(self, fw, name, sem):
        self.fw = fw
        self.name = name
        self.sem = sem
        self.count = 0
        self.seen = {}
        self.prog = []
        self.snaps = {0: {}}
        self.nwait = 0
        self.ninst = 0

    def _deps(self, reads, writes):
        deps = []
        for v in reads:
            if v.buf.w is not None:
                deps.append(v.buf.w)
        for v in writes:
            if v.buf.w is not None:
                deps.append(v.buf.w)
            deps.extend(v.buf.r.values())
        need = {}
        for (key, val, sem) in deps:
            if key == self.name and (self.name == 'pe' or not SAME_ENGINE_SYNC):
                continue
            if val > self.seen.get(key, 0) and val > need.get(key, (0, None))[0]:
                need[key] = (val, sem)
        for key, (val, sem) in need.items():
            self.prog.append(('wait', sem, val))
            self.nwait += 1
            self.seen[key] = val
            if key in self.fw.E and key != self.name:
                snap = self.fw.E[key].snaps.get(val)
                if snap:
                    for k2, v2 in snap.items():
                        if v2 > self.seen.get(k2, 0):
                            self.seen[k2] = v2

    def issue(self, fn, reads=(), writes=(), signal=True):
        self._deps(reads, writes)
        self.ninst += 1
        if signal:
            self.count += 1
            self.prog.append(('inst', fn, True))
            self.snaps[self.count] = {k: v for k, v in self.seen.items() if k in CE}
            tok = (self.name, self.count, self.sem)
        else:
            self.prog.append(('inst', fn, False))
            tok = (self.name, self.count + 1, self.sem)
        for v in reads:
            v.buf.r[self.name] = tok
        for v in writes:
            v.buf.w = tok
            v.buf.r = {}
        return tok

    def dma(self, out, in_, **kw):
        return self.dma_group([(out, in_)], **kw)

    def dma_group(self, pairs, **kw):
        reads = [i for (o, i) in pairs if isinstance(i, V)]
        writes = [o for (o, i) in pairs if isinstance(o, V)]
        self._deps(reads, writes)
        buf = (writes[0].buf if writes else reads[0].buf)
        rec = self.fw.dsem.get(buf.semname)
        if rec is None:
            rec = [self.fw.new_sem('d_' + buf.semname), 0]
            self.fw.dsem[buf.semname] = rec
        for (o, i) in pairs:
            rec[1] += 1
            oa = o.ap if isinstance(o, V) else o
            ia = i.ap if isinstance(i, V) else i
            self.prog.append(('dma', oa, ia, rec[0], kw))
        tok = (('d', buf.semname), 16 * rec[1], rec[0])
        for v in reads:
            v.buf.r[tok[0]] = tok
        for v in writes:
            v.buf.w = tok
            v.buf.r = {}
        return tok

    def wait_tok(self, tok):
        key, val, sem = tok
        if val > self.seen.get(key, 0):
            self.prog.append(('wait', sem, val))
            self.seen[key] = val

    def replay(self, e):
        for item in self.prog:
            if item[0] == 'wait':
                e.wait_ge(item[1], item[2])
            elif item[0] == 'inst':
                ins = item[1](e)
                if item[2]:
                    ins.then_inc(self.sem, 1)
            else:
                _, oa, ia, sem, kw = item
                e.dma_start(out=oa, in_=ia, **kw).then_inc(sem, 16)


class FW:
    def __init__(self, nc, stack):
        self.nc = nc
        self.stack = stack
        self.nsem = 0
        self.dsem = {}
        self.dma_toks = []
        self.E = {}
        for name in CE:
            self.E[name] = Eng(self, name, self.new_sem('c_' + name))
        self.sp = Eng(self, 'sp', None)
        self.pe, self.act, self.dve, self.pool = (self.E[n] for n in CE)

    def new_sem(self, name):
        self.nsem += 1
        return self.stack.enter_context(self.nc.semaphore(name))

    def sbuf(self, name, shape, dtype):
        t = self.stack.enter_context(self.nc.sbuf_tensor(name, list(shape), dtype))
        return Buf(t, name)

    def psum(self, name, shape, dtype):
        t = self.stack.enter_context(self.nc.psum_tensor(name, list(shape), dtype))
        return Buf(t, name)

    def barrier(self):
        toks = [(n, self.E[n].count, self.E[n].sem) for n in CE if self.E[n].count > 0]
        for e in list(self.E.values()) + [self.sp]:
            for tok in toks:
                if tok[0] != e.name:
                    e.wait_tok(tok)
            for tok in self.dma_toks:
                e.wait_tok(tok)
        self.dma_toks = []

    def check(self):
        engs = list(self.E.values()) + [self.sp]
        pc = {e.name: 0 for e in engs}
        sems = {}
        progress = True
        while progress:
            progress = False
            for e in engs:
                while pc[e.name] < len(e.prog):
                    it = e.prog[pc[e.name]]
                    if it[0] == 'wait':
                        if sems.get(id(it[1]), 0) < it[2]:
                            break
                    elif it[0] == 'inst':
                        if it[2]:
                            sems[id(e.sem)] = sems.get(id(e.sem), 0) + 1
                    else:
                        sems[id(it[3])] = sems.get(id(it[3]), 0) + 16
                    pc[e.name] += 1
                    progress = True
        stuck = {}
        for e in engs:
            if pc[e.name] < len(e.prog):
                it = e.prog[pc[e.name]]
                who = [n for n, x in self.E.items() if x.sem is it[1]] or [k for k, r in self.dsem.items() if r[0] is it[1]]
                stuck[e.name] = (pc[e.name], len(e.prog), who, it[2], sems.get(id(it[1]), 0))
        return stuck

    def emit(self):
        st = self.check()
        assert not st, f"DEADLOCK: {st}"
        with self.nc.Block() as block:
            @block.sync
            def _(e):
                self.sp.replay(e)

            @block.tensor
            def _(e):
                self.pe.replay(e)

            @block.scalar
            def _(e):
                self.act.replay(e)

            @block.vector
            def _(e):
                self.dve.replay(e)

            @block.gpsimd
            def _(e):
                self.pool.replay(e)

    def mm(self, out, lhsT, rhs, start=True, stop=True, signal=None, **kw):
        if signal is None:
            signal = stop
        return self.pe.issue(
            lambda e: e.matmul(out.ap, lhsT.ap, rhs.ap, start=start, stop=stop, **kw),
            reads=[lhsT, rhs], writes=[out], signal=signal)

    def transpose(self, out, in_, ident, signal=True):
        return self.pe.issue(
            lambda e: e.transpose(out.ap, in_.ap, ident.ap),
            reads=[in_, ident], writes=[out], signal=signal)

    def activation(self, out, in_, func, scale=1.0, bias=0.0, accum_out=None, eng=None):
        reads = [in_]
        sc = scale
        bi = bias
        if isinstance(scale, V):
            reads.append(scale)
            sc = scale.ap
        if isinstance(bias, V):
            reads.append(bias)
            bi = bias.ap
        writes = [out]
        kw = {}
        if accum_out is not None:
            writes.append(accum_out)
            kw['accum_out'] = accum_out.ap
        return self.act.issue(
            lambda e: e.activation(out=out.ap, in_=in_.ap, func=func, scale=sc, bias=bi, **kw),
            reads=reads, writes=writes)

    def tt(self, eng, out, in0, in1, op):
        return eng.issue(lambda e: e.tensor_tensor(out=out.ap, in0=in0.ap, in1=in1.ap, op=op),
                         reads=[in0, in1], writes=[out])

    def ts(self, eng, out, in0, s1, op0, s2=None, op1=None, accum_out=None):
        reads = [in0]
        a1 = s1
        a2 = s2
        if isinstance(s1, V):
            reads.append(s1)
            a1 = s1.ap
        if isinstance(s2, V):
            reads.append(s2)
            a2 = s2.ap
        kw = {}
        writes = [out]
        if op1 is not None:
            kw['op1'] = op1
        if accum_out is not None:
            kw['accum_out'] = accum_out.ap
            writes.append(accum_out)
        return eng.issue(lambda e: e.tensor_scalar(out=out.ap, in0=in0.ap, scalar1=a1, scalar2=a2, op0=op0, **kw),
                         reads=reads, writes=writes)

    def stt(self, out, in0, scalar, in1, op0, op1):
        reads = [in0, in1]
        a = scalar
        if isinstance(scalar, V):
            reads.append(scalar)
            a = scalar.ap
        return self.dve.issue(
            lambda e: e.scalar_tensor_tensor(out=out.ap, in0=in0.ap, scalar=a, in1=in1.ap, op0=op0, op1=op1),
            reads=reads, writes=[out])

    def copy(self, eng, out, in_):
        if eng is self.act:
            return eng.issue(lambda e: e.copy(out=out.ap, in_=in_.ap), reads=[in_], writes=[out])
        return eng.issue(lambda e: e.tensor_copy(out=out.ap, in_=in_.ap), reads=[in_], writes=[out])

    def memset(self, eng, out, val):
        return eng.issue(lambda e: e.memset(out.ap, val), writes=[out])

D = 1024
S = 2048
DEPTH = 4
NCORES = 8
IN_W = 2720
FH = 2816
OFF_AQ, OFF_AK, OFF_AV = 0, 384, 768
OFF_BQ, OFF_BKV, OFF_BKR = 1152, 1408, 1536
OFF_CQ, OFF_CK, OFF_CV, OFF_CG = 1568, 1760, 1952, 2336
EPS = 1e-6
NEG = -30000.0
SLAB = 209984
FFN_GROUPS = [(0, 6), (6, 6), (12, 5), (17, 5)]


def _t5_bucket(dist):
    n_buckets, max_distance = 32, 2048
    max_exact = n_buckets // 2
    safe = np.maximum(dist, 1).astype(np.float32)
    large = max_exact + (np.log(safe / max_exact) / np.log(max_distance / max_exact) * (n_buckets - max_exact)).astype(np.int32)
    large = np.minimum(large, n_buckets - 1)
    return np.where(dist < max_exact, dist, large).astype(np.int32)


def host_consts(rel_bias):
    f32 = np.float32
    k = np.arange(128)[:, None]
    ql = np.arange(256)[None, :]
    q3 = np.arange(128)[None, :]
    d1 = ql - k
    v1 = (d1 >= 0) & (d1 <= 128)
    i1 = _t5_bucket(np.clip(d1, 0, 128))
    v2 = v1
    i2 = _t5_bucket(np.clip(4 * d1, 0, 512))
    d3 = q3 - k
    v3 = d3 >= 0
    i3 = _t5_bucket(np.clip(16 * d3, 0, 2048))
    bmb = np.zeros((128, 6, 640), f32)
    for h in range(6):
        bmb[:, h, 0:256] = np.where(v1, rel_bias[i1, h], 0.0)
        bmb[:, h, 256:512] = np.where(v2, rel_bias[i2, h], 0.0)
        bmb[:, h, 512:640] = np.where(v3, rel_bias[i3, h], 0.0)
    bmm = np.concatenate([np.where(v1, 0.0, NEG), np.where(v2, 0.0, NEG), np.where(v3, 0.0, NEG)], axis=1).astype(f32)
    cm = np.where(q3 >= k, 0.0, NEG).astype(f32)
    H = 6
    log_g = np.log(1.0 - 2.0 ** (-5.0 - np.arange(H))).astype(f32)
    i = np.arange(128, dtype=f32)
    rel = i[:, None] - i[None, :]
    decay_intra = (np.exp(np.maximum(rel, 0.0)[None] * log_g[:, None, None]) * (rel >= 0)[None]).astype(f32)
    xi = np.exp((i + 1.0)[None, :] * log_g[:, None]).astype(f32)
    zeta = np.exp((128 - 1.0 - i)[None, :] * log_g[:, None]).astype(f32)
    cd = np.exp(128 * log_g).astype(f32)
    s = f32(32 ** -0.5)
    decT = np.zeros((128, 6, 128), f32)
    for h in range(H):
        decT[:, h, :] = decay_intra[h].T * s
    xi_t = np.zeros((64, 3, 128), f32)
    zeta_t = np.zeros((128, 3, 64), f32)
    gam = np.zeros((64, 3), f32)
    for p in range(3):
        for hh in range(2):
            h = 2 * p + hh
            xi_t[hh * 32:(hh + 1) * 32, p, :] = xi[h][None, :] * s
            zeta_t[:, p, hh * 32:(hh + 1) * 32] = zeta[h][:, None]
            gam[hh * 32:(hh + 1) * 32, p] = cd[h]
    half = 16
    inv_freq = (1.0 / (10000.0 ** (np.arange(half, dtype=f32) / half))).astype(f32)
    invf = np.tile(inv_freq, 8).reshape(128, 1).astype(f32)
    return dict(bmb=bmb, bmm=bmm, cm=cm, decT=decT, xi=xi_t, zeta=zeta_t, gam=gam, invf=invf)


class Arena:
    def __init__(self, nc, base, size, name):
        self.nc, self.base, self.size, self.name = nc, base, size, name
        self.off = 0
        self.n = 0

    def reset(self):
        self.off = 0

    def alloc(self, fw, name, shape, dtype):
        nbytes = int(np.prod(shape[1:])) * mybir.dt.size(dtype)
        nbytes = (nbytes + 31) // 32 * 32
        assert self.off + nbytes <= self.size, (self.name, name, self.off, nbytes, self.size)
        self.n += 1
        t = self.nc.alloc_sbuf_tensor_at(f"{self.name}_{name}_{self.n}", list(shape), dtype, offset=self.base + self.off)
        self.off += nbytes
        return Buf(t, f"{self.name}_{name}_{self.n}", semname=f"{self.name}_{name}")


def build(depth=DEPTH, dbg=None, skip=()):
    nc = bass.Bass("TRN2", target_bir_lowering=False)
    dbg = dbg or []
    dbg_out = {}

    def din(name, shape, dt=F32):
        return nc.dram_tensor(name, list(shape), dt, kind="ExternalInput").ap()

    x_d = din("x", [S, D])
    cT_d = din("cT", [128, 8])
    pos_d = din("posb", [1, S], I32)
    invf_d = din("invf", [128, 1])
    bmb_d = din("bmb", [128, 6, 640])
    bmm_d = din("bmm", [128, 640])
    cm_d = din("cm", [128, 128])
    decT_d = din("decT", [128, 6, 128])
    xi_d = din("xi", [64, 3, 128])
    zeta_d = din("zeta", [128, 3, 64])
    gam_d = din("gam", [64, 3])
    adab_d = din("adabT", [128, DEPTH, 48])
    n1g_d = din("n1gT", [128, DEPTH, 8])
    n2g_d = din("n2gT", [128, DEPTH, 8])
    fng_d = din("fngT", [128, 8])
    mg_d = din("mgT", [128, DEPTH, 8])
    mqn_d = din("mqnT", [128, DEPTH, 2])
    mkvn_d = din("mkvnT", [128, DEPTH, 1])
    if depth > 0:
        adaw_d = din("ada_w", [DEPTH, D, 6 * D])
        win_d = din("w_in", [DEPTH, D, IN_W])
        wuq_d = din("w_uq", [DEPTH, 256, 384])
        wukv_d = din("w_ukv", [DEPTH, 128, 512])
        wout_d = din("w_out", [DEPTH, D, D])
        wg_d = din("w_gate", [DEPTH, D, FH])
        wu_d = din("w_up", [DEPTH, D, FH])
        wd_d = din("w_down", [DEPTH, FH, D])
        xsp_d = nc.dram_tensor("xspill", [128, 8, S], F32, kind="Internal").ap()
    y_d = nc.dram_tensor("y", [S, D], F32, kind="ExternalOutput").ap()

    st = ExitStack()
    with st:
        fw = FW(nc, st)
        pe, act, dve, pool, sp = fw.pe, fw.act, fw.dve, fw.pool, fw.sp
        base0 = (nc.sbuf_base + 31) // 32 * 32
        st.enter_context(nc.sbuf_tensor("slab", [128, SLAB], mybir.dt.uint8))
        assert nc.sbuf_base == base0 + SLAB, (nc.sbuf_base, base0)
        o = base0
        SZ_HT, SZ_X, SZ_Y, SZ_ADAW, SZ_W = 32768, 65536, 32768, 12288, 36864
        SZ_C = SLAB - (SZ_HT + SZ_X + SZ_Y + SZ_ADAW + SZ_W)
        AC = Arena(nc, o, SZ_C, "c"); o += SZ_C
        AH = Arena(nc, o, SZ_HT, "h"); o += SZ_HT
        AX = Arena(nc, o, SZ_X, "x"); o += SZ_X
        AY = Arena(nc, o, SZ_Y, "y"); o += SZ_Y
        AA = Arena(nc, o, SZ_ADAW, "a"); o += SZ_ADAW
        AW = Arena(nc, o, SZ_W, "w"); o += SZ_W

        PS = [fw.psum(f"ps{i}", [128, 512], F32) for i in range(8)]

        def sub(buf, name):
            return Buf(buf.t, name)

        ident_f = AC.alloc(fw, "identf", [128, 128], F32)
        ident_b = AC.alloc(fw, "identb", [128, 128], BF16)
        ones_b = AC.alloc(fw, "onesb", [128, 128], BF16)
        BD = AC.alloc(fw, "bd", [128, 128], BF16)
        WNe = AC.alloc(fw, "wne", [128, 64], BF16)
        WNo = AC.alloc(fw, "wno", [128, 64], BF16)
        FCOS = AC.alloc(fw, "fcos", [128, S], BF16)
        FSIN = AC.alloc(fw, "fsin", [128, S], BF16)
        BM = AC.alloc(fw, "bm", [128, 6, 640], BF16)
        CM = AC.alloc(fw, "cm", [128, 128], F32)
        DECT = AC.alloc(fw, "dect", [128, 6, 128], F32)
        XI = AC.alloc(fw, "xi", [64, 3, 128], F32)
        ZETA = AC.alloc(fw, "zeta", [128, 3, 64], F32)
        GAM = AC.alloc(fw, "gam", [64, 3], F32)
        MOD = AC.alloc(fw, "mod", [128, DEPTH, 48], F32)
        A1 = AC.alloc(fw, "a1", [128, DEPTH, 8], F32)
        A2 = AC.alloc(fw, "a2", [128, DEPTH, 8], F32)
        ADAB = AC.alloc(fw, "adab", [128, DEPTH, 48], F32)
        N1G = AC.alloc(fw, "n1g", [128, DEPTH, 8], F32)
        N2G = AC.alloc(fw, "n2g", [128, DEPTH, 8], F32)
        FNG = AC.alloc(fw, "fng", [128, 8], F32)
        MG = AC.alloc(fw, "mg", [128, DEPTH, 8], F32)
        MQN = AC.alloc(fw, "mqn", [128, DEPTH, 2], F32)
        MKVN = AC.alloc(fw, "mkvn", [128, DEPTH, 1], F32)
        CTF = AC.alloc(fw, "ctf", [128, 8], F32)
        CTB = AC.alloc(fw, "ctb", [128, 8], BF16)
        INVF = AC.alloc(fw, "invf", [128, 1], F32)
        hT = AH.alloc(fw, "hT", [128, 8, S], BF16)
        adaw = [AA.alloc(fw, f"adaw{i}", [128, 3072], BF16) for i in range(2)]

        def dump(name, view, shape, dt):
            if name not in dbg:
                return
            t = nc.dram_tensor("dbg_" + name, list(shape), dt, kind="ExternalOutput").ap()
            dbg_out[name] = sp.dma(t, view)
            fw.dma_toks.append(dbg_out[name])

        fw.memset(dve, ident_f[:], 1.0)
        pool.issue(lambda e: e.affine_select(out=ident_f.t[:], in_=ident_f.t[:], pattern=[[-1, 128]],
                                             compare_op=ALU.is_equal, fill=0.0, base=0, channel_multiplier=1),
                   reads=[ident_f[:]], writes=[ident_f[:]])
        fw.copy(dve, ident_b[:], ident_f[:])
        fw.memset(dve, ones_b[:], 1.0)
        fw.memset(dve, BD[:], 0.0)
        fw.memset(dve, BD[0:64, 0:64], 1.0 / 64)
        fw.memset(dve, BD[64:128, 64:128], 1.0 / 64)
        fw.memset(dve, WNe[:], 0.0)
        fw.memset(dve, WNe[0:64, :], 1.0)
        if 'wn' not in skip:
            fw.memset(dve, WNe[64:65, :], 64 * EPS)
        fw.memset(dve, WNo[:], 0.0)
        fw.memset(dve, WNo[64:128, :], 1.0)
        if 'wn' not in skip:
            fw.memset(dve, WNo[0:1, :], 64 * EPS)
        for (b, d_) in [] if 'small' in skip else [(CM, cm_d), (DECT, decT_d), (XI, xi_d), (ZETA, zeta_d), (GAM, gam_d), (ADAB, adab_d),
                        (N1G, n1g_d), (N2G, n2g_d), (FNG, fng_d), (MG, mg_d), (MQN, mqn_d), (MKVN, mkvn_d),
                        (CTF, cT_d), (INVF, invf_d)]:
            sp.dma(b[:], d_)
        if 'silu' not in skip:
            fw.activation(CTB[:], CTF[:], AF.Silu)
        AY.reset()
        bmb_s = AY.alloc(fw, "bmb", [128, 6, 640], F32)
        bmm_s = AY.alloc(fw, "bmm", [128, 640], F32)
        if 'bm' not in skip:
            sp.dma(bmb_s[:], bmb_d)
            sp.dma(bmm_s[:], bmm_d)
            for h in range(6):
                fw.tt(dve, BM[:, h, :], bmb_s[:, h, :], bmm_s[:], ALU.add)
        fw.barrier()
        AY.reset()
        posi = AY.alloc(fw, "posi", [128, S], I32)
        ang = AY.alloc(fw, "ang", [128, S], F32)
        t1 = AY.alloc(fw, "t1", [128, S], F32)
        t2 = AY.alloc(fw, "t2", [128, S], F32)
        if 'tables' not in skip:
            sp.dma(posi[:], pos_d.partition_broadcast(128))
            fw.copy(dve, t1[:], posi[:])
            fw.ts(dve, ang[:], t1[:], INVF[:, 0:1], ALU.mult)
            TWO_PI = 2.0 * np.pi
            C1 = 6.28125
            C2 = float(np.float32(TWO_PI - C1))
            fw.ts(dve, t1[:], ang[:], 1.0 / TWO_PI, ALU.mult)
            fw.copy(dve, posi[:], t1[:])
            fw.copy(dve, t1[:], posi[:])
            fw.stt(t2[:], t1[:], -C1, ang[:], ALU.mult, ALU.add)
            fw.stt(t2[:], t1[:], -C2, t2[:], ALU.mult, ALU.add)

            def wrap(dst, src, tmp):
                fw.ts(dve, tmp, src, float(np.pi), ALU.is_gt)
                fw.stt(dst, tmp, -TWO_PI, src, ALU.mult, ALU.add)
                fw.ts(dve, tmp, dst, -float(np.pi), ALU.is_lt)
                fw.stt(dst, tmp, TWO_PI, dst, ALU.mult, ALU.add)
                fw.ts(dve, dst, dst, 3.1415925, ALU.min, -3.1415925, ALU.max)

            wrap(t2[:], t2[:], t1[:])
            fw.activation(FSIN[:], t2[:], AF.Sin)
            fw.ts(dve, ang[:], t2[:], float(np.pi / 2), ALU.add)
            wrap(ang[:], ang[:], t1[:])
            fw.activation(FCOS[:], ang[:], AF.Sin)
        fw.barrier()

        AX.reset()
        xT = AX.alloc(fw, "xT", [128, 8, S], F32)
        AW.reset()
        io = [AW.alloc(fw, f"io{i}", [128, D], F32) for i in range(2)]
        for n in range(16):
            sp.dma(io[n % 2][:], x_d[n * 128:(n + 1) * 128, :])
            for g in range(2):
                pb = PS[(2 * n + g) % 4]
                for j in range(4):
                    kc = 4 * g + j
                    fw.transpose(pb[:, j * 128:(j + 1) * 128], io[n % 2][:, kc * 128:(kc + 1) * 128], ident_f[:], signal=(j == 3))
                src = pb.v(pb.t[:, :].rearrange("p (j t) -> p j t", j=4))
                dst = xT.v(xT.t[:, 4 * g:4 * g + 4, n * 128:(n + 1) * 128])
                fw.copy(act if (n + g) % 2 else dve, dst, src)
        fw.barrier()

        def mod_pieces(l):
            mp = PS[7]
            for piece in range(16):
                kc, half = piece // 2, piece % 2
                slot = adaw[piece % 2]
                pool.dma(slot[:], adaw_d[l, kc * 128:(kc + 1) * 128, half * 3072:(half + 1) * 3072], max_dma_last_dim=4096)
                for j in range(24):
                    jj = half * 24 + j
                    fw.mm(mp[:, jj:jj + 1], slot[:, j * 128:(j + 1) * 128], CTB[:, kc:kc + 1],
                          start=(piece == 0 and j == 0), stop=(kc == 7), signal=(j == 23), skip_group_check=True)
                yield
            fw.tt(dve, MOD[:, l, :], mp[:, 0:48], ADAB[:, l, :], ALU.add)
            fw.stt(A1[:, l, :], MOD[:, l, 8:16], 1.0, N1G[:, l, :], ALU.add, ALU.mult)
            fw.stt(A2[:, l, :], MOD[:, l, 32:40], 1.0, N2G[:, l, :], ALU.add, ALU.mult)
            yield

        EPSC = AC.alloc(fw, "epsc", [128, 1], F32)
        fw.memset(dve, EPSC[:], EPS)

        def norm_to_hT(l, a_view, b_col0, ar):
            ar.reset()
            sq = [ar.alloc(fw, f"nsq{i}", [128, 8, 512], BF16) for i in range(2)]
            lnt = ar.alloc(fw, "nln", [128, 512], F32)
            rs = [ar.alloc(fw, f"nrs{i}", [128, 512], F32) for i in range(2)]
            tm = [ar.alloc(fw, f"ntm{i}", [128, 512], F32) for i in range(3)]
            for tb in range(4):
                ts_ = slice(tb * 512, (tb + 1) * 512)
                fw.activation(sq[tb % 2][:], xT[:, :, ts_], AF.Square)
                pb = PS[tb % 2]
                for kc in range(8):
                    fw.mm(pb[:], ones_b[:], sq[tb % 2][:, kc, :], start=(kc == 0), stop=(kc == 7))
                fw.activation(lnt[:], pb[:], AF.Ln, scale=1.0 / D, bias=EPSC[:, 0:1])
                fw.activation(rs[tb % 2][:], lnt[:], AF.Exp, scale=-0.5)
                for kc in range(8):
                    t = tm[kc % 3]
                    fw.tt(dve, t[:], xT[:, kc, ts_], rs[tb % 2][:], ALU.mult)
                    fw.activation(hT[:, kc, ts_], t[:], AF.Identity, scale=a_view(kc), bias=MOD[:, l, b_col0 + kc:b_col0 + kc + 1])

        def load_w(buf_view, dram_ap):
            pool.dma(buf_view, dram_ap, max_dma_last_dim=4096)

        def proj_fm(out_fn, w_fn, M, nk=8, rhs_fn=None, banks=(6, 7), out_p0=0):
            for tb in range(4):
                pb = PS[banks[tb % len(banks)]]
                ts_ = slice(tb * 512, (tb + 1) * 512)
                pv = pb[out_p0:out_p0 + M, :]
                for kc in range(nk):
                    rhs = rhs_fn(kc, ts_) if rhs_fn else hT[:, kc, ts_]
                    fw.mm(pv, w_fn(kc), rhs, start=(kc == 0), stop=(kc == nk - 1))
                out_fn(tb, ts_, pv)

        def finalize_head(bank, nr, mixb, chunk, tb, fin):
            SQb, LNb, RSb = fin
            ts_ = slice(tb * 512, (tb + 1) * 512)
            fw.activation(SQb[:], bank[:], AF.Square)
            pf = PS[6 + (tb % 2)]
            wn = WNe if nr == 0 else WNo
            fw.mm(pf[nr:nr + 64, :], wn[:], SQb[:], start=True, stop=True)
            fw.activation(LNb[nr:nr + 64, :], pf[nr:nr + 64, :], AF.Ln, scale=1.0 / 64)
            fw.activation(RSb[nr:nr + 64, :], LNb[nr:nr + 64, :], AF.Exp, scale=-0.5)
            fw.tt(dve, mixb[nr:nr + 64, chunk, ts_], bank[nr:nr + 64, :], RSb[nr:nr + 64, :], ALU.mult)

        def v_lhsT(vbuf, tile_off, ones_off, even):
            t = vbuf.t
            pstride = t[:, :].ap[0][0]
            if even:
                return vbuf.v(bass.AP(t, tile_off, [[pstride, 128], [ones_off - tile_off, 2], [1, 64]]))
            return vbuf.v(bass.AP(t, ones_off, [[pstride, 128], [tile_off - ones_off, 2], [1, 64]]))

        for l in range(depth):
            if l == 0:
                for _ in mod_pieces(0):
                    pass
            AY.reset()
            norm_to_hT(l, lambda kc: A1[:, l, kc:kc + 1], 0, AY)
            dump(f"hT{l}", hT[:], [128, 8, S], BF16)
            for kc in range(8):
                sp.dma(xsp_d[:, kc, :], xT[:, kc, :])
            fw.barrier()
            _rec = fw.dsem[xT.semname]
            spill_tok = (("d", xT.semname), 16 * _rec[1], _rec[0])
            for e in (pe, act, dve, pool):
                e.wait_tok(spill_tok)

            AY.reset()
            mixT = AY.alloc(fw, "mixT", [128, 8, S], BF16)

            AX.reset()
            qT = AX.alloc(fw, "qT", [128, S], BF16)
            kT = AX.alloc(fw, "kT", [128, S], BF16)
            VA = AX.alloc(fw, "VA", [128, 48, 2, 128], BF16)
            PT = [AX.alloc(fw, f"pt{i}", [128, 256], BF16) for i in range(6)]
            TS = [AX.alloc(fw, f"ts{i}", [128, 256], F32) for i in range(6)]
            fin = (AX.alloc(fw, "fsq", [128, 512], BF16), AX.alloc(fw, "fln", [128, 512], F32), AX.alloc(fw, "frs", [128, 512], F32))
            AW.reset()
            wq = AW.alloc(fw, "wq", [128, 8, 128], BF16)
            wk = AW.alloc(fw, "wk", [128, 8, 128], BF16)
            wv = AW.alloc(fw, "wv", [128, 8, 128], BF16)
            SS = [sub(PS[4], "ss0"), sub(PS[4], "ss1"), sub(PS[5], "ss2"), sub(PS[5], "ss3")]
            fw.memset(pool, VA[:, :, 0, 64:128], 1.0)
            fw.memset(pool, VA[:, :, 1, 0:64], 1.0)
            winv = win_d[l].rearrange("(k p) n -> p k n", p=128)
            blk = [0]
            for c in (range(3) if 'A' not in skip else []):
                load_w(wq[:], winv[:, :, OFF_AQ + c * 128:OFF_AQ + (c + 1) * 128])
                load_w(wk[:], winv[:, :, OFF_AK + c * 128:OFF_AK + (c + 1) * 128])
                load_w(wv[:], winv[:, :, OFF_AV + c * 128:OFF_AV + (c + 1) * 128])
                proj_fm(lambda tb, ts_, pv: fw.activation(qT[:, ts_], pv, AF.Identity, scale=0.125),
                        lambda kc: wq[:, kc, :], 128)
                proj_fm(lambda tb, ts_, pv: fw.copy(dve, kT[:, ts_], pv), lambda kc: wk[:, kc, :], 128)
                def tok_ap(ordr, tile, kc):
                    if ordr == 0:
                        return hT[:, kc, tile * 128:(tile + 1) * 128]
                    if ordr == 1:
                        r, b = tile // 4, tile % 4
                        return hT[:, kc, 512 * b + r:512 * (b + 1):4]
                    return hT[:, kc, tile::16]
                for ordr in range(3):
                    for tg in range(4):
                        pb = PS[6 + (tg % 2)]
                        for tt_ in range(4):
                            tile = tg * 4 + tt_
                            for kc in range(8):
                                fw.mm(pb[:, tt_ * 128:(tt_ + 1) * 128], tok_ap(ordr, tile, kc), wv[:, kc, :],
                                      start=(kc == 0), stop=(kc == 7), signal=(kc == 7 and tt_ == 3))
                        t0_ = ordr * 16 + tg * 4
                        pv4 = pb.t[:, :].rearrange("p (n c) -> p n c", n=4)
                        ev_ = act if tg % 2 else dve
                        fw.copy(ev_, VA[:, t0_:t0_ + 4, 0, 0:64], pb.v(pv4[:, :, 0:64]))
                        fw.copy(ev_, VA[:, t0_:t0_ + 4, 1, 64:128], pb.v(pv4[:, :, 64:128]))
                if c == 0:
                    dump(f"qTA{l}", qT[:], [128, S], BF16)
                    dump(f"kTA{l}", kT[:], [128, S], BF16)
                    dump(f"VA{l}", VA[:], [128, 48, 2, 128], BF16)
                for hh in range(2):
                    h = 2 * c + hh
                    ro = 64 * hh
                    started = [False] * 4

                    def sblock(kcols, qcols, nq, bm_view, pv_list):
                        i = blk[0]
                        blk[0] += 1
                        ss = PS[4 + i % 4]
                        ssv = ss[:, 0:nq]
                        fw.mm(ssv, kcols, qcols, start=True, stop=True)
                        tsb = TS[i % 6]
                        fw.tt(dve, tsb[:, 0:nq], ssv, bm_view, ALU.add)
                        pt = PT[i % 6]
                        fw.activation(pt[:, 0:nq], tsb[:, 0:nq], AF.Exp)
                        for (qs, vtile, tbk, ocols) in pv_list:
                            lhs = VA[:, vtile, hh, :]
                            fw.mm(ocols, lhs, pt[:, qs], start=(not started[tbk]), stop=False, signal=True, skip_group_check=True)
                            started[tbk] = True

                    for kt in range(16):
                        nqt = 2 if kt < 15 else 1
                        pvl = []
                        for j in range(nqt):
                            qt = kt + j
                            pvl.append((slice(j * 128, (j + 1) * 128), 0 * 16 + kt, qt // 4,
                                        PS[qt // 4][:, (qt % 4) * 128:(qt % 4 + 1) * 128]))
                        sblock(kT[ro:ro + 64, kt * 128:(kt + 1) * 128], qT[ro:ro + 64, kt * 128:kt * 128 + nqt * 128],
                               nqt * 128, BM[:, h, 0:nqt * 128], pvl)
                    for r in range(4):
                        for b in range(4):
                            nqt = 2 if b < 3 else 1
                            pvl = []
                            for j in range(nqt):
                                pvl.append((slice(j * 128, (j + 1) * 128), 16 + r * 4 + b, b + j,
                                            PS[b + j][:, r::4]))
                            sblock(kT[ro:ro + 64, 512 * b + r:512 * (b + 1):4],
                                   qT[ro:ro + 64, 512 * b + r:512 * (b + nqt):4],
                                   nqt * 128, BM[:, h, 256:256 + nqt * 128], pvl)
                    for r in range(16):
                        pvl = []
                        for tbk in range(4):
                            pvl.append((slice(tbk * 32, (tbk + 1) * 32), 32 + r, tbk, PS[tbk][:, r::16]))
                        sblock(kT[ro:ro + 64, r::16], qT[ro:ro + 64, r::16], 128, BM[:, h, 512:640], pvl)
                    for tb in range(4):
                        finalize_head(PS[tb], ro, mixT, c, tb, fin)
            dump(f"mixA{l}", mixT[:, 0:3, :], [128, 3, S], BF16)
            fw.barrier()

            AX.reset()
            qlatT = AX.alloc(fw, "qlatT", [128, 2, S], BF16)
            kvlatT = AX.alloc(fw, "kvlatT", [128, S], BF16)
            sqb = [AX.alloc(fw, "sqb0", [128, 2, 512], BF16)] * 2
            rsq = AX.alloc(fw, "rsq", [128, S], F32)
            rskv = AX.alloc(fw, "rskv", [128, S], F32)
            rstok = AX.alloc(fw, "rstok", [128, 16], F32)
            qTh = AX.alloc(fw, "qTh", [128, S], BF16)
            KTh = AX.alloc(fw, "KTh", [128, S], BF16)
            VBs = [AX.alloc(fw, f"VB{i}", [128, 16, 128], BF16) for i in range(2)]
            PTB = [AX.alloc(fw, f"ptb{i}", [128, 512], BF16) for i in range(3)]
            TSB = [AX.alloc(fw, f"tsb{i}", [128, 128], F32) for i in range(2)]
            RT = [AX.alloc(fw, f"rt{i}", [128, 512], F32) for i in range(3)]
            fin = (AX.alloc(fw, "fsq", [128, 512], BF16), AX.alloc(fw, "fln", [128, 512], F32), AX.alloc(fw, "frs", [128, 512], F32))
            AW.reset()
            winB = AW.alloc(fw, "winB", [128, 8, 416], BF16)
            wkrr = AW.alloc(fw, "wkrr", [128, 8, 32], BF16)
            wuq_f = AW.alloc(fw, "wuqf", [128, 2, 384], F32)
            wuq_b = AW.alloc(fw, "wuqb", [128, 2, 384], BF16)
            wuq_r = AW.alloc(fw, "wuqr", [128, 2, 4, 32], BF16)
            wukv_f = AW.alloc(fw, "wukvf", [128, 512], F32)
            wukv_b = AW.alloc(fw, "wukvb", [128, 512], BF16)
            fw.memset(pool, VBs[0][:, :, 64:128], 1.0)
            fw.memset(pool, VBs[1][:, :, 0:64], 1.0)
            load_w(winB[:], winv[:, :, OFF_BQ:OFF_BQ + 416])
            pool.dma_group([(wkrr[:, :, 0:16], winv[:, :, OFF_BKR + 16:OFF_BKR + 32]),
                            (wkrr[:, :, 16:32], winv[:, :, OFF_BKR:OFF_BKR + 16])], max_dma_last_dim=4096)
            fw.activation(wkrr[:, :, 0:16], wkrr[:, :, 0:16], AF.Identity, scale=-1.0)
            sp.dma(wuq_f[:], wuq_d[l].rearrange("(k p) n -> p k n", p=128))
            sp.dma(wukv_f[:], wukv_d[l])
            for kc in range(2):
                fw.activation(wuq_b[:, kc, :], wuq_f[:, kc, :], AF.Identity, scale=MQN[:, l, kc:kc + 1])
            fw.activation(wukv_b[:], wukv_f[:], AF.Identity, scale=MKVN[:, l, 0:1])
            wq4 = wuq_b.t[:, :, :].rearrange("p k (h e) -> p k h e", h=4)
            fw.activation(wuq_r.v(wuq_r.t[:, :, :, 0:16]), wuq_b.v(wq4[:, :, :, 80:96]), AF.Identity, scale=-1.0)
            fw.copy(dve, wuq_r.v(wuq_r.t[:, :, :, 16:32]), wuq_b.v(wq4[:, :, :, 64:80]))

            for c2 in range(2):
                proj_fm(lambda tb, ts_, pv, c2=c2: fw.copy(dve, qlatT[:, c2, ts_], pv),
                        lambda kc, c2=c2: winB[:, kc, c2 * 128:(c2 + 1) * 128], 128)
            for tb in range(4):
                ts_ = slice(tb * 512, (tb + 1) * 512)
                fw.activation(sqb[tb % 2][:], qlatT[:, :, ts_], AF.Square)
                pb = PS[4 + tb % 2]
                for c2 in range(2):
                    fw.mm(pb[:], ones_b[:], sqb[tb % 2][:, c2, :], start=(c2 == 0), stop=(c2 == 1))
                fw.activation(RT[0][:], pb[:], AF.Ln, scale=1.0 / 256, bias=EPSC[:, 0:1])
                fw.activation(rsq[:, ts_], RT[0][:], AF.Exp, scale=-0.5)
            proj_fm(lambda tb, ts_, pv: fw.copy(dve, kvlatT[:, ts_], pv), lambda kc: winB[:, kc, 256:384], 128)
            for tb in range(4):
                ts_ = slice(tb * 512, (tb + 1) * 512)
                fw.activation(sqb[tb % 2][:, 0, :], kvlatT[:, ts_], AF.Square)
                pb = PS[4 + tb % 2]
                fw.mm(pb[:], ones_b[:], sqb[tb % 2][:, 0, :], start=True, stop=True)
                fw.activation(RT[0][:], pb[:], AF.Ln, scale=1.0 / 128, bias=EPSC[:, 0:1])
                fw.activation(rskv[:, ts_], RT[0][:], AF.Exp, scale=-0.5)
            identb4 = ident_f.v(bass.AP(ident_f.t, 0, [[128, 128], [0, 4], [1, 128]]))
            for g4 in range(4):
                tdv = RT[0].v(RT[0].t[:, :].rearrange("p (n q) -> p n q", n=4))
                fw.tt(dve, tdv, rskv.v(rskv.t[:, g4 * 512:(g4 + 1) * 512].rearrange("p (n q) -> p n q", n=4)), identb4, ALU.mult)
                _o = rstok.t[:, g4 * 4:(g4 + 1) * 4]
                _i = RT[0].t[:, :].rearrange("p (n q) -> p n q", n=4)
                dve.issue(lambda e, _o=_o, _i=_i: e.tensor_reduce(out=_o, in_=_i, axis=AX_X, op=ALU.add),
                          reads=[RT[0][:]], writes=[rstok[:]])
            for tb in range(4):
                ts_ = slice(tb * 512, (tb + 1) * 512)
                p1, p2 = PS[6], PS[7]
                for kc in range(8):
                    fw.mm(p1[64:96, :], winB[:, kc, 384:416], hT[:, kc, ts_], start=(kc == 0), stop=(kc == 7))
                for kc in range(8):
                    fw.mm(p2[64:96, :], wkrr[:, kc, :], hT[:, kc, ts_], start=(kc == 0), stop=(kc == 7))
                fw.tt(dve, RT[0][64:96, :], p1[64:96, :], FCOS[64:96, ts_], ALU.mult)
                fw.tt(dve, RT[1][64:96, :], p2[64:96, :], FSIN[64:96, ts_], ALU.mult)
                fw.tt(pool, KTh[64:96, ts_], RT[0][64:96, :], RT[1][64:96, :], ALU.add)
            dump(f"rsq{l}", rsq[:], [128, S], F32)
            SCB = float((64 + 32) ** -0.5)
            for h in (range(4) if 'B' not in skip else []):
                hh = h % 2
                nr = 64 * hh
                for tb in range(4):
                    ts_ = slice(tb * 512, (tb + 1) * 512)
                    p1, p2 = PS[6], PS[7]
                    for kc in range(2):
                        fw.mm(p1[0:96, :], wuq_b[:, kc, h * 96:(h + 1) * 96], qlatT[:, kc, ts_], start=(kc == 0), stop=(kc == 1))
                    for kc in range(2):
                        fw.mm(p2[64:96, :], wuq_r.v(wuq_r.t[:, kc, h, :]), qlatT[:, kc, ts_], start=(kc == 0), stop=(kc == 1))
                    fw.stt(qTh[0:64, ts_], p1[0:64, :], SCB, rsq[0:64, ts_], ALU.mult, ALU.mult)
                    fw.tt(dve, RT[0][64:96, :], p1[64:96, :], FCOS[64:96, ts_], ALU.mult)
                    fw.tt(dve, RT[1][64:96, :], p2[64:96, :], FSIN[64:96, ts_], ALU.mult)
                    fw.tt(pool, RT[2][64:96, :], RT[0][64:96, :], RT[1][64:96, :], ALU.add)
                    fw.stt(qTh[64:96, ts_], RT[2][64:96, :], SCB, rsq[64:96, ts_], ALU.mult, ALU.mult)
                    p3 = PS[4 + tb % 2]
                    fw.mm(p3[0:64, :], wukv_b[:, h * 128:h * 128 + 64], kvlatT[:, ts_], start=True, stop=True)
                    fw.tt(dve, KTh[0:64, ts_], p3[0:64, :], rskv[0:64, ts_], ALU.mult)
                for tg in range(2):
                    pb = PS[4 + tg]
                    for t8 in range(8):
                        tile = tg * 8 + t8
                        fw.mm(pb[:, t8 * 64:(t8 + 1) * 64], kvlatT[:, tile * 128:(tile + 1) * 128],
                              wukv_b[:, h * 128 + 64:(h + 1) * 128], start=True, stop=True, signal=(t8 == 7))
                    for t8 in range(8):
                        tile = tg * 8 + t8
                        fw.activation(VBs[hh][:, tile, 64 * hh:64 * hh + 64], pb[:, t8 * 64:(t8 + 1) * 64], AF.Identity,
                                      scale=rstok[:, tile:tile + 1])
                if h == 0:
                    dump(f"qTh{l}", qTh[0:96, :], [96, S], BF16)
                    dump(f"KTh{l}", KTh[0:96, :], [96, S], BF16)
                    dump(f"VB{l}", VBs[0][:], [128, 16, 128], BF16)
                started = [False] * 4
                bi = 0
                for kt in range(16):
                    for tb in range(kt // 4, 4):
                        q0 = max(128 * kt, 512 * tb)
                        q1 = 512 * (tb + 1)
                        nq = q1 - q0
                        sbk = PS[4 + bi % 2]
                        pt = PTB[bi % 3]
                        bi += 1
                        fw.mm(sbk[:, 0:nq], KTh[0:96, kt * 128:(kt + 1) * 128], qTh[0:96, q0:q1], start=True, stop=True)
                        if q0 == 128 * kt:
                            tsb = TSB[kt % 2]
                            fw.tt(dve, tsb[:], sbk[:, 0:128], CM[:], ALU.add)
                            fw.activation(pt[:, 0:128], tsb[:], AF.Exp)
                            if nq > 128:
                                fw.activation(pt[:, 128:nq], sbk[:, 128:nq], AF.Exp)
                        else:
                            fw.activation(pt[:, 0:nq], sbk[:, 0:nq], AF.Exp)
                        lhs = VBs[hh][:, kt, :]
                        fw.mm(PS[tb][:, q0 - 512 * tb:q1 - 512 * tb], lhs, pt[:, 0:nq], start=(not started[tb]), stop=False,
                              signal=True, skip_group_check=True)
                        started[tb] = True
                for tb in range(4):
                    finalize_head(PS[tb], nr, mixT, 3 + h // 2, tb, fin)
            dump(f"mixB{l}", mixT[:, 3:5, :], [128, 2, S], BF16)
            fw.barrier()

            AX.reset()
            qTc = AX.alloc(fw, "qTc", [128, S], BF16)
            kTc = AX.alloc(fw, "kTc", [128, S], BF16)
            qxT = AX.alloc(fw, "qxT", [128, S], BF16)
            kz = AX.alloc(fw, "kz", [128, 16, 64], BF16)
            VC = AX.alloc(fw, "VC", [128, 16, 128], BF16)
            gT = AX.alloc(fw, "gT", [128, S], BF16)
            STf = AX.alloc(fw, "stf", [64, 16, 64], F32)
            STb = AX.alloc(fw, "stb", [64, 16, 64], BF16)
            PTC = [AX.alloc(fw, f"ptc{i}", [128, 128], BF16) for i in range(4)]
            RT = [AX.alloc(fw, f"rtc{i}", [128, 512], F32) for i in range(3)]
            CP = AX.alloc(fw, "ccp", [128, 512], BF16)
            CSQ = AX.alloc(fw, "csq", [128, 512], BF16)
            CMEAN = AX.alloc(fw, "cmean", [128, 512], F32)
            CMSQ = AX.alloc(fw, "cmsq", [128, 512], F32)
            CDV = AX.alloc(fw, "cdv", [128, 512], F32)
            CVAR = AX.alloc(fw, "cvar", [128, 512], F32)
            CRS = AX.alloc(fw, "crs", [128, 512], F32)
            AW.reset()
            wcq = AW.alloc(fw, "wcq", [128, 8, 64], BF16)
            wcqr = AW.alloc(fw, "wcqr", [128, 8, 2, 32], BF16)
            wck = AW.alloc(fw, "wck", [128, 8, 64], BF16)
            wckr = AW.alloc(fw, "wckr", [128, 8, 2, 32], BF16)
            wcv = AW.alloc(fw, "wcv", [128, 8, 128], BF16)
            wcg = AW.alloc(fw, "wcg", [128, 8, 128], BF16)
            SC_ = [sub(PS[4], "sc0"), sub(PS[4], "sc1"), sub(PS[5], "sc2"), sub(PS[5], "sc3")]
            for c in (range(3) if 'C' not in skip else []):
                def load_rot(dst, off):
                    prs = []
                    for h2 in range(2):
                        prs.append((dst.v(dst.t[:, :, h2, 0:16]), winv[:, :, off + h2 * 32 + 16:off + h2 * 32 + 32]))
                        prs.append((dst.v(dst.t[:, :, h2, 16:32]), winv[:, :, off + h2 * 32:off + h2 * 32 + 16]))
                    pool.dma_group(prs, max_dma_last_dim=4096)
                    fw.activation(dst.v(dst.t[:, :, :, 0:16]), dst.v(dst.t[:, :, :, 0:16]), AF.Identity, scale=-1.0)
                load_w(wcq[:], winv[:, :, OFF_CQ + c * 64:OFF_CQ + (c + 1) * 64])
                load_rot(wcqr, OFF_CQ + c * 64)
                load_w(wck[:], winv[:, :, OFF_CK + c * 64:OFF_CK + (c + 1) * 64])
                load_rot(wckr, OFF_CK + c * 64)
                load_w(wcv[:], winv[:, :, OFF_CV + c * 128:OFF_CV + (c + 1) * 128])
                load_w(wcg[:], winv[:, :, OFF_CG + c * 128:OFF_CG + (c + 1) * 128])
                for (w_, wr_, dst, isq) in [(wcq, wcqr, qTc, True), (wck, wckr, kTc, False)]:
                    for tb in range(4):
                        ts_ = slice(tb * 512, (tb + 1) * 512)
                        p1, p2 = PS[6], PS[7]
                        for kc in range(8):
                            fw.mm(p1[0:64, :], w_[:, kc, :], hT[:, kc, ts_], start=(kc == 0), stop=(kc == 7))
                        for kc in range(8):
                            fw.mm(p2[0:64, :], wr_.v(wr_.t[:, kc, :, :].rearrange("p h e -> p (h e)")), hT[:, kc, ts_],
                                  start=(kc == 0), stop=(kc == 7))
                        fw.tt(dve, RT[0][0:64, :], p1[0:64, :], FCOS[0:64, ts_], ALU.mult)
                        fw.tt(dve, RT[1][0:64, :], p2[0:64, :], FSIN[0:64, ts_], ALU.mult)
                        if isq:
                            fw.tt(pool, RT[2][0:64, :], RT[0][0:64, :], RT[1][0:64, :], ALU.add)
                            fw.copy(act, dst[0:64, ts_], RT[2][0:64, :])
                            xib = XI.v(bass.AP(XI.t, c * 128, [[3 * 128, 64], [0, 4], [1, 128]]))
                            fw.tt(pool, qxT.v(qxT.t[0:64, ts_].rearrange("p (n q) -> p n q", n=4)),
                                  RT[2].v(RT[2].t[0:64, :].rearrange("p (n q) -> p n q", n=4)), xib, ALU.mult)
                        else:
                            fw.tt(pool, dst[0:64, ts_], RT[0][0:64, :], RT[1][0:64, :], ALU.add)
                proj_fm(lambda tb, ts_, pv: fw.activation(gT[:, ts_], pv, AF.Silu), lambda kc: wcg[:, kc, :], 128)
                for tg in range(4):
                    pb = PS[6 + tg % 2]
                    for t4 in range(4):
                        tile = tg * 4 + t4
                        for kc in range(8):
                            fw.mm(pb[:, t4 * 128:(t4 + 1) * 128], hT[:, kc, tile * 128:(tile + 1) * 128], wcv[:, kc, :],
                                  start=(kc == 0), stop=(kc == 7), signal=(kc == 7 and t4 == 3))
                    fw.copy(act if tg % 2 else dve, VC.v(VC.t[:, tg * 4:tg * 4 + 4, :].rearrange("p n c -> p (n c)")), pb[:])
                for tg in range(2):
                    pb = PS[6 + tg]
                    tb16 = pb.t.bitcast(BF16)
                    for t8 in range(8):
                        tile = tg * 8 + t8
                        fw.transpose(pb.v(tb16[:, t8 * 64:(t8 + 1) * 64]), kTc[0:64, tile * 128:(tile + 1) * 128],
                                     ident_b[0:64, 0:64], signal=(t8 == 7))
                    zb = ZETA.v(bass.AP(ZETA.t, c * 64, [[3 * 64, 128], [0, 8], [1, 64]]))
                    fw.tt(dve, kz[:, tg * 8:(tg + 1) * 8, :],
                          pb.v(tb16[:, 0:512].rearrange("p (n c) -> p n c", n=8)), zb, ALU.mult)
                for hh in range(2):
                    for n in range(15):
                        pu = PS[6 + (n // 8)]
                        fw.mm(pu[32 * hh:32 * hh + 32, (n % 8) * 64:(n % 8 + 1) * 64], kz[:, n, 32 * hh:32 * hh + 32],
                              VC[:, n, 64 * hh:64 * hh + 64], start=True, stop=True, signal=(n in (7, 14)))
                fw.copy(dve, STf[:, 1, :], PS[6][0:64, 0:64])
                for n in range(1, 15):
                    pu = PS[6 + (n // 8)]
                    fw.stt(STf[:, n + 1, :], STf[:, n, :], GAM[:, c:c + 1], pu[0:64, (n % 8) * 64:(n % 8 + 1) * 64], ALU.mult, ALU.add)
                fw.copy(act, STb[:, 1:16, :], STf[:, 1:16, :])
                if c == 0:
                    dump(f"qTc{l}", qTc[0:64, :], [64, S], BF16)
                    dump(f"kTc{l}", kTc[0:64, :], [64, S], BF16)
                    dump(f"qxT{l}", qxT[0:64, :], [64, S], BF16)
                    dump(f"kz{l}", kz[:], [128, 16, 64], BF16)
                    dump(f"VC{l}", VC[:], [128, 16, 128], BF16)
                    dump(f"STf{l}", STf[:, 1:16, :], [64, 15, 64], F32)
                    dump(f"gT{l}", gT[:], [128, S], BF16)
                bi = 0
                for n in range(16):
                    for hh in range(2):
                        h = 2 * c + hh
                        ss = PS[4 + bi % 4]
                        ssv = ss[:, 0:128]
                        pt = PTC[bi % 4]
                        bi += 1
                        cs = slice(n * 128, (n + 1) * 128)
                        fw.mm(ssv, kTc[32 * hh:32 * hh + 32, cs], qTc[32 * hh:32 * hh + 32, cs], start=True, stop=True)
                        fw.tt(dve, pt[:], ssv, DECT[:, h, :], ALU.mult)
                        ob = PS[n // 4][64 * hh:64 * hh + 64, (n % 4) * 128:(n % 4 + 1) * 128]
                        fw.mm(ob, VC[:, n, 64 * hh:64 * hh + 64], pt[:], start=True, stop=(n == 0), signal=True, skip_group_check=True)
                        if n > 0:
                            fw.mm(ob, STb[32 * hh:32 * hh + 32, n, :], qxT[32 * hh:32 * hh + 32, cs], start=False, stop=True,
                                  signal=True, skip_group_check=True)
                for tb in range(4):
                    ts_ = slice(tb * 512, (tb + 1) * 512)
                    ob = PS[tb]
                    fw.copy(act, CP[:], ob[:])
                    fw.activation(CSQ[:], ob[:], AF.Square)
                    pm, p2_ = PS[6], PS[7]
                    fw.mm(pm[:], BD[:], CP[:], start=True, stop=True)
                    fw.mm(p2_[:], BD[:], CSQ[:], start=True, stop=True)
                    fw.copy(act, CMEAN[:], pm[:])
                    fw.activation(CMSQ[:], pm[:], AF.Square)
                    fw.tt(dve, CDV[:], ob[:], CMEAN[:], ALU.subtract)
                    fw.tt(dve, CVAR[:], p2_[:], CMSQ[:], ALU.subtract)
                    fw.ts(dve, CVAR[:], CVAR[:], 0.0, ALU.max)
                    fw.activation(CVAR[:], CVAR[:], AF.Ln, bias=EPSC[:, 0:1])
                    fw.activation(CRS[:], CVAR[:], AF.Exp, scale=-0.5)
                    fw.tt(dve, CDV[:], CDV[:], CRS[:], ALU.mult)
                    fw.tt(pool, mixT[:, 5 + c, ts_], CDV[:], gT[:, ts_], ALU.mult)
            dump(f"mixC{l}", mixT[:, 5:8, :], [128, 3, S], BF16)
            fw.barrier()

            AX.reset()
            xT = AX.alloc(fw, "xT", [128, 8, S], F32)
            sp.dma_group([(xT[:, kc, :], xsp_d[:, kc, :]) for kc in range(8)])
            AW.reset()
            wo = AW.alloc(fw, "wo", [128, 8, D], BF16)
            wov = wout_d[l].rearrange("(k p) n -> p k n", p=128)
            load_w(wo[:], wov)
            for kc in range(8):
                fw.activation(wo[:, kc, :], wo[:, kc, :], AF.Identity, scale=MG[:, l, kc:kc + 1])
            for d_ in range(8):
                for tb in range(4):
                    ts_ = slice(tb * 512, (tb + 1) * 512)
                    pb = PS[(d_ * 4 + tb) % 4]
                    for kc in range(8):
                        fw.mm(pb[:], wo[:, kc, d_ * 128:(d_ + 1) * 128], mixT[:, kc, ts_], start=(kc == 0), stop=(kc == 7))
                    fw.stt(xT[:, d_, ts_], pb[:], MOD[:, l, 16 + d_:17 + d_], xT[:, d_, ts_], ALU.mult, ALU.add)
            dump(f"xmid{l}", xT[:], [128, 8, S], F32)
            fw.barrier()

            AY.reset()
            norm_to_hT(l, lambda kc: A2[:, l, kc:kc + 1], 24, AY)
            fw.barrier()
            AY.reset()
            actT = AY.alloc(fw, "actT", [128, 6, S], BF16)
            sgt = [AY.alloc(fw, f"sg{i}", [128, 512], BF16) for i in range(2)]
            AW.reset()
            wdn = [AW.alloc(fw, f"wdn{i}", [128, 6, D], BF16) for i in range(2)]
            gu = [AW.alloc(fw, f"gu{i}", [128, 2, 8, 128], BF16) for i in range(3)]
            wgv = wg_d[l].rearrange("(k p) n -> p k n", p=128)
            wuv = wu_d[l].rearrange("(k p) n -> p k n", p=128)
            ji = 0
            modgen = mod_pieces(l + 1) if l + 1 < depth else iter(())
            for gi, (j0, nj) in enumerate(FFN_GROUPS):
                wdb = wdn[gi % 2]
                load_w(wdb[:, 0:nj, :], wd_d[l, j0 * 128:(j0 + nj) * 128, :].rearrange("(j p) n -> p j n", p=128))
                for jj in range(nj):
                    j = j0 + jj
                    g = gu[ji % 3]
                    ji += 1
                    pool.dma_group([(g[:, 0, :, :], wgv[:, :, j * 128:(j + 1) * 128]),
                                    (g[:, 1, :, :], wuv[:, :, j * 128:(j + 1) * 128])], max_dma_last_dim=4096)
                    for tb in range(4):
                        ts_ = slice(tb * 512, (tb + 1) * 512)
                        pg, pu = PS[(tb % 2) * 2], PS[(tb % 2) * 2 + 1]
                        for kc in range(8):
                            fw.mm(pg[:], g[:, 0, kc, :], hT[:, kc, ts_], start=(kc == 0), stop=(kc == 7))
                        for kc in range(8):
                            fw.mm(pu[:], g[:, 1, kc, :], hT[:, kc, ts_], start=(kc == 0), stop=(kc == 7))
                        fw.activation(sgt[tb % 2][:], pg[:], AF.Silu)
                        fw.tt(dve, actT[:, jj, ts_], pu[:], sgt[tb % 2][:], ALU.mult)
                    next(modgen, None)
                for d_ in range(8):
                    for tb in range(4):
                        ts_ = slice(tb * 512, (tb + 1) * 512)
                        pb = PS[4 + (d_ * 4 + tb) % 3]
                        for jj in range(nj):
                            fw.mm(pb[:], wdb[:, jj, d_ * 128:(d_ + 1) * 128], actT[:, jj, ts_], start=(jj == 0), stop=(jj == nj - 1))
                        fw.stt(xT[:, d_, ts_], pb[:], MOD[:, l, 40 + d_:41 + d_], xT[:, d_, ts_], ALU.mult, ALU.add)
            for _ in modgen:
                pass
            dump(f"xout{l}", xT[:], [128, 8, S], F32)
            fw.barrier()

        AY.reset()
        sq = [AY.alloc(fw, f"fsq{i}", [128, 8, 512], BF16) for i in range(2)]
        lnt = AY.alloc(fw, "fln", [128, 512], F32)
        rs = AY.alloc(fw, "frs", [128, 512], F32)
        yt = [AY.alloc(fw, f"fyt{i}", [128, 512], F32) for i in range(3)]
        AW.reset()
        OUT = [AW.alloc(fw, f"oo{i}", [128, 4, D], F32) for i in range(2)]
        stores = []
        for tb in (range(4) if 'final' not in skip else []):
            ts_ = slice(tb * 512, (tb + 1) * 512)
            fw.activation(sq[tb % 2][:], xT[:, :, ts_], AF.Square)
            pb = PS[tb % 2]
            for kc in range(8):
                fw.mm(pb[:], ones_b[:], sq[tb % 2][:, kc, :], start=(kc == 0), stop=(kc == 7))
            fw.activation(lnt[:], pb[:], AF.Ln, scale=1.0 / D, bias=EPSC[:, 0:1])
            fw.activation(rs[:], lnt[:], AF.Exp, scale=-0.5)
            ob_ = OUT[tb % 2]
            for kc in range(8):
                t = yt[kc % 3]
                fw.stt(t[:], xT[:, kc, ts_], FNG[:, kc:kc + 1], rs[:], ALU.mult, ALU.mult)
                pt_ = PS[2 + kc % 4]
                for q in range(4):
                    fw.transpose(pt_[:, q * 128:(q + 1) * 128], t[:, q * 128:(q + 1) * 128], ident_f[:], signal=(q == 3))
                fw.copy(act if kc % 2 else dve, ob_[:, :, kc * 128:(kc + 1) * 128],
                        pt_.v(pt_.t[:, :].rearrange("p (q c) -> p q c", q=4)))
            stores.append(sp.dma_group([(y_d[(tb * 4 + q) * 128:(tb * 4 + q + 1) * 128, :], ob_[:, q, :]) for q in range(4)]))
        for tok in stores:
            sp.wait_tok(tok)
        for tok in dbg_out.values():
            sp.wait_tok(tok)
        fw.emit()
        info = {n: (e.ninst, e.nwait) for n, e in fw.E.items()}
        info["sp"] = (fw.sp.ninst, fw.sp.nwait)
        info["sems"] = fw.nsem
        print("MK build:", info)
    return nc


AX_X = mybir.AxisListType.X
_NC_CACHE = {}


def prep_inputs(inputs, b):
    f32 = np.float32
    g = lambda k: np.asarray(inputs[k])
    hc = _HC_CACHE.get("hc")
    if hc is None:
        hc = host_consts(g("rel_bias").astype(f32))
        _HC_CACHE["hc"] = hc
    m = dict(hc)
    m["x"] = np.ascontiguousarray(g("x")[b].astype(f32))
    m["cT"] = np.ascontiguousarray(g("c")[b].astype(f32).reshape(8, 128).T)
    m["posb"] = np.ascontiguousarray(g("positions")[b].astype(np.int32).reshape(1, S))
    sh = _HC_CACHE.get("shared")
    if sh is None:
        def fm(a, nch):
            a = np.asarray(a, f32)
            return np.ascontiguousarray(a.reshape(a.shape[0], nch, 128).transpose(2, 0, 1))
        sh = dict(
            ada_w=np.ascontiguousarray(g("ada_w").astype(f32)),
            adabT=fm(g("ada_b"), 48), n1gT=fm(g("norm1_g"), 8), n2gT=fm(g("norm2_g"), 8),
            fngT=np.ascontiguousarray(g("final_norm").astype(f32).reshape(8, 128).T),
            mgT=fm(g("mix_gain"), 8), mqnT=fm(g("mla_q_norm"), 2), mkvnT=fm(g("mla_kv_norm"), 1),
            w_in=np.ascontiguousarray(g("w_in").astype(f32)), w_uq=np.ascontiguousarray(g("mla_w_uq").astype(f32)),
            w_ukv=np.ascontiguousarray(g("mla_w_ukv").astype(f32)), w_out=np.ascontiguousarray(g("w_out").astype(f32)),
            w_gate=np.ascontiguousarray(g("ffn_w_gate").astype(f32)), w_up=np.ascontiguousarray(g("ffn_w_up").astype(f32)),
            w_down=np.ascontiguousarray(g("ffn_w_down").astype(f32)),
        )
        _HC_CACHE["shared"] = sh
    m.update(sh)
    return m


_HC_CACHE = {}


def kernel(**inputs):
    _HC_CACHE.clear()
    nc = build()
    in_maps = [prep_inputs(inputs, b) for b in range(NCORES)]
    res = run_bass_kernel_spmd(nc, in_maps, core_ids=list(range(NCORES)))
    out = np.stack([np.asarray(r["y"], dtype=np.float32) for r in res.results], axis=0)
    return out
```

```python
import numpy as np
import concourse.bass as bass
import concourse.mybir as mybir
from concourse.bass_utils import run_bass_kernel_spmd
from contextlib import ExitStack

F32 = mybir.dt.float32
BF16 = mybir.dt.bfloat16
I32 = mybir.dt.int32
AF = mybir.ActivationFunctionType
ALU = mybir.AluOpType
AX = mybir.AxisListType

CE = ('pe', 'act', 'dve', 'pool')
SAME_ENGINE_SYNC = True


class Buf:
    __slots__ = ('t', 'name', 'w', 'r', 'semname')

    def __init__(self, t, name, semname=None):
        self.t = t
        self.name = name
        self.w = None
        self.r = {}
        self.semname = semname or name

    def __getitem__(self, idx):
        return V(self, self.t[idx])

    def v(self, ap):
        return V(self, ap)


class V:
    __slots__ = ('buf', 'ap')

    def __init__(self, buf, ap):
        self.buf = buf
        self.ap = ap


class Eng:
    def __init__(self, fw, name, sem):
        self.fw = fw
        self.name = name
        self.sem = sem
        self.count = 0
        self.seen = {}
        self.prog = []
        self.snaps = {0: {}}
        self.nwait = 0
        self.ninst = 0

    def _deps(self, reads, writes):
        deps = []
        for v in reads:
            if v.buf.w is not None:
                deps.append(v.buf.w)
        for v in writes:
            if v.buf.w is not None:
                deps.append(v.buf.w)
            deps.extend(v.buf.r.values())
        need = {}
        for (key, val, sem) in deps:
            if key == self.name and (self.name == 'pe' or not SAME_ENGINE_SYNC):
                continue
            if val > self.seen.get(key, 0) and val > need.get(key, (0, None))[0]:
                need[key] = (val, sem)
        for key, (val, sem) in need.items():
            self.prog.append(('wait', sem, val))
            self.nwait += 1
            self.seen[key] = val
            if key in self.fw.E and key != self.name:
                snap = self.fw.E[key].snaps.get(val)
                if snap:
                    for k2, v2 in snap.items():
                        if v2 > self.seen.get(k2, 0):
                            self.seen[k2] = v2

    def issue(self, fn, reads=(), writes=(), signal=True):
        self._deps(reads, writes)
        self.ninst += 1
        if signal:
            self.count += 1
            self.prog.append(('inst', fn, True))
            self.snaps[self.count] = {k: v for k, v in self.seen.items() if k in CE}
            tok = (self.name, self.count, self.sem)
        else:
            self.prog.append(('inst', fn, False))
            tok = (self.name, self.count + 1, self.sem)
        for v in reads:
            v.buf.r[self.name] = tok
        for v in writes:
            v.buf.w = tok
            v.buf.r = {}
        return tok

    def dma(self, out, in_, **kw):
        return self.dma_group([(out, in_)], **kw)

    def dma_group(self, pairs, **kw):
        reads = [i for (o, i) in pairs if isinstance(i, V)]
        writes = [o for (o, i) in pairs if isinstance(o, V)]
        self._deps(reads, writes)
        buf = (writes[0].buf if writes else reads[0].buf)
        rec = self.fw.dsem.get(buf.semname)
        if rec is None:
            rec = [self.fw.new_sem('d_' + buf.semname), 0]
            self.fw.dsem[buf.semname] = rec
        for (o, i) in pairs:
            rec[1] += 1
            oa = o.ap if isinstance(o, V) else o
            ia = i.ap if isinstance(i, V) else i
            self.prog.append(('dma', oa, ia, rec[0], kw))
        tok = (('d', buf.semname), 16 * rec[1], rec[0])
        for v in reads:
            v.buf.r[tok[0]] = tok
        for v in writes:
            v.buf.w = tok
            v.buf.r = {}
        return tok

    def wait_tok(self, tok):
        key, val, sem = tok
        if val > self.seen.get(key, 0):
            self.prog.append(('wait', sem, val))
            self.seen[key] = val

    def replay(self, e):
        for item in self.prog:
            if item[0] == 'wait':
                e.wait_ge(item[1], item[2])
            elif item[0] == 'inst':
                ins = item[1](e)
                if item[2]:
                    ins.then_inc(self.sem, 1)
            else:
                _, oa, ia, sem, kw = item
                e.dma_start(out=oa, in_=ia, **kw).then_inc(sem, 16)


class FW:
    def __init__(self, nc, stack):
        self.nc = nc
        self.stack = stack
        self.nsem = 0
        self.dsem = {}
        self.dma_toks = []
        self.E = {}
        for name in CE:
            self.E[name] = Eng(self, name, self.new_sem('c_' + name))
        self.sp = Eng(self, 'sp', None)
        self.pe, self.act, self.dve, self.pool = (self.E[n] for n in CE)

    def new_sem(self, name):
        self.nsem += 1
        return self.stack.enter_context(self.nc.semaphore(name))

    def sbuf(self, name, shape, dtype):
        t = self.stack.enter_context(self.nc.sbuf_tensor(name, list(shape), dtype))
        return Buf(t, name)

    def psum(self, name, shape, dtype):
        t = self.stack.enter_context(self.nc.psum_tensor(name, list(shape), dtype))
        return Buf(t, name)

    def barrier(self):
        toks = [(n, self.E[n].count, self.E[n].sem) for n in CE if self.E[n].count > 0]
        for e in list(self.E.values()) + [self.sp]:
            for tok in toks:
                if tok[0] != e.name:
                    e.wait_tok(tok)
            for tok in self.dma_toks:
                e.wait_tok(tok)
        self.dma_toks = []

    def check(self):
        engs = list(self.E.values()) + [self.sp]
        pc = {e.name: 0 for e in engs}
        sems = {}
        progress = True
        while progress:
            progress = False
            for e in engs:
                while pc[e.name] < len(e.prog):
                    it = e.prog[pc[e.name]]
                    if it[0] == 'wait':
                        if sems.get(id(it[1]), 0) < it[2]:
                            break
                    elif it[0] == 'inst':
                        if it[2]:
                            sems[id(e.sem)] = sems.get(id(e.sem), 0) + 1
                    else:
                        sems[id(it[3])] = sems.get(id(it[3]), 0) + 16
                    pc[e.name] += 1
                    progress = True
        stuck = {}
        for e in engs:
            if pc[e.name] < len(e.prog):
                it = e.prog[pc[e.name]]
                who = [n for n, x in self.E.items() if x.sem is it[1]] or [k for k, r in self.dsem.items() if r[0] is it[1]]
                stuck[e.name] = (pc[e.name], len(e.prog), who, it[2], sems.get(id(it[1]), 0))
        return stuck

    def emit(self):
        st = self.check()
        assert not st, f"DEADLOCK: {st}"
        with self.nc.Block() as block:
            @block.sync
            def _(e):
                self.sp.replay(e)

            @block.tensor
            def _(e):
                self.pe.replay(e)

            @block.scalar
            def _(e):
                self.act.replay(e)

            @block.vector
            def _(e):
                self.dve.replay(e)

            @block.gpsimd
            def _(e):
                self.pool.replay(e)

    def mm(self, out, lhsT, rhs, start=True, stop=True, signal=None, **kw):
        if signal is None:
            signal = stop
        return self.pe.issue(
            lambda e: e.matmul(out.ap, lhsT.ap, rhs.ap, start=start, stop=stop, **kw),
            reads=[lhsT, rhs], writes=[out], signal=signal)

    def transpose(self, out, in_, ident, signal=True):
        return self.pe.issue(
            lambda e: e.transpose(out.ap, in_.ap, ident.ap),
            reads=[in_, ident], writes=[out], signal=signal)

    def activation(self, out, in_, func, scale=1.0, bias=0.0, accum_out=None, eng=None):
        reads = [in_]
        sc = scale
        bi = bias
        if isinstance(scale, V):
            reads.append(scale)
            sc = scale.ap
        if isinstance(bias, V):
            reads.append(bias)
            bi = bias.ap
        writes = [out]
        kw = {}
        if accum_out is not None:
            writes.append(accum_out)
            kw['accum_out'] = accum_out.ap
        return self.act.issue(
            lambda e: e.activation(out=out.ap, in_=in_.ap, func=func, scale=sc, bias=bi, **kw),
            reads=reads, writes=writes)

    def tt(self, eng, out, in0, in1, op):
        return eng.issue(lambda e: e.tensor_tensor(out=out.ap, in0=in0.ap, in1=in1.ap, op=op),
                         reads=[in0, in1], writes=[out])

    def ts(self, eng, out, in0, s1, op0, s2=None, op1=None, accum_out=None):
        reads = [in0]
        a1 = s1
        a2 = s2
        if isinstance(s1, V):
            reads.append(s1)
            a1 = s1.ap
        if isinstance(s2, V):
            reads.append(s2)
            a2 = s2.ap
        kw = {}
        writes = [out]
        if op1 is not None:
            kw['op1'] = op1
        if accum_out is not None:
            kw['accum_out'] = accum_out.ap
            writes.append(accum_out)
        return eng.issue(lambda e: e.tensor_scalar(out=out.ap, in0=in0.ap, scalar1=a1, scalar2=a2, op0=op0, **kw),
                         reads=reads, writes=writes)

    def stt(self, out, in0, scalar, in1, op0, op1):
        reads = [in0, in1]
        a = scalar
        if isinstance(scalar, V):
            reads.append(scalar)
            a = scalar.ap
        return self.dve.issue(
            lambda e: e.scalar_tensor_tensor(out=out.ap, in0=in0.ap, scalar=a, in1=in1.ap, op0=op0, op1=op1),
            reads=reads, writes=[out])

    def copy(self, eng, out, in_):
        if eng is self.act:
            return eng.issue(lambda e: e.copy(out=out.ap, in_=in_.ap), reads=[in_], writes=[out])
        return eng.issue(lambda e: e.tensor_copy(out=out.ap, in_=in_.ap), reads=[in_], writes=[out])

    def memset(self, eng, out, val):
        return eng.issue(lambda e: e.memset(out.ap, val), writes=[out])

D = 1024
S = 2048
DEPTH = 4
NCORES = 8
IN_W = 2720
FH = 2816
OFF_AQ, OFF_AK, OFF_AV = 0, 384, 768
OFF_BQ, OFF_BKV, OFF_BKR = 1152, 1408, 1536
OFF_CQ, OFF_CK, OFF_CV, OFF_CG = 1568, 1760, 1952, 2336
EPS = 1e-6
NEG = -30000.0
SLAB = 209984
FFN_GROUPS = [(0, 6), (6, 6), (12, 5), (17, 5)]


def _t5_bucket(dist):
    n_buckets, max_distance = 32, 2048
    max_exact = n_buckets // 2
    safe = np.maximum(dist, 1).astype(np.float32)
    large = max_exact + (np.log(safe / max_exact) / np.log(max_distance / max_exact) * (n_buckets - max_exact)).astype(np.int32)
    large = np.minimum(large, n_buckets - 1)
    return np.where(dist < max_exact, dist, large).astype(np.int32)


def host_consts(rel_bias):
    f32 = np.float32
    k = np.arange(128)[:, None]
    ql = np.arange(256)[None, :]
    q3 = np.arange(128)[None, :]
    d1 = ql - k
    v1 = (d1 >= 0) & (d1 <= 128)
    i1 = _t5_bucket(np.clip(d1, 0, 128))
    v2 = v1
    i2 = _t5_bucket(np.clip(4 * d1, 0, 512))
    d3 = q3 - k
    v3 = d3 >= 0
    i3 = _t5_bucket(np.clip(16 * d3, 0, 2048))
    bmb = np.zeros((128, 6, 640), f32)
    for h in range(6):
        bmb[:, h, 0:256] = np.where(v1, rel_bias[i1, h], 0.0)
        bmb[:, h, 256:512] = np.where(v2, rel_bias[i2, h], 0.0)
        bmb[:, h, 512:640] = np.where(v3, rel_bias[i3, h], 0.0)
    bmm = np.concatenate([np.where(v1, 0.0, NEG), np.where(v2, 0.0, NEG), np.where(v3, 0.0, NEG)], axis=1).astype(f32)
    cm = np.where(q3 >= k, 0.0, NEG).astype(f32)
    H = 6
    log_g = np.log(1.0 - 2.0 ** (-5.0 - np.arange(H))).astype(f32)
    i = np.arange(128, dtype=f32)
    rel = i[:, None] - i[None, :]
    decay_intra = (np.exp(np.maximum(rel, 0.0)[None] * log_g[:, None, None]) * (rel >= 0)[None]).astype(f32)
    xi = np.exp((i + 1.0)[None, :] * log_g[:, None]).astype(f32)
    zeta = np.exp((128 - 1.0 - i)[None, :] * log_g[:, None]).astype(f32)
    cd = np.exp(128 * log_g).astype(f32)
    s = f32(32 ** -0.5)
    decT = np.zeros((128, 6, 128), f32)
    for h in range(H):
        decT[:, h, :] = decay_intra[h].T * s
    xi_t = np.zeros((64, 3, 128), f32)
    zeta_t = np.zeros((128, 3, 64), f32)
    gam = np.zeros((64, 3), f32)
    for p in range(3):
        for hh in range(2):
            h = 2 * p + hh
            xi_t[hh * 32:(hh + 1) * 32, p, :] = xi[h][None, :] * s
            zeta_t[:, p, hh * 32:(hh + 1) * 32] = zeta[h][:, None]
            gam[hh * 32:(hh + 1) * 32, p] = cd[h]
    half = 16
    inv_freq = (1.0 / (10000.0 ** (np.arange(half, dtype=f32) / half))).astype(f32)
    invf = np.tile(inv_freq, 8).reshape(128, 1).astype(f32)
    return dict(bmb=bmb, bmm=bmm, cm=cm, decT=decT, xi=xi_t, zeta=zeta_t, gam=gam, invf=invf)


class Arena:
    def __init__(self, nc, base, size, name):
        self.nc, self.base, self.size, self.name = nc, base, size, name
        self.off = 0
        self.n = 0

    def reset(self):
        self.off = 0

    def alloc(self, fw, name, shape, dtype):
        nbytes = int(np.prod(shape[1:])) * mybir.dt.size(dtype)
        nbytes = (nbytes + 31) // 32 * 32
        assert self.off + nbytes <= self.size, (self.name, name, self.off, nbytes, self.size)
        self.n += 1
        t = self.nc.alloc_sbuf_tensor_at(f"{self.name}_{name}_{self.n}", list(shape), dtype, offset=self.base + self.off)
        self.off += nbytes
        return Buf(t, f"{self.name}_{name}_{self.n}", semname=f"{self.name}_{name}")


def build(depth=DEPTH, dbg=None, skip=()):
    nc = bass.Bass("TRN2", target_bir_lowering=False)
    dbg = dbg or []
    dbg_out = {}
    MARKS.clear()

    def din(name, shape, dt=F32):
        return nc.dram_tensor(name, list(shape), dt, kind="ExternalInput").ap()

    x_d = din("x", [S, D])
    cT_d = din("cT", [128, 8])
    pos_d = din("posb", [1, S], I32)
    invf_d = din("invf", [128, 1])
    bmb_d = din("bmb", [128, 6, 640])
    bmm_d = din("bmm", [128, 640])
    cm_d = din("cm", [128, 128])
    decT_d = din("decT", [128, 6, 128])
    xi_d = din("xi", [64, 3, 128])
    zeta_d = din("zeta", [128, 3, 64])
    gam_d = din("gam", [64, 3])
    adab_d = din("adabT", [128, DEPTH, 48])
    n1g_d = din("n1gT", [128, DEPTH, 8])
    n2g_d = din("n2gT", [128, DEPTH, 8])
    fng_d = din("fngT", [128, 8])
    mg_d = din("mgT", [128, DEPTH, 8])
    mqn_d = din("mqnT", [128, DEPTH, 2])
    mkvn_d = din("mkvnT", [128, DEPTH, 1])
    if depth > 0:
        adaw_d = din("ada_w", [DEPTH, D, 6 * D])
        win_d = din("w_in", [DEPTH, D, IN_W])
        wuq_d = din("w_uq", [DEPTH, 256, 384])
        wukv_d = din("w_ukv", [DEPTH, 128, 512])
        wout_d = din("w_out", [DEPTH, D, D])
        wg_d = din("w_gate", [DEPTH, D, FH])
        wu_d = din("w_up", [DEPTH, D, FH])
        wd_d = din("w_down", [DEPTH, FH, D])
        xsp_d = nc.dram_tensor("xspill", [128, 8, S], F32, kind="Internal").ap()
    y_d = nc.dram_tensor("y", [S, D], F32, kind="ExternalOutput").ap()

    st = ExitStack()
    with st:
        fw = FW(nc, st)
        pe, act, dve, pool, sp = fw.pe, fw.act, fw.dve, fw.pool, fw.sp
        base0 = (nc.sbuf_base + 31) // 32 * 32
        st.enter_context(nc.sbuf_tensor("slab", [128, SLAB], mybir.dt.uint8))
        assert nc.sbuf_base == base0 + SLAB, (nc.sbuf_base, base0)
        o = base0
        SZ_HT, SZ_X, SZ_Y, SZ_ADAW, SZ_W = 32768, 65536, 32768, 12288, 36864
        SZ_C = SLAB - (SZ_HT + SZ_X + SZ_Y + SZ_ADAW + SZ_W)
        AC = Arena(nc, o, SZ_C, "c"); o += SZ_C
        AH = Arena(nc, o, SZ_HT, "h"); o += SZ_HT
        AX = Arena(nc, o, SZ_X, "x"); o += SZ_X
        AY = Arena(nc, o, SZ_Y, "y"); o += SZ_Y
        AA = Arena(nc, o, SZ_ADAW, "a"); o += SZ_ADAW
        AW = Arena(nc, o, SZ_W, "w"); o += SZ_W

        PS = [fw.psum(f"ps{i}", [128, 512], F32) for i in range(8)]

        def sub(buf, name):
            return Buf(buf.t, name)

        ident_f = AC.alloc(fw, "identf", [128, 128], F32)
        ident_b = AC.alloc(fw, "identb", [128, 128], BF16)
        ones_b = AC.alloc(fw, "onesb", [128, 128], BF16)
        BD = AC.alloc(fw, "bd", [128, 128], BF16)
        WNe = AC.alloc(fw, "wne", [128, 64], BF16)
        WNo = AC.alloc(fw, "wno", [128, 64], BF16)
        FCOS = AC.alloc(fw, "fcos", [128, S], BF16)
        FSIN = AC.alloc(fw, "fsin", [128, S], BF16)
        BM = AC.alloc(fw, "bm", [128, 6, 640], BF16)
        CM = AC.alloc(fw, "cm", [128, 128], F32)
        DECT = AC.alloc(fw, "dect", [128, 6, 128], F32)
        XI = AC.alloc(fw, "xi", [64, 3, 128], F32)
        ZETA = AC.alloc(fw, "zeta", [128, 3, 64], F32)
        GAM = AC.alloc(fw, "gam", [64, 3], F32)
        MOD = AC.alloc(fw, "mod", [128, DEPTH, 48], F32)
        A1 = AC.alloc(fw, "a1", [128, DEPTH, 8], F32)
        A2 = AC.alloc(fw, "a2", [128, DEPTH, 8], F32)
        ADAB = AC.alloc(fw, "adab", [128, DEPTH, 48], F32)
        N1G = AC.alloc(fw, "n1g", [128, DEPTH, 8], F32)
        N2G = AC.alloc(fw, "n2g", [128, DEPTH, 8], F32)
        FNG = AC.alloc(fw, "fng", [128, 8], F32)
        MG = AC.alloc(fw, "mg", [128, DEPTH, 8], F32)
        MQN = AC.alloc(fw, "mqn", [128, DEPTH, 2], F32)
        MKVN = AC.alloc(fw, "mkvn", [128, DEPTH, 1], F32)
        CTF = AC.alloc(fw, "ctf", [128, 8], F32)
        CTB = AC.alloc(fw, "ctb", [128, 8], BF16)
        INVF = AC.alloc(fw, "invf", [128, 1], F32)
        hT = AH.alloc(fw, "hT", [128, 8, S], BF16)
        adaw = [AA.alloc(fw, f"adaw{i}", [128, 3072], BF16) for i in range(2)]

        def dump(name, view, shape, dt):
            if name not in dbg:
                return
            t = nc.dram_tensor("dbg_" + name, list(shape), dt, kind="ExternalOutput").ap()
            dbg_out[name] = sp.dma(t, view)
            fw.dma_toks.append(dbg_out[name])

        fw.memset(dve, ident_f[:], 1.0)
        pool.issue(lambda e: e.affine_select(out=ident_f.t[:], in_=ident_f.t[:], pattern=[[-1, 128]],
                                             compare_op=ALU.is_equal, fill=0.0, base=0, channel_multiplier=1),
                   reads=[ident_f[:]], writes=[ident_f[:]])
        fw.copy(dve, ident_b[:], ident_f[:])
        fw.memset(dve, ones_b[:], 1.0)
        fw.memset(dve, BD[:], 0.0)
        fw.memset(dve, BD[0:64, 0:64], 1.0 / 64)
        fw.memset(dve, BD[64:128, 64:128], 1.0 / 64)
        fw.memset(dve, WNe[:], 0.0)
        fw.memset(dve, WNe[0:64, :], 1.0)
        if 'wn' not in skip:
            fw.memset(dve, WNe[64:65, :], 64 * EPS)
        fw.memset(dve, WNo[:], 0.0)
        fw.memset(dve, WNo[64:128, :], 1.0)
        if 'wn' not in skip:
            fw.memset(dve, WNo[0:1, :], 64 * EPS)
        for (b, d_) in [] if 'small' in skip else [(CM, cm_d), (DECT, decT_d), (XI, xi_d), (ZETA, zeta_d), (GAM, gam_d), (ADAB, adab_d),
                        (N1G, n1g_d), (N2G, n2g_d), (FNG, fng_d), (MG, mg_d), (MQN, mqn_d), (MKVN, mkvn_d),
                        (CTF, cT_d), (INVF, invf_d)]:
            sp.dma(b[:], d_)
        if 'silu' not in skip:
            fw.activation(CTB[:], CTF[:], AF.Silu)
        AY.reset()
        bmb_s = AY.alloc(fw, "bmb", [128, 6, 640], F32)
        bmm_s = AY.alloc(fw, "bmm", [128, 640], F32)
        if 'bm' not in skip:
            sp.dma(bmb_s[:], bmb_d)
            sp.dma(bmm_s[:], bmm_d)
            for h in range(6):
                fw.tt(dve, BM[:, h, :], bmb_s[:, h, :], bmm_s[:], ALU.add)
        fw.barrier()
        AY.reset()
        posi = AY.alloc(fw, "posi", [128, S], I32)
        ang = AY.alloc(fw, "ang", [128, S], F32)
        t1 = AY.alloc(fw, "t1", [128, S], F32)
        t2 = AY.alloc(fw, "t2", [128, S], F32)
        if 'tables' not in skip:
            sp.dma(posi[:], pos_d.partition_broadcast(128))
            fw.copy(dve, t1[:], posi[:])
            fw.ts(dve, ang[:], t1[:], INVF[:, 0:1], ALU.mult)
            TWO_PI = 2.0 * np.pi
            C1 = 6.28125
            C2 = float(np.float32(TWO_PI - C1))
            fw.ts(dve, t1[:], ang[:], 1.0 / TWO_PI, ALU.mult)
            fw.copy(dve, posi[:], t1[:])
            fw.copy(dve, t1[:], posi[:])
            fw.stt(t2[:], t1[:], -C1, ang[:], ALU.mult, ALU.add)
            fw.stt(t2[:], t1[:], -C2, t2[:], ALU.mult, ALU.add)

            def wrap(dst, src, tmp):
                fw.ts(dve, tmp, src, float(np.pi), ALU.is_gt)
                fw.stt(dst, tmp, -TWO_PI, src, ALU.mult, ALU.add)
                fw.ts(dve, tmp, dst, -float(np.pi), ALU.is_lt)
                fw.stt(dst, tmp, TWO_PI, dst, ALU.mult, ALU.add)
                fw.ts(dve, dst, dst, 3.1415925, ALU.min, -3.1415925, ALU.max)

            wrap(t2[:], t2[:], t1[:])
            fw.activation(FSIN[:], t2[:], AF.Sin)
            fw.ts(dve, ang[:], t2[:], float(np.pi / 2), ALU.add)
            wrap(ang[:], ang[:], t1[:])
            fw.activation(FCOS[:], ang[:], AF.Sin)
        fw.barrier()

        AX.reset()
        xT = AX.alloc(fw, "xT", [128, 8, S], F32)
        AW.reset()
        io = [AW.alloc(fw, f"io{i}", [128, D], F32) for i in range(2)]
        for n in range(16):
            sp.dma(io[n % 2][:], x_d[n * 128:(n + 1) * 128, :])
            for g in range(2):
                pb = PS[(2 * n + g) % 4]
                for j in range(4):
                    kc = 4 * g + j
                    fw.transpose(pb[:, j * 128:(j + 1) * 128], io[n % 2][:, kc * 128:(kc + 1) * 128], ident_f[:], signal=(j == 3))
                src = pb.v(pb.t[:, :].rearrange("p (j t) -> p j t", j=4))
                dst = xT.v(xT.t[:, 4 * g:4 * g + 4, n * 128:(n + 1) * 128])
                fw.copy(act if (n + g) % 2 else dve, dst, src)
        fw.barrier()

        def mod_pieces(l):
            mp = PS[7]
            for piece in range(16):
                kc, half = piece // 2, piece % 2
                slot = adaw[piece % 2]
                pool.dma(slot[:], adaw_d[l, kc * 128:(kc + 1) * 128, half * 3072:(half + 1) * 3072], max_dma_last_dim=4096)
                for j in range(24):
                    jj = half * 24 + j
                    fw.mm(mp[:, jj:jj + 1], slot[:, j * 128:(j + 1) * 128], CTB[:, kc:kc + 1],
                          start=(piece == 0 and j == 0), stop=(kc == 7), signal=(j == 23), skip_group_check=True)
                yield
            fw.tt(dve, MOD[:, l, :], mp[:, 0:48], ADAB[:, l, :], ALU.add)
            fw.stt(A1[:, l, :], MOD[:, l, 8:16], 1.0, N1G[:, l, :], ALU.add, ALU.mult)
            fw.stt(A2[:, l, :], MOD[:, l, 32:40], 1.0, N2G[:, l, :], ALU.add, ALU.mult)
            yield

        EPSC = AC.alloc(fw, "epsc", [128, 1], F32)
        fw.memset(dve, EPSC[:], EPS)

        def norm_to_hT(l, a_view, b_col0, ar):
            ar.reset()
            sq = [ar.alloc(fw, f"nsq{i}", [128, 8, 512], BF16) for i in range(2)]
            lnt = ar.alloc(fw, "nln", [128, 512], F32)
            rs = [ar.alloc(fw, f"nrs{i}", [128, 512], F32) for i in range(2)]
            tm = [ar.alloc(fw, f"ntm{i}", [128, 512], F32) for i in range(3)]
            for tb in range(4):
                ts_ = slice(tb * 512, (tb + 1) * 512)
                fw.activation(sq[tb % 2][:], xT[:, :, ts_], AF.Square)
                pb = PS[tb % 2]
                for kc in range(8):
                    fw.mm(pb[:], ones_b[:], sq[tb % 2][:, kc, :], start=(kc == 0), stop=(kc == 7))
                fw.activation(lnt[:], pb[:], AF.Ln, scale=1.0 / D, bias=EPSC[:, 0:1])
                fw.activation(rs[tb % 2][:], lnt[:], AF.Exp, scale=-0.5)
                for kc in range(8):
                    t = tm[kc % 3]
                    fw.tt(dve, t[:], xT[:, kc, ts_], rs[tb % 2][:], ALU.mult)
                    fw.activation(hT[:, kc, ts_], t[:], AF.Identity, scale=a_view(kc), bias=MOD[:, l, b_col0 + kc:b_col0 + kc + 1])

        def load_w(buf_view, dram_ap):
            pool.dma(buf_view, dram_ap, max_dma_last_dim=4096)

        def proj_fm(out_fn, w_fn, M, nk=8, rhs_fn=None, banks=(6, 7), out_p0=0):
            for tb in range(4):
                pb = PS[banks[tb % len(banks)]]
                ts_ = slice(tb * 512, (tb + 1) * 512)
                pv = pb[out_p0:out_p0 + M, :]
                for kc in range(nk):
                    rhs = rhs_fn(kc, ts_) if rhs_fn else hT[:, kc, ts_]
                    fw.mm(pv, w_fn(kc), rhs, start=(kc == 0), stop=(kc == nk - 1))
                out_fn(tb, ts_, pv)

        def finalize_head(bank, nr, mixb, chunk, tb, fin):
            SQb, LNb, RSb = fin
            ts_ = slice(tb * 512, (tb + 1) * 512)
            fw.activation(SQb[:], bank[:], AF.Square)
            pf = PS[6 + (tb % 2)]
            wn = WNe if nr == 0 else WNo
            fw.mm(pf[nr:nr + 64, :], wn[:], SQb[:], start=True, stop=True)
            fw.activation(LNb[nr:nr + 64, :], pf[nr:nr + 64, :], AF.Ln, scale=1.0 / 64)
            fw.activation(RSb[nr:nr + 64, :], LNb[nr:nr + 64, :], AF.Exp, scale=-0.5)
            fw.tt(dve, mixb[nr:nr + 64, chunk, ts_], bank[nr:nr + 64, :], RSb[nr:nr + 64, :], ALU.mult)

        def v_lhsT(vbuf, tile_off, ones_off, even):
            t = vbuf.t
            pstride = t[:, :].ap[0][0]
            if even:
                return vbuf.v(bass.AP(t, tile_off, [[pstride, 128], [ones_off - tile_off, 2], [1, 64]]))
            return vbuf.v(bass.AP(t, ones_off, [[pstride, 128], [tile_off - ones_off, 2], [1, 64]]))

        for l in range(depth):
            MARKS.append(('mod', l, pe.ninst))
            if l == 0:
                for _ in mod_pieces(0):
                    pass
            MARKS.append(('norm1', l, pe.ninst))
            AY.reset()
            norm_to_hT(l, lambda kc: A1[:, l, kc:kc + 1], 0, AY)
            dump(f"hT{l}", hT[:], [128, 8, S], BF16)
            for kc in range(8):
                sp.dma(xsp_d[:, kc, :], xT[:, kc, :])
            fw.barrier()
            _rec = fw.dsem[xT.semname]
            spill_tok = (("d", xT.semname), 16 * _rec[1], _rec[0])
            for e in (pe, act, dve, pool):
                e.wait_tok(spill_tok)

            AY.reset()
            mixT = AY.alloc(fw, "mixT", [128, 8, S], BF16)

            MARKS.append(('A', l, pe.ninst))
            AX.reset()
            qT = AX.alloc(fw, "qT", [128, S], BF16)
            kT = AX.alloc(fw, "kT", [128, S], BF16)
            VA = AX.alloc(fw, "VA", [128, 48, 2, 128], BF16)
            PT = [AX.alloc(fw, f"pt{i}", [128, 512], BF16) for i in range(4)]
            TS = [AX.alloc(fw, f"ts{i}", [128, 512], F32) for i in range(4)]
            fin = (AX.alloc(fw, "fsq", [128, 512], BF16), AX.alloc(fw, "fln", [128, 512], F32), AX.alloc(fw, "frs", [128, 512], F32))
            AW.reset()
            wq = AW.alloc(fw, "wq", [128, 8, 128], BF16)
            wk = AW.alloc(fw, "wk", [128, 8, 128], BF16)
            wv = AW.alloc(fw, "wv", [128, 8, 128], BF16)
            SS = [sub(PS[4], "ss0"), sub(PS[4], "ss1"), sub(PS[5], "ss2"), sub(PS[5], "ss3")]
            fw.memset(pool, VA[:, :, 0, 64:128], 1.0)
            fw.memset(pool, VA[:, :, 1, 0:64], 1.0)
            winv = win_d[l].rearrange("(k p) n -> p k n", p=128)
            blk = [0]
            for c in (range(3) if 'A' not in skip else []):
                load_w(wq[:], winv[:, :, OFF_AQ + c * 128:OFF_AQ + (c + 1) * 128])
                load_w(wk[:], winv[:, :, OFF_AK + c * 128:OFF_AK + (c + 1) * 128])
                load_w(wv[:], winv[:, :, OFF_AV + c * 128:OFF_AV + (c + 1) * 128])
                proj_fm(lambda tb, ts_, pv: fw.activation(qT[:, ts_], pv, AF.Identity, scale=0.125),
                        lambda kc: wq[:, kc, :], 128)
                proj_fm(lambda tb, ts_, pv: fw.copy(dve, kT[:, ts_], pv), lambda kc: wk[:, kc, :], 128)
                def tok_ap(ordr, tile, kc):
                    if ordr == 0:
                        return hT[:, kc, tile * 128:(tile + 1) * 128]
                    if ordr == 1:
                        r, b = tile // 4, tile % 4
                        return hT[:, kc, 512 * b + r:512 * (b + 1):4]
                    return hT[:, kc, tile::16]
                for ordr in range(3):
                    for tg in range(4):
                        pb = PS[6 + (tg % 2)]
                        for tt_ in range(4):
                            tile = tg * 4 + tt_
                            for kc in range(8):
                                fw.mm(pb[:, tt_ * 128:(tt_ + 1) * 128], tok_ap(ordr, tile, kc), wv[:, kc, :],
                                      start=(kc == 0), stop=(kc == 7), signal=(kc == 7 and tt_ == 3))
                        t0_ = ordr * 16 + tg * 4
                        pv4 = pb.t[:, :].rearrange("p (n c) -> p n c", n=4)
                        ev_ = act if tg % 2 else dve
                        fw.copy(ev_, VA[:, t0_:t0_ + 4, 0, 0:64], pb.v(pv4[:, :, 0:64]))
                        fw.copy(ev_, VA[:, t0_:t0_ + 4, 1, 64:128], pb.v(pv4[:, :, 64:128]))
                if c == 0:
                    dump(f"qTA{l}", qT[:], [128, S], BF16)
                    dump(f"kTA{l}", kT[:], [128, S], BF16)
                    dump(f"VA{l}", VA[:], [128, 48, 2, 128], BF16)
                for hh in range(2):
                    h = 2 * c + hh
                    ro = 64 * hh
                    started = [False] * 4

                    def sunit(subs, bm_off, w):
                        i = blk[0]
                        blk[0] += 1
                        ss = PS[4 + i % 4]
                        tsb = TS[i % 4]
                        pt = PT[i % 4]
                        off = 0
                        offs = []
                        for (kcols, qcols, nq, pvl) in subs:
                            fw.mm(ss[:, off:off + nq], kcols, qcols, start=True, stop=True)
                            offs.append(off)
                            off += nq
                        tot = off
                        nfull = sum(1 for s_ in subs if s_[2] == w)
                        bmv = BM.v(bass.AP(BM.t, h * 640 + bm_off, [[6 * 640, 128], [0, nfull], [1, w]]))
                        fw.tt(dve, tsb.v(tsb.t[:, 0:nfull * w].rearrange("p (n q) -> p n q", n=nfull)),
                              ss.v(ss.t[:, 0:nfull * w].rearrange("p (n q) -> p n q", n=nfull)), bmv, ALU.add)
                        if nfull < len(subs):
                            nql = subs[-1][2]
                            fw.tt(dve, tsb[:, nfull * w:nfull * w + nql], ss[:, nfull * w:nfull * w + nql],
                                  BM[:, h, bm_off:bm_off + nql], ALU.add)
                        fw.activation(pt[:, 0:tot], tsb[:, 0:tot], AF.Exp)
                        for (kcols, qcols, nq, pvl), o_ in zip(subs, offs):
                            for (qs, vtile, tbk, ocols) in pvl:
                                lhs = VA[:, vtile, hh, :]
                                fw.mm(ocols, lhs, pt[:, o_ + qs.start:o_ + qs.stop], start=(not started[tbk]), stop=False,
                                      signal=True, skip_group_check=True)
                                started[tbk] = True

                    def p1_sub(kt):
                        nqt = 2 if kt < 15 else 1
                        pvl = []
                        for j in range(nqt):
                            qt = kt + j
                            pvl.append((slice(j * 128, (j + 1) * 128), 0 * 16 + kt, qt // 4,
                                        PS[qt // 4][:, (qt % 4) * 128:(qt % 4 + 1) * 128]))
                        return (kT[ro:ro + 64, kt * 128:(kt + 1) * 128], qT[ro:ro + 64, kt * 128:kt * 128 + nqt * 128], nqt * 128, pvl)

                    def p2_sub(r, b_):
                        nqt = 2 if b_ < 3 else 1
                        pvl = []
                        for j in range(nqt):
                            pvl.append((slice(j * 128, (j + 1) * 128), 16 + r * 4 + b_, b_ + j, PS[b_ + j][:, r::4]))
                        return (kT[ro:ro + 64, 512 * b_ + r:512 * (b_ + 1):4], qT[ro:ro + 64, 512 * b_ + r:512 * (b_ + nqt):4],
                                nqt * 128, pvl)

                    def p3_sub(r):
                        pvl = []
                        for tbk in range(4):
                            pvl.append((slice(tbk * 32, (tbk + 1) * 32), 32 + r, tbk, PS[tbk][:, r::16]))
                        return (kT[ro:ro + 64, r::16], qT[ro:ro + 64, r::16], 128, pvl)

                    for kt in range(0, 16, 2):
                        sunit([p1_sub(kt), p1_sub(kt + 1)], 0, 256)
                    for r in range(4):
                        for b_ in (0, 2):
                            sunit([p2_sub(r, b_), p2_sub(r, b_ + 1)], 256, 256)
                    for r in range(0, 16, 4):
                        sunit([p3_sub(r + j) for j in range(4)], 512, 128)
                    for tb in range(4):
                        finalize_head(PS[tb], ro, mixT, c, tb, fin)
            dump(f"mixA{l}", mixT[:, 0:3, :], [128, 3, S], BF16)
            fw.barrier()

            MARKS.append(('B', l, pe.ninst))
            AX.reset()
            qlatT = AX.alloc(fw, "qlatT", [128, 2, S], BF16)
            kvlatT = AX.alloc(fw, "kvlatT", [128, S], BF16)
            sqb = [AX.alloc(fw, "sqb0", [128, 2, 512], BF16)] * 2
            rsq = AX.alloc(fw, "rsq", [128, S], F32)
            rskv = AX.alloc(fw, "rskv", [128, S], F32)
            rstok = AX.alloc(fw, "rstok", [128, 16], F32)
            qTh = AX.alloc(fw, "qTh", [128, S], BF16)
            KTh = AX.alloc(fw, "KTh", [128, S], BF16)
            VBs = [AX.alloc(fw, f"VB{i}", [128, 16, 128], BF16) for i in range(2)]
            PTB = [AX.alloc(fw, f"ptb{i}", [128, 512], BF16) for i in range(4)]
            TSB = [AX.alloc(fw, f"tsb{i}", [128, 128], F32) for i in range(2)]
            RT = [AX.alloc(fw, f"rt{i}", [128, 512], F32) for i in range(3)]
            fin = (AX.alloc(fw, "fsq", [128, 512], BF16), AX.alloc(fw, "fln", [128, 512], F32), AX.alloc(fw, "frs", [128, 512], F32))
            AW.reset()
            winB = AW.alloc(fw, "winB", [128, 8, 416], BF16)
            wkrr = AW.alloc(fw, "wkrr", [128, 8, 32], BF16)
            wuq_f = AW.alloc(fw, "wuqf", [128, 2, 384], F32)
            wuq_b = AW.alloc(fw, "wuqb", [128, 2, 384], BF16)
            wuq_r = AW.alloc(fw, "wuqr", [128, 2, 4, 32], BF16)
            wukv_f = AW.alloc(fw, "wukvf", [128, 512], F32)
            wukv_b = AW.alloc(fw, "wukvb", [128, 512], BF16)
            fw.memset(pool, VBs[0][:, :, 64:128], 1.0)
            fw.memset(pool, VBs[1][:, :, 0:64], 1.0)
            load_w(winB[:], winv[:, :, OFF_BQ:OFF_BQ + 416])
            pool.dma_group([(wkrr[:, :, 0:16], winv[:, :, OFF_BKR + 16:OFF_BKR + 32]),
                            (wkrr[:, :, 16:32], winv[:, :, OFF_BKR:OFF_BKR + 16])], max_dma_last_dim=4096)
            fw.activation(wkrr[:, :, 0:16], wkrr[:, :, 0:16], AF.Identity, scale=-1.0)
            sp.dma(wuq_f[:], wuq_d[l].rearrange("(k p) n -> p k n", p=128))
            sp.dma(wukv_f[:], wukv_d[l])
            for kc in range(2):
                fw.activation(wuq_b[:, kc, :], wuq_f[:, kc, :], AF.Identity, scale=MQN[:, l, kc:kc + 1])
            fw.activation(wukv_b[:], wukv_f[:], AF.Identity, scale=MKVN[:, l, 0:1])
            wq4 = wuq_b.t[:, :, :].rearrange("p k (h e) -> p k h e", h=4)
            fw.activation(wuq_r.v(wuq_r.t[:, :, :, 0:16]), wuq_b.v(wq4[:, :, :, 80:96]), AF.Identity, scale=-1.0)
            fw.copy(dve, wuq_r.v(wuq_r.t[:, :, :, 16:32]), wuq_b.v(wq4[:, :, :, 64:80]))

            for c2 in range(2):
                proj_fm(lambda tb, ts_, pv, c2=c2: fw.copy(dve, qlatT[:, c2, ts_], pv),
                        lambda kc, c2=c2: winB[:, kc, c2 * 128:(c2 + 1) * 128], 128)
            for tb in range(4):
                ts_ = slice(tb * 512, (tb + 1) * 512)
                fw.activation(sqb[tb % 2][:], qlatT[:, :, ts_], AF.Square)
                pb = PS[4 + tb % 2]
                for c2 in range(2):
                    fw.mm(pb[:], ones_b[:], sqb[tb % 2][:, c2, :], start=(c2 == 0), stop=(c2 == 1))
                fw.activation(RT[0][:], pb[:], AF.Ln, scale=1.0 / 256, bias=EPSC[:, 0:1])
                fw.activation(rsq[:, ts_], RT[0][:], AF.Exp, scale=-0.5)
            proj_fm(lambda tb, ts_, pv: fw.copy(dve, kvlatT[:, ts_], pv), lambda kc: winB[:, kc, 256:384], 128)
            for tb in range(4):
                ts_ = slice(tb * 512, (tb + 1) * 512)
                fw.activation(sqb[tb % 2][:, 0, :], kvlatT[:, ts_], AF.Square)
                pb = PS[4 + tb % 2]
                fw.mm(pb[:], ones_b[:], sqb[tb % 2][:, 0, :], start=True, stop=True)
                fw.activation(RT[0][:], pb[:], AF.Ln, scale=1.0 / 128, bias=EPSC[:, 0:1])
                fw.activation(rskv[:, ts_], RT[0][:], AF.Exp, scale=-0.5)
            identb4 = ident_f.v(bass.AP(ident_f.t, 0, [[128, 128], [0, 4], [1, 128]]))
            for g4 in range(4):
                tdv = RT[0].v(RT[0].t[:, :].rearrange("p (n q) -> p n q", n=4))
                fw.tt(dve, tdv, rskv.v(rskv.t[:, g4 * 512:(g4 + 1) * 512].rearrange("p (n q) -> p n q", n=4)), identb4, ALU.mult)
                _o = rstok.t[:, g4 * 4:(g4 + 1) * 4]
                _i = RT[0].t[:, :].rearrange("p (n q) -> p n q", n=4)
                dve.issue(lambda e, _o=_o, _i=_i: e.tensor_reduce(out=_o, in_=_i, axis=AX_X, op=ALU.add),
                          reads=[RT[0][:]], writes=[rstok[:]])
            for tb in range(4):
                ts_ = slice(tb * 512, (tb + 1) * 512)
                p1, p2 = PS[6], PS[7]
                for kc in range(8):
                    fw.mm(p1[64:96, :], winB[:, kc, 384:416], hT[:, kc, ts_], start=(kc == 0), stop=(kc == 7))
                for kc in range(8):
                    fw.mm(p2[64:96, :], wkrr[:, kc, :], hT[:, kc, ts_], start=(kc == 0), stop=(kc == 7))
                fw.tt(dve, RT[0][64:96, :], p1[64:96, :], FCOS[64:96, ts_], ALU.mult)
                fw.tt(dve, RT[1][64:96, :], p2[64:96, :], FSIN[64:96, ts_], ALU.mult)
                fw.tt(pool, KTh[64:96, ts_], RT[0][64:96, :], RT[1][64:96, :], ALU.add)
            dump(f"rsq{l}", rsq[:], [128, S], F32)
            SCB = float((64 + 32) ** -0.5)
            for h in (range(4) if 'B' not in skip else []):
                hh = h % 2
                nr = 64 * hh
                for tb in range(4):
                    ts_ = slice(tb * 512, (tb + 1) * 512)
                    p1, p2 = PS[6], PS[7]
                    for kc in range(2):
                        fw.mm(p1[0:96, :], wuq_b[:, kc, h * 96:(h + 1) * 96], qlatT[:, kc, ts_], start=(kc == 0), stop=(kc == 1))
                    for kc in range(2):
                        fw.mm(p2[64:96, :], wuq_r.v(wuq_r.t[:, kc, h, :]), qlatT[:, kc, ts_], start=(kc == 0), stop=(kc == 1))
                    fw.stt(qTh[0:64, ts_], p1[0:64, :], SCB, rsq[0:64, ts_], ALU.mult, ALU.mult)
                    fw.tt(dve, RT[0][64:96, :], p1[64:96, :], FCOS[64:96, ts_], ALU.mult)
                    fw.tt(dve, RT[1][64:96, :], p2[64:96, :], FSIN[64:96, ts_], ALU.mult)
                    fw.tt(pool, RT[2][64:96, :], RT[0][64:96, :], RT[1][64:96, :], ALU.add)
                    fw.stt(qTh[64:96, ts_], RT[2][64:96, :], SCB, rsq[64:96, ts_], ALU.mult, ALU.mult)
                    p3 = PS[4 + tb % 2]
                    fw.mm(p3[0:64, :], wukv_b[:, h * 128:h * 128 + 64], kvlatT[:, ts_], start=True, stop=True)
                    fw.tt(dve, KTh[0:64, ts_], p3[0:64, :], rskv[0:64, ts_], ALU.mult)
                for tg in range(2):
                    pb = PS[4 + tg]
                    for t8 in range(8):
                        tile = tg * 8 + t8
                        fw.mm(pb[:, t8 * 64:(t8 + 1) * 64], kvlatT[:, tile * 128:(tile + 1) * 128],
                              wukv_b[:, h * 128 + 64:(h + 1) * 128], start=True, stop=True, signal=(t8 == 7))
                    for t8 in range(8):
                        tile = tg * 8 + t8
                        fw.activation(VBs[hh][:, tile, 64 * hh:64 * hh + 64], pb[:, t8 * 64:(t8 + 1) * 64], AF.Identity,
                                      scale=rstok[:, tile:tile + 1])
                if h == 0:
                    dump(f"qTh{l}", qTh[0:96, :], [96, S], BF16)
                    dump(f"KTh{l}", KTh[0:96, :], [96, S], BF16)
                    dump(f"VB{l}", VBs[0][:], [128, 16, 128], BF16)
                started = [False] * 4
                bi = 0
                for kt in range(16):
                    for tb in range(kt // 4, 4):
                        q0 = max(128 * kt, 512 * tb)
                        q1 = 512 * (tb + 1)
                        nq = q1 - q0
                        sbk = PS[4 + bi % 4]
                        pt = PTB[bi % 4]
                        bi += 1
                        fw.mm(sbk[:, 0:nq], KTh[0:96, kt * 128:(kt + 1) * 128], qTh[0:96, q0:q1], start=True, stop=True)
                        if q0 == 128 * kt:
                            tsb = TSB[kt % 2]
                            fw.tt(dve, tsb[:], sbk[:, 0:128], CM[:], ALU.add)
                            fw.activation(pt[:, 0:128], tsb[:], AF.Exp)
                            if nq > 128:
                                fw.activation(pt[:, 128:nq], sbk[:, 128:nq], AF.Exp)
                        else:
                            fw.activation(pt[:, 0:nq], sbk[:, 0:nq], AF.Exp)
                        lhs = VBs[hh][:, kt, :]
                        fw.mm(PS[tb][:, q0 - 512 * tb:q1 - 512 * tb], lhs, pt[:, 0:nq], start=(not started[tb]), stop=False,
                              signal=True, skip_group_check=True)
                        started[tb] = True
                for tb in range(4):
                    finalize_head(PS[tb], nr, mixT, 3 + h // 2, tb, fin)
            dump(f"mixB{l}", mixT[:, 3:5, :], [128, 2, S], BF16)
            fw.barrier()

            MARKS.append(('C', l, pe.ninst))
            AX.reset()
            qTc = AX.alloc(fw, "qTc", [128, S], BF16)
            kTc = AX.alloc(fw, "kTc", [128, S], BF16)
            qxT = AX.alloc(fw, "qxT", [128, S], BF16)
            kz = AX.alloc(fw, "kz", [128, 16, 64], BF16)
            VC = AX.alloc(fw, "VC", [128, 16, 128], BF16)
            gT = AX.alloc(fw, "gT", [128, S], BF16)
            STf = AX.alloc(fw, "stf", [64, 16, 64], F32)
            STb = AX.alloc(fw, "stb", [64, 16, 64], BF16)
            PTC = [AX.alloc(fw, f"ptc{i}", [128, 128], BF16) for i in range(4)]
            RT = [AX.alloc(fw, f"rtc{i}", [128, 512], F32) for i in range(3)]
            CP = AX.alloc(fw, "ccp", [128, 512], BF16)
            CSQ = AX.alloc(fw, "csq", [128, 512], BF16)
            CMEAN = AX.alloc(fw, "cmean", [128, 512], F32)
            CMSQ = AX.alloc(fw, "cmsq", [128, 512], F32)
            CDV = AX.alloc(fw, "cdv", [128, 512], F32)
            CVAR = AX.alloc(fw, "cvar", [128, 512], F32)
            CRS = AX.alloc(fw, "crs", [128, 512], F32)
            AW.reset()
            wcq = AW.alloc(fw, "wcq", [128, 8, 64], BF16)
            wcqr = AW.alloc(fw, "wcqr", [128, 8, 2, 32], BF16)
            wck = AW.alloc(fw, "wck", [128, 8, 64], BF16)
            wckr = AW.alloc(fw, "wckr", [128, 8, 2, 32], BF16)
            wcv = AW.alloc(fw, "wcv", [128, 8, 128], BF16)
            wcg = AW.alloc(fw, "wcg", [128, 8, 128], BF16)
            SC_ = [sub(PS[4], "sc0"), sub(PS[4], "sc1"), sub(PS[5], "sc2"), sub(PS[5], "sc3")]
            for c in (range(3) if 'C' not in skip else []):
                def load_rot(dst, off):
                    prs = []
                    for h2 in range(2):
                        prs.append((dst.v(dst.t[:, :, h2, 0:16]), winv[:, :, off + h2 * 32 + 16:off + h2 * 32 + 32]))
                        prs.append((dst.v(dst.t[:, :, h2, 16:32]), winv[:, :, off + h2 * 32:off + h2 * 32 + 16]))
                    pool.dma_group(prs, max_dma_last_dim=4096)
                    fw.activation(dst.v(dst.t[:, :, :, 0:16]), dst.v(dst.t[:, :, :, 0:16]), AF.Identity, scale=-1.0)
                load_w(wcq[:], winv[:, :, OFF_CQ + c * 64:OFF_CQ + (c + 1) * 64])
                load_rot(wcqr, OFF_CQ + c * 64)
                load_w(wck[:], winv[:, :, OFF_CK + c * 64:OFF_CK + (c + 1) * 64])
                load_rot(wckr, OFF_CK + c * 64)
                load_w(wcv[:], winv[:, :, OFF_CV + c * 128:OFF_CV + (c + 1) * 128])
                load_w(wcg[:], winv[:, :, OFF_CG + c * 128:OFF_CG + (c + 1) * 128])
                for (w_, wr_, dst, isq) in [(wcq, wcqr, qTc, True), (wck, wckr, kTc, False)]:
                    for tb in range(4):
                        ts_ = slice(tb * 512, (tb + 1) * 512)
                        p1, p2 = PS[6], PS[7]
                        for kc in range(8):
                            fw.mm(p1[0:64, :], w_[:, kc, :], hT[:, kc, ts_], start=(kc == 0), stop=(kc == 7))
                        for kc in range(8):
                            fw.mm(p2[0:64, :], wr_.v(wr_.t[:, kc, :, :].rearrange("p h e -> p (h e)")), hT[:, kc, ts_],
                                  start=(kc == 0), stop=(kc == 7))
                        fw.tt(dve, RT[0][0:64, :], p1[0:64, :], FCOS[0:64, ts_], ALU.mult)
                        fw.tt(dve, RT[1][0:64, :], p2[0:64, :], FSIN[0:64, ts_], ALU.mult)
                        if isq:
                            fw.tt(pool, RT[2][0:64, :], RT[0][0:64, :], RT[1][0:64, :], ALU.add)
                            fw.copy(act, dst[0:64, ts_], RT[2][0:64, :])
                            xib = XI.v(bass.AP(XI.t, c * 128, [[3 * 128, 64], [0, 4], [1, 128]]))
                            fw.tt(pool, qxT.v(qxT.t[0:64, ts_].rearrange("p (n q) -> p n q", n=4)),
                                  RT[2].v(RT[2].t[0:64, :].rearrange("p (n q) -> p n q", n=4)), xib, ALU.mult)
                        else:
                            fw.tt(pool, dst[0:64, ts_], RT[0][0:64, :], RT[1][0:64, :], ALU.add)
                proj_fm(lambda tb, ts_, pv: fw.activation(gT[:, ts_], pv, AF.Silu), lambda kc: wcg[:, kc, :], 128)
                for tg in range(4):
                    pb = PS[6 + tg % 2]
                    for t4 in range(4):
                        tile = tg * 4 + t4
                        for kc in range(8):
                            fw.mm(pb[:, t4 * 128:(t4 + 1) * 128], hT[:, kc, tile * 128:(tile + 1) * 128], wcv[:, kc, :],
                                  start=(kc == 0), stop=(kc == 7), signal=(kc == 7 and t4 == 3))
                    fw.copy(act if tg % 2 else dve, VC.v(VC.t[:, tg * 4:tg * 4 + 4, :].rearrange("p n c -> p (n c)")), pb[:])
                for tg in range(2):
                    pb = PS[6 + tg]
                    tb16 = pb.t.bitcast(BF16)
                    for t8 in range(8):
                        tile = tg * 8 + t8
                        fw.transpose(pb.v(tb16[:, t8 * 64:(t8 + 1) * 64]), kTc[0:64, tile * 128:(tile + 1) * 128],
                                     ident_b[0:64, 0:64], signal=(t8 == 7))
                    zb = ZETA.v(bass.AP(ZETA.t, c * 64, [[3 * 64, 128], [0, 8], [1, 64]]))
                    fw.tt(dve, kz[:, tg * 8:(tg + 1) * 8, :],
                          pb.v(tb16[:, 0:512].rearrange("p (n c) -> p n c", n=8)), zb, ALU.mult)
                for hh in range(2):
                    for n in range(15):
                        pu = PS[6 + (n // 8)]
                        fw.mm(pu[32 * hh:32 * hh + 32, (n % 8) * 64:(n % 8 + 1) * 64], kz[:, n, 32 * hh:32 * hh + 32],
                              VC[:, n, 64 * hh:64 * hh + 64], start=True, stop=True, signal=(n in (7, 14)))
                fw.copy(dve, STf[:, 1, :], PS[6][0:64, 0:64])
                for n in range(1, 15):
                    pu = PS[6 + (n // 8)]
                    fw.stt(STf[:, n + 1, :], STf[:, n, :], GAM[:, c:c + 1], pu[0:64, (n % 8) * 64:(n % 8 + 1) * 64], ALU.mult, ALU.add)
                fw.copy(act, STb[:, 1:16, :], STf[:, 1:16, :])
                if c == 0:
                    dump(f"qTc{l}", qTc[0:64, :], [64, S], BF16)
                    dump(f"kTc{l}", kTc[0:64, :], [64, S], BF16)
                    dump(f"qxT{l}", qxT[0:64, :], [64, S], BF16)
                    dump(f"kz{l}", kz[:], [128, 16, 64], BF16)
                    dump(f"VC{l}", VC[:], [128, 16, 128], BF16)
                    dump(f"STf{l}", STf[:, 1:16, :], [64, 15, 64], F32)
                    dump(f"gT{l}", gT[:], [128, S], BF16)
                bi = 0
                for n in range(16):
                    for hh in range(2):
                        h = 2 * c + hh
                        ss = PS[4 + bi % 4]
                        ssv = ss[:, 0:128]
                        pt = PTC[bi % 4]
                        bi += 1
                        cs = slice(n * 128, (n + 1) * 128)
                        fw.mm(ssv, kTc[32 * hh:32 * hh + 32, cs], qTc[32 * hh:32 * hh + 32, cs], start=True, stop=True)
                        fw.tt(dve, pt[:], ssv, DECT[:, h, :], ALU.mult)
                        ob = PS[n // 4][64 * hh:64 * hh + 64, (n % 4) * 128:(n % 4 + 1) * 128]
                        fw.mm(ob, VC[:, n, 64 * hh:64 * hh + 64], pt[:], start=True, stop=(n == 0), signal=True, skip_group_check=True)
                        if n > 0:
                            fw.mm(ob, STb[32 * hh:32 * hh + 32, n, :], qxT[32 * hh:32 * hh + 32, cs], start=False, stop=True,
                                  signal=True, skip_group_check=True)
                for tb in range(4):
                    ts_ = slice(tb * 512, (tb + 1) * 512)
                    ob = PS[tb]
                    fw.copy(act, CP[:], ob[:])
                    fw.activation(CSQ[:], ob[:], AF.Square)
                    pm, p2_ = PS[6], PS[7]
                    fw.mm(pm[:], BD[:], CP[:], start=True, stop=True)
                    fw.mm(p2_[:], BD[:], CSQ[:], start=True, stop=True)
                    fw.copy(act, CMEAN[:], pm[:])
                    fw.activation(CMSQ[:], pm[:], AF.Square)
                    fw.tt(dve, CDV[:], ob[:], CMEAN[:], ALU.subtract)
                    fw.tt(dve, CVAR[:], p2_[:], CMSQ[:], ALU.subtract)
                    fw.ts(dve, CVAR[:], CVAR[:], 0.0, ALU.max)
                    fw.activation(CVAR[:], CVAR[:], AF.Ln, bias=EPSC[:, 0:1])
                    fw.activation(CRS[:], CVAR[:], AF.Exp, scale=-0.5)
                    fw.tt(dve, CDV[:], CDV[:], CRS[:], ALU.mult)
                    fw.tt(pool, mixT[:, 5 + c, ts_], CDV[:], gT[:, ts_], ALU.mult)
            dump(f"mixC{l}", mixT[:, 5:8, :], [128, 3, S], BF16)
            fw.barrier()

            MARKS.append(('wout', l, pe.ninst))
            AX.reset()
            xT = AX.alloc(fw, "xT", [128, 8, S], F32)
            sp.dma_group([(xT[:, kc, :], xsp_d[:, kc, :]) for kc in range(8)])
            AW.reset()
            wo = AW.alloc(fw, "wo", [128, 8, D], BF16)
            wov = wout_d[l].rearrange("(k p) n -> p k n", p=128)
            load_w(wo[:], wov)
            for kc in range(8):
                fw.activation(wo[:, kc, :], wo[:, kc, :], AF.Identity, scale=MG[:, l, kc:kc + 1])
            for d_ in range(8):
                for tb in range(4):
                    ts_ = slice(tb * 512, (tb + 1) * 512)
                    pb = PS[(d_ * 4 + tb) % 4]
                    for kc in range(8):
                        fw.mm(pb[:], wo[:, kc, d_ * 128:(d_ + 1) * 128], mixT[:, kc, ts_], start=(kc == 0), stop=(kc == 7))
                    fw.stt(xT[:, d_, ts_], pb[:], MOD[:, l, 16 + d_:17 + d_], xT[:, d_, ts_], ALU.mult, ALU.add)
            dump(f"xmid{l}", xT[:], [128, 8, S], F32)
            fw.barrier()

            MARKS.append(('ffn', l, pe.ninst))
            AY.reset()
            norm_to_hT(l, lambda kc: A2[:, l, kc:kc + 1], 24, AY)
            fw.barrier()
            AY.reset()
            actT = AY.alloc(fw, "actT", [128, 6, S], BF16)
            sgt = [AY.alloc(fw, f"sg{i}", [128, 512], BF16) for i in range(2)]
            AW.reset()
            wdn = [AW.alloc(fw, f"wdn{i}", [128, 6, D], BF16) for i in range(2)]
            gu = [AW.alloc(fw, f"gu{i}", [128, 2, 8, 128], BF16) for i in range(3)]
            wgv = wg_d[l].rearrange("(k p) n -> p k n", p=128)
            wuv = wu_d[l].rearrange("(k p) n -> p k n", p=128)
            ji = 0
            modgen = mod_pieces(l + 1) if l + 1 < depth else iter(())
            for gi, (j0, nj) in enumerate(FFN_GROUPS):
                wdb = wdn[gi % 2]
                load_w(wdb[:, 0:nj, :], wd_d[l, j0 * 128:(j0 + nj) * 128, :].rearrange("(j p) n -> p j n", p=128))
                for jj in range(nj):
                    j = j0 + jj
                    g = gu[ji % 3]
                    ji += 1
                    pool.dma_group([(g[:, 0, :, :], wgv[:, :, j * 128:(j + 1) * 128]),
                                    (g[:, 1, :, :], wuv[:, :, j * 128:(j + 1) * 128])], max_dma_last_dim=4096)
                    for tb in range(4):
                        ts_ = slice(tb * 512, (tb + 1) * 512)
                        pg, pu = PS[(tb % 2) * 2], PS[(tb % 2) * 2 + 1]
                        for kc in range(8):
                            fw.mm(pg[:], g[:, 0, kc, :], hT[:, kc, ts_], start=(kc == 0), stop=(kc == 7))
                        for kc in range(8):
                            fw.mm(pu[:], g[:, 1, kc, :], hT[:, kc, ts_], start=(kc == 0), stop=(kc == 7))
                        fw.activation(sgt[tb % 2][:], pg[:], AF.Silu)
                        fw.tt(dve, actT[:, jj, ts_], pu[:], sgt[tb % 2][:], ALU.mult)
                    next(modgen, None)
                for d_ in range(8):
                    for tb in range(4):
                        ts_ = slice(tb * 512, (tb + 1) * 512)
                        pb = PS[4 + (d_ * 4 + tb) % 3]
                        for jj in range(nj):
                            fw.mm(pb[:], wdb[:, jj, d_ * 128:(d_ + 1) * 128], actT[:, jj, ts_], start=(jj == 0), stop=(jj == nj - 1))
                        fw.stt(xT[:, d_, ts_], pb[:], MOD[:, l, 40 + d_:41 + d_], xT[:, d_, ts_], ALU.mult, ALU.add)
            for _ in modgen:
                pass
            dump(f"xout{l}", xT[:], [128, 8, S], F32)
            fw.barrier()

        MARKS.append(('final', depth, pe.ninst))
        AY.reset()
        sq = [AY.alloc(fw, f"fsq{i}", [128, 8, 512], BF16) for i in range(2)]
        lnt = AY.alloc(fw, "fln", [128, 512], F32)
        rs = AY.alloc(fw, "frs", [128, 512], F32)
        yt = [AY.alloc(fw, f"fyt{i}", [128, 512], F32) for i in range(3)]
        AW.reset()
        OUT = [AW.alloc(fw, f"oo{i}", [128, 4, D], F32) for i in range(2)]
        stores = []
        for tb in (range(4) if 'final' not in skip else []):
            ts_ = slice(tb * 512, (tb + 1) * 512)
            fw.activation(sq[tb % 2][:], xT[:, :, ts_], AF.Square)
            pb = PS[tb % 2]
            for kc in range(8):
                fw.mm(pb[:], ones_b[:], sq[tb % 2][:, kc, :], start=(kc == 0), stop=(kc == 7))
            fw.activation(lnt[:], pb[:], AF.Ln, scale=1.0 / D, bias=EPSC[:, 0:1])
            fw.activation(rs[:], lnt[:], AF.Exp, scale=-0.5)
            ob_ = OUT[tb % 2]
            for kc in range(8):
                t = yt[kc % 3]
                fw.stt(t[:], xT[:, kc, ts_], FNG[:, kc:kc + 1], rs[:], ALU.mult, ALU.mult)
                pt_ = PS[2 + kc % 4]
                for q in range(4):
                    fw.transpose(pt_[:, q * 128:(q + 1) * 128], t[:, q * 128:(q + 1) * 128], ident_f[:], signal=(q == 3))
                fw.copy(act if kc % 2 else dve, ob_[:, :, kc * 128:(kc + 1) * 128],
                        pt_.v(pt_.t[:, :].rearrange("p (q c) -> p q c", q=4)))
            stores.append(sp.dma_group([(y_d[(tb * 4 + q) * 128:(tb * 4 + q + 1) * 128, :], ob_[:, q, :]) for q in range(4)]))
        for tok in stores:
            sp.wait_tok(tok)
        for tok in dbg_out.values():
            sp.wait_tok(tok)
        fw.emit()
        info = {n: (e.ninst, e.nwait) for n, e in fw.E.items()}
        info["sp"] = (fw.sp.ninst, fw.sp.nwait)
        info["sems"] = fw.nsem
        print("MK build:", info)
    return nc


AX_X = mybir.AxisListType.X
MARKS = []
_NC_CACHE = {}


def prep_inputs(inputs, b):
    f32 = np.float32
    g = lambda k: np.asarray(inputs[k])
    hc = _HC_CACHE.get("hc")
    if hc is None:
        hc = host_consts(g("rel_bias").astype(f32))
        _HC_CACHE["hc"] = hc
    m = dict(hc)
    m["x"] = np.ascontiguousarray(g("x")[b].astype(f32))
    m["cT"] = np.ascontiguousarray(g("c")[b].astype(f32).reshape(8, 128).T)
    m["posb"] = np.ascontiguousarray(g("positions")[b].astype(np.int32).reshape(1, S))
    sh = _HC_CACHE.get("shared")
    if sh is None:
        def fm(a, nch):
            a = np.asarray(a, f32)
            return np.ascontiguousarray(a.reshape(a.shape[0], nch, 128).transpose(2, 0, 1))
        sh = dict(
            ada_w=np.ascontiguousarray(g("ada_w").astype(f32)),
            adabT=fm(g("ada_b"), 48), n1gT=fm(g("norm1_g"), 8), n2gT=fm(g("norm2_g"), 8),
            fngT=np.ascontiguousarray(g("final_norm").astype(f32).reshape(8, 128).T),
            mgT=fm(g("mix_gain"), 8), mqnT=fm(g("mla_q_norm"), 2), mkvnT=fm(g("mla_kv_norm"), 1),
            w_in=np.ascontiguousarray(g("w_in").astype(f32)), w_uq=np.ascontiguousarray(g("mla_w_uq").astype(f32)),
            w_ukv=np.ascontiguousarray(g("mla_w_ukv").astype(f32)), w_out=np.ascontiguousarray(g("w_out").astype(f32)),
            w_gate=np.ascontiguousarray(g("ffn_w_gate").astype(f32)), w_up=np.ascontiguousarray(g("ffn_w_up").astype(f32)),
            w_down=np.ascontiguousarray(g("ffn_w_down").astype(f32)),
        )
        _HC_CACHE["shared"] = sh
    m.update(sh)
    return m


_HC_CACHE = {}


def kernel(**inputs):
    _HC_CACHE.clear()
    nc = build()
    in_maps = [prep_inputs(inputs, b) for b in range(NCORES)]
    res = run_bass_kernel_spmd(nc, in_maps, core_ids=list(range(NCORES)))
    out = np.stack([np.asarray(r["y"], dtype=np.float32) for r in res.results], axis=0)
    return out
```

```python
import numpy as np
import concourse.bass as bass
import concourse.mybir as mybir
from concourse.bass_utils import run_bass_kernel_spmd
from contextlib import ExitStack

F32 = mybir.dt.float32
BF16 = mybir.dt.bfloat16
I32 = mybir.dt.int32
AF = mybir.ActivationFunctionType
ALU = mybir.AluOpType
AX = mybir.AxisListType

CE = ('pe', 'act', 'dve', 'pool')
SAME_ENGINE_SYNC = True


class Buf:
    __slots__ = ('t', 'name', 'w', 'r', 'semname')

    def __init__(self, t, name, semname=None):
        self.t = t
        self.name = name
        self.w = None
        self.r = {}
        self.semname = semname or name

    def __getitem__(self, idx):
        return V(self, self.t[idx])

    def v(self, ap):
        return V(self, ap)


class V:
    __slots__ = ('buf', 'ap')

    def __init__(self, buf, ap):
        self.buf = buf
        self.ap = ap


class Eng:
    def __init__(self, fw, name, sem):
        self.fw = fw
        self.name = name
        self.sem = sem
        self.count = 0
        self.seen = {}
        self.prog = []
        self.snaps = {0: {}}
        self.nwait = 0
        self.ninst = 0

    def _deps(self, reads, writes):
        deps = []
        for v in reads:
            if v.buf.w is not None:
                deps.append(v.buf.w)
        for v in writes:
            if v.buf.w is not None:
                deps.append(v.buf.w)
            deps.extend(v.buf.r.values())
        need = {}
        for (key, val, sem) in deps:
            if key == self.name and (self.name == 'pe' or not SAME_ENGINE_SYNC):
                continue
            if val > self.seen.get(key, 0) and val > need.get(key, (0, None))[0]:
                need[key] = (val, sem)
        for key, (val, sem) in need.items():
            self.prog.append(('wait', sem, val))
            self.nwait += 1
            self.seen[key] = val
            if key in self.fw.E and key != self.name:
                snap = self.fw.E[key].snaps.get(val)
                if snap:
                    for k2, v2 in snap.items():
                        if v2 > self.seen.get(k2, 0):
                            self.seen[k2] = v2

    def issue(self, fn, reads=(), writes=(), signal=True):
        self._deps(reads, writes)
        self.ninst += 1
        if signal:
            self.count += 1
            self.prog.append(('inst', fn, True))
            self.snaps[self.count] = {k: v for k, v in self.seen.items() if k in CE}
            tok = (self.name, self.count, self.sem)
        else:
            self.prog.append(('inst', fn, False))
            tok = (self.name, self.count + 1, self.sem)
        for v in reads:
            v.buf.r[self.name] = tok
        for v in writes:
            v.buf.w = tok
            v.buf.r = {}
        return tok

    def dma(self, out, in_, **kw):
        return self.dma_group([(out, in_)], **kw)

    def dma_group(self, pairs, **kw):
        reads = [i for (o, i) in pairs if isinstance(i, V)]
        writes = [o for (o, i) in pairs if isinstance(o, V)]
        self._deps(reads, writes)
        buf = (writes[0].buf if writes else reads[0].buf)
        rec = self.fw.dsem.get(buf.semname)
        if rec is None:
            rec = [self.fw.new_sem('d_' + buf.semname), 0]
            self.fw.dsem[buf.semname] = rec
        for (o, i) in pairs:
            rec[1] += 1
            oa = o.ap if isinstance(o, V) else o
            ia = i.ap if isinstance(i, V) else i
            self.prog.append(('dma', oa, ia, rec[0], kw))
        tok = (('d', buf.semname), 16 * rec[1], rec[0])
        for v in reads:
            v.buf.r[tok[0]] = tok
        for v in writes:
            v.buf.w = tok
            v.buf.r = {}
        return tok

    def wait_tok(self, tok):
        key, val, sem = tok
        if val > self.seen.get(key, 0):
            self.prog.append(('wait', sem, val))
            self.seen[key] = val

    def replay(self, e):
        for item in self.prog:
            if item[0] == 'wait':
                e.wait_ge(item[1], item[2])
            elif item[0] == 'inst':
                ins = item[1](e)
                if item[2]:
                    ins.then_inc(self.sem, 1)
            else:
                _, oa, ia, sem, kw = item
                e.dma_start(out=oa, in_=ia, **kw).then_inc(sem, 16)


class FW:
    def __init__(self, nc, stack):
        self.nc = nc
        self.stack = stack
        self.nsem = 0
        self.dsem = {}
        self.dma_toks = []
        self.E = {}
        for name in CE:
            self.E[name] = Eng(self, name, self.new_sem('c_' + name))
        self.sp = Eng(self, 'sp', None)
        self.pe, self.act, self.dve, self.pool = (self.E[n] for n in CE)

    def new_sem(self, name):
        self.nsem += 1
        return self.stack.enter_context(self.nc.semaphore(name))

    def sbuf(self, name, shape, dtype):
        t = self.stack.enter_context(self.nc.sbuf_tensor(name, list(shape), dtype))
        return Buf(t, name)

    def psum(self, name, shape, dtype):
        t = self.stack.enter_context(self.nc.psum_tensor(name, list(shape), dtype))
        return Buf(t, name)

    def barrier(self):
        toks = [(n, self.E[n].count, self.E[n].sem) for n in CE if self.E[n].count > 0]
        for e in list(self.E.values()) + [self.sp]:
            for tok in toks:
                if tok[0] != e.name:
                    e.wait_tok(tok)
            for tok in self.dma_toks:
                e.wait_tok(tok)
        self.dma_toks = []

    def check(self):
        engs = list(self.E.values()) + [self.sp]
        pc = {e.name: 0 for e in engs}
        sems = {}
        progress = True
        while progress:
            progress = False
            for e in engs:
                while pc[e.name] < len(e.prog):
                    it = e.prog[pc[e.name]]
                    if it[0] == 'wait':
                        if sems.get(id(it[1]), 0) < it[2]:
                            break
                    elif it[0] == 'inst':
                        if it[2]:
                            sems[id(e.sem)] = sems.get(id(e.sem), 0) + 1
                    else:
                        sems[id(it[3])] = sems.get(id(it[3]), 0) + 16
                    pc[e.name] += 1
                    progress = True
        stuck = {}
        for e in engs:
            if pc[e.name] < len(e.prog):
                it = e.prog[pc[e.name]]
                who = [n for n, x in self.E.items() if x.sem is it[1]] or [k for k, r in self.dsem.items() if r[0] is it[1]]
                stuck[e.name] = (pc[e.name], len(e.prog), who, it[2], sems.get(id(it[1]), 0))
        return stuck

    def emit(self):
        st = self.check()
        assert not st, f"DEADLOCK: {st}"
        with self.nc.Block() as block:
            @block.sync
            def _(e):
                self.sp.replay(e)

            @block.tensor
            def _(e):
                self.pe.replay(e)

            @block.scalar
            def _(e):
                self.act.replay(e)

            @block.vector
            def _(e):
                self.dve.replay(e)

            @block.gpsimd
            def _(e):
                self.pool.replay(e)

    def mm(self, out, lhsT, rhs, start=True, stop=True, signal=None, **kw):
        if signal is None:
            signal = stop
        return self.pe.issue(
            lambda e: e.matmul(out.ap, lhsT.ap, rhs.ap, start=start, stop=stop, **kw),
            reads=[lhsT, rhs], writes=[out], signal=signal)

    def transpose(self, out, in_, ident, signal=True):
        return self.pe.issue(
            lambda e: e.transpose(out.ap, in_.ap, ident.ap),
            reads=[in_, ident], writes=[out], signal=signal)

    def activation(self, out, in_, func, scale=1.0, bias=0.0, accum_out=None, eng=None):
        reads = [in_]
        sc = scale
        bi = bias
        if isinstance(scale, V):
            reads.append(scale)
            sc = scale.ap
        if isinstance(bias, V):
            reads.append(bias)
            bi = bias.ap
        writes = [out]
        kw = {}
        if accum_out is not None:
            writes.append(accum_out)
            kw['accum_out'] = accum_out.ap
        return self.act.issue(
            lambda e: e.activation(out=out.ap, in_=in_.ap, func=func, scale=sc, bias=bi, **kw),
            reads=reads, writes=writes)

    def tt(self, eng, out, in0, in1, op):
        return eng.issue(lambda e: e.tensor_tensor(out=out.ap, in0=in0.ap, in1=in1.ap, op=op),
                         reads=[in0, in1], writes=[out])

    def ts(self, eng, out, in0, s1, op0, s2=None, op1=None, accum_out=None):
        reads = [in0]
        a1 = s1
        a2 = s2
        if isinstance(s1, V):
            reads.append(s1)
            a1 = s1.ap
        if isinstance(s2, V):
            reads.append(s2)
            a2 = s2.ap
        kw = {}
        writes = [out]
        if op1 is not None:
            kw['op1'] = op1
        if accum_out is not None:
            kw['accum_out'] = accum_out.ap
            writes.append(accum_out)
        return eng.issue(lambda e: e.tensor_scalar(out=out.ap, in0=in0.ap, scalar1=a1, scalar2=a2, op0=op0, **kw),
                         reads=reads, writes=writes)

    def stt(self, out, in0, scalar, in1, op0, op1):
        reads = [in0, in1]
        a = scalar
        if isinstance(scalar, V):
            reads.append(scalar)
            a = scalar.ap
        return self.dve.issue(
            lambda e: e.scalar_tensor_tensor(out=out.ap, in0=in0.ap, scalar=a, in1=in1.ap, op0=op0, op1=op1),
            reads=reads, writes=[out])

    def copy(self, eng, out, in_):
        if eng is self.act:
            return eng.issue(lambda e: e.copy(out=out.ap, in_=in_.ap), reads=[in_], writes=[out])
        return eng.issue(lambda e: e.tensor_copy(out=out.ap, in_=in_.ap), reads=[in_], writes=[out])

    def memset(self, eng, out, val):
        return eng.issue(lambda e: e.memset(out.ap, val), writes=[out])

D = 1024
S = 2048
DEPTH = 4
NCORES = 8
IN_W = 2720
FH = 2816
OFF_AQ, OFF_AK, OFF_AV = 0, 384, 768
OFF_BQ, OFF_BKV, OFF_BKR = 1152, 1408, 1536
OFF_CQ, OFF_CK, OFF_CV, OFF_CG = 1568, 1760, 1952, 2336
EPS = 1e-6
NEG = -30000.0
SLAB = 209984
FFN_GROUPS = [(0, 6), (6, 6), (12, 5), (17, 5)]


def _t5_bucket(dist):
    n_buckets, max_distance = 32, 2048
    max_exact = n_buckets // 2
    safe = np.maximum(dist, 1).astype(np.float32)
    large = max_exact + (np.log(safe / max_exact) / np.log(max_distance / max_exact) * (n_buckets - max_exact)).astype(np.int32)
    large = np.minimum(large, n_buckets - 1)
    return np.where(dist < max_exact, dist, large).astype(np.int32)


def host_consts(rel_bias):
    f32 = np.float32
    k = np.arange(128)[:, None]
    ql = np.arange(256)[None, :]
    q3 = np.arange(128)[None, :]
    d1 = ql - k
    v1 = (d1 >= 0) & (d1 <= 128)
    i1 = _t5_bucket(np.clip(d1, 0, 128))
    v2 = v1
    i2 = _t5_bucket(np.clip(4 * d1, 0, 512))
    d3 = q3 - k
    v3 = d3 >= 0
    i3 = _t5_bucket(np.clip(16 * d3, 0, 2048))
    bmb = np.zeros((128, 6, 640), f32)
    for h in range(6):
        bmb[:, h, 0:256] = np.where(v1, rel_bias[i1, h], 0.0)
        bmb[:, h, 256:512] = np.where(v2, rel_bias[i2, h], 0.0)
        bmb[:, h, 512:640] = np.where(v3, rel_bias[i3, h], 0.0)
    bmm = np.concatenate([np.where(v1, 0.0, NEG), np.where(v2, 0.0, NEG), np.where(v3, 0.0, NEG)], axis=1).astype(f32)
    cm = np.where(q3 >= k, 0.0, NEG).astype(f32)
    H = 6
    log_g = np.log(1.0 - 2.0 ** (-5.0 - np.arange(H))).astype(f32)
    i = np.arange(128, dtype=f32)
    rel = i[:, None] - i[None, :]
    decay_intra = (np.exp(np.maximum(rel, 0.0)[None] * log_g[:, None, None]) * (rel >= 0)[None]).astype(f32)
    xi = np.exp((i + 1.0)[None, :] * log_g[:, None]).astype(f32)
    zeta = np.exp((128 - 1.0 - i)[None, :] * log_g[:, None]).astype(f32)
    cd = np.exp(128 * log_g).astype(f32)
    s = f32(32 ** -0.5)
    decT = np.zeros((128, 6, 128), f32)
    for h in range(H):
        decT[:, h, :] = decay_intra[h].T * s
    xi_t = np.zeros((64, 3, 128), f32)
    zeta_t = np.zeros((128, 3, 64), f32)
    gam = np.zeros((64, 3), f32)
    for p in range(3):
        for hh in range(2):
            h = 2 * p + hh
            xi_t[hh * 32:(hh + 1) * 32, p, :] = xi[h][None, :] * s
            zeta_t[:, p, hh * 32:(hh + 1) * 32] = zeta[h][:, None]
            gam[hh * 32:(hh + 1) * 32, p] = cd[h]
    half = 16
    inv_freq = (1.0 / (10000.0 ** (np.arange(half, dtype=f32) / half))).astype(f32)
    invf = np.tile(inv_freq, 8).reshape(128, 1).astype(f32)
    return dict(bmb=bmb, bmm=bmm, cm=cm, decT=decT, xi=xi_t, zeta=zeta_t, gam=gam, invf=invf)


class Arena:
    def __init__(self, nc, base, size, name):
        self.nc, self.base, self.size, self.name = nc, base, size, name
        self.off = 0
        self.n = 0

    def reset(self):
        self.off = 0

    def alloc(self, fw, name, shape, dtype):
        nbytes = int(np.prod(shape[1:])) * mybir.dt.size(dtype)
        nbytes = (nbytes + 31) // 32 * 32
        assert self.off + nbytes <= self.size, (self.name, name, self.off, nbytes, self.size)
        self.n += 1
        t = self.nc.alloc_sbuf_tensor_at(f"{self.name}_{name}_{self.n}", list(shape), dtype, offset=self.base + self.off)
        self.off += nbytes
        return Buf(t, f"{self.name}_{name}_{self.n}", semname=f"{self.name}_{name}")


def build(depth=DEPTH, dbg=None, skip=()):
    nc = bass.Bass("TRN2", target_bir_lowering=False)
    dbg = dbg or []
    dbg_out = {}
    MARKS.clear()

    def din(name, shape, dt=F32):
        return nc.dram_tensor(name, list(shape), dt, kind="ExternalInput").ap()

    x_d = din("x", [S, D])
    cT_d = din("cT", [128, 8])
    pos_d = din("posb", [1, S], I32)
    invf_d = din("invf", [128, 1])
    bmb_d = din("bmb", [128, 6, 640])
    bmm_d = din("bmm", [128, 640])
    cm_d = din("cm", [128, 128])
    decT_d = din("decT", [128, 6, 128])
    xi_d = din("xi", [64, 3, 128])
    zeta_d = din("zeta", [128, 3, 64])
    gam_d = din("gam", [64, 3])
    adab_d = din("adabT", [128, DEPTH, 48])
    n1g_d = din("n1gT", [128, DEPTH, 8])
    n2g_d = din("n2gT", [128, DEPTH, 8])
    fng_d = din("fngT", [128, 8])
    mg_d = din("mgT", [128, DEPTH, 8])
    mqn_d = din("mqnT", [128, DEPTH, 2])
    mkvn_d = din("mkvnT", [128, DEPTH, 1])
    if depth > 0:
        adaw_d = din("ada_w", [DEPTH, D, 6 * D])
        win_d = din("w_in", [DEPTH, D, IN_W])
        wuq_d = din("w_uq", [DEPTH, 256, 384])
        wukv_d = din("w_ukv", [DEPTH, 128, 512])
        wout_d = din("w_out", [DEPTH, D, D])
        wg_d = din("w_gate", [DEPTH, D, FH])
        wu_d = din("w_up", [DEPTH, D, FH])
        wd_d = din("w_down", [DEPTH, FH, D])
        xsp_d = nc.dram_tensor("xspill", [128, 8, S], F32, kind="Internal").ap()
    y_d = nc.dram_tensor("y", [S, D], F32, kind="ExternalOutput").ap()

    st = ExitStack()
    with st:
        fw = FW(nc, st)
        pe, act, dve, pool, sp = fw.pe, fw.act, fw.dve, fw.pool, fw.sp
        base0 = (nc.sbuf_base + 31) // 32 * 32
        st.enter_context(nc.sbuf_tensor("slab", [128, SLAB], mybir.dt.uint8))
        assert nc.sbuf_base == base0 + SLAB, (nc.sbuf_base, base0)
        o = base0
        SZ_HT, SZ_X, SZ_Y, SZ_ADAW, SZ_W = 32768, 65536, 32768, 12288, 36864
        SZ_C = SLAB - (SZ_HT + SZ_X + SZ_Y + SZ_ADAW + SZ_W)
        AC = Arena(nc, o, SZ_C, "c"); o += SZ_C
        AH = Arena(nc, o, SZ_HT, "h"); o += SZ_HT
        AX = Arena(nc, o, SZ_X, "x"); o += SZ_X
        AY = Arena(nc, o, SZ_Y, "y"); o += SZ_Y
        AA = Arena(nc, o, SZ_ADAW, "a"); o += SZ_ADAW
        AW = Arena(nc, o, SZ_W, "w"); o += SZ_W

        PS = [fw.psum(f"ps{i}", [128, 512], F32) for i in range(8)]

        def sub(buf, name):
            return Buf(buf.t, name)

        ident_f = AC.alloc(fw, "identf", [128, 128], F32)
        ident_b = AC.alloc(fw, "identb", [128, 128], BF16)
        ones_b = AC.alloc(fw, "onesb", [128, 128], BF16)
        BD = AC.alloc(fw, "bd", [128, 128], BF16)
        WNe = AC.alloc(fw, "wne", [128, 64], BF16)
        WNo = AC.alloc(fw, "wno", [128, 64], BF16)
        FCOS = AC.alloc(fw, "fcos", [128, S], BF16)
        FSIN = AC.alloc(fw, "fsin", [128, S], BF16)
        BM = AC.alloc(fw, "bm", [128, 6, 640], BF16)
        CM = AC.alloc(fw, "cm", [128, 128], F32)
        DECT = AC.alloc(fw, "dect", [128, 6, 128], F32)
        XI = AC.alloc(fw, "xi", [64, 3, 128], F32)
        ZETA = AC.alloc(fw, "zeta", [128, 3, 64], F32)
        GAM = AC.alloc(fw, "gam", [64, 3], F32)
        MOD = AC.alloc(fw, "mod", [128, DEPTH, 48], F32)
        A1 = AC.alloc(fw, "a1", [128, DEPTH, 8], F32)
        A2 = AC.alloc(fw, "a2", [128, DEPTH, 8], F32)
        ADAB = AC.alloc(fw, "adab", [128, DEPTH, 48], F32)
        N1G = AC.alloc(fw, "n1g", [128, DEPTH, 8], F32)
        N2G = AC.alloc(fw, "n2g", [128, DEPTH, 8], F32)
        FNG = AC.alloc(fw, "fng", [128, 8], F32)
        MG = AC.alloc(fw, "mg", [128, DEPTH, 8], F32)
        MQN = AC.alloc(fw, "mqn", [128, DEPTH, 2], F32)
        MKVN = AC.alloc(fw, "mkvn", [128, DEPTH, 1], F32)
        CTF = AC.alloc(fw, "ctf", [128, 8], F32)
        CTB = AC.alloc(fw, "ctb", [128, 8], BF16)
        INVF = AC.alloc(fw, "invf", [128, 1], F32)
        hT = AH.alloc(fw, "hT", [128, 8, S], BF16)
        adaw = [AA.alloc(fw, f"adaw{i}", [128, 3072], BF16) for i in range(2)]

        def dump(name, view, shape, dt):
            if name not in dbg:
                return
            t = nc.dram_tensor("dbg_" + name, list(shape), dt, kind="ExternalOutput").ap()
            dbg_out[name] = sp.dma(t, view)
            fw.dma_toks.append(dbg_out[name])

        fw.memset(dve, ident_f[:], 1.0)
        pool.issue(lambda e: e.affine_select(out=ident_f.t[:], in_=ident_f.t[:], pattern=[[-1, 128]],
                                             compare_op=ALU.is_equal, fill=0.0, base=0, channel_multiplier=1),
                   reads=[ident_f[:]], writes=[ident_f[:]])
        fw.copy(dve, ident_b[:], ident_f[:])
        fw.memset(dve, ones_b[:], 1.0)
        fw.memset(dve, BD[:], 0.0)
        fw.memset(dve, BD[0:64, 0:64], 1.0 / 64)
        fw.memset(dve, BD[64:128, 64:128], 1.0 / 64)
        fw.memset(dve, WNe[:], 0.0)
        fw.memset(dve, WNe[0:64, :], 1.0)
        if 'wn' not in skip:
            fw.memset(dve, WNe[64:65, :], 64 * EPS)
        fw.memset(dve, WNo[:], 0.0)
        fw.memset(dve, WNo[64:128, :], 1.0)
        if 'wn' not in skip:
            fw.memset(dve, WNo[0:1, :], 64 * EPS)
        for (b, d_) in [] if 'small' in skip else [(CM, cm_d), (DECT, decT_d), (XI, xi_d), (ZETA, zeta_d), (GAM, gam_d), (ADAB, adab_d),
                        (N1G, n1g_d), (N2G, n2g_d), (FNG, fng_d), (MG, mg_d), (MQN, mqn_d), (MKVN, mkvn_d),
                        (CTF, cT_d), (INVF, invf_d)]:
            sp.dma(b[:], d_)
        if 'silu' not in skip:
            fw.activation(CTB[:], CTF[:], AF.Silu)
        AY.reset()
        bmb_s = AY.alloc(fw, "bmb", [128, 6, 640], F32)
        bmm_s = AY.alloc(fw, "bmm", [128, 640], F32)
        if 'bm' not in skip:
            sp.dma(bmb_s[:], bmb_d)
            sp.dma(bmm_s[:], bmm_d)
            for h in range(6):
                fw.tt(dve, BM[:, h, :], bmb_s[:, h, :], bmm_s[:], ALU.add)
        fw.barrier()
        AY.reset()
        posi = AY.alloc(fw, "posi", [128, S], I32)
        ang = AY.alloc(fw, "ang", [128, S], F32)
        t1 = AY.alloc(fw, "t1", [128, S], F32)
        t2 = AY.alloc(fw, "t2", [128, S], F32)
        if 'tables' not in skip:
            sp.dma(posi[:], pos_d.partition_broadcast(128))
            fw.copy(dve, t1[:], posi[:])
            fw.ts(dve, ang[:], t1[:], INVF[:, 0:1], ALU.mult)
            TWO_PI = 2.0 * np.pi
            C1 = 6.28125
            C2 = float(np.float32(TWO_PI - C1))
            fw.ts(dve, t1[:], ang[:], 1.0 / TWO_PI, ALU.mult)
            fw.copy(dve, posi[:], t1[:])
            fw.copy(dve, t1[:], posi[:])
            fw.stt(t2[:], t1[:], -C1, ang[:], ALU.mult, ALU.add)
            fw.stt(t2[:], t1[:], -C2, t2[:], ALU.mult, ALU.add)

            def wrap(dst, src, tmp):
                fw.ts(dve, tmp, src, float(np.pi), ALU.is_gt)
                fw.stt(dst, tmp, -TWO_PI, src, ALU.mult, ALU.add)
                fw.ts(dve, tmp, dst, -float(np.pi), ALU.is_lt)
                fw.stt(dst, tmp, TWO_PI, dst, ALU.mult, ALU.add)
                fw.ts(dve, dst, dst, 3.1415925, ALU.min, -3.1415925, ALU.max)

            wrap(t2[:], t2[:], t1[:])
            fw.activation(FSIN[:], t2[:], AF.Sin)
            fw.ts(dve, ang[:], t2[:], float(np.pi / 2), ALU.add)
            wrap(ang[:], ang[:], t1[:])
            fw.activation(FCOS[:], ang[:], AF.Sin)
        fw.barrier()

        AX.reset()
        xT = AX.alloc(fw, "xT", [128, 8, S], F32)
        AW.reset()
        io = [AW.alloc(fw, f"io{i}", [128, D], F32) for i in range(2)]
        for n in range(16):
            sp.dma(io[n % 2][:], x_d[n * 128:(n + 1) * 128, :])
            for g in range(2):
                pb = PS[(2 * n + g) % 4]
                for j in range(4):
                    kc = 4 * g + j
                    fw.transpose(pb[:, j * 128:(j + 1) * 128], io[n % 2][:, kc * 128:(kc + 1) * 128], ident_f[:], signal=(j == 3))
                src = pb.v(pb.t[:, :].rearrange("p (j t) -> p j t", j=4))
                dst = xT.v(xT.t[:, 4 * g:4 * g + 4, n * 128:(n + 1) * 128])
                fw.copy(act if (n + g) % 2 else dve, dst, src)
        fw.barrier()

        def mod_pieces(l):
            mp = PS[7]
            for piece in range(16):
                kc, half = piece // 2, piece % 2
                slot = adaw[piece % 2]
                pool.dma(slot[:], adaw_d[l, kc * 128:(kc + 1) * 128, half * 3072:(half + 1) * 3072], max_dma_last_dim=4096)
                for j in range(24):
                    jj = half * 24 + j
                    fw.mm(mp[:, jj:jj + 1], slot[:, j * 128:(j + 1) * 128], CTB[:, kc:kc + 1],
                          start=(piece == 0 and j == 0), stop=(kc == 7), signal=(j == 23), skip_group_check=True)
                yield
            fw.tt(dve, MOD[:, l, :], mp[:, 0:48], ADAB[:, l, :], ALU.add)
            fw.stt(A1[:, l, :], MOD[:, l, 8:16], 1.0, N1G[:, l, :], ALU.add, ALU.mult)
            fw.stt(A2[:, l, :], MOD[:, l, 32:40], 1.0, N2G[:, l, :], ALU.add, ALU.mult)
            yield

        EPSC = AC.alloc(fw, "epsc", [128, 1], F32)
        fw.memset(dve, EPSC[:], EPS)

        def norm_to_hT(l, a_view, b_col0, ar):
            ar.reset()
            sq = [ar.alloc(fw, f"nsq{i}", [128, 8, 512], BF16) for i in range(2)]
            lnt = ar.alloc(fw, "nln", [128, 512], F32)
            rs = [ar.alloc(fw, f"nrs{i}", [128, 512], F32) for i in range(2)]
            tm = [ar.alloc(fw, f"ntm{i}", [128, 512], F32) for i in range(3)]
            for tb in range(4):
                ts_ = slice(tb * 512, (tb + 1) * 512)
                fw.activation(sq[tb % 2][:], xT[:, :, ts_], AF.Square)
                pb = PS[tb % 2]
                for kc in range(8):
                    fw.mm(pb[:], ones_b[:], sq[tb % 2][:, kc, :], start=(kc == 0), stop=(kc == 7))
                fw.activation(lnt[:], pb[:], AF.Ln, scale=1.0 / D, bias=EPSC[:, 0:1])
                fw.activation(rs[tb % 2][:], lnt[:], AF.Exp, scale=-0.5)
                for kc in range(8):
                    t = tm[kc % 3]
                    fw.tt(dve, t[:], xT[:, kc, ts_], rs[tb % 2][:], ALU.mult)
                    fw.activation(hT[:, kc, ts_], t[:], AF.Identity, scale=a_view(kc), bias=MOD[:, l, b_col0 + kc:b_col0 + kc + 1])

        def load_w(buf_view, dram_ap):
            pool.dma(buf_view, dram_ap, max_dma_last_dim=4096)

        def proj_fm(out_fn, w_fn, M, nk=8, rhs_fn=None, banks=(6, 7), out_p0=0):
            for tb in range(4):
                pb = PS[banks[tb % len(banks)]]
                ts_ = slice(tb * 512, (tb + 1) * 512)
                pv = pb[out_p0:out_p0 + M, :]
                for kc in range(nk):
                    rhs = rhs_fn(kc, ts_) if rhs_fn else hT[:, kc, ts_]
                    fw.mm(pv, w_fn(kc), rhs, start=(kc == 0), stop=(kc == nk - 1))
                out_fn(tb, ts_, pv)

        def finalize_head(bank, nr, mixb, chunk, tb, fin):
            SQb, LNb, RSb = fin
            ts_ = slice(tb * 512, (tb + 1) * 512)
            fw.activation(SQb[:], bank[:], AF.Square)
            pf = PS[6 + (tb % 2)]
            wn = WNe if nr == 0 else WNo
            fw.mm(pf[nr:nr + 64, :], wn[:], SQb[:], start=True, stop=True)
            fw.activation(LNb[nr:nr + 64, :], pf[nr:nr + 64, :], AF.Ln, scale=1.0 / 64)
            fw.activation(RSb[nr:nr + 64, :], LNb[nr:nr + 64, :], AF.Exp, scale=-0.5)
            fw.tt(dve, mixb[nr:nr + 64, chunk, ts_], bank[nr:nr + 64, :], RSb[nr:nr + 64, :], ALU.mult)

        def v_lhsT(vbuf, tile_off, ones_off, even):
            t = vbuf.t
            pstride = t[:, :].ap[0][0]
            if even:
                return vbuf.v(bass.AP(t, tile_off, [[pstride, 128], [ones_off - tile_off, 2], [1, 64]]))
            return vbuf.v(bass.AP(t, ones_off, [[pstride, 128], [tile_off - ones_off, 2], [1, 64]]))

        def run_pipe(units, la=2):
            nU = len(units)
            for i in range(nU + la):
                if i < nU:
                    units[i][0]()
                if i - la >= 0:
                    units[i - la][1]()

        for l in range(depth):
            MARKS.append(('mod', l, pe.ninst))
            if l == 0:
                for _ in mod_pieces(0):
                    pass
            MARKS.append(('norm1', l, pe.ninst))
            AY.reset()
            norm_to_hT(l, lambda kc: A1[:, l, kc:kc + 1], 0, AY)
            dump(f"hT{l}", hT[:], [128, 8, S], BF16)
            for kc in range(8):
                sp.dma(xsp_d[:, kc, :], xT[:, kc, :])
            fw.barrier()
            _rec = fw.dsem[xT.semname]
            spill_tok = (("d", xT.semname), 16 * _rec[1], _rec[0])
            for e in (pe, act, dve, pool):
                e.wait_tok(spill_tok)

            AY.reset()
            mixT = AY.alloc(fw, "mixT", [128, 8, S], BF16)

            MARKS.append(('A', l, pe.ninst))
            AX.reset()
            qT = AX.alloc(fw, "qT", [128, S], BF16)
            kT = AX.alloc(fw, "kT", [128, S], BF16)
            VA = AX.alloc(fw, "VA", [128, 48, 2, 128], BF16)
            PT = [AX.alloc(fw, f"pt{i}", [128, 512], BF16) for i in range(4)]
            TS = [AX.alloc(fw, f"ts{i}", [128, 512], F32) for i in range(4)]
            fin = (AX.alloc(fw, "fsq", [128, 512], BF16), AX.alloc(fw, "fln", [128, 512], F32), AX.alloc(fw, "frs", [128, 512], F32))
            AW.reset()
            wq = AW.alloc(fw, "wq", [128, 8, 128], BF16)
            wk = AW.alloc(fw, "wk", [128, 8, 128], BF16)
            wv = AW.alloc(fw, "wv", [128, 8, 128], BF16)
            SS = [sub(PS[4], "ss0"), sub(PS[4], "ss1"), sub(PS[5], "ss2"), sub(PS[5], "ss3")]
            fw.memset(pool, VA[:, :, 0, 64:128], 1.0)
            fw.memset(pool, VA[:, :, 1, 0:64], 1.0)
            winv = win_d[l].rearrange("(k p) n -> p k n", p=128)
            blk = [0]
            for c in (range(3) if 'A' not in skip else []):
                load_w(wq[:], winv[:, :, OFF_AQ + c * 128:OFF_AQ + (c + 1) * 128])
                load_w(wk[:], winv[:, :, OFF_AK + c * 128:OFF_AK + (c + 1) * 128])
                load_w(wv[:], winv[:, :, OFF_AV + c * 128:OFF_AV + (c + 1) * 128])
                proj_fm(lambda tb, ts_, pv: fw.activation(qT[:, ts_], pv, AF.Identity, scale=0.125),
                        lambda kc: wq[:, kc, :], 128)
                proj_fm(lambda tb, ts_, pv: fw.copy(dve, kT[:, ts_], pv), lambda kc: wk[:, kc, :], 128)
                def tok_ap(ordr, tile, kc):
                    if ordr == 0:
                        return hT[:, kc, tile * 128:(tile + 1) * 128]
                    if ordr == 1:
                        r, b = tile // 4, tile % 4
                        return hT[:, kc, 512 * b + r:512 * (b + 1):4]
                    return hT[:, kc, tile::16]
                for ordr in range(3):
                    for tg in range(4):
                        pb = PS[6 + (tg % 2)]
                        for tt_ in range(4):
                            tile = tg * 4 + tt_
                            for kc in range(8):
                                fw.mm(pb[:, tt_ * 128:(tt_ + 1) * 128], tok_ap(ordr, tile, kc), wv[:, kc, :],
                                      start=(kc == 0), stop=(kc == 7), signal=(kc == 7 and tt_ == 3))
                        t0_ = ordr * 16 + tg * 4
                        pv4 = pb.t[:, :].rearrange("p (n c) -> p n c", n=4)
                        ev_ = act if tg % 2 else dve
                        fw.copy(ev_, VA[:, t0_:t0_ + 4, 0, 0:64], pb.v(pv4[:, :, 0:64]))
                        fw.copy(ev_, VA[:, t0_:t0_ + 4, 1, 64:128], pb.v(pv4[:, :, 64:128]))
                if c == 0:
                    dump(f"qTA{l}", qT[:], [128, S], BF16)
                    dump(f"kTA{l}", kT[:], [128, S], BF16)
                    dump(f"VA{l}", VA[:], [128, 48, 2, 128], BF16)
                for hh in range(2):
                    h = 2 * c + hh
                    ro = 64 * hh
                    started = [False] * 4

                    unitsA = []

                    def sunit(subs, bm_off, w):
                        i = blk[0]
                        blk[0] += 1
                        ss = PS[4 + i % 4]
                        tsb = TS[i % 4]
                        pt = PT[i % 4]
                        offs = []
                        off = 0
                        for s_ in subs:
                            offs.append(off)
                            off += s_[2]
                        tot = off
                        nfull = sum(1 for s_ in subs if s_[2] == w)

                        def st1():
                            for (kcols, qcols, nq, pvl), o_ in zip(subs, offs):
                                fw.mm(ss[:, o_:o_ + nq], kcols, qcols, start=True, stop=True)
                            bmv = BM.v(bass.AP(BM.t, h * 640 + bm_off, [[6 * 640, 128], [0, nfull], [1, w]]))
                            fw.tt(dve, tsb.v(tsb.t[:, 0:nfull * w].rearrange("p (n q) -> p n q", n=nfull)),
                                  ss.v(ss.t[:, 0:nfull * w].rearrange("p (n q) -> p n q", n=nfull)), bmv, ALU.add)
                            if nfull < len(subs):
                                nql = subs[-1][2]
                                fw.tt(dve, tsb[:, nfull * w:nfull * w + nql], ss[:, nfull * w:nfull * w + nql],
                                      BM[:, h, bm_off:bm_off + nql], ALU.add)
                            fw.activation(pt[:, 0:tot], tsb[:, 0:tot], AF.Exp)

                        def st2():
                            for (kcols, qcols, nq, pvl), o_ in zip(subs, offs):
                                for (qs, vtile, tbk, ocols) in pvl:
                                    fw.mm(ocols, VA[:, vtile, hh, :], pt[:, o_ + qs.start:o_ + qs.stop], start=(not started[tbk]),
                                          stop=False, signal=True, skip_group_check=True)
                                    started[tbk] = True
                        unitsA.append((st1, st2))

                    def p1_sub(kt):
                        nqt = 2 if kt < 15 else 1
                        pvl = []
                        for j in range(nqt):
                            qt = kt + j
                            pvl.append((slice(j * 128, (j + 1) * 128), 0 * 16 + kt, qt // 4,
                                        PS[qt // 4][:, (qt % 4) * 128:(qt % 4 + 1) * 128]))
                        return (kT[ro:ro + 64, kt * 128:(kt + 1) * 128], qT[ro:ro + 64, kt * 128:kt * 128 + nqt * 128], nqt * 128, pvl)

                    def p2_sub(r, b_):
                        nqt = 2 if b_ < 3 else 1
                        pvl = []
                        for j in range(nqt):
                            pvl.append((slice(j * 128, (j + 1) * 128), 16 + r * 4 + b_, b_ + j, PS[b_ + j][:, r::4]))
                        return (kT[ro:ro + 64, 512 * b_ + r:512 * (b_ + 1):4], qT[ro:ro + 64, 512 * b_ + r:512 * (b_ + nqt):4],
                                nqt * 128, pvl)

                    def p3_sub(r):
                        pvl = []
                        for tbk in range(4):
                            pvl.append((slice(tbk * 32, (tbk + 1) * 32), 32 + r, tbk, PS[tbk][:, r::16]))
                        return (kT[ro:ro + 64, r::16], qT[ro:ro + 64, r::16], 128, pvl)

                    for kt in range(0, 16, 2):
                        sunit([p1_sub(kt), p1_sub(kt + 1)], 0, 256)
                    for r in range(4):
                        for b_ in (0, 2):
                            sunit([p2_sub(r, b_), p2_sub(r, b_ + 1)], 256, 256)
                    for r in range(0, 16, 4):
                        sunit([p3_sub(r + j) for j in range(4)], 512, 128)
                    run_pipe(unitsA)
                    for tb in range(4):
                        finalize_head(PS[tb], ro, mixT, c, tb, fin)
            dump(f"mixA{l}", mixT[:, 0:3, :], [128, 3, S], BF16)
            fw.barrier()

            MARKS.append(('B', l, pe.ninst))
            AX.reset()
            qlatT = AX.alloc(fw, "qlatT", [128, 2, S], BF16)
            kvlatT = AX.alloc(fw, "kvlatT", [128, S], BF16)
            sqb = [AX.alloc(fw, "sqb0", [128, 2, 512], BF16)] * 2
            rsq = AX.alloc(fw, "rsq", [128, S], F32)
            rskv = AX.alloc(fw, "rskv", [128, S], F32)
            rstok = AX.alloc(fw, "rstok", [128, 16], F32)
            qTh = AX.alloc(fw, "qTh", [128, S], BF16)
            KTh = AX.alloc(fw, "KTh", [128, S], BF16)
            VBs = [AX.alloc(fw, f"VB{i}", [128, 16, 128], BF16) for i in range(2)]
            PTB = [AX.alloc(fw, f"ptb{i}", [128, 512], BF16) for i in range(4)]
            TSB = [AX.alloc(fw, f"tsb{i}", [128, 128], F32) for i in range(2)]
            RT = [AX.alloc(fw, f"rt{i}", [128, 512], F32) for i in range(3)]
            fin = (AX.alloc(fw, "fsq", [128, 512], BF16), AX.alloc(fw, "fln", [128, 512], F32), AX.alloc(fw, "frs", [128, 512], F32))
            AW.reset()
            winB = AW.alloc(fw, "winB", [128, 8, 416], BF16)
            wkrr = AW.alloc(fw, "wkrr", [128, 8, 32], BF16)
            wuq_f = AW.alloc(fw, "wuqf", [128, 2, 384], F32)
            wuq_b = AW.alloc(fw, "wuqb", [128, 2, 384], BF16)
            wuq_r = AW.alloc(fw, "wuqr", [128, 2, 4, 32], BF16)
            wukv_f = AW.alloc(fw, "wukvf", [128, 512], F32)
            wukv_b = AW.alloc(fw, "wukvb", [128, 512], BF16)
            fw.memset(pool, VBs[0][:, :, 64:128], 1.0)
            fw.memset(pool, VBs[1][:, :, 0:64], 1.0)
            load_w(winB[:], winv[:, :, OFF_BQ:OFF_BQ + 416])
            pool.dma_group([(wkrr[:, :, 0:16], winv[:, :, OFF_BKR + 16:OFF_BKR + 32]),
                            (wkrr[:, :, 16:32], winv[:, :, OFF_BKR:OFF_BKR + 16])], max_dma_last_dim=4096)
            fw.activation(wkrr[:, :, 0:16], wkrr[:, :, 0:16], AF.Identity, scale=-1.0)
            sp.dma(wuq_f[:], wuq_d[l].rearrange("(k p) n -> p k n", p=128))
            sp.dma(wukv_f[:], wukv_d[l])
            for kc in range(2):
                fw.activation(wuq_b[:, kc, :], wuq_f[:, kc, :], AF.Identity, scale=MQN[:, l, kc:kc + 1])
            fw.activation(wukv_b[:], wukv_f[:], AF.Identity, scale=MKVN[:, l, 0:1])
            wq4 = wuq_b.t[:, :, :].rearrange("p k (h e) -> p k h e", h=4)
            fw.activation(wuq_r.v(wuq_r.t[:, :, :, 0:16]), wuq_b.v(wq4[:, :, :, 80:96]), AF.Identity, scale=-1.0)
            fw.copy(dve, wuq_r.v(wuq_r.t[:, :, :, 16:32]), wuq_b.v(wq4[:, :, :, 64:80]))

            for c2 in range(2):
                proj_fm(lambda tb, ts_, pv, c2=c2: fw.copy(dve, qlatT[:, c2, ts_], pv),
                        lambda kc, c2=c2: winB[:, kc, c2 * 128:(c2 + 1) * 128], 128)
            for tb in range(4):
                ts_ = slice(tb * 512, (tb + 1) * 512)
                fw.activation(sqb[tb % 2][:], qlatT[:, :, ts_], AF.Square)
                pb = PS[4 + tb % 2]
                for c2 in range(2):
                    fw.mm(pb[:], ones_b[:], sqb[tb % 2][:, c2, :], start=(c2 == 0), stop=(c2 == 1))
                fw.activation(RT[0][:], pb[:], AF.Ln, scale=1.0 / 256, bias=EPSC[:, 0:1])
                fw.activation(rsq[:, ts_], RT[0][:], AF.Exp, scale=-0.5)
            proj_fm(lambda tb, ts_, pv: fw.copy(dve, kvlatT[:, ts_], pv), lambda kc: winB[:, kc, 256:384], 128)
            for tb in range(4):
                ts_ = slice(tb * 512, (tb + 1) * 512)
                fw.activation(sqb[tb % 2][:, 0, :], kvlatT[:, ts_], AF.Square)
                pb = PS[4 + tb % 2]
                fw.mm(pb[:], ones_b[:], sqb[tb % 2][:, 0, :], start=True, stop=True)
                fw.activation(RT[0][:], pb[:], AF.Ln, scale=1.0 / 128, bias=EPSC[:, 0:1])
                fw.activation(rskv[:, ts_], RT[0][:], AF.Exp, scale=-0.5)
            identb4 = ident_f.v(bass.AP(ident_f.t, 0, [[128, 128], [0, 4], [1, 128]]))
            for g4 in range(4):
                tdv = RT[0].v(RT[0].t[:, :].rearrange("p (n q) -> p n q", n=4))
                fw.tt(dve, tdv, rskv.v(rskv.t[:, g4 * 512:(g4 + 1) * 512].rearrange("p (n q) -> p n q", n=4)), identb4, ALU.mult)
                _o = rstok.t[:, g4 * 4:(g4 + 1) * 4]
                _i = RT[0].t[:, :].rearrange("p (n q) -> p n q", n=4)
                dve.issue(lambda e, _o=_o, _i=_i: e.tensor_reduce(out=_o, in_=_i, axis=AX_X, op=ALU.add),
                          reads=[RT[0][:]], writes=[rstok[:]])
            for tb in range(4):
                ts_ = slice(tb * 512, (tb + 1) * 512)
                p1, p2 = PS[6], PS[7]
                for kc in range(8):
                    fw.mm(p1[64:96, :], winB[:, kc, 384:416], hT[:, kc, ts_], start=(kc == 0), stop=(kc == 7))
                for kc in range(8):
                    fw.mm(p2[64:96, :], wkrr[:, kc, :], hT[:, kc, ts_], start=(kc == 0), stop=(kc == 7))
                fw.tt(dve, RT[0][64:96, :], p1[64:96, :], FCOS[64:96, ts_], ALU.mult)
                fw.tt(dve, RT[1][64:96, :], p2[64:96, :], FSIN[64:96, ts_], ALU.mult)
                fw.tt(pool, KTh[64:96, ts_], RT[0][64:96, :], RT[1][64:96, :], ALU.add)
            dump(f"rsq{l}", rsq[:], [128, S], F32)
            SCB = float((64 + 32) ** -0.5)
            for h in (range(4) if 'B' not in skip else []):
                hh = h % 2
                nr = 64 * hh
                for tb in range(4):
                    ts_ = slice(tb * 512, (tb + 1) * 512)
                    p1, p2 = PS[6], PS[7]
                    for kc in range(2):
                        fw.mm(p1[0:96, :], wuq_b[:, kc, h * 96:(h + 1) * 96], qlatT[:, kc, ts_], start=(kc == 0), stop=(kc == 1))
                    for kc in range(2):
                        fw.mm(p2[64:96, :], wuq_r.v(wuq_r.t[:, kc, h, :]), qlatT[:, kc, ts_], start=(kc == 0), stop=(kc == 1))
                    fw.stt(qTh[0:64, ts_], p1[0:64, :], SCB, rsq[0:64, ts_], ALU.mult, ALU.mult)
                    fw.tt(dve, RT[0][64:96, :], p1[64:96, :], FCOS[64:96, ts_], ALU.mult)
                    fw.tt(dve, RT[1][64:96, :], p2[64:96, :], FSIN[64:96, ts_], ALU.mult)
                    fw.tt(pool, RT[2][64:96, :], RT[0][64:96, :], RT[1][64:96, :], ALU.add)
                    fw.stt(qTh[64:96, ts_], RT[2][64:96, :], SCB, rsq[64:96, ts_], ALU.mult, ALU.mult)
                    p3 = PS[4 + tb % 2]
                    fw.mm(p3[0:64, :], wukv_b[:, h * 128:h * 128 + 64], kvlatT[:, ts_], start=True, stop=True)
                    fw.tt(dve, KTh[0:64, ts_], p3[0:64, :], rskv[0:64, ts_], ALU.mult)
                for tg in range(2):
                    pb = PS[4 + tg]
                    for t8 in range(8):
                        tile = tg * 8 + t8
                        fw.mm(pb[:, t8 * 64:(t8 + 1) * 64], kvlatT[:, tile * 128:(tile + 1) * 128],
                              wukv_b[:, h * 128 + 64:(h + 1) * 128], start=True, stop=True, signal=(t8 == 7))
                    for t8 in range(8):
                        tile = tg * 8 + t8
                        fw.activation(VBs[hh][:, tile, 64 * hh:64 * hh + 64], pb[:, t8 * 64:(t8 + 1) * 64], AF.Identity,
                                      scale=rstok[:, tile:tile + 1])
                if h == 0:
                    dump(f"qTh{l}", qTh[0:96, :], [96, S], BF16)
                    dump(f"KTh{l}", KTh[0:96, :], [96, S], BF16)
                    dump(f"VB{l}", VBs[0][:], [128, 16, 128], BF16)
                started = [False] * 4
                unitsB = []
                bi = 0
                for kt in range(16):
                    for tb in range(kt // 4, 4):
                        q0 = max(128 * kt, 512 * tb)
                        q1 = 512 * (tb + 1)

                        def mk(kt=kt, tb=tb, q0=q0, q1=q1, bi=bi):
                            nq = q1 - q0
                            sbk = PS[4 + bi % 4]
                            pt = PTB[bi % 4]

                            def st1():
                                fw.mm(sbk[:, 0:nq], KTh[0:96, kt * 128:(kt + 1) * 128], qTh[0:96, q0:q1], start=True, stop=True)
                                if q0 == 128 * kt:
                                    tsb = TSB[kt % 2]
                                    fw.tt(dve, tsb[:], sbk[:, 0:128], CM[:], ALU.add)
                                    fw.activation(pt[:, 0:128], tsb[:], AF.Exp)
                                    if nq > 128:
                                        fw.activation(pt[:, 128:nq], sbk[:, 128:nq], AF.Exp)
                                else:
                                    fw.activation(pt[:, 0:nq], sbk[:, 0:nq], AF.Exp)

                            def st2():
                                fw.mm(PS[tb][:, q0 - 512 * tb:q1 - 512 * tb], VBs[hh][:, kt, :], pt[:, 0:nq], start=(not started[tb]),
                                      stop=False, signal=True, skip_group_check=True)
                                started[tb] = True
                            return (st1, st2)
                        unitsB.append(mk())
                        bi += 1
                run_pipe(unitsB)
                for tb in range(4):
                    finalize_head(PS[tb], nr, mixT, 3 + h // 2, tb, fin)
            dump(f"mixB{l}", mixT[:, 3:5, :], [128, 2, S], BF16)
            fw.barrier()

            MARKS.append(('C', l, pe.ninst))
            AX.reset()
            qTc = AX.alloc(fw, "qTc", [128, S], BF16)
            kTc = AX.alloc(fw, "kTc", [128, S], BF16)
            qxT = AX.alloc(fw, "qxT", [128, S], BF16)
            kz = AX.alloc(fw, "kz", [128, 16, 64], BF16)
            VC = AX.alloc(fw, "VC", [128, 16, 128], BF16)
            gT = AX.alloc(fw, "gT", [128, S], BF16)
            STf = AX.alloc(fw, "stf", [64, 16, 64], F32)
            STb = AX.alloc(fw, "stb", [64, 16, 64], BF16)
            PTC = [AX.alloc(fw, f"ptc{i}", [128, 128], BF16) for i in range(4)]
            RT = [AX.alloc(fw, f"rtc{i}", [128, 512], F32) for i in range(3)]
            CP = AX.alloc(fw, "ccp", [128, 512], BF16)
            CSQ = AX.alloc(fw, "csq", [128, 512], BF16)
            CMEAN = AX.alloc(fw, "cmean", [128, 512], F32)
            CMSQ = AX.alloc(fw, "cmsq", [128, 512], F32)
            CDV = AX.alloc(fw, "cdv", [128, 512], F32)
            CVAR = AX.alloc(fw, "cvar", [128, 512], F32)
            CRS = AX.alloc(fw, "crs", [128, 512], F32)
            AW.reset()
            wcq = AW.alloc(fw, "wcq", [128, 8, 64], BF16)
            wcqr = AW.alloc(fw, "wcqr", [128, 8, 2, 32], BF16)
            wck = AW.alloc(fw, "wck", [128, 8, 64], BF16)
            wckr = AW.alloc(fw, "wckr", [128, 8, 2, 32], BF16)
            wcv = AW.alloc(fw, "wcv", [128, 8, 128], BF16)
            wcg = AW.alloc(fw, "wcg", [128, 8, 128], BF16)
            SC_ = [sub(PS[4], "sc0"), sub(PS[4], "sc1"), sub(PS[5], "sc2"), sub(PS[5], "sc3")]
            for c in (range(3) if 'C' not in skip else []):
                def load_rot(dst, off):
                    prs = []
                    for h2 in range(2):
                        prs.append((dst.v(dst.t[:, :, h2, 0:16]), winv[:, :, off + h2 * 32 + 16:off + h2 * 32 + 32]))
                        prs.append((dst.v(dst.t[:, :, h2, 16:32]), winv[:, :, off + h2 * 32:off + h2 * 32 + 16]))
                    pool.dma_group(prs, max_dma_last_dim=4096)
                    fw.activation(dst.v(dst.t[:, :, :, 0:16]), dst.v(dst.t[:, :, :, 0:16]), AF.Identity, scale=-1.0)
                load_w(wcq[:], winv[:, :, OFF_CQ + c * 64:OFF_CQ + (c + 1) * 64])
                load_rot(wcqr, OFF_CQ + c * 64)
                load_w(wck[:], winv[:, :, OFF_CK + c * 64:OFF_CK + (c + 1) * 64])
                load_rot(wckr, OFF_CK + c * 64)
                load_w(wcv[:], winv[:, :, OFF_CV + c * 128:OFF_CV + (c + 1) * 128])
                load_w(wcg[:], winv[:, :, OFF_CG + c * 128:OFF_CG + (c + 1) * 128])
                for (w_, wr_, dst, isq) in [(wcq, wcqr, qTc, True), (wck, wckr, kTc, False)]:
                    for tb in range(4):
                        ts_ = slice(tb * 512, (tb + 1) * 512)
                        p1, p2 = PS[6], PS[7]
                        for kc in range(8):
                            fw.mm(p1[0:64, :], w_[:, kc, :], hT[:, kc, ts_], start=(kc == 0), stop=(kc == 7))
                        for kc in range(8):
                            fw.mm(p2[0:64, :], wr_.v(wr_.t[:, kc, :, :].rearrange("p h e -> p (h e)")), hT[:, kc, ts_],
                                  start=(kc == 0), stop=(kc == 7))
                        fw.tt(dve, RT[0][0:64, :], p1[0:64, :], FCOS[0:64, ts_], ALU.mult)
                        fw.tt(dve, RT[1][0:64, :], p2[0:64, :], FSIN[0:64, ts_], ALU.mult)
                        if isq:
                            fw.tt(pool, RT[2][0:64, :], RT[0][0:64, :], RT[1][0:64, :], ALU.add)
                            fw.copy(act, dst[0:64, ts_], RT[2][0:64, :])
                            xib = XI.v(bass.AP(XI.t, c * 128, [[3 * 128, 64], [0, 4], [1, 128]]))
                            fw.tt(pool, qxT.v(qxT.t[0:64, ts_].rearrange("p (n q) -> p n q", n=4)),
                                  RT[2].v(RT[2].t[0:64, :].rearrange("p (n q) -> p n q", n=4)), xib, ALU.mult)
                        else:
                            fw.tt(pool, dst[0:64, ts_], RT[0][0:64, :], RT[1][0:64, :], ALU.add)
                proj_fm(lambda tb, ts_, pv: fw.activation(gT[:, ts_], pv, AF.Silu), lambda kc: wcg[:, kc, :], 128)
                for tg in range(4):
                    pb = PS[6 + tg % 2]
                    for t4 in range(4):
                        tile = tg * 4 + t4
                        for kc in range(8):
                            fw.mm(pb[:, t4 * 128:(t4 + 1) * 128], hT[:, kc, tile * 128:(tile + 1) * 128], wcv[:, kc, :],
                                  start=(kc == 0), stop=(kc == 7), signal=(kc == 7 and t4 == 3))
                    fw.copy(act if tg % 2 else dve, VC.v(VC.t[:, tg * 4:tg * 4 + 4, :].rearrange("p n c -> p (n c)")), pb[:])
                for tg in range(2):
                    pb = PS[6 + tg]
                    tb16 = pb.t.bitcast(BF16)
                    for t8 in range(8):
                        tile = tg * 8 + t8
                        fw.transpose(pb.v(tb16[:, t8 * 64:(t8 + 1) * 64]), kTc[0:64, tile * 128:(tile + 1) * 128],
                                     ident_b[0:64, 0:64], signal=(t8 == 7))
                    zb = ZETA.v(bass.AP(ZETA.t, c * 64, [[3 * 64, 128], [0, 8], [1, 64]]))
                    fw.tt(dve, kz[:, tg * 8:(tg + 1) * 8, :],
                          pb.v(tb16[:, 0:512].rearrange("p (n c) -> p n c", n=8)), zb, ALU.mult)
                for hh in range(2):
                    for n in range(15):
                        pu = PS[6 + (n // 8)]
                        fw.mm(pu[32 * hh:32 * hh + 32, (n % 8) * 64:(n % 8 + 1) * 64], kz[:, n, 32 * hh:32 * hh + 32],
                              VC[:, n, 64 * hh:64 * hh + 64], start=True, stop=True, signal=(n in (7, 14)))
                fw.copy(dve, STf[:, 1, :], PS[6][0:64, 0:64])
                for n in range(1, 15):
                    pu = PS[6 + (n // 8)]
                    fw.stt(STf[:, n + 1, :], STf[:, n, :], GAM[:, c:c + 1], pu[0:64, (n % 8) * 64:(n % 8 + 1) * 64], ALU.mult, ALU.add)
                fw.copy(act, STb[:, 1:16, :], STf[:, 1:16, :])
                if c == 0:
                    dump(f"qTc{l}", qTc[0:64, :], [64, S], BF16)
                    dump(f"kTc{l}", kTc[0:64, :], [64, S], BF16)
                    dump(f"qxT{l}", qxT[0:64, :], [64, S], BF16)
                    dump(f"kz{l}", kz[:], [128, 16, 64], BF16)
                    dump(f"VC{l}", VC[:], [128, 16, 128], BF16)
                    dump(f"STf{l}", STf[:, 1:16, :], [64, 15, 64], F32)
                    dump(f"gT{l}", gT[:], [128, S], BF16)
                unitsC = []
                bi = 0
                for n in range(16):
                    for hh in range(2):
                        def mk(n=n, hh=hh, bi=bi):
                            h = 2 * c + hh
                            ss = PS[4 + bi % 4]
                            ssv = ss[:, 0:128]
                            pt = PTC[bi % 4]
                            cs = slice(n * 128, (n + 1) * 128)
                            ob = PS[n // 4][64 * hh:64 * hh + 64, (n % 4) * 128:(n % 4 + 1) * 128]

                            def st1():
                                fw.mm(ssv, kTc[32 * hh:32 * hh + 32, cs], qTc[32 * hh:32 * hh + 32, cs], start=True, stop=True)
                                fw.tt(dve, pt[:], ssv, DECT[:, h, :], ALU.mult)

                            def st2():
                                fw.mm(ob, VC[:, n, 64 * hh:64 * hh + 64], pt[:], start=True, stop=(n == 0), signal=True, skip_group_check=True)
                                if n > 0:
                                    fw.mm(ob, STb[32 * hh:32 * hh + 32, n, :], qxT[32 * hh:32 * hh + 32, cs], start=False, stop=True,
                                          signal=True, skip_group_check=True)
                            return (st1, st2)
                        unitsC.append(mk())
                        bi += 1
                run_pipe(unitsC)
                for tb in range(4):
                    ts_ = slice(tb * 512, (tb + 1) * 512)
                    ob = PS[tb]
                    fw.copy(act, CP[:], ob[:])
                    fw.activation(CSQ[:], ob[:], AF.Square)
                    pm, p2_ = PS[6], PS[7]
                    fw.mm(pm[:], BD[:], CP[:], start=True, stop=True)
                    fw.mm(p2_[:], BD[:], CSQ[:], start=True, stop=True)
                    fw.copy(act, CMEAN[:], pm[:])
                    fw.activation(CMSQ[:], pm[:], AF.Square)
                    fw.tt(dve, CDV[:], ob[:], CMEAN[:], ALU.subtract)
                    fw.tt(dve, CVAR[:], p2_[:], CMSQ[:], ALU.subtract)
                    fw.ts(dve, CVAR[:], CVAR[:], 0.0, ALU.max)
                    fw.activation(CVAR[:], CVAR[:], AF.Ln, bias=EPSC[:, 0:1])
                    fw.activation(CRS[:], CVAR[:], AF.Exp, scale=-0.5)
                    fw.tt(dve, CDV[:], CDV[:], CRS[:], ALU.mult)
                    fw.tt(pool, mixT[:, 5 + c, ts_], CDV[:], gT[:, ts_], ALU.mult)
            dump(f"mixC{l}", mixT[:, 5:8, :], [128, 3, S], BF16)
            fw.barrier()

            MARKS.append(('wout', l, pe.ninst))
            AX.reset()
            xT = AX.alloc(fw, "xT", [128, 8, S], F32)
            sp.dma_group([(xT[:, kc, :], xsp_d[:, kc, :]) for kc in range(8)])
            AW.reset()
            wo = AW.alloc(fw, "wo", [128, 8, D], BF16)
            wov = wout_d[l].rearrange("(k p) n -> p k n", p=128)
            load_w(wo[:], wov)
            for kc in range(8):
                fw.activation(wo[:, kc, :], wo[:, kc, :], AF.Identity, scale=MG[:, l, kc:kc + 1])
            for d_ in range(8):
                for tb in range(4):
                    ts_ = slice(tb * 512, (tb + 1) * 512)
                    pb = PS[(d_ * 4 + tb) % 4]
                    for kc in range(8):
                        fw.mm(pb[:], wo[:, kc, d_ * 128:(d_ + 1) * 128], mixT[:, kc, ts_], start=(kc == 0), stop=(kc == 7))
                    fw.stt(xT[:, d_, ts_], pb[:], MOD[:, l, 16 + d_:17 + d_], xT[:, d_, ts_], ALU.mult, ALU.add)
            dump(f"xmid{l}", xT[:], [128, 8, S], F32)
            fw.barrier()

            MARKS.append(('ffn', l, pe.ninst))
            AY.reset()
            norm_to_hT(l, lambda kc: A2[:, l, kc:kc + 1], 24, AY)
            fw.barrier()
            AY.reset()
            actT = AY.alloc(fw, "actT", [128, 6, S], BF16)
            sgt = [AY.alloc(fw, f"sg{i}", [128, 512], BF16) for i in range(2)]
            AW.reset()
            wdn = [AW.alloc(fw, f"wdn{i}", [128, 6, D], BF16) for i in range(2)]
            gu = [AW.alloc(fw, f"gu{i}", [128, 2, 8, 128], BF16) for i in range(3)]
            wgv = wg_d[l].rearrange("(k p) n -> p k n", p=128)
            wuv = wu_d[l].rearrange("(k p) n -> p k n", p=128)
            ji = 0
            modgen = mod_pieces(l + 1) if l + 1 < depth else iter(())
            for gi, (j0, nj) in enumerate(FFN_GROUPS):
                wdb = wdn[gi % 2]
                load_w(wdb[:, 0:nj, :], wd_d[l, j0 * 128:(j0 + nj) * 128, :].rearrange("(j p) n -> p j n", p=128))
                for jj in range(nj):
                    j = j0 + jj
                    g = gu[ji % 3]
                    ji += 1
                    pool.dma_group([(g[:, 0, :, :], wgv[:, :, j * 128:(j + 1) * 128]),
                                    (g[:, 1, :, :], wuv[:, :, j * 128:(j + 1) * 128])], max_dma_last_dim=4096)
                    for tb in range(4):
                        ts_ = slice(tb * 512, (tb + 1) * 512)
                        pg, pu = PS[(tb % 2) * 2], PS[(tb % 2) * 2 + 1]
                        for kc in range(8):
                            fw.mm(pg[:], g[:, 0, kc, :], hT[:, kc, ts_], start=(kc == 0), stop=(kc == 7))
                        for kc in range(8):
                            fw.mm(pu[:], g[:, 1, kc, :], hT[:, kc, ts_], start=(kc == 0), stop=(kc == 7))
                        fw.activation(sgt[tb % 2][:], pg[:], AF.Silu)
                        fw.tt(dve, actT[:, jj, ts_], pu[:], sgt[tb % 2][:], ALU.mult)
                    next(modgen, None)
                for d_ in range(8):
                    for tb in range(4):
                        ts_ = slice(tb * 512, (tb + 1) * 512)
                        pb = PS[4 + (d_ * 4 + tb) % 3]
                        for jj in range(nj):
                            fw.mm(pb[:], wdb[:, jj, d_ * 128:(d_ + 1) * 128], actT[:, jj, ts_], start=(jj == 0), stop=(jj == nj - 1))
                        fw.stt(xT[:, d_, ts_], pb[:], MOD[:, l, 40 + d_:41 + d_], xT[:, d_, ts_], ALU.mult, ALU.add)
            for _ in modgen:
                pass
            dump(f"xout{l}", xT[:], [128, 8, S], F32)
            fw.barrier()

        MARKS.append(('final', depth, pe.ninst))
        AY.reset()
        sq = [AY.alloc(fw, f"fsq{i}", [128, 8, 512], BF16) for i in range(2)]
        lnt = AY.alloc(fw, "fln", [128, 512], F32)
        rs = AY.alloc(fw, "frs", [128, 512], F32)
        yt = [AY.alloc(fw, f"fyt{i}", [128, 512], F32) for i in range(3)]
        AW.reset()
        OUT = [AW.alloc(fw, f"oo{i}", [128, 4, D], F32) for i in range(2)]
        stores = []
        for tb in (range(4) if 'final' not in skip else []):
            ts_ = slice(tb * 512, (tb + 1) * 512)
            fw.activation(sq[tb % 2][:], xT[:, :, ts_], AF.Square)
            pb = PS[tb % 2]
            for kc in range(8):
                fw.mm(pb[:], ones_b[:], sq[tb % 2][:, kc, :], start=(kc == 0), stop=(kc == 7))
            fw.activation(lnt[:], pb[:], AF.Ln, scale=1.0 / D, bias=EPSC[:, 0:1])
            fw.activation(rs[:], lnt[:], AF.Exp, scale=-0.5)
            ob_ = OUT[tb % 2]
            for kc in range(8):
                t = yt[kc % 3]
                fw.stt(t[:], xT[:, kc, ts_], FNG[:, kc:kc + 1], rs[:], ALU.mult, ALU.mult)
                pt_ = PS[2 + kc % 4]
                for q in range(4):
                    fw.transpose(pt_[:, q * 128:(q + 1) * 128], t[:, q * 128:(q + 1) * 128], ident_f[:], signal=(q == 3))
                fw.copy(act if kc % 2 else dve, ob_[:, :, kc * 128:(kc + 1) * 128],
                        pt_.v(pt_.t[:, :].rearrange("p (q c) -> p q c", q=4)))
            stores.append(sp.dma_group([(y_d[(tb * 4 + q) * 128:(tb * 4 + q + 1) * 128, :], ob_[:, q, :]) for q in range(4)]))
        for tok in stores:
            sp.wait_tok(tok)
        for tok in dbg_out.values():
            sp.wait_tok(tok)
        fw.emit()
        info = {n: (e.ninst, e.nwait) for n, e in fw.E.items()}
        info["sp"] = (fw.sp.ninst, fw.sp.nwait)
        info["sems"] = fw.nsem
        print("MK build:", info)
    return nc


AX_X = mybir.AxisListType.X
MARKS = []
_NC_CACHE = {}


def prep_inputs(inputs, b):
    f32 = np.float32
    g = lambda k: np.asarray(inputs[k])
    hc = _HC_CACHE.get("hc")
    if hc is None:
        hc = host_consts(g("rel_bias").astype(f32))
        _HC_CACHE["hc"] = hc
    m = dict(hc)
    m["x"] = np.ascontiguousarray(g("x")[b].astype(f32))
    m["cT"] = np.ascontiguousarray(g("c")[b].astype(f32).reshape(8, 128).T)
    m["posb"] = np.ascontiguousarray(g("positions")[b].astype(np.int32).reshape(1, S))
    sh = _HC_CACHE.get("shared")
    if sh is None:
        def fm(a, nch):
            a = np.asarray(a, f32)
            return np.ascontiguousarray(a.reshape(a.shape[0], nch, 128).transpose(2, 0, 1))
        sh = dict(
            ada_w=np.ascontiguousarray(g("ada_w").astype(f32)),
            adabT=fm(g("ada_b"), 48), n1gT=fm(g("norm1_g"), 8), n2gT=fm(g("norm2_g"), 8),
            fngT=np.ascontiguousarray(g("final_norm").astype(f32).reshape(8, 128).T),
            mgT=fm(g("mix_gain"), 8), mqnT=fm(g("mla_q_norm"), 2), mkvnT=fm(g("mla_kv_norm"), 1),
            w_in=np.ascontiguousarray(g("w_in").astype(f32)), w_uq=np.ascontiguousarray(g("mla_w_uq").astype(f32)),
            w_ukv=np.ascontiguousarray(g("mla_w_ukv").astype(f32)), w_out=np.ascontiguousarray(g("w_out").astype(f32)),
            w_gate=np.ascontiguousarray(g("ffn_w_gate").astype(f32)), w_up=np.ascontiguousarray(g("ffn_w_up").astype(f32)),
            w_down=np.ascontiguousarray(g("ffn_w_down").astype(f32)),
        )
        _HC_CACHE["shared"] = sh
    m.update(sh)
    return m


_HC_CACHE = {}


def kernel(**inputs):
    _HC_CACHE.clear()
    nc = build()
    in_maps = [prep_inputs(inputs, b) for b in range(NCORES)]
    res = run_bass_kernel_spmd(nc, in_maps, core_ids=list(range(NCORES)))
    out = np.stack([np.asarray(r["y"], dtype=np.float32) for r in res.results], axis=0)
    return out
```

```python
import numpy as np
import concourse.bass as bass
import concourse.mybir as mybir
from concourse.bass_utils import run_bass_kernel_spmd
from contextlib import ExitStack

F32 = mybir.dt.float32
BF16 = mybir.dt.bfloat16
I32 = mybir.dt.int32
AF = mybir.ActivationFunctionType
ALU = mybir.AluOpType
AX = mybir.AxisListType

CE = ('pe', 'act', 'dve', 'pool')
SAME_ENGINE_SYNC = True


class Buf:
    __slots__ = ('t', 'name', 'w', 'r', 'semname')

    def __init__(self, t, name, semname=None):
        self.t = t
        self.name = name
        self.w = None
        self.r = {}
        self.semname = semname or name

    def __getitem__(self, idx):
        return V(self, self.t[idx])

    def v(self, ap):
        return V(self, ap)


class V:
    __slots__ = ('buf', 'ap')

    def __init__(self, buf, ap):
        self.buf = buf
        self.ap = ap


class Eng:
    def __init__(self, fw, name, sem):
        self.fw = fw
        self.name = name
        self.sem = sem
        self.count = 0
        self.seen = {}
        self.prog = []
        self.snaps = {0: {}}
        self.nwait = 0
        self.ninst = 0

    def _deps(self, reads, writes):
        deps = []
        for v in reads:
            if v.buf.w is not None:
                deps.append(v.buf.w)
        for v in writes:
            if v.buf.w is not None:
                deps.append(v.buf.w)
            deps.extend(v.buf.r.values())
        need = {}
        for (key, val, sem) in deps:
            if key == self.name and (self.name == 'pe' or not SAME_ENGINE_SYNC):
                continue
            if val > self.seen.get(key, 0) and val > need.get(key, (0, None))[0]:
                need[key] = (val, sem)
        for key, (val, sem) in need.items():
            self.prog.append(('wait', sem, val))
            self.nwait += 1
            self.seen[key] = val
            if key in self.fw.E and key != self.name:
                snap = self.fw.E[key].snaps.get(val)
                if snap:
                    for k2, v2 in snap.items():
                        if v2 > self.seen.get(k2, 0):
                            self.seen[k2] = v2

    def issue(self, fn, reads=(), writes=(), signal=True):
        self._deps(reads, writes)
        self.ninst += 1
        if signal:
            self.count += 1
            self.prog.append(('inst', fn, True))
            self.snaps[self.count] = {k: v for k, v in self.seen.items() if k in CE}
            tok = (self.name, self.count, self.sem)
        else:
            self.prog.append(('inst', fn, False))
            tok = (self.name, self.count + 1, self.sem)
        for v in reads:
            v.buf.r[self.name] = tok
        for v in writes:
            v.buf.w = tok
            v.buf.r = {}
        return tok

    def dma(self, out, in_, **kw):
        return self.dma_group([(out, in_)], **kw)

    def dma_group(self, pairs, **kw):
        reads = [i for (o, i) in pairs if isinstance(i, V)]
        writes = [o for (o, i) in pairs if isinstance(o, V)]
        self._deps(reads, writes)
        buf = (writes[0].buf if writes else reads[0].buf)
        rec = self.fw.dsem.get(buf.semname)
        if rec is None:
            rec = [self.fw.new_sem('d_' + buf.semname), 0]
            self.fw.dsem[buf.semname] = rec
        for (o, i) in pairs:
            rec[1] += 1
            oa = o.ap if isinstance(o, V) else o
            ia = i.ap if isinstance(i, V) else i
            self.prog.append(('dma', oa, ia, rec[0], kw))
        tok = (('d', buf.semname), 16 * rec[1], rec[0])
        for v in reads:
            v.buf.r[tok[0]] = tok
        for v in writes:
            v.buf.w = tok
            v.buf.r = {}
        return tok

    def wait_tok(self, tok):
        key, val, sem = tok
        if val > self.seen.get(key, 0):
            self.prog.append(('wait', sem, val))
            self.seen[key] = val

    def replay(self, e):
        for item in self.prog:
            if item[0] == 'wait':
                e.wait_ge(item[1], item[2])
            elif item[0] == 'inst':
                ins = item[1](e)
                if item[2]:
                    ins.then_inc(self.sem, 1)
            else:
                _, oa, ia, sem, kw = item
                e.dma_start(out=oa, in_=ia, **kw).then_inc(sem, 16)


class FW:
    def __init__(self, nc, stack):
        self.nc = nc
        self.stack = stack
        self.nsem = 0
        self.dsem = {}
        self.dma_toks = []
        self.E = {}
        for name in CE:
            self.E[name] = Eng(self, name, self.new_sem('c_' + name))
        self.sp = Eng(self, 'sp', None)
        self.pe, self.act, self.dve, self.pool = (self.E[n] for n in CE)

    def new_sem(self, name):
        self.nsem += 1
        return self.stack.enter_context(self.nc.semaphore(name))

    def sbuf(self, name, shape, dtype):
        t = self.stack.enter_context(self.nc.sbuf_tensor(name, list(shape), dtype))
        return Buf(t, name)

    def psum(self, name, shape, dtype):
        t = self.stack.enter_context(self.nc.psum_tensor(name, list(shape), dtype))
        return Buf(t, name)

    def barrier(self):
        toks = [(n, self.E[n].count, self.E[n].sem) for n in CE if self.E[n].count > 0]
        for e in list(self.E.values()) + [self.sp]:
            for tok in toks:
                if tok[0] != e.name:
                    e.wait_tok(tok)
            for tok in self.dma_toks:
                e.wait_tok(tok)
        self.dma_toks = []

    def check(self):
        engs = list(self.E.values()) + [self.sp]
        pc = {e.name: 0 for e in engs}
        sems = {}
        progress = True
        while progress:
            progress = False
            for e in engs:
                while pc[e.name] < len(e.prog):
                    it = e.prog[pc[e.name]]
                    if it[0] == 'wait':
                        if sems.get(id(it[1]), 0) < it[2]:
                            break
                    elif it[0] == 'inst':
                        if it[2]:
                            sems[id(e.sem)] = sems.get(id(e.sem), 0) + 1
                    else:
                        sems[id(it[3])] = sems.get(id(it[3]), 0) + 16
                    pc[e.name] += 1
                    progress = True
        stuck = {}
        for e in engs:
            if pc[e.name] < len(e.prog):
                it = e.prog[pc[e.name]]
                who = [n for n, x in self.E.items() if x.sem is it[1]] or [k for k, r in self.dsem.items() if r[0] is it[1]]
                stuck[e.name] = (pc[e.name], len(e.prog), who, it[2], sems.get(id(it[1]), 0))
        return stuck

    def emit(self):
        st = self.check()
        assert not st, f"DEADLOCK: {st}"
        with self.nc.Block() as block:
            @block.sync
            def _(e):
                self.sp.replay(e)

            @block.tensor
            def _(e):
                self.pe.replay(e)

            @block.scalar
            def _(e):
                self.act.replay(e)

            @block.vector
            def _(e):
                self.dve.replay(e)

            @block.gpsimd
            def _(e):
                self.pool.replay(e)

    def mm(self, out, lhsT, rhs, start=True, stop=True, signal=None, **kw):
        if signal is None:
            signal = stop
        return self.pe.issue(
            lambda e: e.matmul(out.ap, lhsT.ap, rhs.ap, start=start, stop=stop, **kw),
            reads=[lhsT, rhs], writes=[out], signal=signal)

    def transpose(self, out, in_, ident, signal=True):
        return self.pe.issue(
            lambda e: e.transpose(out.ap, in_.ap, ident.ap),
            reads=[in_, ident], writes=[out], signal=signal)

    def activation(self, out, in_, func, scale=1.0, bias=0.0, accum_out=None, eng=None):
        reads = [in_]
        sc = scale
        bi = bias
        if isinstance(scale, V):
            reads.append(scale)
            sc = scale.ap
        if isinstance(bias, V):
            reads.append(bias)
            bi = bias.ap
        writes = [out]
        kw = {}
        if accum_out is not None:
            writes.append(accum_out)
            kw['accum_out'] = accum_out.ap
        return self.act.issue(
            lambda e: e.activation(out=out.ap, in_=in_.ap, func=func, scale=sc, bias=bi, **kw),
            reads=reads, writes=writes)

    def tt(self, eng, out, in0, in1, op):
        return eng.issue(lambda e: e.tensor_tensor(out=out.ap, in0=in0.ap, in1=in1.ap, op=op),
                         reads=[in0, in1], writes=[out])

    def ts(self, eng, out, in0, s1, op0, s2=None, op1=None, accum_out=None):
        reads = [in0]
        a1 = s1
        a2 = s2
        if isinstance(s1, V):
            reads.append(s1)
            a1 = s1.ap
        if isinstance(s2, V):
            reads.append(s2)
            a2 = s2.ap
        kw = {}
        writes = [out]
        if op1 is not None:
            kw['op1'] = op1
        if accum_out is not None:
            kw['accum_out'] = accum_out.ap
            writes.append(accum_out)
        return eng.issue(lambda e: e.tensor_scalar(out=out.ap, in0=in0.ap, scalar1=a1, scalar2=a2, op0=op0, **kw),
                         reads=reads, writes=writes)

    def stt(self, out, in0, scalar, in1, op0, op1):
        reads = [in0, in1]
        a = scalar
        if isinstance(scalar, V):
            reads.append(scalar)
            a = scalar.ap
        return self.dve.issue(
            lambda e: e.scalar_tensor_tensor(out=out.ap, in0=in0.ap, scalar=a, in1=in1.ap, op0=op0, op1=op1),
            reads=reads, writes=[out])

    def copy(self, eng, out, in_):
        if eng is self.act:
            return eng.issue(lambda e: e.copy(out=out.ap, in_=in_.ap), reads=[in_], writes=[out])
        return eng.issue(lambda e: e.tensor_copy(out=out.ap, in_=in_.ap), reads=[in_], writes=[out])

    def memset(self, eng, out, val):
        return eng.issue(lambda e: e.memset(out.ap, val), writes=[out])

D = 1024
S = 2048
DEPTH = 4
NCORES = 8
IN_W = 2720
FH = 2816
OFF_AQ, OFF_AK, OFF_AV = 0, 384, 768
OFF_BQ, OFF_BKV, OFF_BKR = 1152, 1408, 1536
OFF_CQ, OFF_CK, OFF_CV, OFF_CG = 1568, 1760, 1952, 2336
EPS = 1e-6
NEG = -30000.0
SLAB = 209984
FFN_GROUPS = [(0, 6), (6, 6), (12, 5), (17, 5)]


def _t5_bucket(dist):
    n_buckets, max_distance = 32, 2048
    max_exact = n_buckets // 2
    safe = np.maximum(dist, 1).astype(np.float32)
    large = max_exact + (np.log(safe / max_exact) / np.log(max_distance / max_exact) * (n_buckets - max_exact)).astype(np.int32)
    large = np.minimum(large, n_buckets - 1)
    return np.where(dist < max_exact, dist, large).astype(np.int32)


def host_consts(rel_bias):
    f32 = np.float32
    k = np.arange(128)[:, None]
    ql = np.arange(256)[None, :]
    q3 = np.arange(128)[None, :]
    d1 = ql - k
    v1 = (d1 >= 0) & (d1 <= 128)
    i1 = _t5_bucket(np.clip(d1, 0, 128))
    v2 = v1
    i2 = _t5_bucket(np.clip(4 * d1, 0, 512))
    d3 = q3 - k
    v3 = d3 >= 0
    i3 = _t5_bucket(np.clip(16 * d3, 0, 2048))
    bmb = np.zeros((128, 6, 640), f32)
    for h in range(6):
        bmb[:, h, 0:256] = np.where(v1, rel_bias[i1, h], 0.0)
        bmb[:, h, 256:512] = np.where(v2, rel_bias[i2, h], 0.0)
        bmb[:, h, 512:640] = np.where(v3, rel_bias[i3, h], 0.0)
    bmm = np.concatenate([np.where(v1, 0.0, NEG), np.where(v2, 0.0, NEG), np.where(v3, 0.0, NEG)], axis=1).astype(f32)
    cm = np.where(q3 >= k, 0.0, NEG).astype(f32)
    H = 6
    log_g = np.log(1.0 - 2.0 ** (-5.0 - np.arange(H))).astype(f32)
    i = np.arange(128, dtype=f32)
    rel = i[:, None] - i[None, :]
    decay_intra = (np.exp(np.maximum(rel, 0.0)[None] * log_g[:, None, None]) * (rel >= 0)[None]).astype(f32)
    xi = np.exp((i + 1.0)[None, :] * log_g[:, None]).astype(f32)
    zeta = np.exp((128 - 1.0 - i)[None, :] * log_g[:, None]).astype(f32)
    cd = np.exp(128 * log_g).astype(f32)
    s = f32(32 ** -0.5)
    decT = np.zeros((128, 6, 128), f32)
    for h in range(H):
        decT[:, h, :] = decay_intra[h].T * s
    xi_t = np.zeros((64, 3, 128), f32)
    zeta_t = np.zeros((128, 3, 64), f32)
    gam = np.zeros((64, 3), f32)
    for p in range(3):
        for hh in range(2):
            h = 2 * p + hh
            xi_t[hh * 32:(hh + 1) * 32, p, :] = xi[h][None, :] * s
            zeta_t[:, p, hh * 32:(hh + 1) * 32] = zeta[h][:, None]
            gam[hh * 32:(hh + 1) * 32, p] = cd[h]
    half = 16
    inv_freq = (1.0 / (10000.0 ** (np.arange(half, dtype=f32) / half))).astype(f32)
    invf = np.tile(inv_freq, 8).reshape(128, 1).astype(f32)
    return dict(bmb=bmb, bmm=bmm, cm=cm, decT=decT, xi=xi_t, zeta=zeta_t, gam=gam, invf=invf)


class Arena:
    def __init__(self, nc, base, size, name):
        self.nc, self.base, self.size, self.name = nc, base, size, name
        self.off = 0
        self.n = 0

    def reset(self):
        self.off = 0

    def alloc(self, fw, name, shape, dtype):
        nbytes = int(np.prod(shape[1:])) * mybir.dt.size(dtype)
        nbytes = (nbytes + 31) // 32 * 32
        assert self.off + nbytes <= self.size, (self.name, name, self.off, nbytes, self.size)
        self.n += 1
        t = self.nc.alloc_sbuf_tensor_at(f"{self.name}_{name}_{self.n}", list(shape), dtype, offset=self.base + self.off)
        self.off += nbytes
        return Buf(t, f"{self.name}_{name}_{self.n}", semname=f"{self.name}_{name}")


def build(depth=DEPTH, dbg=None, skip=()):
    nc = bass.Bass("TRN2", target_bir_lowering=False)
    dbg = dbg or []
    dbg_out = {}
    MARKS.clear()

    def din(name, shape, dt=F32):
        return nc.dram_tensor(name, list(shape), dt, kind="ExternalInput").ap()

    x_d = din("x", [S, D])
    cT_d = din("cT", [128, 8])
    pos_d = din("posb", [1, S], I32)
    invf_d = din("invf", [128, 1])
    bmb_d = din("bmb", [128, 6, 640])
    bmm_d = din("bmm", [128, 640])
    cm_d = din("cm", [128, 128])
    decT_d = din("decT", [128, 6, 128])
    xi_d = din("xi", [64, 3, 128])
    zeta_d = din("zeta", [128, 3, 64])
    gam_d = din("gam", [64, 3])
    adab_d = din("adabT", [128, DEPTH, 48])
    n1g_d = din("n1gT", [128, DEPTH, 8])
    n2g_d = din("n2gT", [128, DEPTH, 8])
    fng_d = din("fngT", [128, 8])
    mg_d = din("mgT", [128, DEPTH, 8])
    mqn_d = din("mqnT", [128, DEPTH, 2])
    mkvn_d = din("mkvnT", [128, DEPTH, 1])
    if depth > 0:
        adaw_d = din("ada_w", [DEPTH, D, 6 * D])
        win_d = din("w_in", [DEPTH, D, IN_W])
        wuq_d = din("w_uq", [DEPTH, 256, 384])
        wukv_d = din("w_ukv", [DEPTH, 128, 512])
        wout_d = din("w_out", [DEPTH, D, D])
        wg_d = din("w_gate", [DEPTH, D, FH])
        wu_d = din("w_up", [DEPTH, D, FH])
        wd_d = din("w_down", [DEPTH, FH, D])
        xsp_d = nc.dram_tensor("xspill", [128, 8, S], F32, kind="Internal").ap()
    y_d = nc.dram_tensor("y", [S, D], F32, kind="ExternalOutput").ap()

    st = ExitStack()
    with st:
        fw = FW(nc, st)
        pe, act, dve, pool, sp = fw.pe, fw.act, fw.dve, fw.pool, fw.sp
        base0 = (nc.sbuf_base + 31) // 32 * 32
        st.enter_context(nc.sbuf_tensor("slab", [128, SLAB], mybir.dt.uint8))
        assert nc.sbuf_base == base0 + SLAB, (nc.sbuf_base, base0)
        o = base0
        SZ_HT, SZ_X, SZ_Y, SZ_ADAW, SZ_W = 32768, 65536, 32768, 12288, 36864
        SZ_C = SLAB - (SZ_HT + SZ_X + SZ_Y + SZ_ADAW + SZ_W)
        AC = Arena(nc, o, SZ_C, "c"); o += SZ_C
        AH = Arena(nc, o, SZ_HT, "h"); o += SZ_HT
        AX = Arena(nc, o, SZ_X, "x"); o += SZ_X
        AY = Arena(nc, o, SZ_Y, "y"); o += SZ_Y
        AA = Arena(nc, o, SZ_ADAW, "a"); o += SZ_ADAW
        AW = Arena(nc, o, SZ_W, "w"); o += SZ_W

        PS = [fw.psum(f"ps{i}", [128, 512], F32) for i in range(8)]

        def sub(buf, name):
            return Buf(buf.t, name)

        ident_f = AC.alloc(fw, "identf", [128, 128], F32)
        ident_b = AC.alloc(fw, "identb", [128, 128], BF16)
        ones_b = AC.alloc(fw, "onesb", [128, 128], BF16)
        BD = AC.alloc(fw, "bd", [128, 128], BF16)
        WNe = AC.alloc(fw, "wne", [128, 64], BF16)
        WNo = AC.alloc(fw, "wno", [128, 64], BF16)
        FCOS = AC.alloc(fw, "fcos", [128, S], BF16)
        FSIN = AC.alloc(fw, "fsin", [128, S], BF16)
        BM = AC.alloc(fw, "bm", [128, 6, 640], BF16)
        CM = AC.alloc(fw, "cm", [128, 128], F32)
        DECT = AC.alloc(fw, "dect", [128, 6, 128], F32)
        XI = AC.alloc(fw, "xi", [64, 3, 128], F32)
        ZETA = AC.alloc(fw, "zeta", [128, 3, 64], F32)
        GAM = AC.alloc(fw, "gam", [64, 3], F32)
        MOD = AC.alloc(fw, "mod", [128, DEPTH, 48], F32)
        A1 = AC.alloc(fw, "a1", [128, DEPTH, 8], F32)
        A2 = AC.alloc(fw, "a2", [128, DEPTH, 8], F32)
        ADAB = AC.alloc(fw, "adab", [128, DEPTH, 48], F32)
        N1G = AC.alloc(fw, "n1g", [128, DEPTH, 8], F32)
        N2G = AC.alloc(fw, "n2g", [128, DEPTH, 8], F32)
        FNG = AC.alloc(fw, "fng", [128, 8], F32)
        MG = AC.alloc(fw, "mg", [128, DEPTH, 8], F32)
        MQN = AC.alloc(fw, "mqn", [128, DEPTH, 2], F32)
        MKVN = AC.alloc(fw, "mkvn", [128, DEPTH, 1], F32)
        CTF = AC.alloc(fw, "ctf", [128, 8], F32)
        CTB = AC.alloc(fw, "ctb", [128, 8], BF16)
        INVF = AC.alloc(fw, "invf", [128, 1], F32)
        hT = AH.alloc(fw, "hT", [128, 8, S], BF16)
        adaw = [AA.alloc(fw, f"adaw{i}", [128, 3072], BF16) for i in range(2)]

        def dump(name, view, shape, dt):
            if name not in dbg:
                return
            t = nc.dram_tensor("dbg_" + name, list(shape), dt, kind="ExternalOutput").ap()
            dbg_out[name] = sp.dma(t, view)
            fw.dma_toks.append(dbg_out[name])

        fw.memset(dve, ident_f[:], 1.0)
        pool.issue(lambda e: e.affine_select(out=ident_f.t[:], in_=ident_f.t[:], pattern=[[-1, 128]],
                                             compare_op=ALU.is_equal, fill=0.0, base=0, channel_multiplier=1),
                   reads=[ident_f[:]], writes=[ident_f[:]])
        fw.copy(dve, ident_b[:], ident_f[:])
        fw.memset(dve, ones_b[:], 1.0)
        fw.memset(dve, BD[:], 0.0)
        fw.memset(dve, BD[0:64, 0:64], 1.0 / 64)
        fw.memset(dve, BD[64:128, 64:128], 1.0 / 64)
        fw.memset(dve, WNe[:], 0.0)
        fw.memset(dve, WNe[0:64, :], 1.0)
        if 'wn' not in skip:
            fw.memset(dve, WNe[64:65, :], 64 * EPS)
        fw.memset(dve, WNo[:], 0.0)
        fw.memset(dve, WNo[64:128, :], 1.0)
        if 'wn' not in skip:
            fw.memset(dve, WNo[0:1, :], 64 * EPS)
        for (b, d_) in [] if 'small' in skip else [(CM, cm_d), (DECT, decT_d), (XI, xi_d), (ZETA, zeta_d), (GAM, gam_d), (ADAB, adab_d),
                        (N1G, n1g_d), (N2G, n2g_d), (FNG, fng_d), (MG, mg_d), (MQN, mqn_d), (MKVN, mkvn_d),
                        (CTF, cT_d), (INVF, invf_d)]:
            sp.dma(b[:], d_)
        if 'silu' not in skip:
            fw.activation(CTB[:], CTF[:], AF.Silu)
        AY.reset()
        bmb_s = AY.alloc(fw, "bmb", [128, 6, 640], F32)
        bmm_s = AY.alloc(fw, "bmm", [128, 640], F32)
        if 'bm' not in skip:
            sp.dma(bmb_s[:], bmb_d)
            sp.dma(bmm_s[:], bmm_d)
            for h in range(6):
                fw.tt(dve, BM[:, h, :], bmb_s[:, h, :], bmm_s[:], ALU.add)
        fw.barrier()
        AY.reset()
        posi = AY.alloc(fw, "posi", [128, S], I32)
        ang = AY.alloc(fw, "ang", [128, S], F32)
        t1 = AY.alloc(fw, "t1", [128, S], F32)
        t2 = AY.alloc(fw, "t2", [128, S], F32)
        if 'tables' not in skip:
            sp.dma(posi[:], pos_d.partition_broadcast(128))
            fw.copy(dve, t1[:], posi[:])
            fw.ts(dve, ang[:], t1[:], INVF[:, 0:1], ALU.mult)
            TWO_PI = 2.0 * np.pi
            C1 = 6.28125
            C2 = float(np.float32(TWO_PI - C1))
            fw.ts(dve, t1[:], ang[:], 1.0 / TWO_PI, ALU.mult)
            fw.copy(dve, posi[:], t1[:])
            fw.copy(dve, t1[:], posi[:])
            fw.stt(t2[:], t1[:], -C1, ang[:], ALU.mult, ALU.add)
            fw.stt(t2[:], t1[:], -C2, t2[:], ALU.mult, ALU.add)

            def wrap(dst, src, tmp):
                fw.ts(dve, tmp, src, float(np.pi), ALU.is_gt)
                fw.stt(dst, tmp, -TWO_PI, src, ALU.mult, ALU.add)
                fw.ts(dve, tmp, dst, -float(np.pi), ALU.is_lt)
                fw.stt(dst, tmp, TWO_PI, dst, ALU.mult, ALU.add)
                fw.ts(dve, dst, dst, 3.1415925, ALU.min, -3.1415925, ALU.max)

            wrap(t2[:], t2[:], t1[:])
            fw.activation(FSIN[:], t2[:], AF.Sin)
            fw.ts(dve, ang[:], t2[:], float(np.pi / 2), ALU.add)
            wrap(ang[:], ang[:], t1[:])
            fw.activation(FCOS[:], ang[:], AF.Sin)
        fw.barrier()

        AX.reset()
        xT = AX.alloc(fw, "xT", [128, 8, S], F32)
        AW.reset()
        io = [AW.alloc(fw, f"io{i}", [128, D], F32) for i in range(2)]
        for n in range(16):
            sp.dma(io[n % 2][:], x_d[n * 128:(n + 1) * 128, :])
            for g in range(2):
                pb = PS[(2 * n + g) % 4]
                for j in range(4):
                    kc = 4 * g + j
                    fw.transpose(pb[:, j * 128:(j + 1) * 128], io[n % 2][:, kc * 128:(kc + 1) * 128], ident_f[:], signal=(j == 3))
                src = pb.v(pb.t[:, :].rearrange("p (j t) -> p j t", j=4))
                dst = xT.v(xT.t[:, 4 * g:4 * g + 4, n * 128:(n + 1) * 128])
                fw.copy(act if (n + g) % 2 else dve, dst, src)
        fw.barrier()

        def mod_pieces(l):
            mp = PS[7]
            for piece in range(16):
                kc, half = piece // 2, piece % 2
                slot = adaw[piece % 2]
                pool.dma(slot[:], adaw_d[l, kc * 128:(kc + 1) * 128, half * 3072:(half + 1) * 3072], max_dma_last_dim=4096)
                for j in range(24):
                    jj = half * 24 + j
                    fw.mm(mp[:, jj:jj + 1], slot[:, j * 128:(j + 1) * 128], CTB[:, kc:kc + 1],
                          start=(piece == 0 and j == 0), stop=(kc == 7), signal=(j == 23), skip_group_check=True)
                yield
            fw.tt(dve, MOD[:, l, :], mp[:, 0:48], ADAB[:, l, :], ALU.add)
            fw.stt(A1[:, l, :], MOD[:, l, 8:16], 1.0, N1G[:, l, :], ALU.add, ALU.mult)
            fw.stt(A2[:, l, :], MOD[:, l, 32:40], 1.0, N2G[:, l, :], ALU.add, ALU.mult)
            yield

        EPSC = AC.alloc(fw, "epsc", [128, 1], F32)
        fw.memset(dve, EPSC[:], EPS)

        def norm_to_hT(l, a_view, b_col0, ar):
            ar.reset()
            sq = [ar.alloc(fw, f"nsq{i}", [128, 8, 512], BF16) for i in range(2)]
            lnt = ar.alloc(fw, "nln", [128, 512], F32)
            rs = [ar.alloc(fw, f"nrs{i}", [128, 512], F32) for i in range(2)]
            tm = [ar.alloc(fw, f"ntm{i}", [128, 512], F32) for i in range(3)]
            for tb in range(4):
                ts_ = slice(tb * 512, (tb + 1) * 512)
                fw.activation(sq[tb % 2][:], xT[:, :, ts_], AF.Square)
                pb = PS[tb % 2]
                for kc in range(8):
                    fw.mm(pb[:], ones_b[:], sq[tb % 2][:, kc, :], start=(kc == 0), stop=(kc == 7))
                fw.activation(lnt[:], pb[:], AF.Ln, scale=1.0 / D, bias=EPSC[:, 0:1])
                fw.activation(rs[tb % 2][:], lnt[:], AF.Exp, scale=-0.5)
                for kc in range(8):
                    t = tm[kc % 3]
                    fw.tt(dve, t[:], xT[:, kc, ts_], rs[tb % 2][:], ALU.mult)
                    fw.activation(hT[:, kc, ts_], t[:], AF.Identity, scale=a_view(kc), bias=MOD[:, l, b_col0 + kc:b_col0 + kc + 1])

        def load_w(buf_view, dram_ap):
            pool.dma(buf_view, dram_ap, max_dma_last_dim=4096)

        def proj_fm(out_fn, w_fn, M, nk=8, rhs_fn=None, banks=(6, 7), out_p0=0):
            for tb in range(4):
                pb = PS[banks[tb % len(banks)]]
                ts_ = slice(tb * 512, (tb + 1) * 512)
                pv = pb[out_p0:out_p0 + M, :]
                for kc in range(nk):
                    rhs = rhs_fn(kc, ts_) if rhs_fn else hT[:, kc, ts_]
                    fw.mm(pv, w_fn(kc), rhs, start=(kc == 0), stop=(kc == nk - 1))
                out_fn(tb, ts_, pv)

        def finalize_head(bank, nr, mixb, chunk, tb, fin):
            SQb, LNb, RSb = fin
            ts_ = slice(tb * 512, (tb + 1) * 512)
            fw.activation(SQb[:], bank[:], AF.Square)
            pf = PS[6 + (tb % 2)]
            wn = WNe if nr == 0 else WNo
            fw.mm(pf[nr:nr + 64, :], wn[:], SQb[:], start=True, stop=True)
            fw.activation(LNb[nr:nr + 64, :], pf[nr:nr + 64, :], AF.Ln, scale=1.0 / 64)
            fw.activation(RSb[nr:nr + 64, :], LNb[nr:nr + 64, :], AF.Exp, scale=-0.5)
            fw.tt(dve, mixb[nr:nr + 64, chunk, ts_], bank[nr:nr + 64, :], RSb[nr:nr + 64, :], ALU.mult)

        def v_lhsT(vbuf, tile_off, ones_off, even):
            t = vbuf.t
            pstride = t[:, :].ap[0][0]
            if even:
                return vbuf.v(bass.AP(t, tile_off, [[pstride, 128], [ones_off - tile_off, 2], [1, 64]]))
            return vbuf.v(bass.AP(t, ones_off, [[pstride, 128], [tile_off - ones_off, 2], [1, 64]]))

        def run_pipe(units, la=2):
            nU = len(units)
            for i in range(nU + la):
                if i < nU:
                    units[i][0]()
                if i - la >= 0:
                    units[i - la][1]()

        for l in range(depth):
            MARKS.append(('mod', l, pe.ninst))
            if l == 0:
                for _ in mod_pieces(0):
                    pass
            MARKS.append(('norm1', l, pe.ninst))
            AY.reset()
            norm_to_hT(l, lambda kc: A1[:, l, kc:kc + 1], 0, AY)
            dump(f"hT{l}", hT[:], [128, 8, S], BF16)
            for kc in range(8):
                sp.dma(xsp_d[:, kc, :], xT[:, kc, :])
            fw.barrier()
            _rec = fw.dsem[xT.semname]
            spill_tok = (("d", xT.semname), 16 * _rec[1], _rec[0])
            for e in (pe, act, dve, pool):
                e.wait_tok(spill_tok)

            AY.reset()
            mixT = AY.alloc(fw, "mixT", [128, 8, S], BF16)

            MARKS.append(('A', l, pe.ninst))
            AX.reset()
            qT = AX.alloc(fw, "qT", [128, S], BF16)
            kT = AX.alloc(fw, "kT", [128, S], BF16)
            VA = AX.alloc(fw, "VA", [128, 48, 2, 128], BF16)
            VT = AX.alloc(fw, "VT", [128, S], BF16)
            PT = [AX.alloc(fw, f"pt{i}", [128, 512], BF16) for i in range(4)]
            TS = [AX.alloc(fw, f"ts{i}", [128, 512], F32) for i in range(4)]
            fin = (AX.alloc(fw, "fsq", [128, 512], BF16), AX.alloc(fw, "fln", [128, 512], F32), AX.alloc(fw, "frs", [128, 512], F32))
            AW.reset()
            wq = AW.alloc(fw, "wq", [128, 8, 128], BF16)
            wk = AW.alloc(fw, "wk", [128, 8, 128], BF16)
            wv = AW.alloc(fw, "wv", [128, 8, 128], BF16)
            SS = [sub(PS[4], "ss0"), sub(PS[4], "ss1"), sub(PS[5], "ss2"), sub(PS[5], "ss3")]
            fw.memset(pool, VA[:, :, 0, 64:128], 1.0)
            fw.memset(pool, VA[:, :, 1, 0:64], 1.0)
            winv = win_d[l].rearrange("(k p) n -> p k n", p=128)
            blk = [0]
            for c in (range(3) if 'A' not in skip else []):
                load_w(wq[:], winv[:, :, OFF_AQ + c * 128:OFF_AQ + (c + 1) * 128])
                load_w(wk[:], winv[:, :, OFF_AK + c * 128:OFF_AK + (c + 1) * 128])
                load_w(wv[:], winv[:, :, OFF_AV + c * 128:OFF_AV + (c + 1) * 128])
                proj_fm(lambda tb, ts_, pv: fw.activation(qT[:, ts_], pv, AF.Identity, scale=0.125),
                        lambda kc: wq[:, kc, :], 128)
                proj_fm(lambda tb, ts_, pv: fw.copy(dve, kT[:, ts_], pv), lambda kc: wk[:, kc, :], 128)
                proj_fm(lambda tb, ts_, pv: fw.copy(act, VT[:, ts_], pv), lambda kc: wv[:, kc, :], 128)

                def tok_cols(ordr, tile):
                    if ordr == 0:
                        return VT[:, tile * 128:(tile + 1) * 128]
                    if ordr == 1:
                        r, b_ = tile // 4, tile % 4
                        return VT[:, 512 * b_ + r:512 * (b_ + 1):4]
                    return VT[:, tile::16]
                for ordr in range(3):
                    for tg in range(4):
                        pb = PS[6 + (tg % 2)]
                        pb16 = pb.t.bitcast(BF16)
                        for tt_ in range(4):
                            fw.transpose(pb.v(pb16[:, tt_ * 128:(tt_ + 1) * 128]), tok_cols(ordr, tg * 4 + tt_), ident_b[:],
                                         signal=(tt_ == 3))
                        t0_ = ordr * 16 + tg * 4
                        pv4 = pb16[:, 0:512].rearrange("p (n c) -> p n c", n=4)
                        ev_ = act if tg % 2 else dve
                        fw.copy(ev_, VA[:, t0_:t0_ + 4, 0, 0:64], pb.v(pv4[:, :, 0:64]))
                        fw.copy(ev_, VA[:, t0_:t0_ + 4, 1, 64:128], pb.v(pv4[:, :, 64:128]))
                if c == 0:
                    dump(f"qTA{l}", qT[:], [128, S], BF16)
                    dump(f"kTA{l}", kT[:], [128, S], BF16)
                    dump(f"VA{l}", VA[:], [128, 48, 2, 128], BF16)
                for hh in range(2):
                    h = 2 * c + hh
                    ro = 64 * hh
                    started = [False] * 4

                    unitsA = []

                    def sunit(subs, bm_off, w):
                        i = blk[0]
                        blk[0] += 1
                        ss = PS[4 + i % 4]
                        tsb = TS[i % 4]
                        pt = PT[i % 4]
                        offs = []
                        off = 0
                        for s_ in subs:
                            offs.append(off)
                            off += s_[2]
                        tot = off
                        nfull = sum(1 for s_ in subs if s_[2] == w)

                        def st1():
                            for (kcols, qcols, nq, pvl), o_ in zip(subs, offs):
                                fw.mm(ss[:, o_:o_ + nq], kcols, qcols, start=True, stop=True)
                            bmv = BM.v(bass.AP(BM.t, h * 640 + bm_off, [[6 * 640, 128], [0, nfull], [1, w]]))
                            fw.tt(dve, tsb.v(tsb.t[:, 0:nfull * w].rearrange("p (n q) -> p n q", n=nfull)),
                                  ss.v(ss.t[:, 0:nfull * w].rearrange("p (n q) -> p n q", n=nfull)), bmv, ALU.add)
                            if nfull < len(subs):
                                nql = subs[-1][2]
                                fw.tt(dve, tsb[:, nfull * w:nfull * w + nql], ss[:, nfull * w:nfull * w + nql],
                                      BM[:, h, bm_off:bm_off + nql], ALU.add)
                            fw.activation(pt[:, 0:tot], tsb[:, 0:tot], AF.Exp)

                        def st2():
                            for (kcols, qcols, nq, pvl), o_ in zip(subs, offs):
                                for (qs, vtile, tbk, ocols) in pvl:
                                    fw.mm(ocols, VA[:, vtile, hh, :], pt[:, o_ + qs.start:o_ + qs.stop], start=(not started[tbk]),
                                          stop=False, signal=True, skip_group_check=True)
                                    started[tbk] = True
                        unitsA.append((st1, st2))

                    def p1_sub(kt):
                        nqt = 2 if kt < 15 else 1
                        pvl = []
                        for j in range(nqt):
                            qt = kt + j
                            pvl.append((slice(j * 128, (j + 1) * 128), 0 * 16 + kt, qt // 4,
                                        PS[qt // 4][:, (qt % 4) * 128:(qt % 4 + 1) * 128]))
                        return (kT[ro:ro + 64, kt * 128:(kt + 1) * 128], qT[ro:ro + 64, kt * 128:kt * 128 + nqt * 128], nqt * 128, pvl)

                    def p2_sub(r, b_):
                        nqt = 2 if b_ < 3 else 1
                        pvl = []
                        for j in range(nqt):
                            pvl.append((slice(j * 128, (j + 1) * 128), 16 + r * 4 + b_, b_ + j, PS[b_ + j][:, r::4]))
                        return (kT[ro:ro + 64, 512 * b_ + r:512 * (b_ + 1):4], qT[ro:ro + 64, 512 * b_ + r:512 * (b_ + nqt):4],
                                nqt * 128, pvl)

                    def p3_sub(r):
                        pvl = []
                        for tbk in range(4):
                            pvl.append((slice(tbk * 32, (tbk + 1) * 32), 32 + r, tbk, PS[tbk][:, r::16]))
                        return (kT[ro:ro + 64, r::16], qT[ro:ro + 64, r::16], 128, pvl)

                    for kt in range(0, 16, 2):
                        sunit([p1_sub(kt), p1_sub(kt + 1)], 0, 256)
                    for r in range(4):
                        for b_ in (0, 2):
                            sunit([p2_sub(r, b_), p2_sub(r, b_ + 1)], 256, 256)
                    for r in range(0, 16, 4):
                        sunit([p3_sub(r + j) for j in range(4)], 512, 128)
                    run_pipe(unitsA)
                    for tb in range(4):
                        finalize_head(PS[tb], ro, mixT, c, tb, fin)
            dump(f"mixA{l}", mixT[:, 0:3, :], [128, 3, S], BF16)
            fw.barrier()

            MARKS.append(('B', l, pe.ninst))
            AX.reset()
            qlatT = AX.alloc(fw, "qlatT", [128, 2, S], BF16)
            kvlatT = AX.alloc(fw, "kvlatT", [128, S], BF16)
            sqb = [AX.alloc(fw, "sqb0", [128, 2, 512], BF16)] * 2
            rsq = AX.alloc(fw, "rsq", [128, S], F32)
            rskv = AX.alloc(fw, "rskv", [128, S], F32)
            rstok = AX.alloc(fw, "rstok", [128, 16], F32)
            qTh = AX.alloc(fw, "qTh", [128, S], BF16)
            KTh = AX.alloc(fw, "KTh", [128, S], BF16)
            VBs = [AX.alloc(fw, f"VB{i}", [128, 16, 128], BF16) for i in range(2)]
            PTB = [AX.alloc(fw, f"ptb{i}", [128, 512], BF16) for i in range(4)]
            TSB = [AX.alloc(fw, f"tsb{i}", [128, 128], F32) for i in range(2)]
            RT = [AX.alloc(fw, f"rt{i}", [128, 512], F32) for i in range(3)]
            fin = (AX.alloc(fw, "fsq", [128, 512], BF16), AX.alloc(fw, "fln", [128, 512], F32), AX.alloc(fw, "frs", [128, 512], F32))
            AW.reset()
            winB = AW.alloc(fw, "winB", [128, 8, 416], BF16)
            wkrr = AW.alloc(fw, "wkrr", [128, 8, 32], BF16)
            wuq_f = AW.alloc(fw, "wuqf", [128, 2, 384], F32)
            wuq_b = AW.alloc(fw, "wuqb", [128, 2, 384], BF16)
            wuq_r = AW.alloc(fw, "wuqr", [128, 2, 4, 32], BF16)
            wukv_f = AW.alloc(fw, "wukvf", [128, 512], F32)
            wukv_b = AW.alloc(fw, "wukvb", [128, 512], BF16)
            fw.memset(pool, VBs[0][:, :, 64:128], 1.0)
            fw.memset(pool, VBs[1][:, :, 0:64], 1.0)
            load_w(winB[:], winv[:, :, OFF_BQ:OFF_BQ + 416])
            pool.dma_group([(wkrr[:, :, 0:16], winv[:, :, OFF_BKR + 16:OFF_BKR + 32]),
                            (wkrr[:, :, 16:32], winv[:, :, OFF_BKR:OFF_BKR + 16])], max_dma_last_dim=4096)
            fw.activation(wkrr[:, :, 0:16], wkrr[:, :, 0:16], AF.Identity, scale=-1.0)
            sp.dma(wuq_f[:], wuq_d[l].rearrange("(k p) n -> p k n", p=128))
            sp.dma(wukv_f[:], wukv_d[l])
            for kc in range(2):
                fw.activation(wuq_b[:, kc, :], wuq_f[:, kc, :], AF.Identity, scale=MQN[:, l, kc:kc + 1])
            fw.activation(wukv_b[:], wukv_f[:], AF.Identity, scale=MKVN[:, l, 0:1])
            wq4 = wuq_b.t[:, :, :].rearrange("p k (h e) -> p k h e", h=4)
            fw.activation(wuq_r.v(wuq_r.t[:, :, :, 0:16]), wuq_b.v(wq4[:, :, :, 80:96]), AF.Identity, scale=-1.0)
            fw.copy(dve, wuq_r.v(wuq_r.t[:, :, :, 16:32]), wuq_b.v(wq4[:, :, :, 64:80]))

            for c2 in range(2):
                proj_fm(lambda tb, ts_, pv, c2=c2: fw.copy(dve, qlatT[:, c2, ts_], pv),
                        lambda kc, c2=c2: winB[:, kc, c2 * 128:(c2 + 1) * 128], 128)
            for tb in range(4):
                ts_ = slice(tb * 512, (tb + 1) * 512)
                fw.activation(sqb[tb % 2][:], qlatT[:, :, ts_], AF.Square)
                pb = PS[4 + tb % 2]
                for c2 in range(2):
                    fw.mm(pb[:], ones_b[:], sqb[tb % 2][:, c2, :], start=(c2 == 0), stop=(c2 == 1))
                fw.activation(RT[0][:], pb[:], AF.Ln, scale=1.0 / 256, bias=EPSC[:, 0:1])
                fw.activation(rsq[:, ts_], RT[0][:], AF.Exp, scale=-0.5)
            proj_fm(lambda tb, ts_, pv: fw.copy(dve, kvlatT[:, ts_], pv), lambda kc: winB[:, kc, 256:384], 128)
            for tb in range(4):
                ts_ = slice(tb * 512, (tb + 1) * 512)
                fw.activation(sqb[tb % 2][:, 0, :], kvlatT[:, ts_], AF.Square)
                pb = PS[4 + tb % 2]
                fw.mm(pb[:], ones_b[:], sqb[tb % 2][:, 0, :], start=True, stop=True)
                fw.activation(RT[0][:], pb[:], AF.Ln, scale=1.0 / 128, bias=EPSC[:, 0:1])
                fw.activation(rskv[:, ts_], RT[0][:], AF.Exp, scale=-0.5)
            identb4 = ident_f.v(bass.AP(ident_f.t, 0, [[128, 128], [0, 4], [1, 128]]))
            for g4 in range(4):
                tdv = RT[0].v(RT[0].t[:, :].rearrange("p (n q) -> p n q", n=4))
                fw.tt(dve, tdv, rskv.v(rskv.t[:, g4 * 512:(g4 + 1) * 512].rearrange("p (n q) -> p n q", n=4)), identb4, ALU.mult)
                _o = rstok.t[:, g4 * 4:(g4 + 1) * 4]
                _i = RT[0].t[:, :].rearrange("p (n q) -> p n q", n=4)
                dve.issue(lambda e, _o=_o, _i=_i: e.tensor_reduce(out=_o, in_=_i, axis=AX_X, op=ALU.add),
                          reads=[RT[0][:]], writes=[rstok[:]])
            for tb in range(4):
                ts_ = slice(tb * 512, (tb + 1) * 512)
                p1, p2 = PS[6], PS[7]
                for kc in range(8):
                    fw.mm(p1[64:96, :], winB[:, kc, 384:416], hT[:, kc, ts_], start=(kc == 0), stop=(kc == 7))
                for kc in range(8):
                    fw.mm(p2[64:96, :], wkrr[:, kc, :], hT[:, kc, ts_], start=(kc == 0), stop=(kc == 7))
                fw.tt(dve, RT[0][64:96, :], p1[64:96, :], FCOS[64:96, ts_], ALU.mult)
                fw.tt(dve, RT[1][64:96, :], p2[64:96, :], FSIN[64:96, ts_], ALU.mult)
                fw.tt(pool, KTh[64:96, ts_], RT[0][64:96, :], RT[1][64:96, :], ALU.add)
            dump(f"rsq{l}", rsq[:], [128, S], F32)
            SCB = float((64 + 32) ** -0.5)
            for h in (range(4) if 'B' not in skip else []):
                hh = h % 2
                nr = 64 * hh
                for tb in range(4):
                    ts_ = slice(tb * 512, (tb + 1) * 512)
                    p1, p2 = PS[6], PS[7]
                    for kc in range(2):
                        fw.mm(p1[0:96, :], wuq_b[:, kc, h * 96:(h + 1) * 96], qlatT[:, kc, ts_], start=(kc == 0), stop=(kc == 1))
                    for kc in range(2):
                        fw.mm(p2[64:96, :], wuq_r.v(wuq_r.t[:, kc, h, :]), qlatT[:, kc, ts_], start=(kc == 0), stop=(kc == 1))
                    fw.stt(qTh[0:64, ts_], p1[0:64, :], SCB, rsq[0:64, ts_], ALU.mult, ALU.mult)
                    fw.tt(dve, RT[0][64:96, :], p1[64:96, :], FCOS[64:96, ts_], ALU.mult)
                    fw.tt(dve, RT[1][64:96, :], p2[64:96, :], FSIN[64:96, ts_], ALU.mult)
                    fw.tt(pool, RT[2][64:96, :], RT[0][64:96, :], RT[1][64:96, :], ALU.add)
                    fw.stt(qTh[64:96, ts_], RT[2][64:96, :], SCB, rsq[64:96, ts_], ALU.mult, ALU.mult)
                    p3 = PS[4 + tb % 2]
                    fw.mm(p3[0:64, :], wukv_b[:, h * 128:h * 128 + 64], kvlatT[:, ts_], start=True, stop=True)
                    fw.tt(dve, KTh[0:64, ts_], p3[0:64, :], rskv[0:64, ts_], ALU.mult)
                for tg in range(2):
                    pb = PS[4 + tg]
                    for t8 in range(8):
                        tile = tg * 8 + t8
                        fw.mm(pb[:, t8 * 64:(t8 + 1) * 64], kvlatT[:, tile * 128:(tile + 1) * 128],
                              wukv_b[:, h * 128 + 64:(h + 1) * 128], start=True, stop=True, signal=(t8 == 7))
                    for t8 in range(8):
                        tile = tg * 8 + t8
                        fw.activation(VBs[hh][:, tile, 64 * hh:64 * hh + 64], pb[:, t8 * 64:(t8 + 1) * 64], AF.Identity,
                                      scale=rstok[:, tile:tile + 1])
                if h == 0:
                    dump(f"qTh{l}", qTh[0:96, :], [96, S], BF16)
                    dump(f"KTh{l}", KTh[0:96, :], [96, S], BF16)
                    dump(f"VB{l}", VBs[0][:], [128, 16, 128], BF16)
                started = [False] * 4
                unitsB = []
                bi = 0
                for kt in range(16):
                    for tb in range(kt // 4, 4):
                        q0 = max(128 * kt, 512 * tb)
                        q1 = 512 * (tb + 1)

                        def mk(kt=kt, tb=tb, q0=q0, q1=q1, bi=bi):
                            nq = q1 - q0
                            sbk = PS[4 + bi % 4]
                            pt = PTB[bi % 4]

                            def st1():
                                fw.mm(sbk[:, 0:nq], KTh[0:96, kt * 128:(kt + 1) * 128], qTh[0:96, q0:q1], start=True, stop=True)
                                if q0 == 128 * kt:
                                    tsb = TSB[kt % 2]
                                    fw.tt(dve, tsb[:], sbk[:, 0:128], CM[:], ALU.add)
                                    fw.activation(pt[:, 0:128], tsb[:], AF.Exp)
                                    if nq > 128:
                                        fw.activation(pt[:, 128:nq], sbk[:, 128:nq], AF.Exp)
                                else:
                                    fw.activation(pt[:, 0:nq], sbk[:, 0:nq], AF.Exp)

                            def st2():
                                fw.mm(PS[tb][:, q0 - 512 * tb:q1 - 512 * tb], VBs[hh][:, kt, :], pt[:, 0:nq], start=(not started[tb]),
                                      stop=False, signal=True, skip_group_check=True)
                                started[tb] = True
                            return (st1, st2)
                        unitsB.append(mk())
                        bi += 1
                run_pipe(unitsB)
                for tb in range(4):
                    finalize_head(PS[tb], nr, mixT, 3 + h // 2, tb, fin)
            dump(f"mixB{l}", mixT[:, 3:5, :], [128, 2, S], BF16)
            fw.barrier()

            MARKS.append(('C', l, pe.ninst))
            AX.reset()
            qTc = AX.alloc(fw, "qTc", [128, S], BF16)
            kTc = AX.alloc(fw, "kTc", [128, S], BF16)
            qxT = AX.alloc(fw, "qxT", [128, S], BF16)
            kz = AX.alloc(fw, "kz", [128, 16, 64], BF16)
            VC = AX.alloc(fw, "VC", [128, 16, 128], BF16)
            gT = AX.alloc(fw, "gT", [128, S], BF16)
            STf = AX.alloc(fw, "stf", [64, 16, 64], F32)
            STb = AX.alloc(fw, "stb", [64, 16, 64], BF16)
            PTC = [AX.alloc(fw, f"ptc{i}", [128, 128], BF16) for i in range(4)]
            RT = [AX.alloc(fw, f"rtc{i}", [128, 512], F32) for i in range(3)]
            CP = AX.alloc(fw, "ccp", [128, 512], BF16)
            CSQ = AX.alloc(fw, "csq", [128, 512], BF16)
            CMEAN = AX.alloc(fw, "cmean", [128, 512], F32)
            CMSQ = AX.alloc(fw, "cmsq", [128, 512], F32)
            CDV = AX.alloc(fw, "cdv", [128, 512], F32)
            CVAR = AX.alloc(fw, "cvar", [128, 512], F32)
            CRS = AX.alloc(fw, "crs", [128, 512], F32)
            AW.reset()
            wo = AW.alloc(fw, "wo", [128, 8, D], BF16)
            wov = wout_d[l].rearrange("(k p) n -> p k n", p=128)
            wcq = AW.alloc(fw, "wcq", [128, 8, 64], BF16)
            wcqr = AW.alloc(fw, "wcqr", [128, 8, 2, 32], BF16)
            wck = AW.alloc(fw, "wck", [128, 8, 64], BF16)
            wckr = AW.alloc(fw, "wckr", [128, 8, 2, 32], BF16)
            wcv = AW.alloc(fw, "wcv", [128, 8, 128], BF16)
            wcg = AW.alloc(fw, "wcg", [128, 8, 128], BF16)
            SC_ = [sub(PS[4], "sc0"), sub(PS[4], "sc1"), sub(PS[5], "sc2"), sub(PS[5], "sc3")]
            for c in (range(3) if 'C' not in skip else []):
                def load_rot(dst, off):
                    prs = []
                    for h2 in range(2):
                        prs.append((dst.v(dst.t[:, :, h2, 0:16]), winv[:, :, off + h2 * 32 + 16:off + h2 * 32 + 32]))
                        prs.append((dst.v(dst.t[:, :, h2, 16:32]), winv[:, :, off + h2 * 32:off + h2 * 32 + 16]))
                    pool.dma_group(prs, max_dma_last_dim=4096)
                    fw.activation(dst.v(dst.t[:, :, :, 0:16]), dst.v(dst.t[:, :, :, 0:16]), AF.Identity, scale=-1.0)
                load_w(wcq[:], winv[:, :, OFF_CQ + c * 64:OFF_CQ + (c + 1) * 64])
                load_rot(wcqr, OFF_CQ + c * 64)
                load_w(wck[:], winv[:, :, OFF_CK + c * 64:OFF_CK + (c + 1) * 64])
                load_rot(wckr, OFF_CK + c * 64)
                load_w(wcv[:], winv[:, :, OFF_CV + c * 128:OFF_CV + (c + 1) * 128])
                load_w(wcg[:], winv[:, :, OFF_CG + c * 128:OFF_CG + (c + 1) * 128])
                if c == 0:
                    load_w(wo[:], wov)
                for (w_, wr_, dst, isq) in [(wcq, wcqr, qTc, True), (wck, wckr, kTc, False)]:
                    for tb in range(4):
                        ts_ = slice(tb * 512, (tb + 1) * 512)
                        p1, p2 = PS[6], PS[7]
                        for kc in range(8):
                            fw.mm(p1[0:64, :], w_[:, kc, :], hT[:, kc, ts_], start=(kc == 0), stop=(kc == 7))
                        for kc in range(8):
                            fw.mm(p2[0:64, :], wr_.v(wr_.t[:, kc, :, :].rearrange("p h e -> p (h e)")), hT[:, kc, ts_],
                                  start=(kc == 0), stop=(kc == 7))
                        fw.tt(dve, RT[0][0:64, :], p1[0:64, :], FCOS[0:64, ts_], ALU.mult)
                        fw.tt(dve, RT[1][0:64, :], p2[0:64, :], FSIN[0:64, ts_], ALU.mult)
                        if isq:
                            fw.tt(pool, RT[2][0:64, :], RT[0][0:64, :], RT[1][0:64, :], ALU.add)
                            fw.copy(act, dst[0:64, ts_], RT[2][0:64, :])
                            xib = XI.v(bass.AP(XI.t, c * 128, [[3 * 128, 64], [0, 4], [1, 128]]))
                            fw.tt(pool, qxT.v(qxT.t[0:64, ts_].rearrange("p (n q) -> p n q", n=4)),
                                  RT[2].v(RT[2].t[0:64, :].rearrange("p (n q) -> p n q", n=4)), xib, ALU.mult)
                        else:
                            fw.tt(pool, dst[0:64, ts_], RT[0][0:64, :], RT[1][0:64, :], ALU.add)
                proj_fm(lambda tb, ts_, pv: fw.activation(gT[:, ts_], pv, AF.Silu), lambda kc: wcg[:, kc, :], 128)
                for tg in range(4):
                    pb = PS[6 + tg % 2]
                    for t4 in range(4):
                        tile = tg * 4 + t4
                        for kc in range(8):
                            fw.mm(pb[:, t4 * 128:(t4 + 1) * 128], hT[:, kc, tile * 128:(tile + 1) * 128], wcv[:, kc, :],
                                  start=(kc == 0), stop=(kc == 7), signal=(kc == 7 and t4 == 3))
                    fw.copy(act if tg % 2 else dve, VC.v(VC.t[:, tg * 4:tg * 4 + 4, :].rearrange("p n c -> p (n c)")), pb[:])
                for tg in range(2):
                    pb = PS[6 + tg]
                    tb16 = pb.t.bitcast(BF16)
                    for t8 in range(8):
                        tile = tg * 8 + t8
                        fw.transpose(pb.v(tb16[:, t8 * 64:(t8 + 1) * 64]), kTc[0:64, tile * 128:(tile + 1) * 128],
                                     ident_b[0:64, 0:64], signal=(t8 == 7))
                    zb = ZETA.v(bass.AP(ZETA.t, c * 64, [[3 * 64, 128], [0, 8], [1, 64]]))
                    fw.tt(dve, kz[:, tg * 8:(tg + 1) * 8, :],
                          pb.v(tb16[:, 0:512].rearrange("p (n c) -> p n c", n=8)), zb, ALU.mult)
                for hh in range(2):
                    for n in range(15):
                        pu = PS[6 + (n // 8)]
                        fw.mm(pu[32 * hh:32 * hh + 32, (n % 8) * 64:(n % 8 + 1) * 64], kz[:, n, 32 * hh:32 * hh + 32],
                              VC[:, n, 64 * hh:64 * hh + 64], start=True, stop=True, signal=(n in (7, 14)))
                fw.copy(dve, STf[:, 1, :], PS[6][0:64, 0:64])
                for n in range(1, 15):
                    pu = PS[6 + (n // 8)]
                    fw.stt(STf[:, n + 1, :], STf[:, n, :], GAM[:, c:c + 1], pu[0:64, (n % 8) * 64:(n % 8 + 1) * 64], ALU.mult, ALU.add)
                fw.copy(act, STb[:, 1:16, :], STf[:, 1:16, :])
                if c == 0:
                    dump(f"qTc{l}", qTc[0:64, :], [64, S], BF16)
                    dump(f"kTc{l}", kTc[0:64, :], [64, S], BF16)
                    dump(f"qxT{l}", qxT[0:64, :], [64, S], BF16)
                    dump(f"kz{l}", kz[:], [128, 16, 64], BF16)
                    dump(f"VC{l}", VC[:], [128, 16, 128], BF16)
                    dump(f"STf{l}", STf[:, 1:16, :], [64, 15, 64], F32)
                    dump(f"gT{l}", gT[:], [128, S], BF16)
                unitsC = []
                bi = 0
                for n in range(16):
                    for hh in range(2):
                        def mk(n=n, hh=hh, bi=bi):
                            h = 2 * c + hh
                            ss = PS[4 + bi % 4]
                            ssv = ss[:, 0:128]
                            pt = PTC[bi % 4]
                            cs = slice(n * 128, (n + 1) * 128)
                            ob = PS[n // 4][64 * hh:64 * hh + 64, (n % 4) * 128:(n % 4 + 1) * 128]

                            def st1():
                                fw.mm(ssv, kTc[32 * hh:32 * hh + 32, cs], qTc[32 * hh:32 * hh + 32, cs], start=True, stop=True)
                                fw.tt(dve, pt[:], ssv, DECT[:, h, :], ALU.mult)

                            def st2():
                                fw.mm(ob, VC[:, n, 64 * hh:64 * hh + 64], pt[:], start=True, stop=(n == 0), signal=True, skip_group_check=True)
                                if n > 0:
                                    fw.mm(ob, STb[32 * hh:32 * hh + 32, n, :], qxT[32 * hh:32 * hh + 32, cs], start=False, stop=True,
                                          signal=True, skip_group_check=True)
                            return (st1, st2)
                        unitsC.append(mk())
                        bi += 1
                run_pipe(unitsC)
                for tb in range(4):
                    ts_ = slice(tb * 512, (tb + 1) * 512)
                    ob = PS[tb]
                    fw.copy(act, CP[:], ob[:])
                    fw.activation(CSQ[:], ob[:], AF.Square)
                    pm, p2_ = PS[6], PS[7]
                    fw.mm(pm[:], BD[:], CP[:], start=True, stop=True)
                    fw.mm(p2_[:], BD[:], CSQ[:], start=True, stop=True)
                    fw.copy(act, CMEAN[:], pm[:])
                    fw.activation(CMSQ[:], pm[:], AF.Square)
                    fw.tt(dve, CDV[:], ob[:], CMEAN[:], ALU.subtract)
                    fw.tt(dve, CVAR[:], p2_[:], CMSQ[:], ALU.subtract)
                    fw.ts(dve, CVAR[:], CVAR[:], 0.0, ALU.max)
                    fw.activation(CVAR[:], CVAR[:], AF.Ln, bias=EPSC[:, 0:1])
                    fw.activation(CRS[:], CVAR[:], AF.Exp, scale=-0.5)
                    fw.tt(dve, CDV[:], CDV[:], CRS[:], ALU.mult)
                    fw.tt(pool, mixT[:, 5 + c, ts_], CDV[:], gT[:, ts_], ALU.mult)
            for kc in range(8):
                fw.activation(wo[:, kc, :], wo[:, kc, :], AF.Identity, scale=MG[:, l, kc:kc + 1])
            dump(f"mixC{l}", mixT[:, 5:8, :], [128, 3, S], BF16)
            fw.barrier()

            MARKS.append(('wout', l, pe.ninst))
            AX.reset()
            xT = AX.alloc(fw, "xT", [128, 8, S], F32)
            sp.dma_group([(xT[:, kc, :], xsp_d[:, kc, :]) for kc in range(8)])
            for d_ in range(8):
                for tb in range(4):
                    ts_ = slice(tb * 512, (tb + 1) * 512)
                    pb = PS[(d_ * 4 + tb) % 4]
                    for kc in range(8):
                        fw.mm(pb[:], wo[:, kc, d_ * 128:(d_ + 1) * 128], mixT[:, kc, ts_], start=(kc == 0), stop=(kc == 7))
                    fw.stt(xT[:, d_, ts_], pb[:], MOD[:, l, 16 + d_:17 + d_], xT[:, d_, ts_], ALU.mult, ALU.add)
            dump(f"xmid{l}", xT[:], [128, 8, S], F32)
            fw.barrier()

            MARKS.append(('ffn', l, pe.ninst))
            AY.reset()
            norm_to_hT(l, lambda kc: A2[:, l, kc:kc + 1], 24, AY)
            fw.barrier()
            AY.reset()
            actT = AY.alloc(fw, "actT", [128, 6, S], BF16)
            sgt = [AY.alloc(fw, f"sg{i}", [128, 512], BF16) for i in range(2)]
            AW.reset()
            wdn = [AW.alloc(fw, f"wdn{i}", [128, 6, D], BF16) for i in range(2)]
            gu = [AW.alloc(fw, f"gu{i}", [128, 2, 8, 128], BF16) for i in range(3)]
            wgv = wg_d[l].rearrange("(k p) n -> p k n", p=128)
            wuv = wu_d[l].rearrange("(k p) n -> p k n", p=128)
            ji = 0
            modgen = mod_pieces(l + 1) if l + 1 < depth else iter(())
            for gi, (j0, nj) in enumerate(FFN_GROUPS):
                wdb = wdn[gi % 2]
                load_w(wdb[:, 0:nj, :], wd_d[l, j0 * 128:(j0 + nj) * 128, :].rearrange("(j p) n -> p j n", p=128))
                for jj in range(nj):
                    j = j0 + jj
                    g = gu[ji % 3]
                    ji += 1
                    pool.dma_group([(g[:, 0, :, :], wgv[:, :, j * 128:(j + 1) * 128]),
                                    (g[:, 1, :, :], wuv[:, :, j * 128:(j + 1) * 128])], max_dma_last_dim=4096)
                    for tb in range(4):
                        ts_ = slice(tb * 512, (tb + 1) * 512)
                        pg, pu = PS[(tb % 2) * 2], PS[(tb % 2) * 2 + 1]
                        for kc in range(8):
                            fw.mm(pg[:], g[:, 0, kc, :], hT[:, kc, ts_], start=(kc == 0), stop=(kc == 7))
                        for kc in range(8):
                            fw.mm(pu[:], g[:, 1, kc, :], hT[:, kc, ts_], start=(kc == 0), stop=(kc == 7))
                        fw.activation(sgt[tb % 2][:], pg[:], AF.Silu)
                        fw.tt(dve, actT[:, jj, ts_], pu[:], sgt[tb % 2][:], ALU.mult)
                    next(modgen, None)
                for d_ in range(8):
                    for tb in range(4):
                        ts_ = slice(tb * 512, (tb + 1) * 512)
                        pb = PS[4 + (d_ * 4 + tb) % 3]
                        for jj in range(nj):
                            fw.mm(pb[:], wdb[:, jj, d_ * 128:(d_ + 1) * 128], actT[:, jj, ts_], start=(jj == 0), stop=(jj == nj - 1))
                        fw.stt(xT[:, d_, ts_], pb[:], MOD[:, l, 40 + d_:41 + d_], xT[:, d_, ts_], ALU.mult, ALU.add)
            for _ in modgen:
                pass
            dump(f"xout{l}", xT[:], [128, 8, S], F32)
            fw.barrier()

        MARKS.append(('final', depth, pe.ninst))
        AY.reset()
        sq = [AY.alloc(fw, f"fsq{i}", [128, 8, 512], BF16) for i in range(2)]
        lnt = AY.alloc(fw, "fln", [128, 512], F32)
        rs = AY.alloc(fw, "frs", [128, 512], F32)
        yt = [AY.alloc(fw, f"fyt{i}", [128, 512], F32) for i in range(3)]
        AW.reset()
        OUT = [AW.alloc(fw, f"oo{i}", [128, 4, D], F32) for i in range(2)]
        stores = []
        for tb in (range(4) if 'final' not in skip else []):
            ts_ = slice(tb * 512, (tb + 1) * 512)
            fw.activation(sq[tb % 2][:], xT[:, :, ts_], AF.Square)
            pb = PS[tb % 2]
            for kc in range(8):
                fw.mm(pb[:], ones_b[:], sq[tb % 2][:, kc, :], start=(kc == 0), stop=(kc == 7))
            fw.activation(lnt[:], pb[:], AF.Ln, scale=1.0 / D, bias=EPSC[:, 0:1])
            fw.activation(rs[:], lnt[:], AF.Exp, scale=-0.5)
            ob_ = OUT[tb % 2]
            for kc in range(8):
                t = yt[kc % 3]
                fw.stt(t[:], xT[:, kc, ts_], FNG[:, kc:kc + 1], rs[:], ALU.mult, ALU.mult)
                pt_ = PS[2 + kc % 4]
                for q in range(4):
                    fw.transpose(pt_[:, q * 128:(q + 1) * 128], t[:, q * 128:(q + 1) * 128], ident_f[:], signal=(q == 3))
                fw.copy(act if kc % 2 else dve, ob_[:, :, kc * 128:(kc + 1) * 128],
                        pt_.v(pt_.t[:, :].rearrange("p (q c) -> p q c", q=4)))
            stores.append(sp.dma_group([(y_d[(tb * 4 + q) * 128:(tb * 4 + q + 1) * 128, :], ob_[:, q, :]) for q in range(4)]))
        for tok in stores:
            sp.wait_tok(tok)
        for tok in dbg_out.values():
            sp.wait_tok(tok)
        fw.emit()
        info = {n: (e.ninst, e.nwait) for n, e in fw.E.items()}
        info["sp"] = (fw.sp.ninst, fw.sp.nwait)
        info["sems"] = fw.nsem
        print("MK build:", info)
    return nc


AX_X = mybir.AxisListType.X
MARKS = []
_NC_CACHE = {}


def prep_inputs(inputs, b):
    f32 = np.float32
    g = lambda k: np.asarray(inputs[k])
    hc = _HC_CACHE.get("hc")
    if hc is None:
        hc = host_consts(g("rel_bias").astype(f32))
        _HC_CACHE["hc"] = hc
    m = dict(hc)
    m["x"] = np.ascontiguousarray(g("x")[b].astype(f32))
    m["cT"] = np.ascontiguousarray(g("c")[b].astype(f32).reshape(8, 128).T)
    m["posb"] = np.ascontiguousarray(g("positions")[b].astype(np.int32).reshape(1, S))
    sh = _HC_CACHE.get("shared")
    if sh is None:
        def fm(a, nch):
            a = np.asarray(a, f32)
            return np.ascontiguousarray(a.reshape(a.shape[0], nch, 128).transpose(2, 0, 1))
        sh = dict(
            ada_w=np.ascontiguousarray(g("ada_w").astype(f32)),
            adabT=fm(g("ada_b"), 48), n1gT=fm(g("norm1_g"), 8), n2gT=fm(g("norm2_g"), 8),
            fngT=np.ascontiguousarray(g("final_norm").astype(f32).reshape(8, 128).T),
            mgT=fm(g("mix_gain"), 8), mqnT=fm(g("mla_q_norm"), 2), mkvnT=fm(g("mla_kv_norm"), 1),
            w_in=np.ascontiguousarray(g("w_in").astype(f32)), w_uq=np.ascontiguousarray(g("mla_w_uq").astype(f32)),
            w_ukv=np.ascontiguousarray(g("mla_w_ukv").astype(f32)), w_out=np.ascontiguousarray(g("w_out").astype(f32)),
            w_gate=np.ascontiguousarray(g("ffn_w_gate").astype(f32)), w_up=np.ascontiguousarray(g("ffn_w_up").astype(f32)),
            w_down=np.ascontiguousarray(g("ffn_w_down").astype(f32)),
        )
        _HC_CACHE["shared"] = sh
    m.update(sh)
    return m


_HC_CACHE = {}


def kernel(**inputs):
    _HC_CACHE.clear()
    nc = build()
    in_maps = [prep_inputs(inputs, b) for b in range(NCORES)]
    res = run_bass_kernel_spmd(nc, in_maps, core_ids=list(range(NCORES)))
    out = np.stack([np.asarray(r["y"], dtype=np.float32) for r in res.results], axis=0)
    return out
```

```python
import numpy as np
import concourse.bass as bass
import concourse.mybir as mybir
from concourse.bass_utils import run_bass_kernel_spmd
from contextlib import ExitStack

F32 = mybir.dt.float32
BF16 = mybir.dt.bfloat16
I32 = mybir.dt.int32
AF = mybir.ActivationFunctionType
ALU = mybir.AluOpType
AX = mybir.AxisListType

CE = ('pe', 'act', 'dve', 'pool')
SAME_ENGINE_SYNC = True


class Buf:
    __slots__ = ('t', 'name', 'w', 'r', 'semname')

    def __init__(self, t, name, semname=None):
        self.t = t
        self.name = name
        self.w = None
        self.r = {}
        self.semname = semname or name

    def __getitem__(self, idx):
        return V(self, self.t[idx])

    def v(self, ap):
        return V(self, ap)


class V:
    __slots__ = ('buf', 'ap')

    def __init__(self, buf, ap):
        self.buf = buf
        self.ap = ap


class Eng:
    def __init__(self, fw, name, sem):
        self.fw = fw
        self.name = name
        self.sem = sem
        self.count = 0
        self.seen = {}
        self.prog = []
        self.snaps = {0: {}}
        self.nwait = 0
        self.ninst = 0

    def _deps(self, reads, writes):
        deps = []
        for v in reads:
            if v.buf.w is not None:
                deps.append(v.buf.w)
        for v in writes:
            if v.buf.w is not None:
                deps.append(v.buf.w)
            deps.extend(v.buf.r.values())
        need = {}
        for (key, val, sem) in deps:
            if key == self.name and (self.name == 'pe' or not SAME_ENGINE_SYNC):
                continue
            if val > self.seen.get(key, 0) and val > need.get(key, (0, None))[0]:
                need[key] = (val, sem)
        for key, (val, sem) in need.items():
            self.prog.append(('wait', sem, val))
            self.nwait += 1
            self.seen[key] = val
            if key in self.fw.E and key != self.name:
                snap = self.fw.E[key].snaps.get(val)
                if snap:
                    for k2, v2 in snap.items():
                        if v2 > self.seen.get(k2, 0):
                            self.seen[k2] = v2

    def issue(self, fn, reads=(), writes=(), signal=True):
        self._deps(reads, writes)
        self.ninst += 1
        if signal:
            self.count += 1
            self.prog.append(('inst', fn, True))
            self.snaps[self.count] = {k: v for k, v in self.seen.items() if k in CE}
            tok = (self.name, self.count, self.sem)
        else:
            self.prog.append(('inst', fn, False))
            tok = (self.name, self.count + 1, self.sem)
        for v in reads:
            v.buf.r[self.name] = tok
        for v in writes:
            v.buf.w = tok
            v.buf.r = {}
        return tok

    def dma(self, out, in_, **kw):
        return self.dma_group([(out, in_)], **kw)

    def dma_group(self, pairs, **kw):
        reads = [i for (o, i) in pairs if isinstance(i, V)]
        writes = [o for (o, i) in pairs if isinstance(o, V)]
        self._deps(reads, writes)
        buf = (writes[0].buf if writes else reads[0].buf)
        rec = self.fw.dsem.get(buf.semname)
        if rec is None:
            rec = [self.fw.new_sem('d_' + buf.semname), 0]
            self.fw.dsem[buf.semname] = rec
        for (o, i) in pairs:
            rec[1] += 1
            oa = o.ap if isinstance(o, V) else o
            ia = i.ap if isinstance(i, V) else i
            self.prog.append(('dma', oa, ia, rec[0], kw))
        tok = (('d', buf.semname), 16 * rec[1], rec[0])
        for v in reads:
            v.buf.r[tok[0]] = tok
        for v in writes:
            v.buf.w = tok
            v.buf.r = {}
        return tok

    def wait_tok(self, tok):
        key, val, sem = tok
        if val > self.seen.get(key, 0):
            self.prog.append(('wait', sem, val))
            self.seen[key] = val

    def replay(self, e):
        for item in self.prog:
            if item[0] == 'wait':
                e.wait_ge(item[1], item[2])
            elif item[0] == 'inst':
                ins = item[1](e)
                if item[2]:
                    ins.then_inc(self.sem, 1)
            else:
                _, oa, ia, sem, kw = item
                e.dma_start(out=oa, in_=ia, **kw).then_inc(sem, 16)


class FW:
    def __init__(self, nc, stack):
        self.nc = nc
        self.stack = stack
        self.nsem = 0
        self.dsem = {}
        self.dma_toks = []
        self.E = {}
        for name in CE:
            self.E[name] = Eng(self, name, self.new_sem('c_' + name))
        self.sp = Eng(self, 'sp', None)
        self.pe, self.act, self.dve, self.pool = (self.E[n] for n in CE)

    def new_sem(self, name):
        self.nsem += 1
        return self.stack.enter_context(self.nc.semaphore(name))

    def sbuf(self, name, shape, dtype):
        t = self.stack.enter_context(self.nc.sbuf_tensor(name, list(shape), dtype))
        return Buf(t, name)

    def psum(self, name, shape, dtype):
        t = self.stack.enter_context(self.nc.psum_tensor(name, list(shape), dtype))
        return Buf(t, name)

    def barrier(self):
        toks = [(n, self.E[n].count, self.E[n].sem) for n in CE if self.E[n].count > 0]
        for e in list(self.E.values()) + [self.sp]:
            for tok in toks:
                if tok[0] != e.name:
                    e.wait_tok(tok)
            for tok in self.dma_toks:
                e.wait_tok(tok)
        self.dma_toks = []

    def check(self):
        engs = list(self.E.values()) + [self.sp]
        pc = {e.name: 0 for e in engs}
        sems = {}
        progress = True
        while progress:
            progress = False
            for e in engs:
                while pc[e.name] < len(e.prog):
                    it = e.prog[pc[e.name]]
                    if it[0] == 'wait':
                        if sems.get(id(it[1]), 0) < it[2]:
                            break
                    elif it[0] == 'inst':
                        if it[2]:
                            sems[id(e.sem)] = sems.get(id(e.sem), 0) + 1
                    else:
                        sems[id(it[3])] = sems.get(id(it[3]), 0) + 16
                    pc[e.name] += 1
                    progress = True
        stuck = {}
        for e in engs:
            if pc[e.name] < len(e.prog):
                it = e.prog[pc[e.name]]
                who = [n for n, x in self.E.items() if x.sem is it[1]] or [k for k, r in self.dsem.items() if r[0] is it[1]]
                stuck[e.name] = (pc[e.name], len(e.prog), who, it[2], sems.get(id(it[1]), 0))
        return stuck

    def emit(self):
        st = self.check()
        assert not st, f"DEADLOCK: {st}"
        with self.nc.Block() as block:
            @block.sync
            def _(e):
                self.sp.replay(e)

            @block.tensor
            def _(e):
                self.pe.replay(e)

            @block.scalar
            def _(e):
                self.act.replay(e)

            @block.vector
            def _(e):
                self.dve.replay(e)

            @block.gpsimd
            def _(e):
                self.pool.replay(e)

    def mm(self, out, lhsT, rhs, start=True, stop=True, signal=None, **kw):
        if signal is None:
            signal = stop
        return self.pe.issue(
            lambda e: e.matmul(out.ap, lhsT.ap, rhs.ap, start=start, stop=stop, **kw),
            reads=[lhsT, rhs], writes=[out], signal=signal)

    def transpose(self, out, in_, ident, signal=True):
        return self.pe.issue(
            lambda e: e.transpose(out.ap, in_.ap, ident.ap),
            reads=[in_, ident], writes=[out], signal=signal)

    def activation(self, out, in_, func, scale=1.0, bias=0.0, accum_out=None, eng=None):
        reads = [in_]
        sc = scale
        bi = bias
        if isinstance(scale, V):
            reads.append(scale)
            sc = scale.ap
        if isinstance(bias, V):
            reads.append(bias)
            bi = bias.ap
        writes = [out]
        kw = {}
        if accum_out is not None:
            writes.append(accum_out)
            kw['accum_out'] = accum_out.ap
        return self.act.issue(
            lambda e: e.activation(out=out.ap, in_=in_.ap, func=func, scale=sc, bias=bi, **kw),
            reads=reads, writes=writes)

    def tt(self, eng, out, in0, in1, op):
        return eng.issue(lambda e: e.tensor_tensor(out=out.ap, in0=in0.ap, in1=in1.ap, op=op),
                         reads=[in0, in1], writes=[out])

    def ts(self, eng, out, in0, s1, op0, s2=None, op1=None, accum_out=None):
        reads = [in0]
        a1 = s1
        a2 = s2
        if isinstance(s1, V):
            reads.append(s1)
            a1 = s1.ap
        if isinstance(s2, V):
            reads.append(s2)
            a2 = s2.ap
        kw = {}
        writes = [out]
        if op1 is not None:
            kw['op1'] = op1
        if accum_out is not None:
            kw['accum_out'] = accum_out.ap
            writes.append(accum_out)
        return eng.issue(lambda e: e.tensor_scalar(out=out.ap, in0=in0.ap, scalar1=a1, scalar2=a2, op0=op0, **kw),
                         reads=reads, writes=writes)

    def stt(self, out, in0, scalar, in1, op0, op1):
        reads = [in0, in1]
        a = scalar
        if isinstance(scalar, V):
            reads.append(scalar)
            a = scalar.ap
        return self.dve.issue(
            lambda e: e.scalar_tensor_tensor(out=out.ap, in0=in0.ap, scalar=a, in1=in1.ap, op0=op0, op1=op1),
            reads=reads, writes=[out])

    def copy(self, eng, out, in_):
        if eng is self.act:
            return eng.issue(lambda e: e.copy(out=out.ap, in_=in_.ap), reads=[in_], writes=[out])
        return eng.issue(lambda e: e.tensor_copy(out=out.ap, in_=in_.ap), reads=[in_], writes=[out])

    def memset(self, eng, out, val):
        return eng.issue(lambda e: e.memset(out.ap, val), writes=[out])

D = 1024
S = 2048
DEPTH = 4
NCORES = 8
IN_W = 2720
FH = 2816
OFF_AQ, OFF_AK, OFF_AV = 0, 384, 768
OFF_BQ, OFF_BKV, OFF_BKR = 1152, 1408, 1536
OFF_CQ, OFF_CK, OFF_CV, OFF_CG = 1568, 1760, 1952, 2336
EPS = 1e-6
NEG = -30000.0
SLAB = 209984
FFN_GROUPS = [(0, 6), (6, 6), (12, 5), (17, 5)]


def _t5_bucket(dist):
    n_buckets, max_distance = 32, 2048
    max_exact = n_buckets // 2
    safe = np.maximum(dist, 1).astype(np.float32)
    large = max_exact + (np.log(safe / max_exact) / np.log(max_distance / max_exact) * (n_buckets - max_exact)).astype(np.int32)
    large = np.minimum(large, n_buckets - 1)
    return np.where(dist < max_exact, dist, large).astype(np.int32)


def host_consts(rel_bias):
    f32 = np.float32
    k = np.arange(128)[:, None]
    ql = np.arange(256)[None, :]
    q3 = np.arange(128)[None, :]
    d1 = ql - k
    v1 = (d1 >= 0) & (d1 <= 128)
    i1 = _t5_bucket(np.clip(d1, 0, 128))
    v2 = v1
    i2 = _t5_bucket(np.clip(4 * d1, 0, 512))
    d3 = q3 - k
    v3 = d3 >= 0
    i3 = _t5_bucket(np.clip(16 * d3, 0, 2048))
    bmb = np.zeros((128, 6, 640), f32)
    for h in range(6):
        bmb[:, h, 0:256] = np.where(v1, rel_bias[i1, h], 0.0)
        bmb[:, h, 256:512] = np.where(v2, rel_bias[i2, h], 0.0)
        bmb[:, h, 512:640] = np.where(v3, rel_bias[i3, h], 0.0)
    bmm = np.concatenate([np.where(v1, 0.0, NEG), np.where(v2, 0.0, NEG), np.where(v3, 0.0, NEG)], axis=1).astype(f32)
    cm = np.where(q3 >= k, 0.0, NEG).astype(f32)
    H = 6
    log_g = np.log(1.0 - 2.0 ** (-5.0 - np.arange(H))).astype(f32)
    i = np.arange(128, dtype=f32)
    rel = i[:, None] - i[None, :]
    decay_intra = (np.exp(np.maximum(rel, 0.0)[None] * log_g[:, None, None]) * (rel >= 0)[None]).astype(f32)
    xi = np.exp((i + 1.0)[None, :] * log_g[:, None]).astype(f32)
    zeta = np.exp((128 - 1.0 - i)[None, :] * log_g[:, None]).astype(f32)
    cd = np.exp(128 * log_g).astype(f32)
    s = f32(32 ** -0.5)
    decT = np.zeros((128, 6, 128), f32)
    for h in range(H):
        decT[:, h, :] = decay_intra[h].T * s
    xi_t = np.zeros((64, 3, 128), f32)
    zeta_t = np.zeros((128, 3, 64), f32)
    gam = np.zeros((64, 3), f32)
    for p in range(3):
        for hh in range(2):
            h = 2 * p + hh
            xi_t[hh * 32:(hh + 1) * 32, p, :] = xi[h][None, :] * s
            zeta_t[:, p, hh * 32:(hh + 1) * 32] = zeta[h][:, None]
            gam[hh * 32:(hh + 1) * 32, p] = cd[h]
    half = 16
    inv_freq = (1.0 / (10000.0 ** (np.arange(half, dtype=f32) / half))).astype(f32)
    invf = np.tile(inv_freq, 8).reshape(128, 1).astype(f32)
    return dict(bmb=bmb, bmm=bmm, cm=cm, decT=decT, xi=xi_t, zeta=zeta_t, gam=gam, invf=invf)


class Arena:
    def __init__(self, nc, base, size, name):
        self.nc, self.base, self.size, self.name = nc, base, size, name
        self.off = 0
        self.n = 0

    def reset(self):
        self.off = 0

    def alloc(self, fw, name, shape, dtype):
        nbytes = int(np.prod(shape[1:])) * mybir.dt.size(dtype)
        nbytes = (nbytes + 31) // 32 * 32
        assert self.off + nbytes <= self.size, (self.name, name, self.off, nbytes, self.size)
        self.n += 1
        t = self.nc.alloc_sbuf_tensor_at(f"{self.name}_{name}_{self.n}", list(shape), dtype, offset=self.base + self.off)
        self.off += nbytes
        return Buf(t, f"{self.name}_{name}_{self.n}", semname=f"{self.name}_{name}")


def build(depth=DEPTH, dbg=None, skip=()):
    nc = bass.Bass("TRN2", target_bir_lowering=False)
    dbg = dbg or []
    dbg_out = {}
    MARKS.clear()

    def din(name, shape, dt=F32):
        return nc.dram_tensor(name, list(shape), dt, kind="ExternalInput").ap()

    x_d = din("x", [S, D])
    cT_d = din("cT", [128, 8])
    pos_d = din("posb", [1, S], I32)
    invf_d = din("invf", [128, 1])
    bmb_d = din("bmb", [128, 6, 640])
    bmm_d = din("bmm", [128, 640])
    cm_d = din("cm", [128, 128])
    decT_d = din("decT", [128, 6, 128])
    xi_d = din("xi", [64, 3, 128])
    zeta_d = din("zeta", [128, 3, 64])
    gam_d = din("gam", [64, 3])
    adab_d = din("adabT", [128, DEPTH, 48])
    n1g_d = din("n1gT", [128, DEPTH, 8])
    n2g_d = din("n2gT", [128, DEPTH, 8])
    fng_d = din("fngT", [128, 8])
    mg_d = din("mgT", [128, DEPTH, 8])
    mqn_d = din("mqnT", [128, DEPTH, 2])
    mkvn_d = din("mkvnT", [128, DEPTH, 1])
    if depth > 0:
        adaw_d = din("ada_w", [DEPTH, D, 6 * D])
        win_d = din("w_in", [DEPTH, D, IN_W])
        wuq_d = din("w_uq", [DEPTH, 256, 384])
        wukv_d = din("w_ukv", [DEPTH, 128, 512])
        wout_d = din("w_out", [DEPTH, D, D])
        wg_d = din("w_gate", [DEPTH, D, FH])
        wu_d = din("w_up", [DEPTH, D, FH])
        wd_d = din("w_down", [DEPTH, FH, D])
        xsp_d = nc.dram_tensor("xspill", [128, 8, S], F32, kind="Internal").ap()
    y_d = nc.dram_tensor("y", [S, D], F32, kind="ExternalOutput").ap()

    st = ExitStack()
    with st:
        fw = FW(nc, st)
        pe, act, dve, pool, sp = fw.pe, fw.act, fw.dve, fw.pool, fw.sp
        base0 = (nc.sbuf_base + 31) // 32 * 32
        st.enter_context(nc.sbuf_tensor("slab", [128, SLAB], mybir.dt.uint8))
        assert nc.sbuf_base == base0 + SLAB, (nc.sbuf_base, base0)
        o = base0
        SZ_HT, SZ_X, SZ_Y, SZ_ADAW, SZ_W = 32768, 65536, 32768, 12288, 36864
        SZ_C = SLAB - (SZ_HT + SZ_X + SZ_Y + SZ_ADAW + SZ_W)
        AC = Arena(nc, o, SZ_C, "c"); o += SZ_C
        AH = Arena(nc, o, SZ_HT, "h"); o += SZ_HT
        AX = Arena(nc, o, SZ_X, "x"); o += SZ_X
        AY = Arena(nc, o, SZ_Y, "y"); o += SZ_Y
        AA = Arena(nc, o, SZ_ADAW, "a"); o += SZ_ADAW
        AW = Arena(nc, o, SZ_W, "w"); o += SZ_W

        PS = [fw.psum(f"ps{i}", [128, 512], F32) for i in range(8)]

        def sub(buf, name):
            return Buf(buf.t, name)

        ident_f = AC.alloc(fw, "identf", [128, 128], F32)
        ident_b = AC.alloc(fw, "identb", [128, 128], BF16)
        ones_b = AC.alloc(fw, "onesb", [128, 128], BF16)
        BD = AC.alloc(fw, "bd", [128, 128], BF16)
        WNe = AC.alloc(fw, "wne", [128, 64], BF16)
        WNo = AC.alloc(fw, "wno", [128, 64], BF16)
        FCOS = AC.alloc(fw, "fcos", [128, S], BF16)
        FSIN = AC.alloc(fw, "fsin", [128, S], BF16)
        BM = AC.alloc(fw, "bm", [128, 6, 640], BF16)
        CM = AC.alloc(fw, "cm", [128, 128], F32)
        DECT = AC.alloc(fw, "dect", [128, 6, 128], F32)
        XI = AC.alloc(fw, "xi", [64, 3, 128], F32)
        ZETA = AC.alloc(fw, "zeta", [128, 3, 64], F32)
        GAM = AC.alloc(fw, "gam", [64, 3], F32)
        MOD = AC.alloc(fw, "mod", [128, DEPTH, 48], F32)
        A1 = AC.alloc(fw, "a1", [128, DEPTH, 8], F32)
        A2 = AC.alloc(fw, "a2", [128, DEPTH, 8], F32)
        ADAB = AC.alloc(fw, "adab", [128, DEPTH, 48], F32)
        N1G = AC.alloc(fw, "n1g", [128, DEPTH, 8], F32)
        N2G = AC.alloc(fw, "n2g", [128, DEPTH, 8], F32)
        FNG = AC.alloc(fw, "fng", [128, 8], F32)
        MG = AC.alloc(fw, "mg", [128, DEPTH, 8], F32)
        MQN = AC.alloc(fw, "mqn", [128, DEPTH, 2], F32)
        MKVN = AC.alloc(fw, "mkvn", [128, DEPTH, 1], F32)
        CTF = AC.alloc(fw, "ctf", [128, 8], F32)
        CTB = AC.alloc(fw, "ctb", [128, 8], BF16)
        INVF = AC.alloc(fw, "invf", [128, 1], F32)
        hT = AH.alloc(fw, "hT", [128, 8, S], BF16)
        adaw = [AA.alloc(fw, f"adaw{i}", [128, 3072], BF16) for i in range(2)]

        def dump(name, view, shape, dt):
            if name not in dbg:
                return
            t = nc.dram_tensor("dbg_" + name, list(shape), dt, kind="ExternalOutput").ap()
            dbg_out[name] = sp.dma(t, view)
            fw.dma_toks.append(dbg_out[name])

        fw.memset(dve, ident_f[:], 1.0)
        pool.issue(lambda e: e.affine_select(out=ident_f.t[:], in_=ident_f.t[:], pattern=[[-1, 128]],
                                             compare_op=ALU.is_equal, fill=0.0, base=0, channel_multiplier=1),
                   reads=[ident_f[:]], writes=[ident_f[:]])
        fw.copy(dve, ident_b[:], ident_f[:])
        fw.memset(dve, ones_b[:], 1.0)
        fw.memset(dve, BD[:], 0.0)
        fw.memset(dve, BD[0:64, 0:64], 1.0 / 64)
        fw.memset(dve, BD[64:128, 64:128], 1.0 / 64)
        fw.memset(dve, WNe[:], 0.0)
        fw.memset(dve, WNe[0:64, :], 1.0)
        if 'wn' not in skip:
            fw.memset(dve, WNe[64:65, :], 64 * EPS)
        fw.memset(dve, WNo[:], 0.0)
        fw.memset(dve, WNo[64:128, :], 1.0)
        if 'wn' not in skip:
            fw.memset(dve, WNo[0:1, :], 64 * EPS)
        for (b, d_) in [] if 'small' in skip else [(CM, cm_d), (DECT, decT_d), (XI, xi_d), (ZETA, zeta_d), (GAM, gam_d), (ADAB, adab_d),
                        (N1G, n1g_d), (N2G, n2g_d), (FNG, fng_d), (MG, mg_d), (MQN, mqn_d), (MKVN, mkvn_d),
                        (CTF, cT_d), (INVF, invf_d)]:
            sp.dma(b[:], d_)
        if 'silu' not in skip:
            fw.activation(CTB[:], CTF[:], AF.Silu)
        AY.reset()
        bmb_s = AY.alloc(fw, "bmb", [128, 6, 640], F32)
        bmm_s = AY.alloc(fw, "bmm", [128, 640], F32)
        if 'bm' not in skip:
            sp.dma(bmb_s[:], bmb_d)
            sp.dma(bmm_s[:], bmm_d)
            for h in range(6):
                fw.tt(dve, BM[:, h, :], bmb_s[:, h, :], bmm_s[:], ALU.add)
        fw.barrier()
        AY.reset()
        posi = AY.alloc(fw, "posi", [128, S], I32)
        ang = AY.alloc(fw, "ang", [128, S], F32)
        t1 = AY.alloc(fw, "t1", [128, S], F32)
        t2 = AY.alloc(fw, "t2", [128, S], F32)
        if 'tables' not in skip:
            sp.dma(posi[:], pos_d.partition_broadcast(128))
            fw.copy(dve, t1[:], posi[:])
            fw.ts(dve, ang[:], t1[:], INVF[:, 0:1], ALU.mult)
            TWO_PI = 2.0 * np.pi
            C1 = 6.28125
            C2 = float(np.float32(TWO_PI - C1))
            fw.ts(dve, t1[:], ang[:], 1.0 / TWO_PI, ALU.mult)
            fw.copy(dve, posi[:], t1[:])
            fw.copy(dve, t1[:], posi[:])
            fw.stt(t2[:], t1[:], -C1, ang[:], ALU.mult, ALU.add)
            fw.stt(t2[:], t1[:], -C2, t2[:], ALU.mult, ALU.add)

            def wrap(dst, src, tmp):
                fw.ts(dve, tmp, src, float(np.pi), ALU.is_gt)
                fw.stt(dst, tmp, -TWO_PI, src, ALU.mult, ALU.add)
                fw.ts(dve, tmp, dst, -float(np.pi), ALU.is_lt)
                fw.stt(dst, tmp, TWO_PI, dst, ALU.mult, ALU.add)
                fw.ts(dve, dst, dst, 3.1415925, ALU.min, -3.1415925, ALU.max)

            wrap(t2[:], t2[:], t1[:])
            fw.activation(FSIN[:], t2[:], AF.Sin)
            fw.ts(dve, ang[:], t2[:], float(np.pi / 2), ALU.add)
            wrap(ang[:], ang[:], t1[:])
            fw.activation(FCOS[:], ang[:], AF.Sin)
        fw.barrier()

        def mod_pieces(l):
            mp = PS[7]
            for piece in range(16):
                kc, half = piece // 2, piece % 2
                slot = adaw[piece % 2]
                pool.dma(slot[:], adaw_d[l, kc * 128:(kc + 1) * 128, half * 3072:(half + 1) * 3072], max_dma_last_dim=4096)
                for j in range(24):
                    jj = half * 24 + j
                    fw.mm(mp[:, jj:jj + 1], slot[:, j * 128:(j + 1) * 128], CTB[:, kc:kc + 1],
                          start=(piece == 0 and j == 0), stop=(kc == 7), signal=(j == 23), skip_group_check=True)
                yield
            fw.tt(dve, MOD[:, l, :], mp[:, 0:48], ADAB[:, l, :], ALU.add)
            fw.stt(A1[:, l, :], MOD[:, l, 8:16], 1.0, N1G[:, l, :], ALU.add, ALU.mult)
            fw.stt(A2[:, l, :], MOD[:, l, 32:40], 1.0, N2G[:, l, :], ALU.add, ALU.mult)
            yield

        EPSC = AC.alloc(fw, "epsc", [128, 1], F32)
        fw.memset(dve, EPSC[:], EPS)

        def norm_to_hT(l, a_view, b_col0, ar):
            ar.reset()
            sq = [ar.alloc(fw, f"nsq{i}", [128, 8, 512], BF16) for i in range(2)]
            lnt = ar.alloc(fw, "nln", [128, 512], F32)
            rs = [ar.alloc(fw, f"nrs{i}", [128, 512], F32) for i in range(2)]
            tm = [ar.alloc(fw, f"ntm{i}", [128, 512], F32) for i in range(4)]

            def stA(tb):
                ts_ = slice(tb * 512, (tb + 1) * 512)
                fw.activation(sq[tb % 2][:], xT[:, :, ts_], AF.Square)
                pb = PS[tb % 2]
                for kc in range(8):
                    fw.mm(pb[:], ones_b[:], sq[tb % 2][:, kc, :], start=(kc == 0), stop=(kc == 7))

            def stB(tb):
                ts_ = slice(tb * 512, (tb + 1) * 512)
                pb = PS[tb % 2]
                fw.activation(lnt[:], pb[:], AF.Ln, scale=1.0 / D, bias=EPSC[:, 0:1])
                fw.activation(rs[tb % 2][:], lnt[:], AF.Exp, scale=-0.5)
                for kc in range(8):
                    t = tm[kc % 4]
                    fw.tt(dve, t[:], xT[:, kc, ts_], rs[tb % 2][:], ALU.mult)
                    if kc % 2 == 0:
                        fw.activation(hT[:, kc, ts_], t[:], AF.Identity, scale=a_view(kc), bias=MOD[:, l, b_col0 + kc:b_col0 + kc + 1])
                    else:
                        fw.ts(pool, hT[:, kc, ts_], t[:], a_view(kc), ALU.mult, MOD[:, l, b_col0 + kc:b_col0 + kc + 1], ALU.add)
            stA(0)
            for tb in range(4):
                if tb + 1 < 4:
                    stA(tb + 1)
                stB(tb)

        def load_w(buf_view, dram_ap):
            pool.dma(buf_view, dram_ap, max_dma_last_dim=4096)

        def proj_fm(out_fn, w_fn, M, nk=8, rhs_fn=None, banks=(6, 7), out_p0=0):
            for tb in range(4):
                pb = PS[banks[tb % len(banks)]]
                ts_ = slice(tb * 512, (tb + 1) * 512)
                pv = pb[out_p0:out_p0 + M, :]
                for kc in range(nk):
                    rhs = rhs_fn(kc, ts_) if rhs_fn else hT[:, kc, ts_]
                    fw.mm(pv, w_fn(kc), rhs, start=(kc == 0), stop=(kc == nk - 1))
                out_fn(tb, ts_, pv)

        def finalize_head(bank, nr, mixb, chunk, tb, fin):
            SQb, LNb, RSb = fin
            ts_ = slice(tb * 512, (tb + 1) * 512)
            fw.activation(SQb[:], bank[:], AF.Square)
            pf = PS[6 + (tb % 2)]
            wn = WNe if nr == 0 else WNo
            fw.mm(pf[nr:nr + 64, :], wn[:], SQb[:], start=True, stop=True)
            fw.activation(LNb[nr:nr + 64, :], pf[nr:nr + 64, :], AF.Ln, scale=1.0 / 64)
            fw.activation(RSb[nr:nr + 64, :], LNb[nr:nr + 64, :], AF.Exp, scale=-0.5)
            fw.tt(dve, mixb[nr:nr + 64, chunk, ts_], bank[nr:nr + 64, :], RSb[nr:nr + 64, :], ALU.mult)

        def v_lhsT(vbuf, tile_off, ones_off, even):
            t = vbuf.t
            pstride = t[:, :].ap[0][0]
            if even:
                return vbuf.v(bass.AP(t, tile_off, [[pstride, 128], [ones_off - tile_off, 2], [1, 64]]))
            return vbuf.v(bass.AP(t, ones_off, [[pstride, 128], [tile_off - ones_off, 2], [1, 64]]))

        AX.reset()
        xT = AX.alloc(fw, "xT", [128, 8, S], F32)
        AW.reset()
        io = [AW.alloc(fw, f"io{i}", [128, D], F32) for i in range(2)]
        modgen0 = mod_pieces(0) if depth > 0 else iter(())
        for n in range(16):
            sp.dma(io[n % 2][:], x_d[n * 128:(n + 1) * 128, :])
            next(modgen0, None)
            for g in range(2):
                pb = PS[(2 * n + g) % 4]
                for j in range(4):
                    kc = 4 * g + j
                    fw.transpose(pb[:, j * 128:(j + 1) * 128], io[n % 2][:, kc * 128:(kc + 1) * 128], ident_f[:], signal=(j == 3))
                src = pb.v(pb.t[:, :].rearrange("p (j t) -> p j t", j=4))
                dst = xT.v(xT.t[:, 4 * g:4 * g + 4, n * 128:(n + 1) * 128])
                fw.copy(act if (n + g) % 2 else dve, dst, src)
        for _ in modgen0:
            pass
        fw.barrier()

        def run_pipe(units, la=2):
            nU = len(units)
            for i in range(nU + la):
                if i < nU:
                    units[i][0]()
                if i - la >= 0:
                    units[i - la][1]()

        for l in range(depth):
            MARKS.append(('mod', l, pe.ninst))
            MARKS.append(('norm1', l, pe.ninst))
            AY.reset()
            norm_to_hT(l, lambda kc: A1[:, l, kc:kc + 1], 0, AY)
            dump(f"hT{l}", hT[:], [128, 8, S], BF16)
            for kc in range(8):
                sp.dma(xsp_d[:, kc, :], xT[:, kc, :])
            fw.barrier()
            _rec = fw.dsem[xT.semname]
            spill_tok = (("d", xT.semname), 16 * _rec[1], _rec[0])
            for e in (pe, act, dve, pool):
                e.wait_tok(spill_tok)

            AY.reset()
            mixT = AY.alloc(fw, "mixT", [128, 8, S], BF16)

            MARKS.append(('A', l, pe.ninst))
            AX.reset()
            qT = AX.alloc(fw, "qT", [128, S], BF16)
            kT = AX.alloc(fw, "kT", [128, S], BF16)
            VA = AX.alloc(fw, "VA", [128, 48, 2, 128], BF16)
            VT = AX.alloc(fw, "VT", [128, S], BF16)
            PT = [AX.alloc(fw, f"pt{i}", [128, 512], BF16) for i in range(4)]
            TS = [AX.alloc(fw, f"ts{i}", [128, 512], F32) for i in range(4)]
            fin = (AX.alloc(fw, "fsq", [128, 512], BF16), AX.alloc(fw, "fln", [128, 512], F32), AX.alloc(fw, "frs", [128, 512], F32))
            AW.reset()
            wq = AW.alloc(fw, "wq", [128, 8, 128], BF16)
            wk = AW.alloc(fw, "wk", [128, 8, 128], BF16)
            wv = AW.alloc(fw, "wv", [128, 8, 128], BF16)
            SS = [sub(PS[4], "ss0"), sub(PS[4], "ss1"), sub(PS[5], "ss2"), sub(PS[5], "ss3")]
            fw.memset(pool, VA[:, :, 0, 64:128], 1.0)
            fw.memset(pool, VA[:, :, 1, 0:64], 1.0)
            winv = win_d[l].rearrange("(k p) n -> p k n", p=128)
            blk = [0]
            for c in (range(3) if 'A' not in skip else []):
                load_w(wq[:], winv[:, :, OFF_AQ + c * 128:OFF_AQ + (c + 1) * 128])
                load_w(wk[:], winv[:, :, OFF_AK + c * 128:OFF_AK + (c + 1) * 128])
                load_w(wv[:], winv[:, :, OFF_AV + c * 128:OFF_AV + (c + 1) * 128])
                proj_fm(lambda tb, ts_, pv: fw.activation(qT[:, ts_], pv, AF.Identity, scale=0.125),
                        lambda kc: wq[:, kc, :], 128)
                proj_fm(lambda tb, ts_, pv: fw.copy(dve, kT[:, ts_], pv), lambda kc: wk[:, kc, :], 128)
                proj_fm(lambda tb, ts_, pv: fw.copy(act, VT[:, ts_], pv), lambda kc: wv[:, kc, :], 128)

                def tok_cols(ordr, tile):
                    if ordr == 0:
                        return VT[:, tile * 128:(tile + 1) * 128]
                    if ordr == 1:
                        r, b_ = tile // 4, tile % 4
                        return VT[:, 512 * b_ + r:512 * (b_ + 1):4]
                    return VT[:, tile::16]
                for ordr in range(3):
                    for tg in range(4):
                        pb = PS[6 + (tg % 2)]
                        pb16 = pb.t.bitcast(BF16)
                        for tt_ in range(4):
                            fw.transpose(pb.v(pb16[:, tt_ * 128:(tt_ + 1) * 128]), tok_cols(ordr, tg * 4 + tt_), ident_b[:],
                                         signal=(tt_ == 3))
                        t0_ = ordr * 16 + tg * 4
                        pv4 = pb16[:, 0:512].rearrange("p (n c) -> p n c", n=4)
                        ev_ = act if tg % 2 else dve
                        fw.copy(ev_, VA[:, t0_:t0_ + 4, 0, 0:64], pb.v(pv4[:, :, 0:64]))
                        fw.copy(ev_, VA[:, t0_:t0_ + 4, 1, 64:128], pb.v(pv4[:, :, 64:128]))
                if c == 0:
                    dump(f"qTA{l}", qT[:], [128, S], BF16)
                    dump(f"kTA{l}", kT[:], [128, S], BF16)
                    dump(f"VA{l}", VA[:], [128, 48, 2, 128], BF16)
                for hh in range(2):
                    h = 2 * c + hh
                    ro = 64 * hh
                    started = [False] * 4

                    unitsA = []

                    def sunit(subs, bm_off, w):
                        i = blk[0]
                        blk[0] += 1
                        ss = PS[4 + i % 4]
                        tsb = TS[i % 4]
                        pt = PT[i % 4]
                        offs = []
                        off = 0
                        for s_ in subs:
                            offs.append(off)
                            off += s_[2]
                        tot = off
                        nfull = sum(1 for s_ in subs if s_[2] == w)

                        def st1():
                            for (kcols, qcols, nq, pvl), o_ in zip(subs, offs):
                                fw.mm(ss[:, o_:o_ + nq], kcols, qcols, start=True, stop=True)
                            bmv = BM.v(bass.AP(BM.t, h * 640 + bm_off, [[6 * 640, 128], [0, nfull], [1, w]]))
                            fw.tt(dve, tsb.v(tsb.t[:, 0:nfull * w].rearrange("p (n q) -> p n q", n=nfull)),
                                  ss.v(ss.t[:, 0:nfull * w].rearrange("p (n q) -> p n q", n=nfull)), bmv, ALU.add)
                            if nfull < len(subs):
                                nql = subs[-1][2]
                                fw.tt(dve, tsb[:, nfull * w:nfull * w + nql], ss[:, nfull * w:nfull * w + nql],
                                      BM[:, h, bm_off:bm_off + nql], ALU.add)
                            fw.activation(pt[:, 0:tot], tsb[:, 0:tot], AF.Exp)

                        def st2():
                            for (kcols, qcols, nq, pvl), o_ in zip(subs, offs):
                                for (qs, vtile, tbk, ocols) in pvl:
                                    fw.mm(ocols, VA[:, vtile, hh, :], pt[:, o_ + qs.start:o_ + qs.stop], start=(not started[tbk]),
                                          stop=False, signal=True, skip_group_check=True)
                                    started[tbk] = True
                        unitsA.append((st1, st2))

                    def p1_sub(kt):
                        nqt = 2 if kt < 15 else 1
                        pvl = []
                        for j in range(nqt):
                            qt = kt + j
                            pvl.append((slice(j * 128, (j + 1) * 128), 0 * 16 + kt, qt // 4,
                                        PS[qt // 4][:, (qt % 4) * 128:(qt % 4 + 1) * 128]))
                        return (kT[ro:ro + 64, kt * 128:(kt + 1) * 128], qT[ro:ro + 64, kt * 128:kt * 128 + nqt * 128], nqt * 128, pvl)

                    def p2_sub(r, b_):
                        nqt = 2 if b_ < 3 else 1
                        pvl = []
                        for j in range(nqt):
                            pvl.append((slice(j * 128, (j + 1) * 128), 16 + r * 4 + b_, b_ + j, PS[b_ + j][:, r::4]))
                        return (kT[ro:ro + 64, 512 * b_ + r:512 * (b_ + 1):4], qT[ro:ro + 64, 512 * b_ + r:512 * (b_ + nqt):4],
                                nqt * 128, pvl)

                    def p3_sub(r):
                        pvl = []
                        for tbk in range(4):
                            pvl.append((slice(tbk * 32, (tbk + 1) * 32), 32 + r, tbk, PS[tbk][:, r::16]))
                        return (kT[ro:ro + 64, r::16], qT[ro:ro + 64, r::16], 128, pvl)

                    for kt in range(0, 16, 2):
                        sunit([p1_sub(kt), p1_sub(kt + 1)], 0, 256)
                    for r in range(4):
                        for b_ in (0, 2):
                            sunit([p2_sub(r, b_), p2_sub(r, b_ + 1)], 256, 256)
                    for r in range(0, 16, 4):
                        sunit([p3_sub(r + j) for j in range(4)], 512, 128)
                    run_pipe(unitsA)
                    for tb in range(4):
                        finalize_head(PS[tb], ro, mixT, c, tb, fin)
            dump(f"mixA{l}", mixT[:, 0:3, :], [128, 3, S], BF16)
            fw.barrier()

            MARKS.append(('B', l, pe.ninst))
            AX.reset()
            qlatT = AX.alloc(fw, "qlatT", [128, 2, S], BF16)
            kvlatT = AX.alloc(fw, "kvlatT", [128, S], BF16)
            sqb = [AX.alloc(fw, "sqb0", [128, 2, 512], BF16)] * 2
            rsq = AX.alloc(fw, "rsq", [128, S], F32)
            rskv = AX.alloc(fw, "rskv", [128, S], F32)
            rstok = AX.alloc(fw, "rstok", [128, 16], F32)
            qTh = AX.alloc(fw, "qTh", [128, S], BF16)
            KTh = AX.alloc(fw, "KTh", [128, S], BF16)
            VBs = [AX.alloc(fw, f"VB{i}", [128, 16, 128], BF16) for i in range(2)]
            PTB = [AX.alloc(fw, f"ptb{i}", [128, 512], BF16) for i in range(4)]
            TSB = [AX.alloc(fw, f"tsb{i}", [128, 128], F32) for i in range(2)]
            RT = [AX.alloc(fw, f"rt{i}", [128, 512], F32) for i in range(3)]
            fin = (AX.alloc(fw, "fsq", [128, 512], BF16), AX.alloc(fw, "fln", [128, 512], F32), AX.alloc(fw, "frs", [128, 512], F32))
            AW.reset()
            winB = AW.alloc(fw, "winB", [128, 8, 416], BF16)
            wkrr = AW.alloc(fw, "wkrr", [128, 8, 32], BF16)
            wuq_f = AW.alloc(fw, "wuqf", [128, 2, 384], F32)
            wuq_b = AW.alloc(fw, "wuqb", [128, 2, 384], BF16)
            wuq_r = AW.alloc(fw, "wuqr", [128, 2, 4, 32], BF16)
            wukv_f = AW.alloc(fw, "wukvf", [128, 512], F32)
            wukv_b = AW.alloc(fw, "wukvb", [128, 512], BF16)
            fw.memset(pool, VBs[0][:, :, 64:128], 1.0)
            fw.memset(pool, VBs[1][:, :, 0:64], 1.0)
            load_w(winB[:], winv[:, :, OFF_BQ:OFF_BQ + 416])
            pool.dma_group([(wkrr[:, :, 0:16], winv[:, :, OFF_BKR + 16:OFF_BKR + 32]),
                            (wkrr[:, :, 16:32], winv[:, :, OFF_BKR:OFF_BKR + 16])], max_dma_last_dim=4096)
            fw.activation(wkrr[:, :, 0:16], wkrr[:, :, 0:16], AF.Identity, scale=-1.0)
            sp.dma(wuq_f[:], wuq_d[l].rearrange("(k p) n -> p k n", p=128))
            sp.dma(wukv_f[:], wukv_d[l])
            for kc in range(2):
                fw.activation(wuq_b[:, kc, :], wuq_f[:, kc, :], AF.Identity, scale=MQN[:, l, kc:kc + 1])
            fw.activation(wukv_b[:], wukv_f[:], AF.Identity, scale=MKVN[:, l, 0:1])
            wq4 = wuq_b.t[:, :, :].rearrange("p k (h e) -> p k h e", h=4)
            fw.activation(wuq_r.v(wuq_r.t[:, :, :, 0:16]), wuq_b.v(wq4[:, :, :, 80:96]), AF.Identity, scale=-1.0)
            fw.copy(dve, wuq_r.v(wuq_r.t[:, :, :, 16:32]), wuq_b.v(wq4[:, :, :, 64:80]))

            for c2 in range(2):
                proj_fm(lambda tb, ts_, pv, c2=c2: fw.copy(dve, qlatT[:, c2, ts_], pv),
                        lambda kc, c2=c2: winB[:, kc, c2 * 128:(c2 + 1) * 128], 128)
            for tb in range(4):
                ts_ = slice(tb * 512, (tb + 1) * 512)
                fw.activation(sqb[tb % 2][:], qlatT[:, :, ts_], AF.Square)
                pb = PS[4 + tb % 2]
                for c2 in range(2):
                    fw.mm(pb[:], ones_b[:], sqb[tb % 2][:, c2, :], start=(c2 == 0), stop=(c2 == 1))
                fw.activation(RT[0][:], pb[:], AF.Ln, scale=1.0 / 256, bias=EPSC[:, 0:1])
                fw.activation(rsq[:, ts_], RT[0][:], AF.Exp, scale=-0.5)
            proj_fm(lambda tb, ts_, pv: fw.copy(dve, kvlatT[:, ts_], pv), lambda kc: winB[:, kc, 256:384], 128)
            for tb in range(4):
                ts_ = slice(tb * 512, (tb + 1) * 512)
                fw.activation(sqb[tb % 2][:, 0, :], kvlatT[:, ts_], AF.Square)
                pb = PS[4 + tb % 2]
                fw.mm(pb[:], ones_b[:], sqb[tb % 2][:, 0, :], start=True, stop=True)
                fw.activation(RT[0][:], pb[:], AF.Ln, scale=1.0 / 128, bias=EPSC[:, 0:1])
                fw.activation(rskv[:, ts_], RT[0][:], AF.Exp, scale=-0.5)
            identb4 = ident_f.v(bass.AP(ident_f.t, 0, [[128, 128], [0, 4], [1, 128]]))
            for g4 in range(4):
                tdv = RT[0].v(RT[0].t[:, :].rearrange("p (n q) -> p n q", n=4))
                fw.tt(dve, tdv, rskv.v(rskv.t[:, g4 * 512:(g4 + 1) * 512].rearrange("p (n q) -> p n q", n=4)), identb4, ALU.mult)
                _o = rstok.t[:, g4 * 4:(g4 + 1) * 4]
                _i = RT[0].t[:, :].rearrange("p (n q) -> p n q", n=4)
                dve.issue(lambda e, _o=_o, _i=_i: e.tensor_reduce(out=_o, in_=_i, axis=AX_X, op=ALU.add),
                          reads=[RT[0][:]], writes=[rstok[:]])
            for tb in range(4):
                ts_ = slice(tb * 512, (tb + 1) * 512)
                p1, p2 = (PS[6], PS[7]) if tb % 2 else (PS[4], PS[5])
                for kc in range(8):
                    fw.mm(p1[64:96, :], winB[:, kc, 384:416], hT[:, kc, ts_], start=(kc == 0), stop=(kc == 7))
                for kc in range(8):
                    fw.mm(p2[64:96, :], wkrr[:, kc, :], hT[:, kc, ts_], start=(kc == 0), stop=(kc == 7))
                fw.tt(dve, RT[0][64:96, :], p1[64:96, :], FCOS[64:96, ts_], ALU.mult)
                fw.tt(dve, RT[1][64:96, :], p2[64:96, :], FSIN[64:96, ts_], ALU.mult)
                fw.tt(pool, KTh[64:96, ts_], RT[0][64:96, :], RT[1][64:96, :], ALU.add)
            dump(f"rsq{l}", rsq[:], [128, S], F32)
            SCB = float((64 + 32) ** -0.5)
            BSETS = [(PS[4], PS[5], PS[5]), (PS[6], PS[7], PS[7])]
            for h in (range(4) if 'B' not in skip else []):
                hh = h % 2
                nr = 64 * hh
                for tb in range(4):
                    ts_ = slice(tb * 512, (tb + 1) * 512)
                    p1, p2, p3 = BSETS[tb % 2]
                    for kc in range(2):
                        fw.mm(p1[0:96, :], wuq_b[:, kc, h * 96:(h + 1) * 96], qlatT[:, kc, ts_], start=(kc == 0), stop=(kc == 1))
                    for kc in range(2):
                        fw.mm(p2[64:96, :], wuq_r.v(wuq_r.t[:, kc, h, :]), qlatT[:, kc, ts_], start=(kc == 0), stop=(kc == 1))
                    fw.stt(qTh[0:64, ts_], p1[0:64, :], SCB, rsq[0:64, ts_], ALU.mult, ALU.mult)
                    fw.tt(dve, RT[0][64:96, :], p1[64:96, :], FCOS[64:96, ts_], ALU.mult)
                    fw.tt(dve, RT[1][64:96, :], p2[64:96, :], FSIN[64:96, ts_], ALU.mult)
                    fw.tt(pool, RT[2][64:96, :], RT[0][64:96, :], RT[1][64:96, :], ALU.add)
                    fw.stt(qTh[64:96, ts_], RT[2][64:96, :], SCB, rsq[64:96, ts_], ALU.mult, ALU.mult)
                    fw.mm(p3[0:64, :], wukv_b[:, h * 128:h * 128 + 64], kvlatT[:, ts_], start=True, stop=True, skip_group_check=True)
                    fw.tt(dve, KTh[0:64, ts_], p3[0:64, :], rskv[0:64, ts_], ALU.mult)
                for tg in range(2):
                    pb = PS[4 + tg]
                    for t8 in range(8):
                        tile = tg * 8 + t8
                        fw.mm(pb[:, t8 * 64:(t8 + 1) * 64], kvlatT[:, tile * 128:(tile + 1) * 128],
                              wukv_b[:, h * 128 + 64:(h + 1) * 128], start=True, stop=True, signal=(t8 == 7))
                    for t8 in range(8):
                        tile = tg * 8 + t8
                        fw.activation(VBs[hh][:, tile, 64 * hh:64 * hh + 64], pb[:, t8 * 64:(t8 + 1) * 64], AF.Identity,
                                      scale=rstok[:, tile:tile + 1])
                if h == 0:
                    dump(f"qTh{l}", qTh[0:96, :], [96, S], BF16)
                    dump(f"KTh{l}", KTh[0:96, :], [96, S], BF16)
                    dump(f"VB{l}", VBs[0][:], [128, 16, 128], BF16)
                started = [False] * 4
                unitsB = []
                bi = 0
                for kt in range(16):
                    for tb in range(kt // 4, 4):
                        q0 = max(128 * kt, 512 * tb)
                        q1 = 512 * (tb + 1)

                        def mk(kt=kt, tb=tb, q0=q0, q1=q1, bi=bi):
                            nq = q1 - q0
                            sbk = PS[4 + bi % 4]
                            pt = PTB[bi % 4]

                            def st1():
                                fw.mm(sbk[:, 0:nq], KTh[0:96, kt * 128:(kt + 1) * 128], qTh[0:96, q0:q1], start=True, stop=True)
                                if q0 == 128 * kt:
                                    tsb = TSB[kt % 2]
                                    fw.tt(dve, tsb[:], sbk[:, 0:128], CM[:], ALU.add)
                                    fw.activation(pt[:, 0:128], tsb[:], AF.Exp)
                                    if nq > 128:
                                        fw.activation(pt[:, 128:nq], sbk[:, 128:nq], AF.Exp)
                                else:
                                    fw.activation(pt[:, 0:nq], sbk[:, 0:nq], AF.Exp)

                            def st2():
                                fw.mm(PS[tb][:, q0 - 512 * tb:q1 - 512 * tb], VBs[hh][:, kt, :], pt[:, 0:nq], start=(not started[tb]),
                                      stop=False, signal=True, skip_group_check=True)
                                started[tb] = True
                            return (st1, st2)
                        unitsB.append(mk())
                        bi += 1
                run_pipe(unitsB)
                for tb in range(4):
                    finalize_head(PS[tb], nr, mixT, 3 + h // 2, tb, fin)
            dump(f"mixB{l}", mixT[:, 3:5, :], [128, 2, S], BF16)
            fw.barrier()

            MARKS.append(('C', l, pe.ninst))
            AX.reset()
            qTc = AX.alloc(fw, "qTc", [128, S], BF16)
            kTc = AX.alloc(fw, "kTc", [128, S], BF16)
            qxT = AX.alloc(fw, "qxT", [128, S], BF16)
            kz = AX.alloc(fw, "kz", [128, 16, 64], BF16)
            VC = AX.alloc(fw, "VC", [128, 16, 128], BF16)
            gT = AX.alloc(fw, "gT", [128, S], BF16)
            STf = AX.alloc(fw, "stf", [64, 16, 64], F32)
            STb = AX.alloc(fw, "stb", [64, 16, 64], BF16)
            PTC = [AX.alloc(fw, f"ptc{i}", [128, 128], BF16) for i in range(4)]
            RT = [AX.alloc(fw, f"rtc{i}", [128, 512], F32) for i in range(3)]
            CP = AX.alloc(fw, "ccp", [128, 512], BF16)
            CSQ = AX.alloc(fw, "csq", [128, 512], BF16)
            CMEAN = AX.alloc(fw, "cmean", [128, 512], F32)
            CMSQ = AX.alloc(fw, "cmsq", [128, 512], F32)
            CDV = AX.alloc(fw, "cdv", [128, 512], F32)
            CVAR = AX.alloc(fw, "cvar", [128, 512], F32)
            CRS = AX.alloc(fw, "crs", [128, 512], F32)
            AW.reset()
            wo = AW.alloc(fw, "wo", [128, 8, D], BF16)
            wov = wout_d[l].rearrange("(k p) n -> p k n", p=128)
            wcq = AW.alloc(fw, "wcq", [128, 8, 64], BF16)
            wcqr = AW.alloc(fw, "wcqr", [128, 8, 2, 32], BF16)
            wck = AW.alloc(fw, "wck", [128, 8, 64], BF16)
            wckr = AW.alloc(fw, "wckr", [128, 8, 2, 32], BF16)
            wcv = AW.alloc(fw, "wcv", [128, 8, 128], BF16)
            wcg = AW.alloc(fw, "wcg", [128, 8, 128], BF16)
            SC_ = [sub(PS[4], "sc0"), sub(PS[4], "sc1"), sub(PS[5], "sc2"), sub(PS[5], "sc3")]
            for c in (range(3) if 'C' not in skip else []):
                def load_rot(dst, off):
                    prs = []
                    for h2 in range(2):
                        prs.append((dst.v(dst.t[:, :, h2, 0:16]), winv[:, :, off + h2 * 32 + 16:off + h2 * 32 + 32]))
                        prs.append((dst.v(dst.t[:, :, h2, 16:32]), winv[:, :, off + h2 * 32:off + h2 * 32 + 16]))
                    pool.dma_group(prs, max_dma_last_dim=4096)
                    fw.activation(dst.v(dst.t[:, :, :, 0:16]), dst.v(dst.t[:, :, :, 0:16]), AF.Identity, scale=-1.0)
                load_w(wcq[:], winv[:, :, OFF_CQ + c * 64:OFF_CQ + (c + 1) * 64])
                load_rot(wcqr, OFF_CQ + c * 64)
                load_w(wck[:], winv[:, :, OFF_CK + c * 64:OFF_CK + (c + 1) * 64])
                load_rot(wckr, OFF_CK + c * 64)
                load_w(wcv[:], winv[:, :, OFF_CV + c * 128:OFF_CV + (c + 1) * 128])
                load_w(wcg[:], winv[:, :, OFF_CG + c * 128:OFF_CG + (c + 1) * 128])
                if c == 0:
                    load_w(wo[:], wov)
                for (w_, wr_, dst, isq) in [(wcq, wcqr, qTc, True), (wck, wckr, kTc, False)]:
                    for tb in range(4):
                        ts_ = slice(tb * 512, (tb + 1) * 512)
                        p1, p2 = (PS[6], PS[7]) if tb % 2 else (PS[4], PS[5])
                        for kc in range(8):
                            fw.mm(p1[0:64, :], w_[:, kc, :], hT[:, kc, ts_], start=(kc == 0), stop=(kc == 7))
                        for kc in range(8):
                            fw.mm(p2[0:64, :], wr_.v(wr_.t[:, kc, :, :].rearrange("p h e -> p (h e)")), hT[:, kc, ts_],
                                  start=(kc == 0), stop=(kc == 7))
                        fw.tt(dve, RT[0][0:64, :], p1[0:64, :], FCOS[0:64, ts_], ALU.mult)
                        fw.tt(dve, RT[1][0:64, :], p2[0:64, :], FSIN[0:64, ts_], ALU.mult)
                        if isq:
                            fw.tt(pool, RT[2][0:64, :], RT[0][0:64, :], RT[1][0:64, :], ALU.add)
                            fw.copy(act, dst[0:64, ts_], RT[2][0:64, :])
                            xib = XI.v(bass.AP(XI.t, c * 128, [[3 * 128, 64], [0, 4], [1, 128]]))
                            fw.tt(pool, qxT.v(qxT.t[0:64, ts_].rearrange("p (n q) -> p n q", n=4)),
                                  RT[2].v(RT[2].t[0:64, :].rearrange("p (n q) -> p n q", n=4)), xib, ALU.mult)
                        else:
                            fw.tt(pool, dst[0:64, ts_], RT[0][0:64, :], RT[1][0:64, :], ALU.add)
                proj_fm(lambda tb, ts_, pv: fw.activation(gT[:, ts_], pv, AF.Silu), lambda kc: wcg[:, kc, :], 128)
                for tg in range(4):
                    pb = PS[6 + tg % 2]
                    for t4 in range(4):
                        tile = tg * 4 + t4
                        for kc in range(8):
                            fw.mm(pb[:, t4 * 128:(t4 + 1) * 128], hT[:, kc, tile * 128:(tile + 1) * 128], wcv[:, kc, :],
                                  start=(kc == 0), stop=(kc == 7), signal=(kc == 7 and t4 == 3))
                    fw.copy(act if tg % 2 else dve, VC.v(VC.t[:, tg * 4:tg * 4 + 4, :].rearrange("p n c -> p (n c)")), pb[:])
                for tg in range(2):
                    pb = PS[6 + tg]
                    tb16 = pb.t.bitcast(BF16)
                    for t8 in range(8):
                        tile = tg * 8 + t8
                        fw.transpose(pb.v(tb16[:, t8 * 64:(t8 + 1) * 64]), kTc[0:64, tile * 128:(tile + 1) * 128],
                                     ident_b[0:64, 0:64], signal=(t8 == 7))
                    zb = ZETA.v(bass.AP(ZETA.t, c * 64, [[3 * 64, 128], [0, 8], [1, 64]]))
                    fw.tt(dve, kz[:, tg * 8:(tg + 1) * 8, :],
                          pb.v(tb16[:, 0:512].rearrange("p (n c) -> p n c", n=8)), zb, ALU.mult)
                for hh in range(2):
                    for n in range(15):
                        pu = PS[6 + (n // 8)]
                        fw.mm(pu[32 * hh:32 * hh + 32, (n % 8) * 64:(n % 8 + 1) * 64], kz[:, n, 32 * hh:32 * hh + 32],
                              VC[:, n, 64 * hh:64 * hh + 64], start=True, stop=True, signal=(n in (7, 14)))
                fw.copy(dve, STf[:, 1, :], PS[6][0:64, 0:64])
                for n in range(1, 15):
                    pu = PS[6 + (n // 8)]
                    fw.stt(STf[:, n + 1, :], STf[:, n, :], GAM[:, c:c + 1], pu[0:64, (n % 8) * 64:(n % 8 + 1) * 64], ALU.mult, ALU.add)
                fw.copy(act, STb[:, 1:16, :], STf[:, 1:16, :])
                if c == 0:
                    dump(f"qTc{l}", qTc[0:64, :], [64, S], BF16)
                    dump(f"kTc{l}", kTc[0:64, :], [64, S], BF16)
                    dump(f"qxT{l}", qxT[0:64, :], [64, S], BF16)
                    dump(f"kz{l}", kz[:], [128, 16, 64], BF16)
                    dump(f"VC{l}", VC[:], [128, 16, 128], BF16)
                    dump(f"STf{l}", STf[:, 1:16, :], [64, 15, 64], F32)
                    dump(f"gT{l}", gT[:], [128, S], BF16)
                unitsC = []
                bi = 0
                for n in range(16):
                    for hh in range(2):
                        def mk(n=n, hh=hh, bi=bi):
                            h = 2 * c + hh
                            ss = PS[4 + bi % 4]
                            ssv = ss[:, 0:128]
                            pt = PTC[bi % 4]
                            cs = slice(n * 128, (n + 1) * 128)
                            ob = PS[n // 4][64 * hh:64 * hh + 64, (n % 4) * 128:(n % 4 + 1) * 128]

                            def st1():
                                fw.mm(ssv, kTc[32 * hh:32 * hh + 32, cs], qTc[32 * hh:32 * hh + 32, cs], start=True, stop=True)
                                fw.tt(dve, pt[:], ssv, DECT[:, h, :], ALU.mult)

                            def st2():
                                fw.mm(ob, VC[:, n, 64 * hh:64 * hh + 64], pt[:], start=True, stop=(n == 0), signal=True, skip_group_check=True)
                                if n > 0:
                                    fw.mm(ob, STb[32 * hh:32 * hh + 32, n, :], qxT[32 * hh:32 * hh + 32, cs], start=False, stop=True,
                                          signal=True, skip_group_check=True)
                            return (st1, st2)
                        unitsC.append(mk())
                        bi += 1
                run_pipe(unitsC)
                for tb in range(4):
                    ts_ = slice(tb * 512, (tb + 1) * 512)
                    ob = PS[tb]
                    fw.copy(act, CP[:], ob[:])
                    fw.activation(CSQ[:], ob[:], AF.Square)
                    pm, p2_ = PS[6], PS[7]
                    fw.mm(pm[:], BD[:], CP[:], start=True, stop=True)
                    fw.mm(p2_[:], BD[:], CSQ[:], start=True, stop=True)
                    fw.copy(act, CMEAN[:], pm[:])
                    fw.activation(CMSQ[:], pm[:], AF.Square)
                    fw.tt(dve, CDV[:], ob[:], CMEAN[:], ALU.subtract)
                    fw.tt(dve, CVAR[:], p2_[:], CMSQ[:], ALU.subtract)
                    fw.ts(dve, CVAR[:], CVAR[:], 0.0, ALU.max)
                    fw.activation(CVAR[:], CVAR[:], AF.Ln, bias=EPSC[:, 0:1])
                    fw.activation(CRS[:], CVAR[:], AF.Exp, scale=-0.5)
                    fw.tt(dve, CDV[:], CDV[:], CRS[:], ALU.mult)
                    fw.tt(pool, mixT[:, 5 + c, ts_], CDV[:], gT[:, ts_], ALU.mult)
            for kc in range(8):
                fw.activation(wo[:, kc, :], wo[:, kc, :], AF.Identity, scale=MG[:, l, kc:kc + 1])
            dump(f"mixC{l}", mixT[:, 5:8, :], [128, 3, S], BF16)
            fw.barrier()

            MARKS.append(('wout', l, pe.ninst))
            AX.reset()
            xT = AX.alloc(fw, "xT", [128, 8, S], F32)
            sp.dma_group([(xT[:, kc, :], xsp_d[:, kc, :]) for kc in range(8)])
            for d_ in range(8):
                for tb in range(4):
                    ts_ = slice(tb * 512, (tb + 1) * 512)
                    pb = PS[(d_ * 4 + tb) % 4]
                    for kc in range(8):
                        fw.mm(pb[:], wo[:, kc, d_ * 128:(d_ + 1) * 128], mixT[:, kc, ts_], start=(kc == 0), stop=(kc == 7))
                    fw.stt(xT[:, d_, ts_], pb[:], MOD[:, l, 16 + d_:17 + d_], xT[:, d_, ts_], ALU.mult, ALU.add)
            dump(f"xmid{l}", xT[:], [128, 8, S], F32)
            fw.barrier()

            MARKS.append(('ffn', l, pe.ninst))
            AY.reset()
            norm_to_hT(l, lambda kc: A2[:, l, kc:kc + 1], 24, AY)
            fw.barrier()
            AY.reset()
            actT = AY.alloc(fw, "actT", [128, 6, S], BF16)
            sgt = [AY.alloc(fw, f"sg{i}", [128, 512], BF16) for i in range(2)]
            AW.reset()
            wdn = [AW.alloc(fw, f"wdn{i}", [128, 6, D], BF16) for i in range(2)]
            gu = [AW.alloc(fw, f"gu{i}", [128, 2, 8, 128], BF16) for i in range(3)]
            wgv = wg_d[l].rearrange("(k p) n -> p k n", p=128)
            wuv = wu_d[l].rearrange("(k p) n -> p k n", p=128)
            ji = 0
            modgen = mod_pieces(l + 1) if l + 1 < depth else iter(())
            for gi, (j0, nj) in enumerate(FFN_GROUPS):
                wdb = wdn[gi % 2]
                load_w(wdb[:, 0:nj, :], wd_d[l, j0 * 128:(j0 + nj) * 128, :].rearrange("(j p) n -> p j n", p=128))
                for jj in range(nj):
                    j = j0 + jj
                    g = gu[ji % 3]
                    ji += 1
                    pool.dma_group([(g[:, 0, :, :], wgv[:, :, j * 128:(j + 1) * 128]),
                                    (g[:, 1, :, :], wuv[:, :, j * 128:(j + 1) * 128])], max_dma_last_dim=4096)
                    for tb in range(4):
                        ts_ = slice(tb * 512, (tb + 1) * 512)
                        pg, pu = PS[(tb % 2) * 2], PS[(tb % 2) * 2 + 1]
                        for kc in range(8):
                            fw.mm(pg[:], g[:, 0, kc, :], hT[:, kc, ts_], start=(kc == 0), stop=(kc == 7))
                        for kc in range(8):
                            fw.mm(pu[:], g[:, 1, kc, :], hT[:, kc, ts_], start=(kc == 0), stop=(kc == 7))
                        fw.activation(sgt[tb % 2][:], pg[:], AF.Silu)
                        fw.tt(dve, actT[:, jj, ts_], pu[:], sgt[tb % 2][:], ALU.mult)
                    next(modgen, None)
                for d_ in range(8):
                    for tb in range(4):
                        ts_ = slice(tb * 512, (tb + 1) * 512)
                        pb = PS[4 + (d_ * 4 + tb) % 3]
                        for jj in range(nj):
                            fw.mm(pb[:], wdb[:, jj, d_ * 128:(d_ + 1) * 128], actT[:, jj, ts_], start=(jj == 0), stop=(jj == nj - 1))
                        fw.stt(xT[:, d_, ts_], pb[:], MOD[:, l, 40 + d_:41 + d_], xT[:, d_, ts_], ALU.mult, ALU.add)
            for _ in modgen:
                pass
            dump(f"xout{l}", xT[:], [128, 8, S], F32)
            fw.barrier()

        MARKS.append(('final', depth, pe.ninst))
        AY.reset()
        sq = [AY.alloc(fw, f"fsq{i}", [128, 8, 512], BF16) for i in range(2)]
        lnt = AY.alloc(fw, "fln", [128, 512], F32)
        rs = AY.alloc(fw, "frs", [128, 512], F32)
        yt = [AY.alloc(fw, f"fyt{i}", [128, 512], F32) for i in range(3)]
        AW.reset()
        OUT = [AW.alloc(fw, f"oo{i}", [128, 4, D], F32) for i in range(2)]
        stores = []
        for tb in (range(4) if 'final' not in skip else []):
            ts_ = slice(tb * 512, (tb + 1) * 512)
            fw.activation(sq[tb % 2][:], xT[:, :, ts_], AF.Square)
            pb = PS[tb % 2]
            for kc in range(8):
                fw.mm(pb[:], ones_b[:], sq[tb % 2][:, kc, :], start=(kc == 0), stop=(kc == 7))
            fw.activation(lnt[:], pb[:], AF.Ln, scale=1.0 / D, bias=EPSC[:, 0:1])
            fw.activation(rs[:], lnt[:], AF.Exp, scale=-0.5)
            ob_ = OUT[tb % 2]
            for kc in range(8):
                t = yt[kc % 3]
                fw.stt(t[:], xT[:, kc, ts_], FNG[:, kc:kc + 1], rs[:], ALU.mult, ALU.mult)
                pt_ = PS[2 + kc % 4]
                for q in range(4):
                    fw.transpose(pt_[:, q * 128:(q + 1) * 128], t[:, q * 128:(q + 1) * 128], ident_f[:], signal=(q == 3))
                fw.copy(act if kc % 2 else dve, ob_[:, :, kc * 128:(kc + 1) * 128],
                        pt_.v(pt_.t[:, :].rearrange("p (q c) -> p q c", q=4)))
            stores.append(sp.dma_group([(y_d[(tb * 4 + q) * 128:(tb * 4 + q + 1) * 128, :], ob_[:, q, :]) for q in range(4)]))
        for tok in stores:
            sp.wait_tok(tok)
        for tok in dbg_out.values():
            sp.wait_tok(tok)
        fw.emit()
        info = {n: (e.ninst, e.nwait) for n, e in fw.E.items()}
        info["sp"] = (fw.sp.ninst, fw.sp.nwait)
        info["sems"] = fw.nsem
        print("MK build:", info)
    return nc


AX_X = mybir.AxisListType.X
MARKS = []
_NC_CACHE = {}


def prep_inputs(inputs, b):
    f32 = np.float32
    g = lambda k: np.asarray(inputs[k])
    hc = _HC_CACHE.get("hc")
    if hc is None:
        hc = host_consts(g("rel_bias").astype(f32))
        _HC_CACHE["hc"] = hc
    m = dict(hc)
    m["x"] = np.ascontiguousarray(g("x")[b].astype(f32))
    m["cT"] = np.ascontiguousarray(g("c")[b].astype(f32).reshape(8, 128).T)
    m["posb"] = np.ascontiguousarray(g("positions")[b].astype(np.int32).reshape(1, S))
    sh = _HC_CACHE.get("shared")
    if sh is None:
        def fm(a, nch):
            a = np.asarray(a, f32)
            return np.ascontiguousarray(a.reshape(a.shape[0], nch, 128).transpose(2, 0, 1))
        sh = dict(
            ada_w=np.ascontiguousarray(g("ada_w").astype(f32)),
            adabT=fm(g("ada_b"), 48), n1gT=fm(g("norm1_g"), 8), n2gT=fm(g("norm2_g"), 8),
            fngT=np.ascontiguousarray(g("final_norm").astype(f32).reshape(8, 128).T),
            mgT=fm(g("mix_gain"), 8), mqnT=fm(g("mla_q_norm"), 2), mkvnT=fm(g("mla_kv_norm"), 1),
            w_in=np.ascontiguousarray(g("w_in").astype(f32)), w_uq=np.ascontiguousarray(g("mla_w_uq").astype(f32)),
            w_ukv=np.ascontiguousarray(g("mla_w_ukv").astype(f32)), w_out=np.ascontiguousarray(g("w_out").astype(f32)),
            w_gate=np.ascontiguousarray(g("ffn_w_gate").astype(f32)), w_up=np.ascontiguousarray(g("ffn_w_up").astype(f32)),
            w_down=np.ascontiguousarray(g("ffn_w_down").astype(f32)),
        )
        _HC_CACHE["shared"] = sh
    m.update(sh)
    return m


_HC_CACHE = {}


def kernel(**inputs):
    _HC_CACHE.clear()
    nc = build()
    in_maps = [prep_inputs(inputs, b) for b in range(NCORES)]
    res = run_bass_kernel_spmd(nc, in_maps, core_ids=list(range(NCORES)))
    out = np.stack([np.asarray(r["y"], dtype=np.float32) for r in res.results], axis=0)
    return out
```

```python
import numpy as np
import concourse.bass as bass
import concourse.mybir as mybir
from concourse.bass_utils import run_bass_kernel_spmd
from contextlib import ExitStack

F32 = mybir.dt.float32
BF16 = mybir.dt.bfloat16
I32 = mybir.dt.int32
AF = mybir.ActivationFunctionType
ALU = mybir.AluOpType
AX = mybir.AxisListType

CE = ('pe', 'act', 'dve', 'pool')
SAME_ENGINE_SYNC = True


class Buf:
    __slots__ = ('t', 'name', 'w', 'r', 'semname')

    def __init__(self, t, name, semname=None):
        self.t = t
        self.name = name
        self.w = None
        self.r = {}
        self.semname = semname or name

    def __getitem__(self, idx):
        return V(self, self.t[idx])

    def v(self, ap):
        return V(self, ap)


class V:
    __slots__ = ('buf', 'ap')

    def __init__(self, buf, ap):
        self.buf = buf
        self.ap = ap


class Eng:
    def __init__(self, fw, name, sem):
        self.fw = fw
        self.name = name
        self.sem = sem
        self.count = 0
        self.seen = {}
        self.prog = []
        self.snaps = {0: {}}
        self.nwait = 0
        self.ninst = 0

    def _deps(self, reads, writes):
        deps = []
        for v in reads:
            if v.buf.w is not None:
                deps.append(v.buf.w)
        for v in writes:
            if v.buf.w is not None:
                deps.append(v.buf.w)
            deps.extend(v.buf.r.values())
        need = {}
        for (key, val, sem) in deps:
            if key == self.name and (self.name == 'pe' or not SAME_ENGINE_SYNC):
                continue
            if val > self.seen.get(key, 0) and val > need.get(key, (0, None))[0]:
                need[key] = (val, sem)
        for key, (val, sem) in need.items():
            self.prog.append(('wait', sem, val))
            self.nwait += 1
            self.seen[key] = val
            if key in self.fw.E and key != self.name:
                snap = self.fw.E[key].snaps.get(val)
                if snap:
                    for k2, v2 in snap.items():
                        if v2 > self.seen.get(k2, 0):
                            self.seen[k2] = v2

    def issue(self, fn, reads=(), writes=(), signal=True):
        self._deps(reads, writes)
        self.ninst += 1
        if signal:
            self.count += 1
            self.prog.append(('inst', fn, True))
            self.snaps[self.count] = {k: v for k, v in self.seen.items() if k in CE}
            tok = (self.name, self.count, self.sem)
        else:
            self.prog.append(('inst', fn, False))
            tok = (self.name, self.count + 1, self.sem)
        for v in reads:
            v.buf.r[self.name] = tok
        for v in writes:
            v.buf.w = tok
            v.buf.r = {}
        return tok

    def dma(self, out, in_, **kw):
        return self.dma_group([(out, in_)], **kw)

    def dma_group(self, pairs, **kw):
        reads = [i for (o, i) in pairs if isinstance(i, V)]
        writes = [o for (o, i) in pairs if isinstance(o, V)]
        self._deps(reads, writes)
        buf = (writes[0].buf if writes else reads[0].buf)
        rec = self.fw.dsem.get(buf.semname)
        if rec is None:
            rec = [self.fw.new_sem('d_' + buf.semname), 0]
            self.fw.dsem[buf.semname] = rec
        for (o, i) in pairs:
            rec[1] += 1
            oa = o.ap if isinstance(o, V) else o
            ia = i.ap if isinstance(i, V) else i
            self.prog.append(('dma', oa, ia, rec[0], kw))
        tok = (('d', buf.semname), 16 * rec[1], rec[0])
        for v in reads:
            v.buf.r[tok[0]] = tok
        for v in writes:
            v.buf.w = tok
            v.buf.r = {}
        return tok

    def wait_tok(self, tok):
        key, val, sem = tok
        if val > self.seen.get(key, 0):
            self.prog.append(('wait', sem, val))
            self.seen[key] = val

    def replay(self, e):
        for item in self.prog:
            if item[0] == 'wait':
                e.wait_ge(item[1], item[2])
            elif item[0] == 'inst':
                ins = item[1](e)
                if item[2]:
                    ins.then_inc(self.sem, 1)
            else:
                _, oa, ia, sem, kw = item
                e.dma_start(out=oa, in_=ia, **kw).then_inc(sem, 16)


class FW:
    def __init__(self, nc, stack):
        self.nc = nc
        self.stack = stack
        self.nsem = 0
        self.dsem = {}
        self.dma_toks = []
        self.E = {}
        for name in CE:
            self.E[name] = Eng(self, name, self.new_sem('c_' + name))
        self.sp = Eng(self, 'sp', None)
        self.pe, self.act, self.dve, self.pool = (self.E[n] for n in CE)

    def new_sem(self, name):
        self.nsem += 1
        return self.stack.enter_context(self.nc.semaphore(name))

    def sbuf(self, name, shape, dtype):
        t = self.stack.enter_context(self.nc.sbuf_tensor(name, list(shape), dtype))
        return Buf(t, name)

    def psum(self, name, shape, dtype):
        t = self.stack.enter_context(self.nc.psum_tensor(name, list(shape), dtype))
        return Buf(t, name)

    def barrier(self):
        toks = [(n, self.E[n].count, self.E[n].sem) for n in CE if self.E[n].count > 0]
        for e in list(self.E.values()) + [self.sp]:
            for tok in toks:
                if tok[0] != e.name:
                    e.wait_tok(tok)
            for tok in self.dma_toks:
                e.wait_tok(tok)
        self.dma_toks = []

    def check(self):
        engs = list(self.E.values()) + [self.sp]
        pc = {e.name: 0 for e in engs}
        sems = {}
        progress = True
        while progress:
            progress = False
            for e in engs:
                while pc[e.name] < len(e.prog):
                    it = e.prog[pc[e.name]]
                    if it[0] == 'wait':
                        if sems.get(id(it[1]), 0) < it[2]:
                            break
                    elif it[0] == 'inst':
                        if it[2]:
                            sems[id(e.sem)] = sems.get(id(e.sem), 0) + 1
                    else:
                        sems[id(it[3])] = sems.get(id(it[3]), 0) + 16
                    pc[e.name] += 1
                    progress = True
        stuck = {}
        for e in engs:
            if pc[e.name] < len(e.prog):
                it = e.prog[pc[e.name]]
                who = [n for n, x in self.E.items() if x.sem is it[1]] or [k for k, r in self.dsem.items() if r[0] is it[1]]
                stuck[e.name] = (pc[e.name], len(e.prog), who, it[2], sems.get(id(it[1]), 0))
        return stuck

    def emit(self):
        st = self.check()
        assert not st, f"DEADLOCK: {st}"
        with self.nc.Block() as block:
            @block.sync
            def _(e):
                self.sp.replay(e)

            @block.tensor
            def _(e):
                self.pe.replay(e)

            @block.scalar
            def _(e):
                self.act.replay(e)

            @block.vector
            def _(e):
                self.dve.replay(e)

            @block.gpsimd
            def _(e):
                self.pool.replay(e)

    def mm(self, out, lhsT, rhs, start=True, stop=True, signal=None, **kw):
        if signal is None:
            signal = stop
        return self.pe.issue(
            lambda e: e.matmul(out.ap, lhsT.ap, rhs.ap, start=start, stop=stop, **kw),
            reads=[lhsT, rhs], writes=[out], signal=signal)

    def transpose(self, out, in_, ident, signal=True):
        return self.pe.issue(
            lambda e: e.transpose(out.ap, in_.ap, ident.ap),
            reads=[in_, ident], writes=[out], signal=signal)

    def activation(self, out, in_, func, scale=1.0, bias=0.0, accum_out=None, eng=None):
        reads = [in_]
        sc = scale
        bi = bias
        if isinstance(scale, V):
            reads.append(scale)
            sc = scale.ap
        if isinstance(bias, V):
            reads.append(bias)
            bi = bias.ap
        writes = [out]
        kw = {}
        if accum_out is not None:
            writes.append(accum_out)
            kw['accum_out'] = accum_out.ap
        return self.act.issue(
            lambda e: e.activation(out=out.ap, in_=in_.ap, func=func, scale=sc, bias=bi, **kw),
            reads=reads, writes=writes)

    def tt(self, eng, out, in0, in1, op):
        return eng.issue(lambda e: e.tensor_tensor(out=out.ap, in0=in0.ap, in1=in1.ap, op=op),
                         reads=[in0, in1], writes=[out])

    def ts(self, eng, out, in0, s1, op0, s2=None, op1=None, accum_out=None):
        reads = [in0]
        a1 = s1
        a2 = s2
        if isinstance(s1, V):
            reads.append(s1)
            a1 = s1.ap
        if isinstance(s2, V):
            reads.append(s2)
            a2 = s2.ap
        kw = {}
        writes = [out]
        if op1 is not None:
            kw['op1'] = op1
        if accum_out is not None:
            kw['accum_out'] = accum_out.ap
            writes.append(accum_out)
        return eng.issue(lambda e: e.tensor_scalar(out=out.ap, in0=in0.ap, scalar1=a1, scalar2=a2, op0=op0, **kw),
                         reads=reads, writes=writes)

    def stt(self, out, in0, scalar, in1, op0, op1):
        reads = [in0, in1]
        a = scalar
        if isinstance(scalar, V):
            reads.append(scalar)
            a = scalar.ap
        return self.dve.issue(
            lambda e: e.scalar_tensor_tensor(out=out.ap, in0=in0.ap, scalar=a, in1=in1.ap, op0=op0, op1=op1),
            reads=reads, writes=[out])

    def copy(self, eng, out, in_):
        if eng is self.act:
            return eng.issue(lambda e: e.copy(out=out.ap, in_=in_.ap), reads=[in_], writes=[out])
        return eng.issue(lambda e: e.tensor_copy(out=out.ap, in_=in_.ap), reads=[in_], writes=[out])

    def memset(self, eng, out, val):
        return eng.issue(lambda e: e.memset(out.ap, val), writes=[out])

D = 1024
S = 2048
DEPTH = 4
NCORES = 8
IN_W = 2720
FH = 2816
OFF_AQ, OFF_AK, OFF_AV = 0, 384, 768
OFF_BQ, OFF_BKV, OFF_BKR = 1152, 1408, 1536
OFF_CQ, OFF_CK, OFF_CV, OFF_CG = 1568, 1760, 1952, 2336
EPS = 1e-6
NEG = -30000.0
SLAB = 209984
FFN_GROUPS = [(0, 6), (6, 6), (12, 5), (17, 5)]


def _t5_bucket(dist):
    n_buckets, max_distance = 32, 2048
    max_exact = n_buckets // 2
    safe = np.maximum(dist, 1).astype(np.float32)
    large = max_exact + (np.log(safe / max_exact) / np.log(max_distance / max_exact) * (n_buckets - max_exact)).astype(np.int32)
    large = np.minimum(large, n_buckets - 1)
    return np.where(dist < max_exact, dist, large).astype(np.int32)


def host_consts(rel_bias):
    f32 = np.float32
    k = np.arange(128)[:, None]
    ql = np.arange(256)[None, :]
    q3 = np.arange(128)[None, :]
    d1 = ql - k
    v1 = (d1 >= 0) & (d1 <= 128)
    i1 = _t5_bucket(np.clip(d1, 0, 128))
    v2 = v1
    i2 = _t5_bucket(np.clip(4 * d1, 0, 512))
    d3 = q3 - k
    v3 = d3 >= 0
    i3 = _t5_bucket(np.clip(16 * d3, 0, 2048))
    bmb = np.zeros((128, 6, 640), f32)
    for h in range(6):
        bmb[:, h, 0:256] = np.where(v1, rel_bias[i1, h], 0.0)
        bmb[:, h, 256:512] = np.where(v2, rel_bias[i2, h], 0.0)
        bmb[:, h, 512:640] = np.where(v3, rel_bias[i3, h], 0.0)
    bmm = np.concatenate([np.where(v1, 0.0, NEG), np.where(v2, 0.0, NEG), np.where(v3, 0.0, NEG)], axis=1).astype(f32)
    cm = np.where(q3 >= k, 0.0, NEG).astype(f32)
    H = 6
    log_g = np.log(1.0 - 2.0 ** (-5.0 - np.arange(H))).astype(f32)
    i = np.arange(128, dtype=f32)
    rel = i[:, None] - i[None, :]
    decay_intra = (np.exp(np.maximum(rel, 0.0)[None] * log_g[:, None, None]) * (rel >= 0)[None]).astype(f32)
    xi = np.exp((i + 1.0)[None, :] * log_g[:, None]).astype(f32)
    zeta = np.exp((128 - 1.0 - i)[None, :] * log_g[:, None]).astype(f32)
    cd = np.exp(128 * log_g).astype(f32)
    s = f32(32 ** -0.5)
    decT = np.zeros((128, 6, 128), f32)
    for h in range(H):
        decT[:, h, :] = decay_intra[h].T * s
    xi_t = np.zeros((64, 3, 128), f32)
    zeta_t = np.zeros((128, 3, 64), f32)
    gam = np.zeros((64, 3), f32)
    for p in range(3):
        for hh in range(2):
            h = 2 * p + hh
            xi_t[hh * 32:(hh + 1) * 32, p, :] = xi[h][None, :] * s
            zeta_t[:, p, hh * 32:(hh + 1) * 32] = zeta[h][:, None]
            gam[hh * 32:(hh + 1) * 32, p] = cd[h]
    half = 16
    inv_freq = (1.0 / (10000.0 ** (np.arange(half, dtype=f32) / half))).astype(f32)
    invf = np.tile(inv_freq, 8).reshape(128, 1).astype(f32)
    return dict(bmb=bmb, bmm=bmm, cm=cm, decT=decT, xi=xi_t, zeta=zeta_t, gam=gam, invf=invf)


class Arena:
    def __init__(self, nc, base, size, name):
        self.nc, self.base, self.size, self.name = nc, base, size, name
        self.off = 0
        self.n = 0

    def reset(self):
        self.off = 0

    def alloc(self, fw, name, shape, dtype):
        nbytes = int(np.prod(shape[1:])) * mybir.dt.size(dtype)
        nbytes = (nbytes + 31) // 32 * 32
        assert self.off + nbytes <= self.size, (self.name, name, self.off, nbytes, self.size)
        self.n += 1
        t = self.nc.alloc_sbuf_tensor_at(f"{self.name}_{name}_{self.n}", list(shape), dtype, offset=self.base + self.off)
        self.off += nbytes
        return Buf(t, f"{self.name}_{name}_{self.n}", semname=f"{self.name}_{name}")


def build(depth=DEPTH, dbg=None, skip=()):
    nc = bass.Bass("TRN2", target_bir_lowering=False)
    dbg = dbg or []
    dbg_out = {}
    MARKS.clear()

    def din(name, shape, dt=F32):
        return nc.dram_tensor(name, list(shape), dt, kind="ExternalInput").ap()

    x_d = din("x", [S, D])
    cT_d = din("cT", [128, 8])
    pos_d = din("posb", [1, S], I32)
    invf_d = din("invf", [128, 1])
    bmb_d = din("bmb", [128, 6, 640])
    bmm_d = din("bmm", [128, 640])
    cm_d = din("cm", [128, 128])
    decT_d = din("decT", [128, 6, 128])
    xi_d = din("xi", [64, 3, 128])
    zeta_d = din("zeta", [128, 3, 64])
    gam_d = din("gam", [64, 3])
    adab_d = din("adabT", [128, DEPTH, 48])
    n1g_d = din("n1gT", [128, DEPTH, 8])
    n2g_d = din("n2gT", [128, DEPTH, 8])
    fng_d = din("fngT", [128, 8])
    mg_d = din("mgT", [128, DEPTH, 8])
    mqn_d = din("mqnT", [128, DEPTH, 2])
    mkvn_d = din("mkvnT", [128, DEPTH, 1])
    if depth > 0:
        adaw_d = din("ada_w", [DEPTH, D, 6 * D])
        win_d = din("w_in", [DEPTH, D, IN_W])
        wuq_d = din("w_uq", [DEPTH, 256, 384])
        wukv_d = din("w_ukv", [DEPTH, 128, 512])
        wout_d = din("w_out", [DEPTH, D, D])
        wg_d = din("w_gate", [DEPTH, D, FH])
        wu_d = din("w_up", [DEPTH, D, FH])
        wd_d = din("w_down", [DEPTH, FH, D])
        xsp_d = nc.dram_tensor("xspill", [128, 8, S], F32, kind="Internal").ap()
    y_d = nc.dram_tensor("y", [S, D], F32, kind="ExternalOutput").ap()

    st = ExitStack()
    with st:
        fw = FW(nc, st)
        pe, act, dve, pool, sp = fw.pe, fw.act, fw.dve, fw.pool, fw.sp
        base0 = (nc.sbuf_base + 31) // 32 * 32
        st.enter_context(nc.sbuf_tensor("slab", [128, SLAB], mybir.dt.uint8))
        assert nc.sbuf_base == base0 + SLAB, (nc.sbuf_base, base0)
        o = base0
        SZ_HT, SZ_X, SZ_Y, SZ_ADAW, SZ_W = 32768, 65536, 32768, 12288, 36864
        SZ_C = SLAB - (SZ_HT + SZ_X + SZ_Y + SZ_ADAW + SZ_W)
        AC = Arena(nc, o, SZ_C, "c"); o += SZ_C
        AH = Arena(nc, o, SZ_HT, "h"); o += SZ_HT
        AX = Arena(nc, o, SZ_X, "x"); o += SZ_X
        AY = Arena(nc, o, SZ_Y, "y"); o += SZ_Y
        AA = Arena(nc, o, SZ_ADAW, "a"); o += SZ_ADAW
        AW = Arena(nc, o, SZ_W, "w"); o += SZ_W

        PS = [fw.psum(f"ps{i}", [128, 512], F32) for i in range(8)]

        def sub(buf, name):
            return Buf(buf.t, name)

        ident_f = AC.alloc(fw, "identf", [128, 128], F32)
        ident_b = AC.alloc(fw, "identb", [128, 128], BF16)
        ones_b = AC.alloc(fw, "onesb", [128, 128], BF16)
        BD = AC.alloc(fw, "bd", [128, 128], BF16)
        WNe = AC.alloc(fw, "wne", [128, 64], BF16)
        WNo = AC.alloc(fw, "wno", [128, 64], BF16)
        FCOS = AC.alloc(fw, "fcos", [128, S], BF16)
        FSIN = AC.alloc(fw, "fsin", [128, S], BF16)
        BM = AC.alloc(fw, "bm", [128, 6, 640], BF16)
        CM = AC.alloc(fw, "cm", [128, 128], F32)
        DECT = AC.alloc(fw, "dect", [128, 6, 128], F32)
        XI = AC.alloc(fw, "xi", [64, 3, 128], F32)
        ZETA = AC.alloc(fw, "zeta", [128, 3, 64], F32)
        GAM = AC.alloc(fw, "gam", [64, 3], F32)
        MOD = AC.alloc(fw, "mod", [128, DEPTH, 48], F32)
        A1 = AC.alloc(fw, "a1", [128, DEPTH, 8], F32)
        A2 = AC.alloc(fw, "a2", [128, DEPTH, 8], F32)
        ADAB = AC.alloc(fw, "adab", [128, DEPTH, 48], F32)
        N1G = AC.alloc(fw, "n1g", [128, DEPTH, 8], F32)
        N2G = AC.alloc(fw, "n2g", [128, DEPTH, 8], F32)
        FNG = AC.alloc(fw, "fng", [128, 8], F32)
        MG = AC.alloc(fw, "mg", [128, DEPTH, 8], F32)
        MQN = AC.alloc(fw, "mqn", [128, DEPTH, 2], F32)
        MKVN = AC.alloc(fw, "mkvn", [128, DEPTH, 1], F32)
        CTF = AC.alloc(fw, "ctf", [128, 8], F32)
        CTB = AC.alloc(fw, "ctb", [128, 8], BF16)
        INVF = AC.alloc(fw, "invf", [128, 1], F32)
        hT = AH.alloc(fw, "hT", [128, 8, S], BF16)
        adaw = [AA.alloc(fw, f"adaw{i}", [128, 3072], BF16) for i in range(2)]

        def dump(name, view, shape, dt):
            if name not in dbg:
                return
            t = nc.dram_tensor("dbg_" + name, list(shape), dt, kind="ExternalOutput").ap()
            dbg_out[name] = sp.dma(t, view)
            fw.dma_toks.append(dbg_out[name])

        fw.memset(dve, ident_f[:], 1.0)
        pool.issue(lambda e: e.affine_select(out=ident_f.t[:], in_=ident_f.t[:], pattern=[[-1, 128]],
                                             compare_op=ALU.is_equal, fill=0.0, base=0, channel_multiplier=1),
                   reads=[ident_f[:]], writes=[ident_f[:]])
        fw.copy(dve, ident_b[:], ident_f[:])
        fw.memset(dve, ones_b[:], 1.0)
        fw.memset(dve, BD[:], 0.0)
        fw.memset(dve, BD[0:64, 0:64], 1.0 / 64)
        fw.memset(dve, BD[64:128, 64:128], 1.0 / 64)
        fw.memset(dve, WNe[:], 0.0)
        fw.memset(dve, WNe[0:64, :], 1.0)
        if 'wn' not in skip:
            fw.memset(dve, WNe[64:65, :], 64 * EPS)
        fw.memset(dve, WNo[:], 0.0)
        fw.memset(dve, WNo[64:128, :], 1.0)
        if 'wn' not in skip:
            fw.memset(dve, WNo[0:1, :], 64 * EPS)
        for (b, d_) in [] if 'small' in skip else [(CM, cm_d), (DECT, decT_d), (XI, xi_d), (ZETA, zeta_d), (GAM, gam_d), (ADAB, adab_d),
                        (N1G, n1g_d), (N2G, n2g_d), (FNG, fng_d), (MG, mg_d), (MQN, mqn_d), (MKVN, mkvn_d),
                        (CTF, cT_d), (INVF, invf_d)]:
            sp.dma(b[:], d_)
        if 'silu' not in skip:
            fw.activation(CTB[:], CTF[:], AF.Silu)
        AY.reset()
        bmb_s = AY.alloc(fw, "bmb", [128, 6, 640], F32)
        bmm_s = AY.alloc(fw, "bmm", [128, 640], F32)
        if 'bm' not in skip:
            sp.dma(bmb_s[:], bmb_d)
            sp.dma(bmm_s[:], bmm_d)
            for h in range(6):
                fw.tt(dve, BM[:, h, :], bmb_s[:, h, :], bmm_s[:], ALU.add)
        fw.barrier()
        AY.reset()
        posi = AY.alloc(fw, "posi", [128, S], I32)
        ang = AY.alloc(fw, "ang", [128, S], F32)
        t1 = AY.alloc(fw, "t1", [128, S], F32)
        t2 = AY.alloc(fw, "t2", [128, S], F32)
        if 'tables' not in skip:
            sp.dma(posi[:], pos_d.partition_broadcast(128))
            fw.copy(dve, t1[:], posi[:])
            fw.ts(dve, ang[:], t1[:], INVF[:, 0:1], ALU.mult)
            TWO_PI = 2.0 * np.pi
            C1 = 6.28125
            C2 = float(np.float32(TWO_PI - C1))
            fw.ts(dve, t1[:], ang[:], 1.0 / TWO_PI, ALU.mult)
            fw.copy(dve, posi[:], t1[:])
            fw.copy(dve, t1[:], posi[:])
            fw.stt(t2[:], t1[:], -C1, ang[:], ALU.mult, ALU.add)
            fw.stt(t2[:], t1[:], -C2, t2[:], ALU.mult, ALU.add)

            def wrap(dst, src, tmp):
                fw.ts(dve, tmp, src, float(np.pi), ALU.is_gt)
                fw.stt(dst, tmp, -TWO_PI, src, ALU.mult, ALU.add)
                fw.ts(dve, tmp, dst, -float(np.pi), ALU.is_lt)
                fw.stt(dst, tmp, TWO_PI, dst, ALU.mult, ALU.add)
                fw.ts(dve, dst, dst, 3.1415925, ALU.min, -3.1415925, ALU.max)

            wrap(t2[:], t2[:], t1[:])
            fw.activation(FSIN[:], t2[:], AF.Sin)
            fw.ts(dve, ang[:], t2[:], float(np.pi / 2), ALU.add)
            wrap(ang[:], ang[:], t1[:])
            fw.activation(FCOS[:], ang[:], AF.Sin)
        fw.barrier()

        def mod_pieces(l):
            mp = PS[7]
            for piece in range(16):
                kc, half = piece // 2, piece % 2
                slot = adaw[piece % 2]
                pool.dma(slot[:], adaw_d[l, kc * 128:(kc + 1) * 128, half * 3072:(half + 1) * 3072], max_dma_last_dim=4096)
                for j in range(24):
                    jj = half * 24 + j
                    fw.mm(mp[:, jj:jj + 1], slot[:, j * 128:(j + 1) * 128], CTB[:, kc:kc + 1],
                          start=(piece == 0 and j == 0), stop=(kc == 7), signal=(j == 23), skip_group_check=True)
                yield
            fw.tt(dve, MOD[:, l, :], mp[:, 0:48], ADAB[:, l, :], ALU.add)
            fw.stt(A1[:, l, :], MOD[:, l, 8:16], 1.0, N1G[:, l, :], ALU.add, ALU.mult)
            fw.stt(A2[:, l, :], MOD[:, l, 32:40], 1.0, N2G[:, l, :], ALU.add, ALU.mult)
            yield

        EPSC = AC.alloc(fw, "epsc", [128, 1], F32)
        fw.memset(dve, EPSC[:], EPS)

        def norm_to_hT(l, a_view, b_col0, ar):
            ar.reset()
            sq = [ar.alloc(fw, f"nsq{i}", [128, 8, 512], BF16) for i in range(2)]
            lnt = ar.alloc(fw, "nln", [128, 512], F32)
            rs = [ar.alloc(fw, f"nrs{i}", [128, 512], F32) for i in range(2)]
            tm = [ar.alloc(fw, f"ntm{i}", [128, 512], F32) for i in range(4)]

            def stA(tb):
                ts_ = slice(tb * 512, (tb + 1) * 512)
                fw.activation(sq[tb % 2][:], xT[:, :, ts_], AF.Square)
                pb = PS[tb % 2]
                for kc in range(8):
                    fw.mm(pb[:], ones_b[:], sq[tb % 2][:, kc, :], start=(kc == 0), stop=(kc == 7))

            def stB(tb):
                ts_ = slice(tb * 512, (tb + 1) * 512)
                pb = PS[tb % 2]
                fw.activation(lnt[:], pb[:], AF.Ln, scale=1.0 / D, bias=EPSC[:, 0:1])
                fw.activation(rs[tb % 2][:], lnt[:], AF.Exp, scale=-0.5)
                for kc in range(8):
                    t = tm[kc % 4]
                    fw.tt(dve, t[:], xT[:, kc, ts_], rs[tb % 2][:], ALU.mult)
                    if kc % 2 == 0:
                        fw.activation(hT[:, kc, ts_], t[:], AF.Identity, scale=a_view(kc), bias=MOD[:, l, b_col0 + kc:b_col0 + kc + 1])
                    else:
                        fw.ts(pool, hT[:, kc, ts_], t[:], a_view(kc), ALU.mult, MOD[:, l, b_col0 + kc:b_col0 + kc + 1], ALU.add)
            stA(0)
            for tb in range(4):
                if tb + 1 < 4:
                    stA(tb + 1)
                stB(tb)

        def load_w(buf_view, dram_ap):
            pool.dma(buf_view, dram_ap, max_dma_last_dim=4096)

        def proj_fm(out_fn, w_fn, M, nk=8, rhs_fn=None, banks=(6, 7), out_p0=0):
            for tb in range(4):
                pb = PS[banks[tb % len(banks)]]
                ts_ = slice(tb * 512, (tb + 1) * 512)
                pv = pb[out_p0:out_p0 + M, :]
                for kc in range(nk):
                    rhs = rhs_fn(kc, ts_) if rhs_fn else hT[:, kc, ts_]
                    fw.mm(pv, w_fn(kc), rhs, start=(kc == 0), stop=(kc == nk - 1))
                out_fn(tb, ts_, pv)

        def finalize_head(bank, nr, mixb, chunk, tb, fin):
            SQb, LNb, RSb = fin
            ts_ = slice(tb * 512, (tb + 1) * 512)
            fw.activation(SQb[:], bank[:], AF.Square)
            pf = PS[6 + (tb % 2)]
            wn = WNe if nr == 0 else WNo
            fw.mm(pf[nr:nr + 64, :], wn[:], SQb[:], start=True, stop=True)
            fw.activation(LNb[nr:nr + 64, :], pf[nr:nr + 64, :], AF.Ln, scale=1.0 / 64)
            fw.activation(RSb[nr:nr + 64, :], LNb[nr:nr + 64, :], AF.Exp, scale=-0.5)
            fw.tt(dve, mixb[nr:nr + 64, chunk, ts_], bank[nr:nr + 64, :], RSb[nr:nr + 64, :], ALU.mult)

        def v_lhsT(vbuf, tile_off, ones_off, even):
            t = vbuf.t
            pstride = t[:, :].ap[0][0]
            if even:
                return vbuf.v(bass.AP(t, tile_off, [[pstride, 128], [ones_off - tile_off, 2], [1, 64]]))
            return vbuf.v(bass.AP(t, ones_off, [[pstride, 128], [tile_off - ones_off, 2], [1, 64]]))

        AX.reset()
        xT = AX.alloc(fw, "xT", [128, 8, S], F32)
        AW.reset()
        io = [AW.alloc(fw, f"io{i}", [128, D], F32) for i in range(2)]
        modgen0 = mod_pieces(0) if depth > 0 else iter(())
        for n in range(16):
            sp.dma(io[n % 2][:], x_d[n * 128:(n + 1) * 128, :])
            next(modgen0, None)
            for g in range(2):
                pb = PS[(2 * n + g) % 4]
                for j in range(4):
                    kc = 4 * g + j
                    fw.transpose(pb[:, j * 128:(j + 1) * 128], io[n % 2][:, kc * 128:(kc + 1) * 128], ident_f[:], signal=(j == 3))
                src = pb.v(pb.t[:, :].rearrange("p (j t) -> p j t", j=4))
                dst = xT.v(xT.t[:, 4 * g:4 * g + 4, n * 128:(n + 1) * 128])
                fw.copy(act if (n + g) % 2 else dve, dst, src)
        for _ in modgen0:
            pass
        fw.barrier()

        def run_pipe(units, la=3):
            nU = len(units)
            for i in range(nU + la):
                if i < nU:
                    units[i][0]()
                if i - la >= 0:
                    units[i - la][1]()

        for l in range(depth):
            MARKS.append(('mod', l, pe.ninst))
            MARKS.append(('norm1', l, pe.ninst))
            AY.reset()
            norm_to_hT(l, lambda kc: A1[:, l, kc:kc + 1], 0, AY)
            dump(f"hT{l}", hT[:], [128, 8, S], BF16)
            for kc in range(8):
                sp.dma(xsp_d[:, kc, :], xT[:, kc, :])
            fw.barrier()
            _rec = fw.dsem[xT.semname]
            spill_tok = (("d", xT.semname), 16 * _rec[1], _rec[0])
            for e in (pe, act, dve, pool):
                e.wait_tok(spill_tok)

            AY.reset()
            mixT = AY.alloc(fw, "mixT", [128, 8, S], BF16)

            MARKS.append(('A', l, pe.ninst))
            AX.reset()
            qT = AX.alloc(fw, "qT", [128, S], BF16)
            kT = AX.alloc(fw, "kT", [128, S], BF16)
            VA = AX.alloc(fw, "VA", [128, 48, 2, 128], BF16)
            VT = AX.alloc(fw, "VT", [128, S], BF16)
            PT = [AX.alloc(fw, f"pt{i}", [128, 512], BF16) for i in range(4)]
            TS = [AX.alloc(fw, f"ts{i}", [128, 512], F32) for i in range(4)]
            fin = (AX.alloc(fw, "fsq", [128, 512], BF16), AX.alloc(fw, "fln", [128, 512], F32), AX.alloc(fw, "frs", [128, 512], F32))
            AW.reset()
            wq = AW.alloc(fw, "wq", [128, 8, 128], BF16)
            wk = AW.alloc(fw, "wk", [128, 8, 128], BF16)
            wv = AW.alloc(fw, "wv", [128, 8, 128], BF16)
            SS = [sub(PS[4], "ss0"), sub(PS[4], "ss1"), sub(PS[5], "ss2"), sub(PS[5], "ss3")]
            fw.memset(pool, VA[:, :, 0, 64:128], 1.0)
            fw.memset(pool, VA[:, :, 1, 0:64], 1.0)
            winv = win_d[l].rearrange("(k p) n -> p k n", p=128)
            blk = [0]
            for c in (range(3) if 'A' not in skip else []):
                load_w(wq[:], winv[:, :, OFF_AQ + c * 128:OFF_AQ + (c + 1) * 128])
                load_w(wk[:], winv[:, :, OFF_AK + c * 128:OFF_AK + (c + 1) * 128])
                load_w(wv[:], winv[:, :, OFF_AV + c * 128:OFF_AV + (c + 1) * 128])
                proj_fm(lambda tb, ts_, pv: fw.activation(qT[:, ts_], pv, AF.Identity, scale=0.125),
                        lambda kc: wq[:, kc, :], 128)
                proj_fm(lambda tb, ts_, pv: fw.copy(dve, kT[:, ts_], pv), lambda kc: wk[:, kc, :], 128)
                proj_fm(lambda tb, ts_, pv: fw.copy(act, VT[:, ts_], pv), lambda kc: wv[:, kc, :], 128)

                def tok_cols(ordr, tile):
                    if ordr == 0:
                        return VT[:, tile * 128:(tile + 1) * 128]
                    if ordr == 1:
                        r, b_ = tile // 4, tile % 4
                        return VT[:, 512 * b_ + r:512 * (b_ + 1):4]
                    return VT[:, tile::16]
                for ordr in range(3):
                    for tg in range(4):
                        pb = PS[6 + (tg % 2)]
                        pb16 = pb.t.bitcast(BF16)
                        for tt_ in range(4):
                            fw.transpose(pb.v(pb16[:, tt_ * 128:(tt_ + 1) * 128]), tok_cols(ordr, tg * 4 + tt_), ident_b[:],
                                         signal=(tt_ == 3))
                        t0_ = ordr * 16 + tg * 4
                        pv4 = pb16[:, 0:512].rearrange("p (n c) -> p n c", n=4)
                        ev_ = act if tg % 2 else dve
                        fw.copy(ev_, VA[:, t0_:t0_ + 4, 0, 0:64], pb.v(pv4[:, :, 0:64]))
                        fw.copy(ev_, VA[:, t0_:t0_ + 4, 1, 64:128], pb.v(pv4[:, :, 64:128]))
                if c == 0:
                    dump(f"qTA{l}", qT[:], [128, S], BF16)
                    dump(f"kTA{l}", kT[:], [128, S], BF16)
                    dump(f"VA{l}", VA[:], [128, 48, 2, 128], BF16)
                for hh in range(2):
                    h = 2 * c + hh
                    ro = 64 * hh
                    started = [False] * 4

                    unitsA = []

                    def sunit(subs, bm_off, w):
                        i = blk[0]
                        blk[0] += 1
                        ss = PS[4 + i % 4]
                        tsb = TS[i % 4]
                        pt = PT[i % 4]
                        offs = []
                        off = 0
                        for s_ in subs:
                            offs.append(off)
                            off += s_[2]
                        tot = off
                        nfull = sum(1 for s_ in subs if s_[2] == w)

                        def st1():
                            for (kcols, qcols, nq, pvl), o_ in zip(subs, offs):
                                fw.mm(ss[:, o_:o_ + nq], kcols, qcols, start=True, stop=True)
                            bmv = BM.v(bass.AP(BM.t, h * 640 + bm_off, [[6 * 640, 128], [0, nfull], [1, w]]))
                            fw.tt(dve, tsb.v(tsb.t[:, 0:nfull * w].rearrange("p (n q) -> p n q", n=nfull)),
                                  ss.v(ss.t[:, 0:nfull * w].rearrange("p (n q) -> p n q", n=nfull)), bmv, ALU.add)
                            if nfull < len(subs):
                                nql = subs[-1][2]
                                fw.tt(dve, tsb[:, nfull * w:nfull * w + nql], ss[:, nfull * w:nfull * w + nql],
                                      BM[:, h, bm_off:bm_off + nql], ALU.add)
                            fw.activation(pt[:, 0:tot], tsb[:, 0:tot], AF.Exp)

                        def st2():
                            for (kcols, qcols, nq, pvl), o_ in zip(subs, offs):
                                for (qs, vtile, tbk, ocols) in pvl:
                                    fw.mm(ocols, VA[:, vtile, hh, :], pt[:, o_ + qs.start:o_ + qs.stop], start=(not started[tbk]),
                                          stop=False, signal=True, skip_group_check=True)
                                    started[tbk] = True
                        unitsA.append((st1, st2))

                    def p1_sub(kt):
                        nqt = 2 if kt < 15 else 1
                        pvl = []
                        for j in range(nqt):
                            qt = kt + j
                            pvl.append((slice(j * 128, (j + 1) * 128), 0 * 16 + kt, qt // 4,
                                        PS[qt // 4][:, (qt % 4) * 128:(qt % 4 + 1) * 128]))
                        return (kT[ro:ro + 64, kt * 128:(kt + 1) * 128], qT[ro:ro + 64, kt * 128:kt * 128 + nqt * 128], nqt * 128, pvl)

                    def p2_sub(r, b_):
                        nqt = 2 if b_ < 3 else 1
                        pvl = []
                        for j in range(nqt):
                            pvl.append((slice(j * 128, (j + 1) * 128), 16 + r * 4 + b_, b_ + j, PS[b_ + j][:, r::4]))
                        return (kT[ro:ro + 64, 512 * b_ + r:512 * (b_ + 1):4], qT[ro:ro + 64, 512 * b_ + r:512 * (b_ + nqt):4],
                                nqt * 128, pvl)

                    def p3_sub(r):
                        pvl = []
                        for tbk in range(4):
                            pvl.append((slice(tbk * 32, (tbk + 1) * 32), 32 + r, tbk, PS[tbk][:, r::16]))
                        return (kT[ro:ro + 64, r::16], qT[ro:ro + 64, r::16], 128, pvl)

                    for kt in range(0, 16, 2):
                        sunit([p1_sub(kt), p1_sub(kt + 1)], 0, 256)
                    for r in range(4):
                        for b_ in (0, 2):
                            sunit([p2_sub(r, b_), p2_sub(r, b_ + 1)], 256, 256)
                    for r in range(0, 16, 4):
                        sunit([p3_sub(r + j) for j in range(4)], 512, 128)
                    run_pipe(unitsA)
                    for tb in range(4):
                        finalize_head(PS[tb], ro, mixT, c, tb, fin)
            dump(f"mixA{l}", mixT[:, 0:3, :], [128, 3, S], BF16)
            fw.barrier()

            MARKS.append(('B', l, pe.ninst))
            AX.reset()
            qlatT = AX.alloc(fw, "qlatT", [128, 2, S], BF16)
            kvlatT = AX.alloc(fw, "kvlatT", [128, S], BF16)
            sqb = [AX.alloc(fw, "sqb0", [128, 2, 512], BF16)] * 2
            rsq = AX.alloc(fw, "rsq", [128, S], F32)
            rskv = AX.alloc(fw, "rskv", [128, S], F32)
            rstok = AX.alloc(fw, "rstok", [128, 16], F32)
            qTh = AX.alloc(fw, "qTh", [128, S], BF16)
            KTh = AX.alloc(fw, "KTh", [128, S], BF16)
            VBs = [AX.alloc(fw, f"VB{i}", [128, 16, 128], BF16) for i in range(2)]
            PTB = [AX.alloc(fw, f"ptb{i}", [128, 512], BF16) for i in range(4)]
            TSB = [AX.alloc(fw, f"tsb{i}", [128, 128], F32) for i in range(2)]
            RT = [AX.alloc(fw, f"rt{i}", [128, 512], F32) for i in range(3)]
            fin = (AX.alloc(fw, "fsq", [128, 512], BF16), AX.alloc(fw, "fln", [128, 512], F32), AX.alloc(fw, "frs", [128, 512], F32))
            AW.reset()
            winB = AW.alloc(fw, "winB", [128, 8, 416], BF16)
            wkrr = AW.alloc(fw, "wkrr", [128, 8, 32], BF16)
            wuq_f = AW.alloc(fw, "wuqf", [128, 2, 384], F32)
            wuq_b = AW.alloc(fw, "wuqb", [128, 2, 384], BF16)
            wuq_r = AW.alloc(fw, "wuqr", [128, 2, 4, 32], BF16)
            wukv_f = AW.alloc(fw, "wukvf", [128, 512], F32)
            wukv_b = AW.alloc(fw, "wukvb", [128, 512], BF16)
            fw.memset(pool, VBs[0][:, :, 64:128], 1.0)
            fw.memset(pool, VBs[1][:, :, 0:64], 1.0)
            load_w(winB[:], winv[:, :, OFF_BQ:OFF_BQ + 416])
            pool.dma_group([(wkrr[:, :, 0:16], winv[:, :, OFF_BKR + 16:OFF_BKR + 32]),
                            (wkrr[:, :, 16:32], winv[:, :, OFF_BKR:OFF_BKR + 16])], max_dma_last_dim=4096)
            fw.activation(wkrr[:, :, 0:16], wkrr[:, :, 0:16], AF.Identity, scale=-1.0)
            sp.dma(wuq_f[:], wuq_d[l].rearrange("(k p) n -> p k n", p=128))
            sp.dma(wukv_f[:], wukv_d[l])
            for kc in range(2):
                fw.activation(wuq_b[:, kc, :], wuq_f[:, kc, :], AF.Identity, scale=MQN[:, l, kc:kc + 1])
            fw.activation(wukv_b[:], wukv_f[:], AF.Identity, scale=MKVN[:, l, 0:1])
            wq4 = wuq_b.t[:, :, :].rearrange("p k (h e) -> p k h e", h=4)
            fw.activation(wuq_r.v(wuq_r.t[:, :, :, 0:16]), wuq_b.v(wq4[:, :, :, 80:96]), AF.Identity, scale=-1.0)
            fw.copy(dve, wuq_r.v(wuq_r.t[:, :, :, 16:32]), wuq_b.v(wq4[:, :, :, 64:80]))

            for c2 in range(2):
                proj_fm(lambda tb, ts_, pv, c2=c2: fw.copy(dve, qlatT[:, c2, ts_], pv),
                        lambda kc, c2=c2: winB[:, kc, c2 * 128:(c2 + 1) * 128], 128)
            for tb in range(4):
                ts_ = slice(tb * 512, (tb + 1) * 512)
                fw.activation(sqb[tb % 2][:], qlatT[:, :, ts_], AF.Square)
                pb = PS[4 + tb % 2]
                for c2 in range(2):
                    fw.mm(pb[:], ones_b[:], sqb[tb % 2][:, c2, :], start=(c2 == 0), stop=(c2 == 1))
                fw.activation(RT[0][:], pb[:], AF.Ln, scale=1.0 / 256, bias=EPSC[:, 0:1])
                fw.activation(rsq[:, ts_], RT[0][:], AF.Exp, scale=-0.5)
            proj_fm(lambda tb, ts_, pv: fw.copy(dve, kvlatT[:, ts_], pv), lambda kc: winB[:, kc, 256:384], 128)
            for tb in range(4):
                ts_ = slice(tb * 512, (tb + 1) * 512)
                fw.activation(sqb[tb % 2][:, 0, :], kvlatT[:, ts_], AF.Square)
                pb = PS[4 + tb % 2]
                fw.mm(pb[:], ones_b[:], sqb[tb % 2][:, 0, :], start=True, stop=True)
                fw.activation(RT[0][:], pb[:], AF.Ln, scale=1.0 / 128, bias=EPSC[:, 0:1])
                fw.activation(rskv[:, ts_], RT[0][:], AF.Exp, scale=-0.5)
            identb4 = ident_f.v(bass.AP(ident_f.t, 0, [[128, 128], [0, 4], [1, 128]]))
            for g4 in range(4):
                tdv = RT[0].v(RT[0].t[:, :].rearrange("p (n q) -> p n q", n=4))
                fw.tt(dve, tdv, rskv.v(rskv.t[:, g4 * 512:(g4 + 1) * 512].rearrange("p (n q) -> p n q", n=4)), identb4, ALU.mult)
                _o = rstok.t[:, g4 * 4:(g4 + 1) * 4]
                _i = RT[0].t[:, :].rearrange("p (n q) -> p n q", n=4)
                dve.issue(lambda e, _o=_o, _i=_i: e.tensor_reduce(out=_o, in_=_i, axis=AX_X, op=ALU.add),
                          reads=[RT[0][:]], writes=[rstok[:]])
            for tb in range(4):
                ts_ = slice(tb * 512, (tb + 1) * 512)
                p1, p2 = (PS[6], PS[7]) if tb % 2 else (PS[4], PS[5])
                for kc in range(8):
                    fw.mm(p1[64:96, :], winB[:, kc, 384:416], hT[:, kc, ts_], start=(kc == 0), stop=(kc == 7))
                for kc in range(8):
                    fw.mm(p2[64:96, :], wkrr[:, kc, :], hT[:, kc, ts_], start=(kc == 0), stop=(kc == 7))
                fw.tt(dve, RT[0][64:96, :], p1[64:96, :], FCOS[64:96, ts_], ALU.mult)
                fw.tt(dve, RT[1][64:96, :], p2[64:96, :], FSIN[64:96, ts_], ALU.mult)
                fw.tt(pool, KTh[64:96, ts_], RT[0][64:96, :], RT[1][64:96, :], ALU.add)
            dump(f"rsq{l}", rsq[:], [128, S], F32)
            SCB = float((64 + 32) ** -0.5)
            BSETS = [(PS[4], PS[5], PS[5]), (PS[6], PS[7], PS[7])]
            for h in (range(4) if 'B' not in skip else []):
                hh = h % 2
                nr = 64 * hh
                for tb in range(4):
                    ts_ = slice(tb * 512, (tb + 1) * 512)
                    p1, p2, p3 = BSETS[tb % 2]
                    for kc in range(2):
                        fw.mm(p1[0:96, :], wuq_b[:, kc, h * 96:(h + 1) * 96], qlatT[:, kc, ts_], start=(kc == 0), stop=(kc == 1))
                    for kc in range(2):
                        fw.mm(p2[64:96, :], wuq_r.v(wuq_r.t[:, kc, h, :]), qlatT[:, kc, ts_], start=(kc == 0), stop=(kc == 1))
                    fw.stt(qTh[0:64, ts_], p1[0:64, :], SCB, rsq[0:64, ts_], ALU.mult, ALU.mult)
                    fw.tt(dve, RT[0][64:96, :], p1[64:96, :], FCOS[64:96, ts_], ALU.mult)
                    fw.tt(dve, RT[1][64:96, :], p2[64:96, :], FSIN[64:96, ts_], ALU.mult)
                    fw.tt(pool, RT[2][64:96, :], RT[0][64:96, :], RT[1][64:96, :], ALU.add)
                    fw.stt(qTh[64:96, ts_], RT[2][64:96, :], SCB, rsq[64:96, ts_], ALU.mult, ALU.mult)
                    fw.mm(p3[0:64, :], wukv_b[:, h * 128:h * 128 + 64], kvlatT[:, ts_], start=True, stop=True, skip_group_check=True)
                    fw.tt(dve, KTh[0:64, ts_], p3[0:64, :], rskv[0:64, ts_], ALU.mult)
                for tg in range(2):
                    pb = PS[4 + tg]
                    for t8 in range(8):
                        tile = tg * 8 + t8
                        fw.mm(pb[:, t8 * 64:(t8 + 1) * 64], kvlatT[:, tile * 128:(tile + 1) * 128],
                              wukv_b[:, h * 128 + 64:(h + 1) * 128], start=True, stop=True, signal=(t8 == 7))
                    for t8 in range(8):
                        tile = tg * 8 + t8
                        fw.activation(VBs[hh][:, tile, 64 * hh:64 * hh + 64], pb[:, t8 * 64:(t8 + 1) * 64], AF.Identity,
                                      scale=rstok[:, tile:tile + 1])
                if h == 0:
                    dump(f"qTh{l}", qTh[0:96, :], [96, S], BF16)
                    dump(f"KTh{l}", KTh[0:96, :], [96, S], BF16)
                    dump(f"VB{l}", VBs[0][:], [128, 16, 128], BF16)
                started = [False] * 4
                unitsB = []
                bi = 0
                for kt in range(16):
                    for tb in range(kt // 4, 4):
                        q0 = max(128 * kt, 512 * tb)
                        q1 = 512 * (tb + 1)

                        def mk(kt=kt, tb=tb, q0=q0, q1=q1, bi=bi):
                            nq = q1 - q0
                            sbk = PS[4 + bi % 4]
                            pt = PTB[bi % 4]

                            def st1():
                                fw.mm(sbk[:, 0:nq], KTh[0:96, kt * 128:(kt + 1) * 128], qTh[0:96, q0:q1], start=True, stop=True)
                                if q0 == 128 * kt:
                                    tsb = TSB[kt % 2]
                                    fw.tt(dve, tsb[:], sbk[:, 0:128], CM[:], ALU.add)
                                    fw.activation(pt[:, 0:128], tsb[:], AF.Exp)
                                    if nq > 128:
                                        fw.activation(pt[:, 128:nq], sbk[:, 128:nq], AF.Exp)
                                else:
                                    fw.activation(pt[:, 0:nq], sbk[:, 0:nq], AF.Exp)

                            def st2():
                                fw.mm(PS[tb][:, q0 - 512 * tb:q1 - 512 * tb], VBs[hh][:, kt, :], pt[:, 0:nq], start=(not started[tb]),
                                      stop=False, signal=True, skip_group_check=True)
                                started[tb] = True
                            return (st1, st2)
                        unitsB.append(mk())
                        bi += 1
                run_pipe(unitsB)
                for tb in range(4):
                    finalize_head(PS[tb], nr, mixT, 3 + h // 2, tb, fin)
            dump(f"mixB{l}", mixT[:, 3:5, :], [128, 2, S], BF16)
            fw.barrier()

            MARKS.append(('C', l, pe.ninst))
            AX.reset()
            qTc = AX.alloc(fw, "qTc", [128, S], BF16)
            kTc = AX.alloc(fw, "kTc", [128, S], BF16)
            qxT = AX.alloc(fw, "qxT", [128, S], BF16)
            kz = AX.alloc(fw, "kz", [128, 16, 64], BF16)
            VC = AX.alloc(fw, "VC", [128, 16, 128], BF16)
            gT = AX.alloc(fw, "gT", [128, S], BF16)
            STf = AX.alloc(fw, "stf", [64, 16, 64], F32)
            STb = AX.alloc(fw, "stb", [64, 16, 64], BF16)
            PTC = [AX.alloc(fw, f"ptc{i}", [128, 128], BF16) for i in range(4)]
            RT = [AX.alloc(fw, f"rtc{i}", [128, 512], F32) for i in range(3)]
            CP = AX.alloc(fw, "ccp", [128, 512], BF16)
            CSQ = AX.alloc(fw, "csq", [128, 512], BF16)
            CMEAN = AX.alloc(fw, "cmean", [128, 512], F32)
            CMSQ = AX.alloc(fw, "cmsq", [128, 512], F32)
            CDV = AX.alloc(fw, "cdv", [128, 512], F32)
            CVAR = AX.alloc(fw, "cvar", [128, 512], F32)
            CRS = AX.alloc(fw, "crs", [128, 512], F32)
            AW.reset()
            wo = AW.alloc(fw, "wo", [128, 8, D], BF16)
            wov = wout_d[l].rearrange("(k p) n -> p k n", p=128)
            wcq = AW.alloc(fw, "wcq", [128, 8, 64], BF16)
            wcqr = AW.alloc(fw, "wcqr", [128, 8, 2, 32], BF16)
            wck = AW.alloc(fw, "wck", [128, 8, 64], BF16)
            wckr = AW.alloc(fw, "wckr", [128, 8, 2, 32], BF16)
            wcv = AW.alloc(fw, "wcv", [128, 8, 128], BF16)
            wcg = AW.alloc(fw, "wcg", [128, 8, 128], BF16)
            SC_ = [sub(PS[4], "sc0"), sub(PS[4], "sc1"), sub(PS[5], "sc2"), sub(PS[5], "sc3")]
            for c in (range(3) if 'C' not in skip else []):
                def load_rot(dst, off):
                    prs = []
                    for h2 in range(2):
                        prs.append((dst.v(dst.t[:, :, h2, 0:16]), winv[:, :, off + h2 * 32 + 16:off + h2 * 32 + 32]))
                        prs.append((dst.v(dst.t[:, :, h2, 16:32]), winv[:, :, off + h2 * 32:off + h2 * 32 + 16]))
                    pool.dma_group(prs, max_dma_last_dim=4096)
                    fw.activation(dst.v(dst.t[:, :, :, 0:16]), dst.v(dst.t[:, :, :, 0:16]), AF.Identity, scale=-1.0)
                load_w(wcq[:], winv[:, :, OFF_CQ + c * 64:OFF_CQ + (c + 1) * 64])
                load_rot(wcqr, OFF_CQ + c * 64)
                load_w(wck[:], winv[:, :, OFF_CK + c * 64:OFF_CK + (c + 1) * 64])
                load_rot(wckr, OFF_CK + c * 64)
                load_w(wcv[:], winv[:, :, OFF_CV + c * 128:OFF_CV + (c + 1) * 128])
                load_w(wcg[:], winv[:, :, OFF_CG + c * 128:OFF_CG + (c + 1) * 128])
                if c == 0:
                    load_w(wo[:], wov)
                for (w_, wr_, dst, isq) in [(wcq, wcqr, qTc, True), (wck, wckr, kTc, False)]:
                    for tb in range(4):
                        ts_ = slice(tb * 512, (tb + 1) * 512)
                        p1, p2 = (PS[6], PS[7]) if tb % 2 else (PS[4], PS[5])
                        for kc in range(8):
                            fw.mm(p1[0:64, :], w_[:, kc, :], hT[:, kc, ts_], start=(kc == 0), stop=(kc == 7))
                        for kc in range(8):
                            fw.mm(p2[0:64, :], wr_.v(wr_.t[:, kc, :, :].rearrange("p h e -> p (h e)")), hT[:, kc, ts_],
                                  start=(kc == 0), stop=(kc == 7))
                        fw.tt(dve, RT[0][0:64, :], p1[0:64, :], FCOS[0:64, ts_], ALU.mult)
                        fw.tt(dve, RT[1][0:64, :], p2[0:64, :], FSIN[0:64, ts_], ALU.mult)
                        if isq:
                            fw.tt(pool, RT[2][0:64, :], RT[0][0:64, :], RT[1][0:64, :], ALU.add)
                            fw.copy(act, dst[0:64, ts_], RT[2][0:64, :])
                            xib = XI.v(bass.AP(XI.t, c * 128, [[3 * 128, 64], [0, 4], [1, 128]]))
                            fw.tt(pool, qxT.v(qxT.t[0:64, ts_].rearrange("p (n q) -> p n q", n=4)),
                                  RT[2].v(RT[2].t[0:64, :].rearrange("p (n q) -> p n q", n=4)), xib, ALU.mult)
                        else:
                            fw.tt(pool, dst[0:64, ts_], RT[0][0:64, :], RT[1][0:64, :], ALU.add)
                proj_fm(lambda tb, ts_, pv: fw.activation(gT[:, ts_], pv, AF.Silu), lambda kc: wcg[:, kc, :], 128)
                for tg in range(4):
                    pb = PS[6 + tg % 2]
                    for t4 in range(4):
                        tile = tg * 4 + t4
                        for kc in range(8):
                            fw.mm(pb[:, t4 * 128:(t4 + 1) * 128], hT[:, kc, tile * 128:(tile + 1) * 128], wcv[:, kc, :],
                                  start=(kc == 0), stop=(kc == 7), signal=(kc == 7 and t4 == 3))
                    fw.copy(act if tg % 2 else dve, VC.v(VC.t[:, tg * 4:tg * 4 + 4, :].rearrange("p n c -> p (n c)")), pb[:])
                for tg in range(2):
                    pb = PS[6 + tg]
                    tb16 = pb.t.bitcast(BF16)
                    for t8 in range(8):
                        tile = tg * 8 + t8
                        fw.transpose(pb.v(tb16[:, t8 * 64:(t8 + 1) * 64]), kTc[0:64, tile * 128:(tile + 1) * 128],
                                     ident_b[0:64, 0:64], signal=(t8 == 7))
                    zb = ZETA.v(bass.AP(ZETA.t, c * 64, [[3 * 64, 128], [0, 8], [1, 64]]))
                    fw.tt(dve, kz[:, tg * 8:(tg + 1) * 8, :],
                          pb.v(tb16[:, 0:512].rearrange("p (n c) -> p n c", n=8)), zb, ALU.mult)
                for hh in range(2):
                    for n in range(15):
                        pu = PS[6 + (n // 8)]
                        fw.mm(pu[32 * hh:32 * hh + 32, (n % 8) * 64:(n % 8 + 1) * 64], kz[:, n, 32 * hh:32 * hh + 32],
                              VC[:, n, 64 * hh:64 * hh + 64], start=True, stop=True, signal=(n in (7, 14)))
                fw.copy(dve, STf[:, 1, :], PS[6][0:64, 0:64])
                for n in range(1, 15):
                    pu = PS[6 + (n // 8)]
                    fw.stt(STf[:, n + 1, :], STf[:, n, :], GAM[:, c:c + 1], pu[0:64, (n % 8) * 64:(n % 8 + 1) * 64], ALU.mult, ALU.add)
                fw.copy(act, STb[:, 1:16, :], STf[:, 1:16, :])
                if c == 0:
                    dump(f"qTc{l}", qTc[0:64, :], [64, S], BF16)
                    dump(f"kTc{l}", kTc[0:64, :], [64, S], BF16)
                    dump(f"qxT{l}", qxT[0:64, :], [64, S], BF16)
                    dump(f"kz{l}", kz[:], [128, 16, 64], BF16)
                    dump(f"VC{l}", VC[:], [128, 16, 128], BF16)
                    dump(f"STf{l}", STf[:, 1:16, :], [64, 15, 64], F32)
                    dump(f"gT{l}", gT[:], [128, S], BF16)
                unitsC = []
                bi = 0
                for n in range(16):
                    for hh in range(2):
                        def mk(n=n, hh=hh, bi=bi):
                            h = 2 * c + hh
                            ss = PS[4 + bi % 4]
                            ssv = ss[:, 0:128]
                            pt = PTC[bi % 4]
                            cs = slice(n * 128, (n + 1) * 128)
                            ob = PS[n // 4][64 * hh:64 * hh + 64, (n % 4) * 128:(n % 4 + 1) * 128]

                            def st1():
                                fw.mm(ssv, kTc[32 * hh:32 * hh + 32, cs], qTc[32 * hh:32 * hh + 32, cs], start=True, stop=True)
                                fw.tt(dve, pt[:], ssv, DECT[:, h, :], ALU.mult)

                            def st2():
                                fw.mm(ob, VC[:, n, 64 * hh:64 * hh + 64], pt[:], start=True, stop=(n == 0), signal=True, skip_group_check=True)
                                if n > 0:
                                    fw.mm(ob, STb[32 * hh:32 * hh + 32, n, :], qxT[32 * hh:32 * hh + 32, cs], start=False, stop=True,
                                          signal=True, skip_group_check=True)
                            return (st1, st2)
                        unitsC.append(mk())
                        bi += 1
                run_pipe(unitsC)
                for tb in range(4):
                    ts_ = slice(tb * 512, (tb + 1) * 512)
                    ob = PS[tb]
                    fw.copy(act, CP[:], ob[:])
                    fw.activation(CSQ[:], ob[:], AF.Square)
                    pm, p2_ = PS[6], PS[7]
                    fw.mm(pm[:], BD[:], CP[:], start=True, stop=True)
                    fw.mm(p2_[:], BD[:], CSQ[:], start=True, stop=True)
                    fw.copy(act, CMEAN[:], pm[:])
                    fw.activation(CMSQ[:], pm[:], AF.Square)
                    fw.tt(dve, CDV[:], ob[:], CMEAN[:], ALU.subtract)
                    fw.tt(dve, CVAR[:], p2_[:], CMSQ[:], ALU.subtract)
                    fw.ts(dve, CVAR[:], CVAR[:], 0.0, ALU.max)
                    fw.activation(CVAR[:], CVAR[:], AF.Ln, bias=EPSC[:, 0:1])
                    fw.activation(CRS[:], CVAR[:], AF.Exp, scale=-0.5)
                    fw.tt(dve, CDV[:], CDV[:], CRS[:], ALU.mult)
                    fw.tt(pool, mixT[:, 5 + c, ts_], CDV[:], gT[:, ts_], ALU.mult)
            for kc in range(8):
                fw.activation(wo[:, kc, :], wo[:, kc, :], AF.Identity, scale=MG[:, l, kc:kc + 1])
            dump(f"mixC{l}", mixT[:, 5:8, :], [128, 3, S], BF16)
            fw.barrier()

            MARKS.append(('wout', l, pe.ninst))
            AX.reset()
            xT = AX.alloc(fw, "xT", [128, 8, S], F32)
            sp.dma_group([(xT[:, kc, :], xsp_d[:, kc, :]) for kc in range(8)])
            for d_ in range(8):
                for tb in range(4):
                    ts_ = slice(tb * 512, (tb + 1) * 512)
                    pb = PS[(d_ * 4 + tb) % 8]
                    for kc in range(8):
                        fw.mm(pb[:], wo[:, kc, d_ * 128:(d_ + 1) * 128], mixT[:, kc, ts_], start=(kc == 0), stop=(kc == 7))
                    fw.stt(xT[:, d_, ts_], pb[:], MOD[:, l, 16 + d_:17 + d_], xT[:, d_, ts_], ALU.mult, ALU.add)
            dump(f"xmid{l}", xT[:], [128, 8, S], F32)
            fw.barrier()

            MARKS.append(('ffn', l, pe.ninst))
            AY.reset()
            norm_to_hT(l, lambda kc: A2[:, l, kc:kc + 1], 24, AY)
            fw.barrier()
            AY.reset()
            actT = AY.alloc(fw, "actT", [128, 6, S], BF16)
            sgt = [AY.alloc(fw, f"sg{i}", [128, 512], BF16) for i in range(2)]
            AW.reset()
            wdn = [AW.alloc(fw, f"wdn{i}", [128, 6, D], BF16) for i in range(2)]
            gu = [AW.alloc(fw, f"gu{i}", [128, 2, 8, 128], BF16) for i in range(3)]
            wgv = wg_d[l].rearrange("(k p) n -> p k n", p=128)
            wuv = wu_d[l].rearrange("(k p) n -> p k n", p=128)
            ji = 0
            modgen = mod_pieces(l + 1) if l + 1 < depth else iter(())
            for gi, (j0, nj) in enumerate(FFN_GROUPS):
                wdb = wdn[gi % 2]
                load_w(wdb[:, 0:nj, :], wd_d[l, j0 * 128:(j0 + nj) * 128, :].rearrange("(j p) n -> p j n", p=128))
                for jj in range(nj):
                    j = j0 + jj
                    g = gu[ji % 3]
                    ji += 1
                    pool.dma_group([(g[:, 0, :, :], wgv[:, :, j * 128:(j + 1) * 128]),
                                    (g[:, 1, :, :], wuv[:, :, j * 128:(j + 1) * 128])], max_dma_last_dim=4096)
                    for tb in range(4):
                        ts_ = slice(tb * 512, (tb + 1) * 512)
                        pg, pu = PS[(tb % 2) * 2], PS[(tb % 2) * 2 + 1]
                        for kc in range(8):
                            fw.mm(pg[:], g[:, 0, kc, :], hT[:, kc, ts_], start=(kc == 0), stop=(kc == 7))
                        for kc in range(8):
                            fw.mm(pu[:], g[:, 1, kc, :], hT[:, kc, ts_], start=(kc == 0), stop=(kc == 7))
                        fw.activation(sgt[tb % 2][:], pg[:], AF.Silu)
                        fw.tt(dve, actT[:, jj, ts_], pu[:], sgt[tb % 2][:], ALU.mult)
                    next(modgen, None)
                for d_ in range(8):
                    for tb in range(4):
                        ts_ = slice(tb * 512, (tb + 1) * 512)
                        pb = PS[4 + (d_ * 4 + tb) % 3]
                        for jj in range(nj):
                            fw.mm(pb[:], wdb[:, jj, d_ * 128:(d_ + 1) * 128], actT[:, jj, ts_], start=(jj == 0), stop=(jj == nj - 1))
                        fw.stt(xT[:, d_, ts_], pb[:], MOD[:, l, 40 + d_:41 + d_], xT[:, d_, ts_], ALU.mult, ALU.add)
            for _ in modgen:
                pass
            dump(f"xout{l}", xT[:], [128, 8, S], F32)
            fw.barrier()

        MARKS.append(('final', depth, pe.ninst))
        AY.reset()
        sq = [AY.alloc(fw, f"fsq{i}", [128, 8, 512], BF16) for i in range(2)]
        lnt = AY.alloc(fw, "fln", [128, 512], F32)
        rs = AY.alloc(fw, "frs", [128, 512], F32)
        yt = [AY.alloc(fw, f"fyt{i}", [128, 512], F32) for i in range(3)]
        AW.reset()
        OUT = [AW.alloc(fw, f"oo{i}", [128, 4, D], F32) for i in range(2)]
        stores = []
        for tb in (range(4) if 'final' not in skip else []):
            ts_ = slice(tb * 512, (tb + 1) * 512)
            fw.activation(sq[tb % 2][:], xT[:, :, ts_], AF.Square)
            pb = PS[tb % 2]
            for kc in range(8):
                fw.mm(pb[:], ones_b[:], sq[tb % 2][:, kc, :], start=(kc == 0), stop=(kc == 7))
            fw.activation(lnt[:], pb[:], AF.Ln, scale=1.0 / D, bias=EPSC[:, 0:1])
            fw.activation(rs[:], lnt[:], AF.Exp, scale=-0.5)
            ob_ = OUT[tb % 2]
            for kc in range(8):
                t = yt[kc % 3]
                fw.stt(t[:], xT[:, kc, ts_], FNG[:, kc:kc + 1], rs[:], ALU.mult, ALU.mult)
                pt_ = PS[2 + kc % 4]
                for q in range(4):
                    fw.transpose(pt_[:, q * 128:(q + 1) * 128], t[:, q * 128:(q + 1) * 128], ident_f[:], signal=(q == 3))
                fw.copy(act if kc % 2 else dve, ob_[:, :, kc * 128:(kc + 1) * 128],
                        pt_.v(pt_.t[:, :].rearrange("p (q c) -> p q c", q=4)))
            stores.append(sp.dma_group([(y_d[(tb * 4 + q) * 128:(tb * 4 + q + 1) * 128, :], ob_[:, q, :]) for q in range(4)]))
        for tok in stores:
            sp.wait_tok(tok)
        for tok in dbg_out.values():
            sp.wait_tok(tok)
        fw.emit()
        info = {n: (e.ninst, e.nwait) for n, e in fw.E.items()}
        info["sp"] = (fw.sp.ninst, fw.sp.nwait)
        info["sems"] = fw.nsem
        print("MK build:", info)
    return nc


AX_X = mybir.AxisListType.X
MARKS = []
_NC_CACHE = {}


def prep_inputs(inputs, b):
    f32 = np.float32
    g = lambda k: np.asarray(inputs[k])
    hc = _HC_CACHE.get("hc")
    if hc is None:
        hc = host_consts(g("rel_bias").astype(f32))
        _HC_CACHE["hc"] = hc
    m = dict(hc)
    m["x"] = np.ascontiguousarray(g("x")[b].astype(f32))
    m["cT"] = np.ascontiguousarray(g("c")[b].astype(f32).reshape(8, 128).T)
    m["posb"] = np.ascontiguousarray(g("positions")[b].astype(np.int32).reshape(1, S))
    sh = _HC_CACHE.get("shared")
    if sh is None:
        def fm(a, nch):
            a = np.asarray(a, f32)
            return np.ascontiguousarray(a.reshape(a.shape[0], nch, 128).transpose(2, 0, 1))
        sh = dict(
            ada_w=np.ascontiguousarray(g("ada_w").astype(f32)),
            adabT=fm(g("ada_b"), 48), n1gT=fm(g("norm1_g"), 8), n2gT=fm(g("norm2_g"), 8),
            fngT=np.ascontiguousarray(g("final_norm").astype(f32).reshape(8, 128).T),
            mgT=fm(g("mix_gain"), 8), mqnT=fm(g("mla_q_norm"), 2), mkvnT=fm(g("mla_kv_norm"), 1),
            w_in=np.ascontiguousarray(g("w_in").astype(f32)), w_uq=np.ascontiguousarray(g("mla_w_uq").astype(f32)),
            w_ukv=np.ascontiguousarray(g("mla_w_ukv").astype(f32)), w_out=np.ascontiguousarray(g("w_out").astype(f32)),
            w_gate=np.ascontiguousarray(g("ffn_w_gate").astype(f32)), w_up=np.ascontiguousarray(g("ffn_w_up").astype(f32)),
            w_down=np.ascontiguousarray(g("ffn_w_down").astype(f32)),
        )
        _HC_CACHE["shared"] = sh
    m.update(sh)
    return m


_HC_CACHE = {}


def kernel(**inputs):
    _HC_CACHE.clear()
    nc = build()
    in_maps = [prep_inputs(inputs, b) for b in range(NCORES)]
    res = run_bass_kernel_spmd(nc, in_maps, core_ids=list(range(NCORES)))
    out = np.stack([np.asarray(r["y"], dtype=np.float32) for r in res.results], axis=0)
    return out
```

```python
import numpy as np
import concourse.bass as bass
import concourse.mybir as mybir
from concourse.bass_utils import run_bass_kernel_spmd
from contextlib import ExitStack

F32 = mybir.dt.float32
BF16 = mybir.dt.bfloat16
I32 = mybir.dt.int32
AF = mybir.ActivationFunctionType
ALU = mybir.AluOpType
AX = mybir.AxisListType

CE = ('pe', 'act', 'dve', 'pool')
SAME_ENGINE_SYNC = True


class Buf:
    __slots__ = ('t', 'name', 'w', 'r', 'semname')

    def __init__(self, t, name, semname=None):
        self.t = t
        self.name = name
        self.w = None
        self.r = {}
        self.semname = semname or name

    def __getitem__(self, idx):
        return V(self, self.t[idx])

    def v(self, ap):
        return V(self, ap)


class V:
    __slots__ = ('buf', 'ap')

    def __init__(self, buf, ap):
        self.buf = buf
        self.ap = ap


class Eng:
    def __init__(self, fw, name, sem):
        self.fw = fw
        self.name = name
        self.sem = sem
        self.count = 0
        self.seen = {}
        self.prog = []
        self.snaps = {0: {}}
        self.nwait = 0
        self.ninst = 0

    def _deps(self, reads, writes):
        deps = []
        for v in reads:
            if v.buf.w is not None:
                deps.append(v.buf.w)
        for v in writes:
            if v.buf.w is not None:
                deps.append(v.buf.w)
            deps.extend(v.buf.r.values())
        need = {}
        for (key, val, sem) in deps:
            if key == self.name and (self.name == 'pe' or not SAME_ENGINE_SYNC):
                continue
            if val > self.seen.get(key, 0) and val > need.get(key, (0, None))[0]:
                need[key] = (val, sem)
        for key, (val, sem) in need.items():
            self.prog.append(('wait', sem, val))
            self.nwait += 1
            self.seen[key] = val
            if key in self.fw.E and key != self.name:
                snap = self.fw.E[key].snaps.get(val)
                if snap:
                    for k2, v2 in snap.items():
                        if v2 > self.seen.get(k2, 0):
                            self.seen[k2] = v2

    def issue(self, fn, reads=(), writes=(), signal=True):
        self._deps(reads, writes)
        self.ninst += 1
        if signal:
            self.count += 1
            self.prog.append(('inst', fn, True))
            self.snaps[self.count] = {k: v for k, v in self.seen.items() if k in CE}
            tok = (self.name, self.count, self.sem)
        else:
            self.prog.append(('inst', fn, False))
            tok = (self.name, self.count + 1, self.sem)
        for v in reads:
            v.buf.r[self.name] = tok
        for v in writes:
            v.buf.w = tok
            v.buf.r = {}
        return tok

    def dma(self, out, in_, **kw):
        return self.dma_group([(out, in_)], **kw)

    def dma_group(self, pairs, **kw):
        reads = [i for (o, i) in pairs if isinstance(i, V)]
        writes = [o for (o, i) in pairs if isinstance(o, V)]
        self._deps(reads, writes)
        buf = (writes[0].buf if writes else reads[0].buf)
        rec = self.fw.dsem.get(buf.semname)
        if rec is None:
            rec = [self.fw.new_sem('d_' + buf.semname), 0]
            self.fw.dsem[buf.semname] = rec
        for (o, i) in pairs:
            rec[1] += 1
            oa = o.ap if isinstance(o, V) else o
            ia = i.ap if isinstance(i, V) else i
            self.prog.append(('dma', oa, ia, rec[0], kw))
        tok = (('d', buf.semname), 16 * rec[1], rec[0])
        for v in reads:
            v.buf.r[tok[0]] = tok
        for v in writes:
            v.buf.w = tok
            v.buf.r = {}
        return tok

    def wait_tok(self, tok):
        key, val, sem = tok
        if val > self.seen.get(key, 0):
            self.prog.append(('wait', sem, val))
            self.seen[key] = val

    def replay(self, e):
        for item in self.prog:
            if item[0] == 'wait':
                e.wait_ge(item[1], item[2])
            elif item[0] == 'inst':
                ins = item[1](e)
                if item[2]:
                    ins.then_inc(self.sem, 1)
            else:
                _, oa, ia, sem, kw = item
                e.dma_start(out=oa, in_=ia, **kw).then_inc(sem, 16)


class FW:
    def __init__(self, nc, stack):
        self.nc = nc
        self.stack = stack
        self.nsem = 0
        self.dsem = {}
        self.dma_toks = []
        self.E = {}
        for name in CE:
            self.E[name] = Eng(self, name, self.new_sem('c_' + name))
        self.sp = Eng(self, 'sp', None)
        self.pe, self.act, self.dve, self.pool = (self.E[n] for n in CE)

    def new_sem(self, name):
        self.nsem += 1
        return self.stack.enter_context(self.nc.semaphore(name))

    def sbuf(self, name, shape, dtype):
        t = self.stack.enter_context(self.nc.sbuf_tensor(name, list(shape), dtype))
        return Buf(t, name)

    def psum(self, name, shape, dtype):
        t = self.stack.enter_context(self.nc.psum_tensor(name, list(shape), dtype))
        return Buf(t, name)

    def barrier(self):
        toks = [(n, self.E[n].count, self.E[n].sem) for n in CE if self.E[n].count > 0]
        for e in list(self.E.values()) + [self.sp]:
            for tok in toks:
                if tok[0] != e.name:
                    e.wait_tok(tok)
            for tok in self.dma_toks:
                e.wait_tok(tok)
        self.dma_toks = []

    def check(self):
        engs = list(self.E.values()) + [self.sp]
        pc = {e.name: 0 for e in engs}
        sems = {}
        progress = True
        while progress:
            progress = False
            for e in engs:
                while pc[e.name] < len(e.prog):
                    it = e.prog[pc[e.name]]
                    if it[0] == 'wait':
                        if sems.get(id(it[1]), 0) < it[2]:
                            break
                    elif it[0] == 'inst':
                        if it[2]:
                            sems[id(e.sem)] = sems.get(id(e.sem), 0) + 1
                    else:
                        sems[id(it[3])] = sems.get(id(it[3]), 0) + 16
                    pc[e.name] += 1
                    progress = True
        stuck = {}
        for e in engs:
            if pc[e.name] < len(e.prog):
                it = e.prog[pc[e.name]]
                who = [n for n, x in self.E.items() if x.sem is it[1]] or [k for k, r in self.dsem.items() if r[0] is it[1]]
                stuck[e.name] = (pc[e.name], len(e.prog), who, it[2], sems.get(id(it[1]), 0))
        return stuck

    def emit(self):
        st = self.check()
        assert not st, f"DEADLOCK: {st}"
        with self.nc.Block() as block:
            @block.sync
            def _(e):
                self.sp.replay(e)

            @block.tensor
            def _(e):
                self.pe.replay(e)

            @block.scalar
            def _(e):
                self.act.replay(e)

            @block.vector
            def _(e):
                self.dve.replay(e)

            @block.gpsimd
            def _(e):
                self.pool.replay(e)

    def mm(self, out, lhsT, rhs, start=True, stop=True, signal=None, **kw):
        if signal is None:
            signal = stop
        return self.pe.issue(
            lambda e: e.matmul(out.ap, lhsT.ap, rhs.ap, start=start, stop=stop, **kw),
            reads=[lhsT, rhs], writes=[out], signal=signal)

    def transpose(self, out, in_, ident, signal=True):
        return self.pe.issue(
            lambda e: e.transpose(out.ap, in_.ap, ident.ap),
            reads=[in_, ident], writes=[out], signal=signal)

    def activation(self, out, in_, func, scale=1.0, bias=0.0, accum_out=None, eng=None):
        reads = [in_]
        sc = scale
        bi = bias
        if isinstance(scale, V):
            reads.append(scale)
            sc = scale.ap
        if isinstance(bias, V):
            reads.append(bias)
            bi = bias.ap
        writes = [out]
        kw = {}
        if accum_out is not None:
            writes.append(accum_out)
            kw['accum_out'] = accum_out.ap
        return self.act.issue(
            lambda e: e.activation(out=out.ap, in_=in_.ap, func=func, scale=sc, bias=bi, **kw),
            reads=reads, writes=writes)

    def tt(self, eng, out, in0, in1, op):
        return eng.issue(lambda e: e.tensor_tensor(out=out.ap, in0=in0.ap, in1=in1.ap, op=op),
                         reads=[in0, in1], writes=[out])

    def ts(self, eng, out, in0, s1, op0, s2=None, op1=None, accum_out=None):
        reads = [in0]
        a1 = s1
        a2 = s2
        if isinstance(s1, V):
            reads.append(s1)
            a1 = s1.ap
        if isinstance(s2, V):
            reads.append(s2)
            a2 = s2.ap
        kw = {}
        writes = [out]
        if op1 is not None:
            kw['op1'] = op1
        if accum_out is not None:
            kw['accum_out'] = accum_out.ap
            writes.append(accum_out)
        return eng.issue(lambda e: e.tensor_scalar(out=out.ap, in0=in0.ap, scalar1=a1, scalar2=a2, op0=op0, **kw),
                         reads=reads, writes=writes)

    def stt(self, out, in0, scalar, in1, op0, op1):
        reads = [in0, in1]
        a = scalar
        if isinstance(scalar, V):
            reads.append(scalar)
            a = scalar.ap
        return self.dve.issue(
            lambda e: e.scalar_tensor_tensor(out=out.ap, in0=in0.ap, scalar=a, in1=in1.ap, op0=op0, op1=op1),
            reads=reads, writes=[out])

    def copy(self, eng, out, in_):
        if eng is self.act:
            return eng.issue(lambda e: e.copy(out=out.ap, in_=in_.ap), reads=[in_], writes=[out])
        return eng.issue(lambda e: e.tensor_copy(out=out.ap, in_=in_.ap), reads=[in_], writes=[out])

    def memset(self, eng, out, val):
        return eng.issue(lambda e: e.memset(out.ap, val), writes=[out])

D = 1024
S = 2048
DEPTH = 4
NCORES = 8
IN_W = 2720
FH = 2816
OFF_AQ, OFF_AK, OFF_AV = 0, 384, 768
OFF_BQ, OFF_BKV, OFF_BKR = 1152, 1408, 1536
OFF_CQ, OFF_CK, OFF_CV, OFF_CG = 1568, 1760, 1952, 2336
EPS = 1e-6
NEG = -30000.0
SLAB = 209984
FFN_GROUPS = [(0, 6), (6, 6), (12, 5), (17, 5)]


def _t5_bucket(dist):
    n_buckets, max_distance = 32, 2048
    max_exact = n_buckets // 2
    safe = np.maximum(dist, 1).astype(np.float32)
    large = max_exact + (np.log(safe / max_exact) / np.log(max_distance / max_exact) * (n_buckets - max_exact)).astype(np.int32)
    large = np.minimum(large, n_buckets - 1)
    return np.where(dist < max_exact, dist, large).astype(np.int32)


def host_consts(rel_bias):
    f32 = np.float32
    k = np.arange(128)[:, None]
    ql = np.arange(256)[None, :]
    q3 = np.arange(128)[None, :]
    d1 = ql - k
    v1 = (d1 >= 0) & (d1 <= 128)
    i1 = _t5_bucket(np.clip(d1, 0, 128))
    v2 = v1
    i2 = _t5_bucket(np.clip(4 * d1, 0, 512))
    d3 = q3 - k
    v3 = d3 >= 0
    i3 = _t5_bucket(np.clip(16 * d3, 0, 2048))
    bmb = np.zeros((128, 6, 640), f32)
    for h in range(6):
        bmb[:, h, 0:256] = np.where(v1, rel_bias[i1, h], 0.0)
        bmb[:, h, 256:512] = np.where(v2, rel_bias[i2, h], 0.0)
        bmb[:, h, 512:640] = np.where(v3, rel_bias[i3, h], 0.0)
    bmm = np.concatenate([np.where(v1, 0.0, NEG), np.where(v2, 0.0, NEG), np.where(v3, 0.0, NEG)], axis=1).astype(f32)
    cm = np.where(q3 >= k, 0.0, NEG).astype(f32)
    H = 6
    log_g = np.log(1.0 - 2.0 ** (-5.0 - np.arange(H))).astype(f32)
    i = np.arange(128, dtype=f32)
    rel = i[:, None] - i[None, :]
    decay_intra = (np.exp(np.maximum(rel, 0.0)[None] * log_g[:, None, None]) * (rel >= 0)[None]).astype(f32)
    xi = np.exp((i + 1.0)[None, :] * log_g[:, None]).astype(f32)
    zeta = np.exp((128 - 1.0 - i)[None, :] * log_g[:, None]).astype(f32)
    cd = np.exp(128 * log_g).astype(f32)
    s = f32(32 ** -0.5)
    decT = np.zeros((128, 6, 128), f32)
    for h in range(H):
        decT[:, h, :] = decay_intra[h].T * s
    xi_t = np.zeros((64, 3, 128), f32)
    zeta_t = np.zeros((128, 3, 64), f32)
    gam = np.zeros((64, 3), f32)
    for p in range(3):
        for hh in range(2):
            h = 2 * p + hh
            xi_t[hh * 32:(hh + 1) * 32, p, :] = xi[h][None, :] * s
            zeta_t[:, p, hh * 32:(hh + 1) * 32] = zeta[h][:, None]
            gam[hh * 32:(hh + 1) * 32, p] = cd[h]
    half = 16
    inv_freq = (1.0 / (10000.0 ** (np.arange(half, dtype=f32) / half))).astype(f32)
    invf = np.tile(inv_freq, 8).reshape(128, 1).astype(f32)
    return dict(bmb=bmb, bmm=bmm, cm=cm, decT=decT, xi=xi_t, zeta=zeta_t, gam=gam, invf=invf)


class Arena:
    def __init__(self, nc, base, size, name):
        self.nc, self.base, self.size, self.name = nc, base, size, name
        self.off = 0
        self.n = 0

    def reset(self):
        self.off = 0

    def alloc_at(self, fw, name, shape, dtype, off):
        self.off = off
        return self.alloc(fw, name, shape, dtype)

    def alloc(self, fw, name, shape, dtype):
        nbytes = int(np.prod(shape[1:])) * mybir.dt.size(dtype)
        nbytes = (nbytes + 31) // 32 * 32
        assert self.off + nbytes <= self.size, (self.name, name, self.off, nbytes, self.size)
        self.n += 1
        t = self.nc.alloc_sbuf_tensor_at(f"{self.name}_{name}_{self.n}", list(shape), dtype, offset=self.base + self.off)
        self.off += nbytes
        return Buf(t, f"{self.name}_{name}_{self.n}", semname=f"{self.name}_{name}")


def build(depth=DEPTH, dbg=None, skip=()):
    nc = bass.Bass("TRN2", target_bir_lowering=False)
    dbg = dbg or []
    dbg_out = {}
    MARKS.clear()

    def din(name, shape, dt=F32):
        return nc.dram_tensor(name, list(shape), dt, kind="ExternalInput").ap()

    x_d = din("x", [S, D])
    cT_d = din("cT", [128, 8])
    pos_d = din("posb", [1, S], I32)
    invf_d = din("invf", [128, 1])
    bmb_d = din("bmb", [128, 6, 640])
    bmm_d = din("bmm", [128, 640])
    cm_d = din("cm", [128, 128])
    decT_d = din("decT", [128, 6, 128])
    xi_d = din("xi", [64, 3, 128])
    zeta_d = din("zeta", [128, 3, 64])
    gam_d = din("gam", [64, 3])
    adab_d = din("adabT", [128, DEPTH, 48])
    n1g_d = din("n1gT", [128, DEPTH, 8])
    n2g_d = din("n2gT", [128, DEPTH, 8])
    fng_d = din("fngT", [128, 8])
    mg_d = din("mgT", [128, DEPTH, 8])
    mqn_d = din("mqnT", [128, DEPTH, 2])
    mkvn_d = din("mkvnT", [128, DEPTH, 1])
    if depth > 0:
        adaw_d = din("ada_w", [DEPTH, D, 6 * D])
        win_d = din("w_in", [DEPTH, D, IN_W])
        wuq_d = din("w_uq", [DEPTH, 256, 384])
        wukv_d = din("w_ukv", [DEPTH, 128, 512])
        wout_d = din("w_out", [DEPTH, D, D])
        wg_d = din("w_gate", [DEPTH, D, FH])
        wu_d = din("w_up", [DEPTH, D, FH])
        wd_d = din("w_down", [DEPTH, FH, D])
        xsp_d = nc.dram_tensor("xspill", [128, 8, S], F32, kind="Internal").ap()
    y_d = nc.dram_tensor("y", [S, D], F32, kind="ExternalOutput").ap()

    st = ExitStack()
    with st:
        fw = FW(nc, st)
        pe, act, dve, pool, sp = fw.pe, fw.act, fw.dve, fw.pool, fw.sp
        base0 = (nc.sbuf_base + 31) // 32 * 32
        st.enter_context(nc.sbuf_tensor("slab", [128, SLAB], mybir.dt.uint8))
        assert nc.sbuf_base == base0 + SLAB, (nc.sbuf_base, base0)
        o = base0
        SZ_HT, SZ_X, SZ_Y, SZ_ADAW, SZ_W = 32768, 65536, 32768, 12288, 36864
        SZ_C = SLAB - (SZ_HT + SZ_X + SZ_Y + SZ_ADAW + SZ_W)
        AC = Arena(nc, o, SZ_C, "c"); o += SZ_C
        AH = Arena(nc, o, SZ_HT, "h"); o += SZ_HT
        AX = Arena(nc, o, SZ_X, "x"); o += SZ_X
        AY = Arena(nc, o, SZ_Y, "y"); o += SZ_Y
        AA = Arena(nc, o, SZ_ADAW, "a"); o += SZ_ADAW
        AW = Arena(nc, o, SZ_W, "w"); o += SZ_W

        PS = [fw.psum(f"ps{i}", [128, 512], F32) for i in range(8)]

        def sub(buf, name):
            return Buf(buf.t, name)

        ident_f = AC.alloc(fw, "identf", [128, 128], F32)
        ident_b = AC.alloc(fw, "identb", [128, 128], BF16)
        ones_b = AC.alloc(fw, "onesb", [128, 128], BF16)
        BD = AC.alloc(fw, "bd", [128, 128], BF16)
        WNe = AC.alloc(fw, "wne", [128, 64], BF16)
        WNo = AC.alloc(fw, "wno", [128, 64], BF16)
        FCOS = AC.alloc(fw, "fcos", [128, S], BF16)
        FSIN = AC.alloc(fw, "fsin", [128, S], BF16)
        BM = AC.alloc(fw, "bm", [128, 6, 640], BF16)
        CM = AC.alloc(fw, "cm", [128, 128], F32)
        DECT = AC.alloc(fw, "dect", [128, 6, 128], F32)
        XI = AC.alloc(fw, "xi", [64, 3, 128], F32)
        ZETA = AC.alloc(fw, "zeta", [128, 3, 64], F32)
        GAM = AC.alloc(fw, "gam", [64, 3], F32)
        MOD = AC.alloc(fw, "mod", [128, DEPTH, 48], F32)
        A1 = AC.alloc(fw, "a1", [128, DEPTH, 8], F32)
        A2 = AC.alloc(fw, "a2", [128, DEPTH, 8], F32)
        ADAB = AC.alloc(fw, "adab", [128, DEPTH, 48], F32)
        N1G = AC.alloc(fw, "n1g", [128, DEPTH, 8], F32)
        N2G = AC.alloc(fw, "n2g", [128, DEPTH, 8], F32)
        FNG = AC.alloc(fw, "fng", [128, 8], F32)
        MG = AC.alloc(fw, "mg", [128, DEPTH, 8], F32)
        MQN = AC.alloc(fw, "mqn", [128, DEPTH, 2], F32)
        MKVN = AC.alloc(fw, "mkvn", [128, DEPTH, 1], F32)
        CTF = AC.alloc(fw, "ctf", [128, 8], F32)
        CTB = AC.alloc(fw, "ctb", [128, 8], BF16)
        INVF = AC.alloc(fw, "invf", [128, 1], F32)
        hT = AH.alloc(fw, "hT", [128, 8, S], BF16)
        adaw = [AA.alloc(fw, f"adaw{i}", [128, 3072], BF16) for i in range(2)]

        def dump(name, view, shape, dt):
            if name not in dbg:
                return
            t = nc.dram_tensor("dbg_" + name, list(shape), dt, kind="ExternalOutput").ap()
            dbg_out[name] = sp.dma(t, view)
            fw.dma_toks.append(dbg_out[name])

        fw.memset(dve, ident_f[:], 1.0)
        pool.issue(lambda e: e.affine_select(out=ident_f.t[:], in_=ident_f.t[:], pattern=[[-1, 128]],
                                             compare_op=ALU.is_equal, fill=0.0, base=0, channel_multiplier=1),
                   reads=[ident_f[:]], writes=[ident_f[:]])
        fw.copy(dve, ident_b[:], ident_f[:])
        fw.memset(dve, ones_b[:], 1.0)
        fw.memset(dve, BD[:], 0.0)
        fw.memset(dve, BD[0:64, 0:64], 1.0 / 64)
        fw.memset(dve, BD[64:128, 64:128], 1.0 / 64)
        fw.memset(dve, WNe[:], 0.0)
        fw.memset(dve, WNe[0:64, :], 1.0)
        if 'wn' not in skip:
            fw.memset(dve, WNe[64:65, :], 64 * EPS)
        fw.memset(dve, WNo[:], 0.0)
        fw.memset(dve, WNo[64:128, :], 1.0)
        if 'wn' not in skip:
            fw.memset(dve, WNo[0:1, :], 64 * EPS)
        for (b, d_) in [] if 'small' in skip else [(CM, cm_d), (DECT, decT_d), (XI, xi_d), (ZETA, zeta_d), (GAM, gam_d), (ADAB, adab_d),
                        (N1G, n1g_d), (N2G, n2g_d), (FNG, fng_d), (MG, mg_d), (MQN, mqn_d), (MKVN, mkvn_d),
                        (CTF, cT_d), (INVF, invf_d)]:
            sp.dma(b[:], d_)
        if 'silu' not in skip:
            fw.activation(CTB[:], CTF[:], AF.Silu)
        AY.reset()
        bmb_s = AY.alloc(fw, "bmb", [128, 6, 640], F32)
        bmm_s = AY.alloc(fw, "bmm", [128, 640], F32)
        if 'bm' not in skip:
            sp.dma(bmb_s[:], bmb_d)
            sp.dma(bmm_s[:], bmm_d)
            for h in range(6):
                fw.tt(dve, BM[:, h, :], bmb_s[:, h, :], bmm_s[:], ALU.add)
        fw.barrier()
        AY.reset()
        posi = AY.alloc(fw, "posi", [128, S], I32)
        ang = AY.alloc(fw, "ang", [128, S], F32)
        t1 = AY.alloc(fw, "t1", [128, S], F32)
        t2 = AY.alloc(fw, "t2", [128, S], F32)
        if 'tables' not in skip:
            sp.dma(posi[:], pos_d.partition_broadcast(128))
            fw.copy(dve, t1[:], posi[:])
            fw.ts(dve, ang[:], t1[:], INVF[:, 0:1], ALU.mult)
            TWO_PI = 2.0 * np.pi
            C1 = 6.28125
            C2 = float(np.float32(TWO_PI - C1))
            fw.ts(dve, t1[:], ang[:], 1.0 / TWO_PI, ALU.mult)
            fw.copy(dve, posi[:], t1[:])
            fw.copy(dve, t1[:], posi[:])
            fw.stt(t2[:], t1[:], -C1, ang[:], ALU.mult, ALU.add)
            fw.stt(t2[:], t1[:], -C2, t2[:], ALU.mult, ALU.add)

            def wrap(dst, src, tmp):
                fw.ts(dve, tmp, src, float(np.pi), ALU.is_gt)
                fw.stt(dst, tmp, -TWO_PI, src, ALU.mult, ALU.add)
                fw.ts(dve, tmp, dst, -float(np.pi), ALU.is_lt)
                fw.stt(dst, tmp, TWO_PI, dst, ALU.mult, ALU.add)
                fw.ts(dve, dst, dst, 3.1415925, ALU.min, -3.1415925, ALU.max)

            wrap(t2[:], t2[:], t1[:])
            fw.activation(FSIN[:], t2[:], AF.Sin)
            fw.ts(dve, ang[:], t2[:], float(np.pi / 2), ALU.add)
            wrap(ang[:], ang[:], t1[:])
            fw.activation(FCOS[:], ang[:], AF.Sin)
        fw.barrier()

        def mod_pieces(l):
            mp = PS[7]
            for piece in range(16):
                kc, half = piece // 2, piece % 2
                slot = adaw[piece % 2]
                pool.dma(slot[:], adaw_d[l, kc * 128:(kc + 1) * 128, half * 3072:(half + 1) * 3072], max_dma_last_dim=4096)
                for j in range(24):
                    jj = half * 24 + j
                    fw.mm(mp[:, jj:jj + 1], slot[:, j * 128:(j + 1) * 128], CTB[:, kc:kc + 1],
                          start=(piece == 0 and j == 0), stop=(kc == 7), signal=(j == 23), skip_group_check=True)
                yield
            fw.tt(dve, MOD[:, l, :], mp[:, 0:48], ADAB[:, l, :], ALU.add)
            fw.stt(A1[:, l, :], MOD[:, l, 8:16], 1.0, N1G[:, l, :], ALU.add, ALU.mult)
            fw.stt(A2[:, l, :], MOD[:, l, 32:40], 1.0, N2G[:, l, :], ALU.add, ALU.mult)
            yield

        EPSC = AC.alloc(fw, "epsc", [128, 1], F32)
        fw.memset(dve, EPSC[:], EPS)

        def norm_to_hT(l, a_view, b_col0, ar):
            ar.reset()
            sq = [ar.alloc(fw, f"nsq{i}", [128, 8, 512], BF16) for i in range(2)]
            lnt = ar.alloc(fw, "nln", [128, 512], F32)
            rs = [ar.alloc(fw, f"nrs{i}", [128, 512], F32) for i in range(2)]
            tm = [ar.alloc(fw, f"ntm{i}", [128, 512], F32) for i in range(4)]

            def stA(tb):
                ts_ = slice(tb * 512, (tb + 1) * 512)
                fw.activation(sq[tb % 2][:], xT[:, :, ts_], AF.Square)
                pb = PS[tb % 2]
                for kc in range(8):
                    fw.mm(pb[:], ones_b[:], sq[tb % 2][:, kc, :], start=(kc == 0), stop=(kc == 7))

            def stB(tb):
                ts_ = slice(tb * 512, (tb + 1) * 512)
                pb = PS[tb % 2]
                fw.activation(lnt[:], pb[:], AF.Ln, scale=1.0 / D, bias=EPSC[:, 0:1])
                fw.activation(rs[tb % 2][:], lnt[:], AF.Exp, scale=-0.5)
                for kc in range(8):
                    t = tm[kc % 4]
                    fw.tt(dve, t[:], xT[:, kc, ts_], rs[tb % 2][:], ALU.mult)
                    if kc % 2 == 0:
                        fw.activation(hT[:, kc, ts_], t[:], AF.Identity, scale=a_view(kc), bias=MOD[:, l, b_col0 + kc:b_col0 + kc + 1])
                    else:
                        fw.ts(pool, hT[:, kc, ts_], t[:], a_view(kc), ALU.mult, MOD[:, l, b_col0 + kc:b_col0 + kc + 1], ALU.add)
            stA(0)
            for tb in range(4):
                if tb + 1 < 4:
                    stA(tb + 1)
                stB(tb)

        def load_w(buf_view, dram_ap):
            pool.dma(buf_view, dram_ap, max_dma_last_dim=4096)

        def proj_fm(out_fn, w_fn, M, nk=8, rhs_fn=None, banks=(6, 7), out_p0=0):
            for tb in range(4):
                pb = PS[banks[tb % len(banks)]]
                ts_ = slice(tb * 512, (tb + 1) * 512)
                pv = pb[out_p0:out_p0 + M, :]
                for kc in range(nk):
                    rhs = rhs_fn(kc, ts_) if rhs_fn else hT[:, kc, ts_]
                    fw.mm(pv, w_fn(kc), rhs, start=(kc == 0), stop=(kc == nk - 1))
                out_fn(tb, ts_, pv)

        def finalize_head(bank, nr, mixb, chunk, tb, fin):
            SQb, LNb, RSb = fin
            ts_ = slice(tb * 512, (tb + 1) * 512)
            fw.activation(SQb[:], bank[:], AF.Square)
            pf = PS[6 + (tb % 2)]
            wn = WNe if nr == 0 else WNo
            fw.mm(pf[nr:nr + 64, :], wn[:], SQb[:], start=True, stop=True)
            fw.activation(LNb[nr:nr + 64, :], pf[nr:nr + 64, :], AF.Ln, scale=1.0 / 64)
            fw.activation(RSb[nr:nr + 64, :], LNb[nr:nr + 64, :], AF.Exp, scale=-0.5)
            fw.tt(dve, mixb[nr:nr + 64, chunk, ts_], bank[nr:nr + 64, :], RSb[nr:nr + 64, :], ALU.mult)

        def v_lhsT(vbuf, tile_off, ones_off, even):
            t = vbuf.t
            pstride = t[:, :].ap[0][0]
            if even:
                return vbuf.v(bass.AP(t, tile_off, [[pstride, 128], [ones_off - tile_off, 2], [1, 64]]))
            return vbuf.v(bass.AP(t, ones_off, [[pstride, 128], [tile_off - ones_off, 2], [1, 64]]))

        AX.reset()
        xT = AX.alloc(fw, "xT", [128, 8, S], F32)
        AW.reset()
        io = [AW.alloc(fw, f"io{i}", [128, D], F32) for i in range(2)]
        modgen0 = mod_pieces(0) if depth > 0 else iter(())
        for n in range(16):
            sp.dma(io[n % 2][:], x_d[n * 128:(n + 1) * 128, :])
            next(modgen0, None)
            for g in range(2):
                pb = PS[(2 * n + g) % 4]
                for j in range(4):
                    kc = 4 * g + j
                    fw.transpose(pb[:, j * 128:(j + 1) * 128], io[n % 2][:, kc * 128:(kc + 1) * 128], ident_f[:], signal=(j == 3))
                src = pb.v(pb.t[:, :].rearrange("p (j t) -> p j t", j=4))
                dst = xT.v(xT.t[:, 4 * g:4 * g + 4, n * 128:(n + 1) * 128])
                fw.copy(act if (n + g) % 2 else dve, dst, src)
        for _ in modgen0:
            pass
        fw.barrier()

        def run_pipe(units, la=3):
            nU = len(units)
            for i in range(nU + la):
                if i < nU:
                    units[i][0]()
                if i - la >= 0:
                    units[i - la][1]()

        for l in range(depth):
            MARKS.append(('mod', l, pe.ninst))
            MARKS.append(('norm1', l, pe.ninst))
            AY.reset()
            norm_to_hT(l, lambda kc: A1[:, l, kc:kc + 1], 0, AY)
            dump(f"hT{l}", hT[:], [128, 8, S], BF16)
            for kc in range(8):
                sp.dma(xsp_d[:, kc, :], xT[:, kc, :])
            fw.barrier()
            _rec = fw.dsem[xT.semname]
            spill_tok = (("d", xT.semname), 16 * _rec[1], _rec[0])
            for e in (pe, act, dve, pool):
                e.wait_tok(spill_tok)

            AY.reset()
            mixT = AY.alloc(fw, "mixT", [128, 8, S], BF16)

            MARKS.append(('A', l, pe.ninst))
            AX.reset()
            qT = AX.alloc(fw, "qT", [128, S], BF16)
            kT = AX.alloc(fw, "kT", [128, S], BF16)
            VA = AX.alloc(fw, "VA", [128, 48, 2, 128], BF16)
            VT = AX.alloc(fw, "VT", [128, S], BF16)
            PT = [AX.alloc(fw, f"pt{i}", [128, 512], BF16) for i in range(4)]
            TS = [AX.alloc(fw, f"ts{i}", [128, 512], F32) for i in range(4)]
            fin = (AX.alloc(fw, "fsq", [128, 512], BF16), AX.alloc(fw, "fln", [128, 512], F32), AX.alloc(fw, "frs", [128, 512], F32))
            AW.reset()
            wq = AW.alloc(fw, "wq", [128, 8, 128], BF16)
            wk = AW.alloc(fw, "wk", [128, 8, 128], BF16)
            wv = AW.alloc(fw, "wv", [128, 8, 128], BF16)
            winB = AW.alloc_at(fw, "winB", [128, 8, 416], BF16, 8192)
            wkrr = AW.alloc(fw, "wkrr", [128, 8, 32], BF16)
            wuq_f = AW.alloc(fw, "wuqf", [128, 2, 384], F32)
            wuq_b = AW.alloc(fw, "wuqb", [128, 2, 384], BF16)
            wuq_r = AW.alloc(fw, "wuqr", [128, 2, 4, 32], BF16)
            wukv_f = AW.alloc(fw, "wukvf", [128, 512], F32)
            wukv_b = AW.alloc(fw, "wukvb", [128, 512], BF16)
            SS = [sub(PS[4], "ss0"), sub(PS[4], "ss1"), sub(PS[5], "ss2"), sub(PS[5], "ss3")]
            fw.memset(pool, VA[:, :, 0, 64:128], 1.0)
            fw.memset(pool, VA[:, :, 1, 0:64], 1.0)
            winv = win_d[l].rearrange("(k p) n -> p k n", p=128)
            blk = [0]
            for c in (range(3) if 'A' not in skip else []):
                load_w(wq[:], winv[:, :, OFF_AQ + c * 128:OFF_AQ + (c + 1) * 128])
                load_w(wk[:], winv[:, :, OFF_AK + c * 128:OFF_AK + (c + 1) * 128])
                load_w(wv[:], winv[:, :, OFF_AV + c * 128:OFF_AV + (c + 1) * 128])
                if c == 0:
                    load_w(winB[:], winv[:, :, OFF_BQ:OFF_BQ + 416])
                    pool.dma_group([(wkrr[:, :, 0:16], winv[:, :, OFF_BKR + 16:OFF_BKR + 32]),
                                    (wkrr[:, :, 16:32], winv[:, :, OFF_BKR:OFF_BKR + 16])], max_dma_last_dim=4096)
                    sp.dma(wuq_f[:], wuq_d[l].rearrange("(k p) n -> p k n", p=128))
                    sp.dma(wukv_f[:], wukv_d[l])
                proj_fm(lambda tb, ts_, pv: fw.activation(qT[:, ts_], pv, AF.Identity, scale=0.125),
                        lambda kc: wq[:, kc, :], 128)
                proj_fm(lambda tb, ts_, pv: fw.copy(dve, kT[:, ts_], pv), lambda kc: wk[:, kc, :], 128)
                proj_fm(lambda tb, ts_, pv: fw.copy(act, VT[:, ts_], pv), lambda kc: wv[:, kc, :], 128)

                def tok_cols(ordr, tile):
                    if ordr == 0:
                        return VT[:, tile * 128:(tile + 1) * 128]
                    if ordr == 1:
                        r, b_ = tile // 4, tile % 4
                        return VT[:, 512 * b_ + r:512 * (b_ + 1):4]
                    return VT[:, tile::16]
                for ordr in range(3):
                    for tg in range(4):
                        pb = PS[6 + (tg % 2)]
                        pb16 = pb.t.bitcast(BF16)
                        for tt_ in range(4):
                            fw.transpose(pb.v(pb16[:, tt_ * 128:(tt_ + 1) * 128]), tok_cols(ordr, tg * 4 + tt_), ident_b[:],
                                         signal=(tt_ == 3))
                        t0_ = ordr * 16 + tg * 4
                        pv4 = pb16[:, 0:512].rearrange("p (n c) -> p n c", n=4)
                        ev_ = act if tg % 2 else dve
                        fw.copy(ev_, VA[:, t0_:t0_ + 4, 0, 0:64], pb.v(pv4[:, :, 0:64]))
                        fw.copy(ev_, VA[:, t0_:t0_ + 4, 1, 64:128], pb.v(pv4[:, :, 64:128]))
                if c == 0:
                    dump(f"qTA{l}", qT[:], [128, S], BF16)
                    dump(f"kTA{l}", kT[:], [128, S], BF16)
                    dump(f"VA{l}", VA[:], [128, 48, 2, 128], BF16)
                for hh in range(2):
                    h = 2 * c + hh
                    ro = 64 * hh
                    started = [False] * 4

                    unitsA = []

                    def sunit(subs, bm_off, w):
                        i = blk[0]
                        blk[0] += 1
                        ss = PS[4 + i % 4]
                        tsb = TS[i % 4]
                        pt = PT[i % 4]
                        offs = []
                        off = 0
                        for s_ in subs:
                            offs.append(off)
                            off += s_[2]
                        tot = off
                        nfull = sum(1 for s_ in subs if s_[2] == w)

                        def st1():
                            for (kcols, qcols, nq, pvl), o_ in zip(subs, offs):
                                fw.mm(ss[:, o_:o_ + nq], kcols, qcols, start=True, stop=True)
                            bmv = BM.v(bass.AP(BM.t, h * 640 + bm_off, [[6 * 640, 128], [0, nfull], [1, w]]))
                            fw.tt(dve, tsb.v(tsb.t[:, 0:nfull * w].rearrange("p (n q) -> p n q", n=nfull)),
                                  ss.v(ss.t[:, 0:nfull * w].rearrange("p (n q) -> p n q", n=nfull)), bmv, ALU.add)
                            if nfull < len(subs):
                                nql = subs[-1][2]
                                fw.tt(dve, tsb[:, nfull * w:nfull * w + nql], ss[:, nfull * w:nfull * w + nql],
                                      BM[:, h, bm_off:bm_off + nql], ALU.add)
                            fw.activation(pt[:, 0:tot], tsb[:, 0:tot], AF.Exp)

                        def st2():
                            for (kcols, qcols, nq, pvl), o_ in zip(subs, offs):
                                for (qs, vtile, tbk, ocols) in pvl:
                                    fw.mm(ocols, VA[:, vtile, hh, :], pt[:, o_ + qs.start:o_ + qs.stop], start=(not started[tbk]),
                                          stop=False, signal=True, skip_group_check=True)
                                    started[tbk] = True
                        unitsA.append((st1, st2))

                    def p1_sub(kt):
                        nqt = 2 if kt < 15 else 1
                        pvl = []
                        for j in range(nqt):
                            qt = kt + j
                            pvl.append((slice(j * 128, (j + 1) * 128), 0 * 16 + kt, qt // 4,
                                        PS[qt // 4][:, (qt % 4) * 128:(qt % 4 + 1) * 128]))
                        return (kT[ro:ro + 64, kt * 128:(kt + 1) * 128], qT[ro:ro + 64, kt * 128:kt * 128 + nqt * 128], nqt * 128, pvl)

                    def p2_sub(r, b_):
                        nqt = 2 if b_ < 3 else 1
                        pvl = []
                        for j in range(nqt):
                            pvl.append((slice(j * 128, (j + 1) * 128), 16 + r * 4 + b_, b_ + j, PS[b_ + j][:, r::4]))
                        return (kT[ro:ro + 64, 512 * b_ + r:512 * (b_ + 1):4], qT[ro:ro + 64, 512 * b_ + r:512 * (b_ + nqt):4],
                                nqt * 128, pvl)

                    def p3_sub(r):
                        pvl = []
                        for tbk in range(4):
                            pvl.append((slice(tbk * 32, (tbk + 1) * 32), 32 + r, tbk, PS[tbk][:, r::16]))
                        return (kT[ro:ro + 64, r::16], qT[ro:ro + 64, r::16], 128, pvl)

                    for kt in range(0, 16, 2):
                        sunit([p1_sub(kt), p1_sub(kt + 1)], 0, 256)
                    for r in range(4):
                        for b_ in (0, 2):
                            sunit([p2_sub(r, b_), p2_sub(r, b_ + 1)], 256, 256)
                    for r in range(0, 16, 4):
                        sunit([p3_sub(r + j) for j in range(4)], 512, 128)
                    run_pipe(unitsA)
                    for tb in range(4):
                        finalize_head(PS[tb], ro, mixT, c, tb, fin)
            dump(f"mixA{l}", mixT[:, 0:3, :], [128, 3, S], BF16)
            fw.barrier()

            MARKS.append(('B', l, pe.ninst))
            AX.reset()
            qlatT = AX.alloc(fw, "qlatT", [128, 2, S], BF16)
            kvlatT = AX.alloc(fw, "kvlatT", [128, S], BF16)
            sqb = [AX.alloc(fw, "sqb0", [128, 2, 512], BF16)] * 2
            rsq = AX.alloc(fw, "rsq", [128, S], F32)
            rskv = AX.alloc(fw, "rskv", [128, S], F32)
            rstok = AX.alloc(fw, "rstok", [128, 16], F32)
            qTh = AX.alloc(fw, "qTh", [128, S], BF16)
            KTh = AX.alloc(fw, "KTh", [128, S], BF16)
            VBs = [AX.alloc(fw, f"VB{i}", [128, 16, 128], BF16) for i in range(2)]
            PTB = [AX.alloc(fw, f"ptb{i}", [128, 512], BF16) for i in range(4)]
            TSB = [AX.alloc(fw, f"tsb{i}", [128, 128], F32) for i in range(2)]
            RT = [AX.alloc(fw, f"rt{i}", [128, 512], F32) for i in range(3)]
            fin = (AX.alloc(fw, "fsq", [128, 512], BF16), AX.alloc(fw, "fln", [128, 512], F32), AX.alloc(fw, "frs", [128, 512], F32))
            fw.memset(pool, VBs[0][:, :, 64:128], 1.0)
            fw.memset(pool, VBs[1][:, :, 0:64], 1.0)
            fw.activation(wkrr[:, :, 0:16], wkrr[:, :, 0:16], AF.Identity, scale=-1.0)
            for kc in range(2):
                fw.activation(wuq_b[:, kc, :], wuq_f[:, kc, :], AF.Identity, scale=MQN[:, l, kc:kc + 1])
            fw.activation(wukv_b[:], wukv_f[:], AF.Identity, scale=MKVN[:, l, 0:1])
            wq4 = wuq_b.t[:, :, :].rearrange("p k (h e) -> p k h e", h=4)
            fw.activation(wuq_r.v(wuq_r.t[:, :, :, 0:16]), wuq_b.v(wq4[:, :, :, 80:96]), AF.Identity, scale=-1.0)
            fw.copy(dve, wuq_r.v(wuq_r.t[:, :, :, 16:32]), wuq_b.v(wq4[:, :, :, 64:80]))

            for c2 in range(2):
                proj_fm(lambda tb, ts_, pv, c2=c2: fw.copy(dve, qlatT[:, c2, ts_], pv),
                        lambda kc, c2=c2: winB[:, kc, c2 * 128:(c2 + 1) * 128], 128)
            for tb in range(4):
                ts_ = slice(tb * 512, (tb + 1) * 512)
                fw.activation(sqb[tb % 2][:], qlatT[:, :, ts_], AF.Square)
                pb = PS[4 + tb % 2]
                for c2 in range(2):
                    fw.mm(pb[:], ones_b[:], sqb[tb % 2][:, c2, :], start=(c2 == 0), stop=(c2 == 1))
                fw.activation(RT[0][:], pb[:], AF.Ln, scale=1.0 / 256, bias=EPSC[:, 0:1])
                fw.activation(rsq[:, ts_], RT[0][:], AF.Exp, scale=-0.5)
            proj_fm(lambda tb, ts_, pv: fw.copy(dve, kvlatT[:, ts_], pv), lambda kc: winB[:, kc, 256:384], 128)
            for tb in range(4):
                ts_ = slice(tb * 512, (tb + 1) * 512)
                fw.activation(sqb[tb % 2][:, 0, :], kvlatT[:, ts_], AF.Square)
                pb = PS[4 + tb % 2]
                fw.mm(pb[:], ones_b[:], sqb[tb % 2][:, 0, :], start=True, stop=True)
                fw.activation(RT[0][:], pb[:], AF.Ln, scale=1.0 / 128, bias=EPSC[:, 0:1])
                fw.activation(rskv[:, ts_], RT[0][:], AF.Exp, scale=-0.5)
            identb4 = ident_f.v(bass.AP(ident_f.t, 0, [[128, 128], [0, 4], [1, 128]]))
            for g4 in range(4):
                tdv = RT[0].v(RT[0].t[:, :].rearrange("p (n q) -> p n q", n=4))
                fw.tt(dve, tdv, rskv.v(rskv.t[:, g4 * 512:(g4 + 1) * 512].rearrange("p (n q) -> p n q", n=4)), identb4, ALU.mult)
                _o = rstok.t[:, g4 * 4:(g4 + 1) * 4]
                _i = RT[0].t[:, :].rearrange("p (n q) -> p n q", n=4)
                dve.issue(lambda e, _o=_o, _i=_i: e.tensor_reduce(out=_o, in_=_i, axis=AX_X, op=ALU.add),
                          reads=[RT[0][:]], writes=[rstok[:]])
            for tb in range(4):
                ts_ = slice(tb * 512, (tb + 1) * 512)
                p1, p2 = (PS[6], PS[7]) if tb % 2 else (PS[4], PS[5])
                for kc in range(8):
                    fw.mm(p1[64:96, :], winB[:, kc, 384:416], hT[:, kc, ts_], start=(kc == 0), stop=(kc == 7))
                for kc in range(8):
                    fw.mm(p2[64:96, :], wkrr[:, kc, :], hT[:, kc, ts_], start=(kc == 0), stop=(kc == 7))
                fw.tt(dve, RT[0][64:96, :], p1[64:96, :], FCOS[64:96, ts_], ALU.mult)
                fw.tt(dve, RT[1][64:96, :], p2[64:96, :], FSIN[64:96, ts_], ALU.mult)
                fw.tt(pool, KTh[64:96, ts_], RT[0][64:96, :], RT[1][64:96, :], ALU.add)
            dump(f"rsq{l}", rsq[:], [128, S], F32)
            SCB = float((64 + 32) ** -0.5)
            BSETS = [(PS[4], PS[5], PS[5]), (PS[6], PS[7], PS[7])]
            for h in (range(4) if 'B' not in skip else []):
                hh = h % 2
                nr = 64 * hh
                for tb in range(4):
                    ts_ = slice(tb * 512, (tb + 1) * 512)
                    p1, p2, p3 = BSETS[tb % 2]
                    for kc in range(2):
                        fw.mm(p1[0:96, :], wuq_b[:, kc, h * 96:(h + 1) * 96], qlatT[:, kc, ts_], start=(kc == 0), stop=(kc == 1))
                    for kc in range(2):
                        fw.mm(p2[64:96, :], wuq_r.v(wuq_r.t[:, kc, h, :]), qlatT[:, kc, ts_], start=(kc == 0), stop=(kc == 1))
                    fw.stt(qTh[0:64, ts_], p1[0:64, :], SCB, rsq[0:64, ts_], ALU.mult, ALU.mult)
                    fw.tt(dve, RT[0][64:96, :], p1[64:96, :], FCOS[64:96, ts_], ALU.mult)
                    fw.tt(dve, RT[1][64:96, :], p2[64:96, :], FSIN[64:96, ts_], ALU.mult)
                    fw.tt(pool, RT[2][64:96, :], RT[0][64:96, :], RT[1][64:96, :], ALU.add)
                    fw.stt(qTh[64:96, ts_], RT[2][64:96, :], SCB, rsq[64:96, ts_], ALU.mult, ALU.mult)
                    fw.mm(p3[0:64, :], wukv_b[:, h * 128:h * 128 + 64], kvlatT[:, ts_], start=True, stop=True, skip_group_check=True)
                    fw.tt(dve, KTh[0:64, ts_], p3[0:64, :], rskv[0:64, ts_], ALU.mult)
                for tg in range(2):
                    pb = PS[4 + tg]
                    for t8 in range(8):
                        tile = tg * 8 + t8
                        fw.mm(pb[:, t8 * 64:(t8 + 1) * 64], kvlatT[:, tile * 128:(tile + 1) * 128],
                              wukv_b[:, h * 128 + 64:(h + 1) * 128], start=True, stop=True, signal=(t8 == 7))
                    for t8 in range(8):
                        tile = tg * 8 + t8
                        fw.activation(VBs[hh][:, tile, 64 * hh:64 * hh + 64], pb[:, t8 * 64:(t8 + 1) * 64], AF.Identity,
                                      scale=rstok[:, tile:tile + 1])
                if h == 0:
                    dump(f"qTh{l}", qTh[0:96, :], [96, S], BF16)
                    dump(f"KTh{l}", KTh[0:96, :], [96, S], BF16)
                    dump(f"VB{l}", VBs[0][:], [128, 16, 128], BF16)
                started = [False] * 4
                unitsB = []
                bi = 0
                for kt in range(16):
                    for tb in range(kt // 4, 4):
                        q0 = max(128 * kt, 512 * tb)
                        q1 = 512 * (tb + 1)

                        def mk(kt=kt, tb=tb, q0=q0, q1=q1, bi=bi):
                            nq = q1 - q0
                            sbk = PS[4 + bi % 4]
                            pt = PTB[bi % 4]

                            def st1():
                                fw.mm(sbk[:, 0:nq], KTh[0:96, kt * 128:(kt + 1) * 128], qTh[0:96, q0:q1], start=True, stop=True)
                                if q0 == 128 * kt:
                                    tsb = TSB[kt % 2]
                                    fw.tt(dve, tsb[:], sbk[:, 0:128], CM[:], ALU.add)
                                    fw.activation(pt[:, 0:128], tsb[:], AF.Exp)
                                    if nq > 128:
                                        fw.activation(pt[:, 128:nq], sbk[:, 128:nq], AF.Exp)
                                else:
                                    fw.activation(pt[:, 0:nq], sbk[:, 0:nq], AF.Exp)

                            def st2():
                                fw.mm(PS[tb][:, q0 - 512 * tb:q1 - 512 * tb], VBs[hh][:, kt, :], pt[:, 0:nq], start=(not started[tb]),
                                      stop=False, signal=True, skip_group_check=True)
                                started[tb] = True
                            return (st1, st2)
                        unitsB.append(mk())
                        bi += 1
                run_pipe(unitsB)
                for tb in range(4):
                    finalize_head(PS[tb], nr, mixT, 3 + h // 2, tb, fin)
            dump(f"mixB{l}", mixT[:, 3:5, :], [128, 2, S], BF16)
            fw.barrier()

            MARKS.append(('C', l, pe.ninst))
            AX.reset()
            qTc = AX.alloc(fw, "qTc", [128, S], BF16)
            kTc = AX.alloc(fw, "kTc", [128, S], BF16)
            qxT = AX.alloc(fw, "qxT", [128, S], BF16)
            kz = AX.alloc(fw, "kz", [128, 16, 64], BF16)
            VC = AX.alloc(fw, "VC", [128, 16, 128], BF16)
            gT = AX.alloc(fw, "gT", [128, S], BF16)
            STf = AX.alloc(fw, "stf", [64, 16, 64], F32)
            STb = AX.alloc(fw, "stb", [64, 16, 64], BF16)
            PTC = [AX.alloc(fw, f"ptc{i}", [128, 128], BF16) for i in range(4)]
            RT = [AX.alloc(fw, f"rtc{i}", [128, 512], F32) for i in range(3)]
            CP = AX.alloc(fw, "ccp", [128, 512], BF16)
            CSQ = AX.alloc(fw, "csq", [128, 512], BF16)
            CMEAN = AX.alloc(fw, "cmean", [128, 512], F32)
            CMSQ = AX.alloc(fw, "cmsq", [128, 512], F32)
            CDV = AX.alloc(fw, "cdv", [128, 512], F32)
            CVAR = AX.alloc(fw, "cvar", [128, 512], F32)
            CRS = AX.alloc(fw, "crs", [128, 512], F32)
            AW.reset()
            WSC = []
            for i_ in range(2):
                WSC.append(dict(q=AW.alloc(fw, f"wcq{i_}", [128, 8, 64], BF16), qr=AW.alloc(fw, f"wcqr{i_}", [128, 8, 2, 32], BF16),
                                k=AW.alloc(fw, f"wck{i_}", [128, 8, 64], BF16), kr=AW.alloc(fw, f"wckr{i_}", [128, 8, 2, 32], BF16),
                                v=AW.alloc(fw, f"wcv{i_}", [128, 8, 128], BF16), g=AW.alloc(fw, f"wcg{i_}", [128, 8, 128], BF16)))
            wo = AW.alloc_at(fw, "wo", [128, 8, D], BF16, 20480)
            wov = wout_d[l].rearrange("(k p) n -> p k n", p=128)

            def load_rot_dma(dst, off):
                prs = []
                for h2 in range(2):
                    prs.append((dst.v(dst.t[:, :, h2, 0:16]), winv[:, :, off + h2 * 32 + 16:off + h2 * 32 + 32]))
                    prs.append((dst.v(dst.t[:, :, h2, 16:32]), winv[:, :, off + h2 * 32:off + h2 * 32 + 16]))
                pool.dma_group(prs, max_dma_last_dim=4096)

            def load_pairC(c_):
                W_ = WSC[c_ % 2]
                load_w(W_["q"][:], winv[:, :, OFF_CQ + c_ * 64:OFF_CQ + (c_ + 1) * 64])
                load_rot_dma(W_["qr"], OFF_CQ + c_ * 64)
                load_w(W_["k"][:], winv[:, :, OFF_CK + c_ * 64:OFF_CK + (c_ + 1) * 64])
                load_rot_dma(W_["kr"], OFF_CK + c_ * 64)
                load_w(W_["v"][:], winv[:, :, OFF_CV + c_ * 128:OFF_CV + (c_ + 1) * 128])
                load_w(W_["g"][:], winv[:, :, OFF_CG + c_ * 128:OFF_CG + (c_ + 1) * 128])
            if 'C' not in skip:
                load_pairC(0)
            SC_ = [sub(PS[4], "sc0"), sub(PS[4], "sc1"), sub(PS[5], "sc2"), sub(PS[5], "sc3")]
            for c in (range(3) if 'C' not in skip else []):
                W_ = WSC[c % 2]
                wcq, wcqr, wck, wckr, wcv, wcg = W_["q"], W_["qr"], W_["k"], W_["kr"], W_["v"], W_["g"]
                if c + 1 < 3:
                    load_pairC(c + 1)
                else:
                    load_w(wo[:], wov)
                for rw_ in (wcqr, wckr):
                    fw.activation(rw_.v(rw_.t[:, :, :, 0:16]), rw_.v(rw_.t[:, :, :, 0:16]), AF.Identity, scale=-1.0)
                for (w_, wr_, dst, isq) in [(wcq, wcqr, qTc, True), (wck, wckr, kTc, False)]:
                    for tb in range(4):
                        ts_ = slice(tb * 512, (tb + 1) * 512)
                        p1, p2 = (PS[6], PS[7]) if tb % 2 else (PS[4], PS[5])
                        for kc in range(8):
                            fw.mm(p1[0:64, :], w_[:, kc, :], hT[:, kc, ts_], start=(kc == 0), stop=(kc == 7))
                        for kc in range(8):
                            fw.mm(p2[0:64, :], wr_.v(wr_.t[:, kc, :, :].rearrange("p h e -> p (h e)")), hT[:, kc, ts_],
                                  start=(kc == 0), stop=(kc == 7))
                        fw.tt(dve, RT[0][0:64, :], p1[0:64, :], FCOS[0:64, ts_], ALU.mult)
                        fw.tt(dve, RT[1][0:64, :], p2[0:64, :], FSIN[0:64, ts_], ALU.mult)
                        if isq:
                            fw.tt(pool, RT[2][0:64, :], RT[0][0:64, :], RT[1][0:64, :], ALU.add)
                            fw.copy(act, dst[0:64, ts_], RT[2][0:64, :])
                            xib = XI.v(bass.AP(XI.t, c * 128, [[3 * 128, 64], [0, 4], [1, 128]]))
                            fw.tt(pool, qxT.v(qxT.t[0:64, ts_].rearrange("p (n q) -> p n q", n=4)),
                                  RT[2].v(RT[2].t[0:64, :].rearrange("p (n q) -> p n q", n=4)), xib, ALU.mult)
                        else:
                            fw.tt(pool, dst[0:64, ts_], RT[0][0:64, :], RT[1][0:64, :], ALU.add)
                proj_fm(lambda tb, ts_, pv: fw.activation(gT[:, ts_], pv, AF.Silu), lambda kc: wcg[:, kc, :], 128)
                for tg in range(4):
                    pb = PS[6 + tg % 2]
                    for t4 in range(4):
                        tile = tg * 4 + t4
                        for kc in range(8):
                            fw.mm(pb[:, t4 * 128:(t4 + 1) * 128], hT[:, kc, tile * 128:(tile + 1) * 128], wcv[:, kc, :],
                                  start=(kc == 0), stop=(kc == 7), signal=(kc == 7 and t4 == 3))
                    fw.copy(act if tg % 2 else dve, VC.v(VC.t[:, tg * 4:tg * 4 + 4, :].rearrange("p n c -> p (n c)")), pb[:])
                for tg in range(2):
                    pb = PS[6 + tg]
                    tb16 = pb.t.bitcast(BF16)
                    for t8 in range(8):
                        tile = tg * 8 + t8
                        fw.transpose(pb.v(tb16[:, t8 * 64:(t8 + 1) * 64]), kTc[0:64, tile * 128:(tile + 1) * 128],
                                     ident_b[0:64, 0:64], signal=(t8 == 7))
                    zb = ZETA.v(bass.AP(ZETA.t, c * 64, [[3 * 64, 128], [0, 8], [1, 64]]))
                    fw.tt(dve, kz[:, tg * 8:(tg + 1) * 8, :],
                          pb.v(tb16[:, 0:512].rearrange("p (n c) -> p n c", n=8)), zb, ALU.mult)
                for hh in range(2):
                    for n in range(15):
                        pu = PS[6 + (n // 8)]
                        fw.mm(pu[32 * hh:32 * hh + 32, (n % 8) * 64:(n % 8 + 1) * 64], kz[:, n, 32 * hh:32 * hh + 32],
                              VC[:, n, 64 * hh:64 * hh + 64], start=True, stop=True, signal=(n in (7, 14)))
                fw.copy(dve, STf[:, 1, :], PS[6][0:64, 0:64])
                for n in range(1, 15):
                    pu = PS[6 + (n // 8)]
                    fw.stt(STf[:, n + 1, :], STf[:, n, :], GAM[:, c:c + 1], pu[0:64, (n % 8) * 64:(n % 8 + 1) * 64], ALU.mult, ALU.add)
                fw.copy(act, STb[:, 1:16, :], STf[:, 1:16, :])
                if c == 0:
                    dump(f"qTc{l}", qTc[0:64, :], [64, S], BF16)
                    dump(f"kTc{l}", kTc[0:64, :], [64, S], BF16)
                    dump(f"qxT{l}", qxT[0:64, :], [64, S], BF16)
                    dump(f"kz{l}", kz[:], [128, 16, 64], BF16)
                    dump(f"VC{l}", VC[:], [128, 16, 128], BF16)
                    dump(f"STf{l}", STf[:, 1:16, :], [64, 15, 64], F32)
                    dump(f"gT{l}", gT[:], [128, S], BF16)
                unitsC = []
                bi = 0
                for n in range(16):
                    for hh in range(2):
                        def mk(n=n, hh=hh, bi=bi):
                            h = 2 * c + hh
                            ss = PS[4 + bi % 4]
                            ssv = ss[:, 0:128]
                            pt = PTC[bi % 4]
                            cs = slice(n * 128, (n + 1) * 128)
                            ob = PS[n // 4][64 * hh:64 * hh + 64, (n % 4) * 128:(n % 4 + 1) * 128]

                            def st1():
                                fw.mm(ssv, kTc[32 * hh:32 * hh + 32, cs], qTc[32 * hh:32 * hh + 32, cs], start=True, stop=True)
                                fw.tt(dve, pt[:], ssv, DECT[:, h, :], ALU.mult)

                            def st2():
                                fw.mm(ob, VC[:, n, 64 * hh:64 * hh + 64], pt[:], start=True, stop=(n == 0), signal=True, skip_group_check=True)
                                if n > 0:
                                    fw.mm(ob, STb[32 * hh:32 * hh + 32, n, :], qxT[32 * hh:32 * hh + 32, cs], start=False, stop=True,
                                          signal=True, skip_group_check=True)
                            return (st1, st2)
                        unitsC.append(mk())
                        bi += 1
                run_pipe(unitsC)
                for tb in range(4):
                    ts_ = slice(tb * 512, (tb + 1) * 512)
                    ob = PS[tb]
                    fw.copy(act, CP[:], ob[:])
                    fw.activation(CSQ[:], ob[:], AF.Square)
                    pm, p2_ = PS[6], PS[7]
                    fw.mm(pm[:], BD[:], CP[:], start=True, stop=True)
                    fw.mm(p2_[:], BD[:], CSQ[:], start=True, stop=True)
                    fw.copy(act, CMEAN[:], pm[:])
                    fw.activation(CMSQ[:], pm[:], AF.Square)
                    fw.tt(dve, CDV[:], ob[:], CMEAN[:], ALU.subtract)
                    fw.tt(dve, CVAR[:], p2_[:], CMSQ[:], ALU.subtract)
                    fw.ts(dve, CVAR[:], CVAR[:], 0.0, ALU.max)
                    fw.activation(CVAR[:], CVAR[:], AF.Ln, bias=EPSC[:, 0:1])
                    fw.activation(CRS[:], CVAR[:], AF.Exp, scale=-0.5)
                    fw.tt(dve, CDV[:], CDV[:], CRS[:], ALU.mult)
                    fw.tt(pool, mixT[:, 5 + c, ts_], CDV[:], gT[:, ts_], ALU.mult)
            for kc in range(8):
                fw.activation(wo[:, kc, :], wo[:, kc, :], AF.Identity, scale=MG[:, l, kc:kc + 1])
            dump(f"mixC{l}", mixT[:, 5:8, :], [128, 3, S], BF16)
            fw.barrier()

            MARKS.append(('wout', l, pe.ninst))
            AX.reset()
            xT = AX.alloc(fw, "xT", [128, 8, S], F32)
            sp.dma_group([(xT[:, kc, :], xsp_d[:, kc, :]) for kc in range(8)])
            gu = [AW.alloc_at(fw, f"gu{i}", [128, 2, 8, 128], BF16, i * 4096) for i in range(3)]
            wgv = wg_d[l].rearrange("(k p) n -> p k n", p=128)
            wuv = wu_d[l].rearrange("(k p) n -> p k n", p=128)

            def load_gu(j_):
                g_ = gu[j_ % 3]
                pool.dma_group([(g_[:, 0, :, :], wgv[:, :, j_ * 128:(j_ + 1) * 128]),
                                (g_[:, 1, :, :], wuv[:, :, j_ * 128:(j_ + 1) * 128])], max_dma_last_dim=4096)
            for j_ in range(3):
                load_gu(j_)
            for d_ in range(8):
                for tb in range(4):
                    ts_ = slice(tb * 512, (tb + 1) * 512)
                    pb = PS[(d_ * 4 + tb) % 8]
                    for kc in range(8):
                        fw.mm(pb[:], wo[:, kc, d_ * 128:(d_ + 1) * 128], mixT[:, kc, ts_], start=(kc == 0), stop=(kc == 7))
                    fw.stt(xT[:, d_, ts_], pb[:], MOD[:, l, 16 + d_:17 + d_], xT[:, d_, ts_], ALU.mult, ALU.add)
            dump(f"xmid{l}", xT[:], [128, 8, S], F32)
            fw.barrier()

            MARKS.append(('ffn', l, pe.ninst))
            AY.reset()
            norm_to_hT(l, lambda kc: A2[:, l, kc:kc + 1], 24, AY)
            fw.barrier()
            AY.reset()
            actT = AY.alloc(fw, "actT", [128, 6, S], BF16)
            sgt = [AY.alloc(fw, f"sg{i}", [128, 512], BF16) for i in range(2)]
            wdn = [AW.alloc_at(fw, f"wdn{i}", [128, 6, D], BF16, 12288 + i * 12288) for i in range(2)]
            ji = 0
            modgen = mod_pieces(l + 1) if l + 1 < depth else iter(())
            for gi, (j0, nj) in enumerate(FFN_GROUPS):
                wdb = wdn[gi % 2]
                load_w(wdb[:, 0:nj, :], wd_d[l, j0 * 128:(j0 + nj) * 128, :].rearrange("(j p) n -> p j n", p=128))
                for jj in range(nj):
                    j = j0 + jj
                    g = gu[j % 3]
                    if j >= 3:
                        load_gu(j)
                    for tb in range(4):
                        ts_ = slice(tb * 512, (tb + 1) * 512)
                        pg, pu = PS[(tb % 2) * 2], PS[(tb % 2) * 2 + 1]
                        for kc in range(8):
                            fw.mm(pg[:], g[:, 0, kc, :], hT[:, kc, ts_], start=(kc == 0), stop=(kc == 7))
                        for kc in range(8):
                            fw.mm(pu[:], g[:, 1, kc, :], hT[:, kc, ts_], start=(kc == 0), stop=(kc == 7))
                        fw.activation(sgt[tb % 2][:], pg[:], AF.Silu)
                        fw.tt(dve, actT[:, jj, ts_], pu[:], sgt[tb % 2][:], ALU.mult)
                    next(modgen, None)
                for d_ in range(8):
                    for tb in range(4):
                        ts_ = slice(tb * 512, (tb + 1) * 512)
                        pb = PS[4 + (d_ * 4 + tb) % 3]
                        for jj in range(nj):
                            fw.mm(pb[:], wdb[:, jj, d_ * 128:(d_ + 1) * 128], actT[:, jj, ts_], start=(jj == 0), stop=(jj == nj - 1))
                        fw.stt(xT[:, d_, ts_], pb[:], MOD[:, l, 40 + d_:41 + d_], xT[:, d_, ts_], ALU.mult, ALU.add)
            for _ in modgen:
                pass
            dump(f"xout{l}", xT[:], [128, 8, S], F32)
            fw.barrier()

        MARKS.append(('final', depth, pe.ninst))
        AY.reset()
        sq = [AY.alloc(fw, f"fsq{i}", [128, 8, 512], BF16) for i in range(2)]
        lnt = AY.alloc(fw, "fln", [128, 512], F32)
        rs = AY.alloc(fw, "frs", [128, 512], F32)
        yt = [AY.alloc(fw, f"fyt{i}", [128, 512], F32) for i in range(3)]
        AW.reset()
        OUT = [AW.alloc(fw, f"oo{i}", [128, 4, D], F32) for i in range(2)]
        stores = []
        for tb in (range(4) if 'final' not in skip else []):
            ts_ = slice(tb * 512, (tb + 1) * 512)
            fw.activation(sq[tb % 2][:], xT[:, :, ts_], AF.Square)
            pb = PS[tb % 2]
            for kc in range(8):
                fw.mm(pb[:], ones_b[:], sq[tb % 2][:, kc, :], start=(kc == 0), stop=(kc == 7))
            fw.activation(lnt[:], pb[:], AF.Ln, scale=1.0 / D, bias=EPSC[:, 0:1])
            fw.activation(rs[:], lnt[:], AF.Exp, scale=-0.5)
            ob_ = OUT[tb % 2]
            for kc in range(8):
                t = yt[kc % 3]
                fw.stt(t[:], xT[:, kc, ts_], FNG[:, kc:kc + 1], rs[:], ALU.mult, ALU.mult)
                pt_ = PS[2 + kc % 4]
                for q in range(4):
                    fw.transpose(pt_[:, q * 128:(q + 1) * 128], t[:, q * 128:(q + 1) * 128], ident_f[:], signal=(q == 3))
                fw.copy(act if kc % 2 else dve, ob_[:, :, kc * 128:(kc + 1) * 128],
                        pt_.v(pt_.t[:, :].rearrange("p (q c) -> p q c", q=4)))
            stores.append(sp.dma_group([(y_d[(tb * 4 + q) * 128:(tb * 4 + q + 1) * 128, :], ob_[:, q, :]) for q in range(4)]))
        for tok in stores:
            sp.wait_tok(tok)
        for tok in dbg_out.values():
            sp.wait_tok(tok)
        fw.emit()
        info = {n: (e.ninst, e.nwait) for n, e in fw.E.items()}
        info["sp"] = (fw.sp.ninst, fw.sp.nwait)
        info["sems"] = fw.nsem
        print("MK build:", info)
    return nc


AX_X = mybir.AxisListType.X
MARKS = []
_NC_CACHE = {}


def prep_inputs(inputs, b):
    f32 = np.float32
    g = lambda k: np.asarray(inputs[k])
    hc = _HC_CACHE.get("hc")
    if hc is None:
        hc = host_consts(g("rel_bias").astype(f32))
        _HC_CACHE["hc"] = hc
    m = dict(hc)
    m["x"] = np.ascontiguousarray(g("x")[b].astype(f32))
    m["cT"] = np.ascontiguousarray(g("c")[b].astype(f32).reshape(8, 128).T)
    m["posb"] = np.ascontiguousarray(g("positions")[b].astype(np.int32).reshape(1, S))
    sh = _HC_CACHE.get("shared")
    if sh is None:
        def fm(a, nch):
            a = np.asarray(a, f32)
            return np.ascontiguousarray(a.reshape(a.shape[0], nch, 128).transpose(2, 0, 1))
        sh = dict(
            ada_w=np.ascontiguousarray(g("ada_w").astype(f32)),
            adabT=fm(g("ada_b"), 48), n1gT=fm(g("norm1_g"), 8), n2gT=fm(g("norm2_g"), 8),
            fngT=np.ascontiguousarray(g("final_norm").astype(f32).reshape(8, 128).T),
            mgT=fm(g("mix_gain"), 8), mqnT=fm(g("mla_q_norm"), 2), mkvnT=fm(g("mla_kv_norm"), 1),
            w_in=np.ascontiguousarray(g("w_in").astype(f32)), w_uq=np.ascontiguousarray(g("mla_w_uq").astype(f32)),
            w_ukv=np.ascontiguousarray(g("mla_w_ukv").astype(f32)), w_out=np.ascontiguousarray(g("w_out").astype(f32)),
            w_gate=np.ascontiguousarray(g("ffn_w_gate").astype(f32)), w_up=np.ascontiguousarray(g("ffn_w_up").astype(f32)),
            w_down=np.ascontiguousarray(g("ffn_w_down").astype(f32)),
        )
        _HC_CACHE["shared"] = sh
    m.update(sh)
    return m


_HC_CACHE = {}


def kernel(**inputs):
    _HC_CACHE.clear()
    nc = build()
    in_maps = [prep_inputs(inputs, b) for b in range(NCORES)]
    res = run_bass_kernel_spmd(nc, in_maps, core_ids=list(range(NCORES)))
    out = np.stack([np.asarray(r["y"], dtype=np.float32) for r in res.results], axis=0)
    return out
```
